# Optimizing a Trainium2 kernel written in Bass

```python
import math
import jax
import jax.numpy as jnp
from jax import lax
import numpy as np

D_MODEL = 1024
BATCH = 16
SEQ = 2048
DEPTH = 2

CTX_LEN = 256
GRID_W = 64
N_MIXERS = 2
N_MOD = 6
EPS = 1e-6

SSD_D_INNER = 2 * D_MODEL
SSD_HEADDIM = 64
SSD_HEADS = SSD_D_INNER // SSD_HEADDIM
SSD_GROUPS = 4
SSD_HPG = SSD_HEADS // SSD_GROUPS
SSD_STATE = 128
SSD_CONV = 5
SSD_CHUNK = 128
SSD_BC_DIM = SSD_GROUPS * SSD_STATE
SSD_CONV_DIM = SSD_D_INNER + 2 * SSD_BC_DIM
SSD_IN_DIM = SSD_D_INNER + SSD_CONV_DIM + 2 * SSD_HEADS
DT_MIN = 1e-3
DT_MAX = 1e-1

S5_CH = 16
S5_GROUPS = D_MODEL // S5_CH
S5_STATE = 64

FFN_DIM = 2816
N_EXPERTS = 8
TOP_K = 2
EXPERT_DIM = 3584

kernel_name = 'hybrid_ssd_s5_moe_diffusion_trunk'


def rmsnorm(x, g):
    xf = x.astype(jnp.float32)
    inv = lax.rsqrt(jnp.mean(xf * xf, axis=-1, keepdims=True) + EPS)
    return (xf * inv).astype(x.dtype) * g


def modulate(h, shift, scale):
    return h * (1.0 + scale) + shift


def swiglu(h, w_in, w_out):
    g, u = jnp.split(h @ w_in, 2, axis=-1)
    return (jax.nn.silu(g) * u) @ w_out


def centred_dwconv(u, w, b):
    pad = (w.shape[0] - 1) // 2
    out = lax.conv_general_dilated(u, w[:, None, :], window_strides=(1,), padding=[(pad, pad)],
                                   dimension_numbers=('NWC', 'WIO', 'NWC'),
                                   feature_group_count=u.shape[-1])
    return out + b


def segsum(a):
    t = a.shape[-1]
    cs = jnp.cumsum(a, axis=-1)
    seg = cs[..., :, None] - cs[..., None, :]
    return jnp.where(jnp.tril(jnp.ones((t, t), dtype=bool)), seg, -jnp.inf)


def ssd_scan(xdt, da, bm, cm, h0):
    bsz, seq, g, e, p = xdt.shape
    nc = seq // SSD_CHUNK
    xc = xdt.reshape(bsz, nc, SSD_CHUNK, g, e, p)
    bc = bm.reshape(bsz, nc, SSD_CHUNK, g, -1)
    cc = cm.reshape(bsz, nc, SSD_CHUNK, g, -1)
    a = jnp.moveaxis(da.reshape(bsz, nc, SSD_CHUNK, g, e), 2, -1)
    a_cum = jnp.cumsum(a, axis=-1)
    decay_in = jnp.exp(segsum(a))
    scores = jnp.einsum('bclgn,bcsgn->bcgls', cc, bc)
    y_diag = jnp.einsum('bcgls,bcgels,bcsgep->bclgep', scores, decay_in, xc)
    decay_to_end = jnp.exp(a_cum[..., -1:] - a_cum)
    states = jnp.einsum('bclgn,bcgel,bclgep->bcgepn', bc, decay_to_end, xc)
    states = jnp.concatenate([h0[:, None].astype(states.dtype), states], axis=1)
    chunk_tot = jnp.pad(a_cum[..., -1], ((0, 0), (1, 0), (0, 0), (0, 0)))
    decay_chunk = jnp.exp(segsum(jnp.moveaxis(chunk_tot, 1, -1)))
    new_states = jnp.einsum('bgezc,bcgepn->bzgepn', decay_chunk, states)
    prev_states, final = new_states[:, :-1], new_states[:, -1]
    y_off = jnp.einsum('bclgn,bcgepn,bcgel->bclgep', cc, prev_states, jnp.exp(a_cum))
    return (y_diag + y_off).reshape(bsz, seq, g, e, p), final


def ssd_mixer(h, w_in, conv_w, conv_b, dt_bias, a_log, d_skip, norm_w, w_out, h0_f, h0_b):
    bsz, seq, _ = h.shape
    proj = h @ w_in
    z = proj[..., :SSD_D_INNER]
    xbc = jax.nn.silu(centred_dwconv(proj[..., SSD_D_INNER:SSD_D_INNER + SSD_CONV_DIM], conv_w, conv_b))
    dt_raw = proj[..., SSD_D_INNER + SSD_CONV_DIM:]
    xs = xbc[..., :SSD_D_INNER].reshape(bsz, seq, SSD_GROUPS, SSD_HPG, SSD_HEADDIM)
    bm = xbc[..., SSD_D_INNER:SSD_D_INNER + SSD_BC_DIM].reshape(bsz, seq, SSD_GROUPS, SSD_STATE)
    cm = xbc[..., SSD_D_INNER + SSD_BC_DIM:].reshape(bsz, seq, SSD_GROUPS, SSD_STATE)
    dt = jax.nn.softplus(dt_raw.reshape(bsz, seq, 2, SSD_HEADS).astype(jnp.float32)
                         + dt_bias.astype(jnp.float32))
    a = -jnp.exp(a_log.astype(jnp.float32))
    da = (dt * a).reshape(bsz, seq, 2, SSD_GROUPS, SSD_HPG)
    dt = dt.reshape(bsz, seq, 2, SSD_GROUPS, SSD_HPG)
    flip = lambda t: jnp.flip(t, axis=1)
    y_f, h_f = ssd_scan(xs * dt[:, :, 0, ..., None], da[:, :, 0], bm, cm, h0_f)
    y_b, h_b = ssd_scan(flip(xs * dt[:, :, 1, ..., None]), flip(da[:, :, 1]), flip(bm), flip(cm), h0_b)
    y = y_f + flip(y_b) + d_skip.reshape(SSD_GROUPS, SSD_HPG, 1) * xs
    y = rmsnorm(y.reshape(bsz, seq, SSD_D_INNER) * jax.nn.silu(z), norm_w)
    return y @ w_out, h_f, h_b


def s5_discretize(lam_re, lam_im, log_step, b_re, b_im):
    step = jnp.exp(log_step)[:, None]
    mag = jnp.exp(lam_re * step)
    ar = mag * jnp.cos(lam_im * step)
    ai = mag * jnp.sin(lam_im * step)
    den = lam_re * lam_re + lam_im * lam_im
    cr = ((ar - 1.0) * lam_re + ai * lam_im) / den
    ci = (ai * lam_re - (ar - 1.0) * lam_im) / den
    bbr = cr[..., None] * b_re - ci[..., None] * b_im
    bbi = cr[..., None] * b_im + ci[..., None] * b_re
    return ar, ai, bbr, bbi


def complex_affine_combine(e1, e2):
    ar1, ai1, br1, bi1 = e1
    ar2, ai2, br2, bi2 = e2
    return (ar2 * ar1 - ai2 * ai1, ar2 * ai1 + ai2 * ar1,
            ar2 * br1 - ai2 * bi1 + br2, ar2 * bi1 + ai2 * br1 + bi2)


def s5_scan(u, ar, ai, bbr, bbi, s0r, s0i, reverse):
    seq = u.shape[1]
    bur = jnp.einsum('btgc,gpc->tbgp', u, bbr)
    bui = jnp.einsum('btgc,gpc->tbgp', u, bbi)
    first = -1 if reverse else 0
    bur = bur.at[first].add(ar * s0r - ai * s0i)
    bui = bui.at[first].add(ar * s0i + ai * s0r)
    a_r = jnp.broadcast_to(ar, (seq, 1) + ar.shape)
    a_i = jnp.broadcast_to(ai, (seq, 1) + ai.shape)
    _, _, sr, si = lax.associative_scan(complex_affine_combine, (a_r, a_i, bur, bui),
                                        reverse=reverse, axis=0)
    return sr, si


def s5_readout(sr, si, c_re, c_im):
    t, b, g, _ = sr.shape
    y = jnp.einsum('tbgp,gcp->btgc', sr, c_re) - jnp.einsum('tbgp,gcp->btgc', si, c_im)
    return y.reshape(b, t, g * S5_CH)


def s5_glu(y, w_glu, b_glu):
    a, g = jnp.split(jax.nn.gelu(y) @ w_glu + b_glu, 2, axis=-1)
    return a * jax.nn.sigmoid(g)


def raster_to_colmajor(h, rows):
    b, t, d = h.shape
    return h.reshape(b, rows, GRID_W, d).transpose(0, 2, 1, 3).reshape(b, t, d)


def colmajor_to_raster(h, rows):
    b, t, d = h.shape
    return h.reshape(b, GRID_W, rows, d).transpose(0, 2, 1, 3).reshape(b, t, d)


def s5_mixer(hx, hc, rows, lam_re, lam_im, log_step, b_re, b_im, c_re, c_im, d_skip, w_glu, b_glu, ctx_needed):
    bsz, seq, _ = hx.shape
    hx_col = raster_to_colmajor(hx, rows)
    u_x = hx_col.reshape(bsz, seq, S5_GROUPS, S5_CH)
    u_c = hc.reshape(bsz, hc.shape[1], S5_GROUPS, S5_CH)
    y_x = d_skip * hx_col
    ys_c = []
    for d in range(2):
        rev = d == 1
        ar, ai, bbr, bbi = s5_discretize(lam_re[d], lam_im[d], log_step[d], b_re[d], b_im[d])
        zeros = jnp.zeros((bsz, S5_GROUPS, S5_STATE), hx.dtype)
        cr, ci = s5_scan(u_c, ar, ai, bbr, bbi, zeros, zeros, rev)
        end = 0 if rev else -1
        sr, si = s5_scan(u_x, ar, ai, bbr, bbi, cr[end], ci[end], rev)
        y_x = y_x + s5_readout(sr, si, c_re[d], c_im[d])
        if ctx_needed:
            ys_c.append(s5_readout(cr, ci, c_re[d], c_im[d]))
    out_x = colmajor_to_raster(s5_glu(y_x, w_glu, b_glu), rows)
    out_c = s5_glu(d_skip * hc + ys_c[0] + ys_c[1], w_glu, b_glu) if ctx_needed else None
    return out_x, out_c


def moe_swiglu(h, router_w, router_b, w_in, w_out):
    bsz, seq, d = h.shape
    t = h.reshape(-1, d)
    logits = (t @ router_w + router_b).astype(jnp.float32)
    top_val, top_idx = lax.top_k(logits, TOP_K)
    gates = jax.nn.softmax(top_val, axis=-1)
    combine = jnp.sum(jax.nn.one_hot(top_idx, N_EXPERTS, dtype=jnp.float32) * gates[..., None], axis=1)
    out = jnp.zeros(t.shape, jnp.float32)
    for e in range(N_EXPERTS):
        out = out + combine[:, e:e + 1] * swiglu(t, w_in[e], w_out[e])
    return out.astype(h.dtype).reshape(bsz, seq, d)


def setup_inputs(seed: int = 0) -> dict:
    key = jax.random.key(seed)
    ks = iter(jax.random.split(key, 40))

    def nrm(shape, scale):
        return scale * jax.random.normal(next(ks), shape, jnp.float32)

    def unif(shape, lo, hi):
        return jax.random.uniform(next(ks), shape, jnp.float32, lo, hi)

    n_a = (DEPTH + 1) // 2
    n_b = DEPTH // 2
    d = D_MODEL
    lam_im0 = jnp.pi * jnp.arange(S5_STATE, dtype=jnp.float32)
    inp = {}
    inp['x'] = nrm((BATCH, SEQ, d), 1.0)
    inp['c'] = nrm((BATCH, d), 1.0)
    inp['ctx'] = nrm((BATCH, CTX_LEN, d), 1.0)
    inp['c_ctx'] = nrm((d,), 1.0)
    inp['ada_w'] = nrm((DEPTH, d, N_MOD * d), 0.5 * d ** -0.5)
    inp['ada_b'] = nrm((DEPTH, N_MOD * d), 0.02)
    inp['norm_mix'] = 1.0 + nrm((DEPTH, d), 0.02)
    inp['norm_ffn'] = 1.0 + nrm((DEPTH, d), 0.02)
    inp['ssd_w_in'] = nrm((n_a, d, SSD_IN_DIM), d ** -0.5)
    inp['ssd_conv_w'] = nrm((n_a, SSD_CONV, SSD_CONV_DIM), SSD_CONV ** -0.5)
    inp['ssd_conv_b'] = nrm((n_a, SSD_CONV_DIM), 0.02)
    dt0 = jnp.exp(unif((n_a, 2, SSD_HEADS), math.log(DT_MIN), math.log(DT_MAX)))
    inp['ssd_dt_bias'] = dt0 + jnp.log(-jnp.expm1(-dt0))
    inp['ssd_a_log'] = jnp.log(unif((n_a, 2, SSD_HEADS), 1.0, 16.0))
    inp['ssd_d'] = 1.0 + nrm((n_a, SSD_HEADS), 0.02)
    inp['ssd_norm'] = 1.0 + nrm((n_a, SSD_D_INNER), 0.02)
    inp['ssd_w_out'] = nrm((n_a, SSD_D_INNER, d), SSD_D_INNER ** -0.5)
    inp['s5_lam_re'] = -0.5 + nrm((n_b, 2, S5_GROUPS, S5_STATE), 0.01)
    inp['s5_lam_im'] = lam_im0 + nrm((n_b, 2, S5_GROUPS, S5_STATE), 0.01)
    inp['s5_log_step'] = unif((n_b, 2, S5_GROUPS), math.log(DT_MIN), math.log(DT_MAX))
    inp['s5_b_re'] = nrm((n_b, 2, S5_GROUPS, S5_STATE, S5_CH), (2 * S5_CH) ** -0.5)
    inp['s5_b_im'] = nrm((n_b, 2, S5_GROUPS, S5_STATE, S5_CH), (2 * S5_CH) ** -0.5)
    inp['s5_c_re'] = nrm((n_b, 2, S5_GROUPS, S5_CH, S5_STATE), S5_STATE ** -0.5)
    inp['s5_c_im'] = nrm((n_b, 2, S5_GROUPS, S5_CH, S5_STATE), S5_STATE ** -0.5)
    inp['s5_d'] = nrm((n_b, d), 0.5)
    inp['s5_w_glu'] = nrm((n_b, d, 2 * d), d ** -0.5)
    inp['s5_b_glu'] = nrm((n_b, 2 * d), 0.02)
    inp['ffn_w_in'] = nrm((n_a, d, 2 * FFN_DIM), d ** -0.5)
    inp['ffn_w_out'] = nrm((n_a, FFN_DIM, d), FFN_DIM ** -0.5)
    inp['moe_router_w'] = nrm((n_b, d, N_EXPERTS), d ** -0.5)
    inp['moe_router_b'] = nrm((n_b, N_EXPERTS), 0.01)
    inp['moe_w_in'] = nrm((n_b, N_EXPERTS, d, 2 * EXPERT_DIM), d ** -0.5)
    inp['moe_w_out'] = nrm((n_b, N_EXPERTS, EXPERT_DIM, d), EXPERT_DIM ** -0.5)
    inp['norm_final'] = 1.0 + nrm((d,), 0.02)
    return inp


def reference(x, c, ctx, c_ctx, ada_w, ada_b, norm_mix, norm_ffn,
              ssd_w_in, ssd_conv_w, ssd_conv_b, ssd_dt_bias, ssd_a_log, ssd_d, ssd_norm, ssd_w_out,
              s5_lam_re, s5_lam_im, s5_log_step, s5_b_re, s5_b_im, s5_c_re, s5_c_im, s5_d,
              s5_w_glu, s5_b_glu, ffn_w_in, ffn_w_out, moe_router_w, moe_router_b, moe_w_in, moe_w_out,
              norm_final):
    bsz, seq, _ = x.shape
    rows = seq // GRID_W
    for i in range(DEPTH):
        j = i // N_MIXERS
        ctx_needed = i < DEPTH - 1
        mod_x = jnp.split((jax.nn.silu(c) @ ada_w[i] + ada_b[i])[:, None, :], N_MOD, axis=-1)
        mod_c = jnp.split((jax.nn.silu(c_ctx) @ ada_w[i] + ada_b[i])[None, None, :], N_MOD, axis=-1)
        hx = modulate(rmsnorm(x, norm_mix[i]), mod_x[0], mod_x[1])
        hc = modulate(rmsnorm(ctx, norm_mix[i]), mod_c[0], mod_c[1])
        if i % N_MIXERS == 0:
            prm = (ssd_w_in[j], ssd_conv_w[j], ssd_conv_b[j], ssd_dt_bias[j], ssd_a_log[j],
                   ssd_d[j], ssd_norm[j], ssd_w_out[j])
            zeros = jnp.zeros((bsz, SSD_GROUPS, SSD_HPG, SSD_HEADDIM, SSD_STATE), x.dtype)
            out_c, h_f, h_b = ssd_mixer(hc, *prm, zeros, zeros)
            out_x, _, _ = ssd_mixer(hx, *prm, h_f, h_b)
        else:
            out_x, out_c = s5_mixer(hx, hc, rows, s5_lam_re[j], s5_lam_im[j], s5_log_step[j],
                                    s5_b_re[j], s5_b_im[j], s5_c_re[j], s5_c_im[j], s5_d[j],
                                    s5_w_glu[j], s5_b_glu[j], ctx_needed)
        x = x + mod_x[2] * out_x
        if ctx_needed:
            ctx = ctx + mod_c[2] * out_c

        def channel_mixer(h):
            if i % 2 == 0:
                return swiglu(h, ffn_w_in[j], ffn_w_out[j])
            return moe_swiglu(h, moe_router_w[j], moe_router_b[j], moe_w_in[j], moe_w_out[j])

        x = x + mod_x[5] * channel_mixer(modulate(rmsnorm(x, norm_ffn[i]), mod_x[3], mod_x[4]))
        if ctx_needed:
            ctx = ctx + mod_c[5] * channel_mixer(modulate(rmsnorm(ctx, norm_ffn[i]), mod_c[3], mod_c[4]))
    return rmsnorm(x, norm_final)
```

```python
import numpy as np
from contextlib import ExitStack
import concourse.bass as bass
import concourse.mybir as mybir
from concourse.bass_utils import run_bass_kernel_spmd

F32 = mybir.dt.float32
BF16 = mybir.dt.bfloat16
I32 = mybir.dt.int32
AF = mybir.ActivationFunctionType
ALU = mybir.AluOpType
AX = mybir.AxisListType
AP = bass.AP

D = 1024
NB = 2
T = 2048
TC = 256
EPS = 1e-6
FFN = 2816
NEXP = 8
EDIM = 3584


class Buf:
    __slots__ = ("name", "w", "r", "ps")

    def __init__(self, name="", ps=False):
        self.name = name
        self.w = {}
        self.r = {}
        self.ps = ps


class KB:
    RING = 8

    def __init__(self, nc, es):
        self.nc = nc
        self.es = es
        self.E = dict(pe=nc.tensor, act=nc.scalar, dve=nc.vector, pool=nc.gpsimd, sp=nc.sync)
        self.sem = {}
        self.cnt = {}
        for e in self.E:
            self.sem[e] = es.enter_context(nc.semaphore("s_" + e))
            self.cnt[e] = 0
        self.ring = {}
        self.ring_i = {}
        for q in ("sp", "pool", "act"):
            self.ring[q] = []
            for i in range(self.RING):
                key = ("d", q, i)
                self.sem[key] = es.enter_context(nc.semaphore("d_%s%d" % (q, i)))
                self.cnt[key] = 0
                self.ring[q].append(key)
            self.ring_i[q] = 0
        self.known = {e: {} for e in self.E}
        self.n_wait = 0

    def _deps(self, eng, reads, writes):
        need = {}

        def add(sk, v):
            if v > need.get(sk, 0):
                need[sk] = v

        for b in reads:
            for sk, v in b.w.items():
                if sk == eng and eng == "pe":
                    continue
                add(sk, v)
        for b in writes:
            for sk, v in b.r.items():
                if sk == eng:
                    continue
                add(sk, v)
            for sk, v in b.w.items():
                if sk == eng:
                    continue
                add(sk, v)
        kn = self.known[eng]
        out = []
        for sk, v in need.items():
            if kn.get(sk, 0) >= v:
                continue
            kn[sk] = v
            out.append((sk, v))
        return out

    def _emit_waits(self, eng, waits):
        E = self.E[eng]
        for sk, v in waits:
            E.wait_ge(self.sem[sk], v)
            self.n_wait += 1

    def _record(self, ev, reads, writes):
        sk, v = ev
        for b in reads:
            if v > b.r.get(sk, 0):
                b.r[sk] = v
        for b in writes:
            if b.r:
                b.w = {sk: v}
                b.r = {}
            else:
                if v > b.w.get(sk, 0):
                    b.w[sk] = v

    def op(self, eng, fn, reads=(), writes=(), inc=True):
        if any(b.ps for b in reads):
            writes = list(writes) + [b for b in reads if b.ps]
            reads = [b for b in reads if not b.ps]
        self._emit_waits(eng, self._deps(eng, reads, writes))
        ins = fn(self.E[eng])
        if inc:
            self.cnt[eng] += 1
            ins.then_inc(self.sem[eng], 1)
            ev = (eng, self.cnt[eng])
        else:
            ev = (eng, self.cnt[eng] + 1)
        self._record(ev, reads, writes)
        return ins

    def dma(self, q, out, in_, reads=(), writes=()):
        key = self.ring[q][self.ring_i[q] % self.RING]
        self.ring_i[q] += 1
        waits = self._deps(q, reads, writes)
        prev = self.cnt[key]
        if prev > 0 and self.known[q].get(key, 0) < prev:
            self.known[q][key] = prev
            waits.append((key, prev))
        self._emit_waits(q, waits)
        ins = self.E[q].dma_start(out=out, in_=in_)
        self.cnt[key] = prev + 16
        ins.then_inc(self.sem[key], 16)
        self._record((key, prev + 16), reads, writes)
        return ins

    def barrier(self):
        for e in self.E:
            waits = []
            for sk, v in self.cnt.items():
                if v == 0 or sk == e and e == "pe":
                    continue
                if self.known[e].get(sk, 0) >= v:
                    continue
                self.known[e][sk] = v
                waits.append((sk, v))
            self._emit_waits(e, waits)

    def final_wait(self):
        waits = []
        for sk, v in self.cnt.items():
            if v and self.known["sp"].get(sk, 0) < v:
                self.known["sp"][sk] = v
                waits.append((sk, v))
        self._emit_waits("sp", waits)


def rev_ap(ap, n):
    a = ap.ap
    assert len(a) == 2 and a[1][1] == n
    return AP(ap.tensor, ap.offset + (n - 1) * a[1][0], [list(a[0]), [-a[1][0], n]])


class Ctx:
    pass


def build_program(phases, kinds):
    nc = bass.Bass("TRN2", target_bir_lowering=False)
    g = Ctx()
    g.nc = nc
    g.phases = phases
    g.kinds = kinds

    def din(name, shape, dt=F32):
        return nc.dram_tensor(name, list(shape), dt, kind="ExternalInput").ap()

    def dmid(name, shape, dt=F32):
        kind = {"in": "ExternalInput", "out": "ExternalOutput", "int": "Internal"}[kinds.get(name, "int")]
        return nc.dram_tensor(name, list(shape), dt, kind=kind).ap()

    g.inputs = {}
    SH = dict(x=[NB, T, D], ctx=[NB, TC, D], cT=[D, 3], ada_w=[2, D, 6 * D], ada_b=[2, 6 * D],
              norm_mix=[2, D], norm_ffn=[2, D], ffn_w_in=[D, 2 * FFN], ffn_w_out=[FFN, D],
              moe_router_w=[D, NEXP], moe_router_b=[1, NEXP], moe_w_in=[NEXP, D, 2 * EDIM],
              moe_w_out=[NEXP, EDIM, D], norm_final=[1, D], ident=[128, 128],
              ssd_w_in=[D, 5184], convT=[3072, 6], ssd_dt_bias=[1, 64], ssd_a_log=[1, 64], ssd_d=[1, 32],
              ssd_norm=[1, 2048], ssd_w_out=[2048, D], cmat=[5, 128, 128],
              s5_lamT=[2, 3, 128, 32], s5_b=[2, 2, 4096, 16], s5_cT=[2, 2, 128, 32, 16], m8=[2, 128, 128],
              s5_d=[1, D], s5_w_glu=[D, 2 * D], s5_b_glu=[1, 2 * D])
    g.SH = SH

    def gin(name):
        if name not in g.inputs:
            g.inputs[name] = din(name, SH[name])
        return g.inputs[name]
    g.gin = gin
    g.x = gin("x")
    g.ctx = gin("ctx")
    g.mod = dmid("mod", [2, 3, 6 * D])
    g.x1 = dmid("x1", [NB, T, D])
    g.c1 = dmid("c1", [NB, TC, D])
    g.x2 = dmid("x2", [NB, T, D])
    g.c2 = dmid("c2", [NB, TC, D])
    g.x3 = dmid("x3", [NB, T, D])
    NTK = T + TC
    g.xbcT = dmid("xbcT", [NB, 3072, NTK], BF16)
    g.zs = dmid("zs", [NB, NTK, 2048], BF16)
    g.dd = dmid("dd", [NB, NTK, 128])
    g.yf = dmid("yf", [NB, NTK, 2048])
    g.s5w = dmid("s5w", [2, 32, 128, 6, 128], BF16)
    g.etab = dmid("etab", [2, 2, 128, 32, 288])
    g.rho = dmid("rho", [2, 128, 32])
    g.out = nc.dram_tensor("out", [NB, T, D], F32, kind="ExternalOutput").ap()
    g.bufs = {n: Buf(n) for n in ("x", "ctx", "mod", "x1", "c1", "x2", "c2", "x3", "out", "xbcT", "zs", "dd", "yf", "s5w", "etab", "rho")}

    with ExitStack() as es:
        k = KB(nc, es)
        g.k = k
        g.ps = [es.enter_context(nc.psum_tensor("ps%d" % i, [128, 512], F32)) for i in range(8)]
        g.psb = [Buf("ps%d" % i, ps=True) for i in range(8)]
        for ph in phases:
            with ExitStack() as pes:
                g.pes = pes
                {"ada": phase_ada, "ffn": phase_ffn, "moe": phase_moe, "ssd": phase_ssd, "s5": phase_s5}[ph](g)
                k.barrier()
        k.final_wait()
    g.kinds = kinds
    return nc, g


def sbt(g, name, shape, dt):
    return g.pes.enter_context(g.nc.sbuf_tensor(name, list(shape), dt))


def bcast_rows(ap_row, nparts):
    a = ap_row.ap
    return AP(ap_row.tensor, ap_row.offset, [[0, nparts]] + [list(x) for x in a[1:]])


def phase_ada(g):
    k, nc = g.k, g.nc
    cT = sbt(g, "a_cT", [128, 8, 3], F32)
    cs = sbt(g, "a_cs", [128, 8, 3], BF16)
    row = sbt(g, "a_row", [3, 6 * D], F32)
    bias = sbt(g, "a_bias", [3, 6 * D], F32)
    nrm = sbt(g, "a_nrm", [3, D], F32)
    wts = [sbt(g, "a_w%d" % i, [128, 8, 512], BF16) for i in range(2)]
    b_cT, b_cs, b_row, b_bias, b_nrm = Buf(), Buf(), Buf(), Buf(), Buf()
    b_w = [Buf(), Buf()]
    k.dma("sp", cT[:], g.gin("cT").rearrange("(k p) m -> p k m", p=128), writes=[b_cT])
    k.op("act", lambda e: e.activation(out=cs[:], in_=cT[:], func=AF.Silu), reads=[b_cT], writes=[b_cs])
    it = 0
    for layer in range(2):
        k.dma("sp", bias[:], bcast_rows(g.gin("ada_b")[layer:layer + 1, :], 3), writes=[b_bias])
        for j in range(12):
            w = wts[it % 2]
            bw = b_w[it % 2]
            it += 1
            k.dma("pool", w[:], g.gin("ada_w")[layer, :, j * 512:(j + 1) * 512].rearrange("(k p) n -> p k n", p=128), writes=[bw])
            ps = g.ps[it % 2]
            pb = g.psb[it % 2]
            for kk in range(8):
                k.op("pe", lambda e, kk=kk: e.matmul(ps[0:3, :], lhsT=cs[:, kk, :], rhs=w[:, kk, :],
                                                     start=(kk == 0), stop=(kk == 7)),
                     reads=[b_cs, bw], writes=[pb], inc=(kk == 7))
            k.op("dve", lambda e: e.tensor_tensor(out=row[:, j * 512:(j + 1) * 512], in0=ps[0:3, :],
                                                   in1=bias[:, j * 512:(j + 1) * 512], op=ALU.add),
                 reads=[pb, b_bias], writes=[b_row])
        for slot, nw in ((1, g.gin("norm_mix")), (4, g.gin("norm_ffn"))):
            k.dma("sp", nrm[:], bcast_rows(nw[layer:layer + 1, :], 3), writes=[b_nrm])
            k.op("dve", lambda e, slot=slot: e.scalar_tensor_tensor(
                out=row[:, slot * D:(slot + 1) * D], in0=row[:, slot * D:(slot + 1) * D], scalar=1.0,
                in1=nrm[:], op0=ALU.add, op1=ALU.mult), reads=[b_row, b_nrm], writes=[b_row])
        k.dma("sp", g.mod[layer], row[:], reads=[b_row], writes=[g.bufs["mod"]])


class NormSet:
    def __init__(self, g, pfx, nbuf=2):
        self.g = g
        self.junk = sbt(g, pfx + "junk", [128, 2 * D], BF16)
        self.b_junk = Buf()
        self.st = [sbt(g, pfx + "st%d" % i, [128, 4], F32) for i in range(nbuf)]
        self.b_st = [Buf() for _ in range(nbuf)]
        self.tmp = [sbt(g, pfx + "tmp0", [128, D], F32)] * nbuf
        self.b_tmp = [Buf()] * nbuf
        self.hb = [sbt(g, pfx + "hb%d" % i, [128, D], BF16) for i in range(nbuf)]
        self.b_hb = [Buf() for _ in range(nbuf)]
        self.ident = sbt(g, pfx + "ident", [128, 128], BF16)
        self.b_ident = Buf()
        g.k.dma("pool", self.ident[:], g.gin("ident")[:, :], writes=[self.b_ident])
        self.i = 0
        self.nbuf = nbuf


def rstd_of(g, ns, i, xt, b_xt, dim=D):
    k = g.k
    P = xt.ap[0][1]
    st, b_st = ns.st[i], ns.b_st[i]
    k.op("act", lambda e: e.activation(out=ns.junk[0:P, 0:dim], in_=xt, func=AF.Square, accum_out=st[0:P, 0:1]),
         reads=[b_xt], writes=[ns.b_junk, b_st])
    k.op("dve", lambda e: e.tensor_scalar(out=st[0:P, 1:2], in0=st[0:P, 0:1], scalar1=1.0 / dim, scalar2=EPS,
                                           op0=ALU.mult, op1=ALU.add), reads=[b_st], writes=[b_st])
    k.op("act", lambda e: e.sqrt(out=st[0:P, 2:3], in_=st[0:P, 1:2]), reads=[b_st], writes=[b_st])
    k.op("dve", lambda e: e.reciprocal(out=st[0:P, 3:4], in_=st[0:P, 2:3]), reads=[b_st], writes=[b_st])
    return st[0:P, 3:4], b_st


def norm_mod_T(g, ns, xt, b_xt, gbc, shbc, b_mod, hT_dst, b_hT, ps, pb):
    k = g.k
    i = ns.i % ns.nbuf
    ns.i += 1
    rstd, b_st = rstd_of(g, ns, i, xt, b_xt)
    tmp, b_tmp, hb, b_hb = ns.tmp[i], ns.b_tmp[i], ns.hb[i], ns.b_hb[i]
    k.op("dve", lambda e: e.scalar_tensor_tensor(out=tmp[:], in0=xt, scalar=rstd, in1=gbc,
                                                  op0=ALU.mult, op1=ALU.mult),
         reads=[b_xt, b_st, b_mod], writes=[b_tmp])
    k.op("pool", lambda e: e.tensor_tensor(out=hb[:], in0=tmp[:], in1=shbc, op=ALU.add),
         reads=[b_tmp, b_mod], writes=[b_hb])
    psv = ps.bitcast(BF16)
    for kk in range(8):
        k.op("pe", lambda e, kk=kk: e.transpose(out=psv[:, kk * 128:(kk + 1) * 128],
                                                in_=hb[:, kk * 128:(kk + 1) * 128], identity=ns.ident[:]),
             reads=[b_hb, ns.b_ident], writes=[pb], inc=(kk == 7))
    k.op("act", lambda e: e.copy(out=hT_dst, in_=psv[:, 0:1024].rearrange("p (k t) -> p k t", t=128)),
         reads=[pb], writes=[b_hT])
    return hb, b_hb


class ModBC:
    def __init__(self, g, pfx, layer, slots):
        self.g, self.layer, self.slots = g, layer, slots
        self.t = [sbt(g, "%s_m%d" % (pfx, s), [128, D], F32) for s in slots]
        self.b = Buf()
        self.cond = None

    def get(self, cond):
        g = self.g
        if cond != self.cond:
            self.cond = cond
            for t, s in zip(self.t, self.slots):
                g.k.dma("sp", t[:], bcast_rows(g.mod[self.layer, cond:cond + 1, s * D:(s + 1) * D], 128),
                        reads=[g.bufs["mod"]], writes=[self.b])
        return self.t, self.b


def seq_list(g, xsrc, csrc, xdst, cdst, with_ctx=True):
    L = []
    for b in range(NB):
        if with_ctx:
            L.append((g.__dict__[csrc][b], g.__dict__[cdst][b], 2, TC, g.bufs.get(csrc), g.bufs.get(cdst)))
        L.append((g.__dict__[xsrc][b], g.__dict__[xdst][b], b, T, g.bufs.get(xsrc), g.bufs.get(xdst)))
    return L


def phase_ffn(g):
    k, nc = g.k, g.nc
    src_x, src_c = ("x1", "c1") if "ssd" in g.phases else ("x", "ctx")
    NT = 256
    NF = FFN // 128
    w1 = sbt(g, "f_w1", [128, 8, 2 * FFN], BF16)
    w2 = sbt(g, "f_w2", [128, NF, D], BF16)
    b_w1, b_w2 = Buf(), Buf()
    for kk in range(8):
        k.dma("pool", w1[:, kk, :], g.gin("ffn_w_in")[kk * 128:(kk + 1) * 128, :], writes=[b_w1])
    for f in range(NF):
        k.dma("pool", w2[:, f, :], g.gin("ffn_w_out")[f * 128:(f + 1) * 128, :], writes=[b_w2])
    ns = NormSet(g, "f_")
    hT = [sbt(g, "f_hT%d" % i, [128, 8, NT], BF16) for i in range(2)]
    b_hT = [Buf(), Buf()]
    aT = [sbt(g, "f_aT0", [128, NF, NT], BF16)] * 2
    b_aT = [Buf()] * 2
    xt = [sbt(g, "f_xt%d" % i, [128, D], F32) for i in range(4)]
    b_xt = [Buf() for _ in range(4)]
    sg = [sbt(g, "f_sg%d" % i, [128, NT], F32) for i in range(2)]
    b_sg = [Buf(), Buf()]
    sg_o = [sbt(g, "f_o%d" % i, [128, 512], F32) for i in range(2)]
    b_sgo = [Buf(), Buf()]
    mods = ModBC(g, "f", 0, (3, 4, 5))
    grp = 0
    oi = 0
    fi = 0
    LIM, STAGE = 1000, 9
    for (src, dst, cond, ntok, bsrc, bdst) in seq_list(g, src_x, src_c, "x2", "c2")[:LIM]:
        (shbc, gbc, gatebc), b_mod = mods.get(cond)
        for t0 in range(0, ntok, NT):
            hi = grp % 2
            for j in range(NT // 128):
                xi = (grp % 2) * 2 + j
                k.dma("sp", xt[xi][:], src[t0 + j * 128:t0 + (j + 1) * 128, :], reads=[bsrc], writes=[b_xt[xi]])
                norm_mod_T(g, ns, xt[xi][:], b_xt[xi], gbc[:], shbc[:], b_mod,
                           hT[hi][:, :, j * 128:(j + 1) * 128], b_hT[hi], g.ps[4 + j], g.psb[4 + j])
            for f in range(NF if STAGE >= 2 else 0):
                pg, pu = g.ps[(fi % 2) * 2], g.ps[(fi % 2) * 2 + 1]
                bg, bu = g.psb[(fi % 2) * 2], g.psb[(fi % 2) * 2 + 1]
                si = fi % 2
                fi += 1
                for kk in range(8):
                    k.op("pe", lambda e, kk=kk: e.matmul(pg[:, 0:NT], lhsT=w1[:, kk, f * 128:(f + 1) * 128],
                                                         rhs=hT[hi][:, kk, :], start=(kk == 0), stop=(kk == 7)),
                         reads=[b_w1, b_hT[hi]], writes=[bg], inc=(kk == 7))
                for kk in range(8):
                    k.op("pe", lambda e, kk=kk: e.matmul(pu[:, 0:NT], lhsT=w1[:, kk, FFN + f * 128:FFN + (f + 1) * 128],
                                                         rhs=hT[hi][:, kk, :], start=(kk == 0), stop=(kk == 7)),
                         reads=[b_w1, b_hT[hi]], writes=[bu], inc=(kk == 7))
                k.op("act", lambda e: e.activation(out=sg[si][:], in_=pg[:, 0:NT], func=AF.Silu),
                     reads=[bg], writes=[b_sg[si]])
                k.op("dve", lambda e: e.tensor_tensor(out=aT[hi][:, f, :], in0=sg[si][:], in1=pu[:, 0:NT], op=ALU.mult),
                     reads=[b_sg[si], bu], writes=[b_aT[hi]])
            for j in range(NT // 128):
                xi = (grp % 2) * 2 + j
                o = sg_o[oi % 2]
                bo = b_sgo[oi % 2]
                oi += 1
                for half in range(2):
                    po, pbo = g.ps[6 + half], g.psb[6 + half]
                    for f in range(NF):
                        k.op("pe", lambda e, f=f: e.matmul(po[:, :], lhsT=aT[hi][:, f, j * 128:(j + 1) * 128],
                                                           rhs=w2[:, f, half * 512:(half + 1) * 512],
                                                           start=(f == 0), stop=(f == NF - 1)),
                             reads=[b_aT[hi], b_w2], writes=[pbo], inc=(f == NF - 1))
                    hs = slice(half * 512, (half + 1) * 512)
                    o = sg_o[oi % 2]
                    bo = b_sgo[oi % 2]
                    oi += 1
                    k.op("dve", lambda e: e.tensor_tensor(out=o[:], in0=po[:, :], in1=gatebc[:, hs], op=ALU.mult),
                         reads=[pbo, b_mod], writes=[bo])
                    k.op("pool", lambda e: e.tensor_tensor(out=xt[xi][:, hs], in0=o[:], in1=xt[xi][:, hs], op=ALU.add),
                         reads=[bo, b_xt[xi]], writes=[b_xt[xi]])
                k.dma("sp", dst[t0 + j * 128:t0 + (j + 1) * 128, :], xt[xi][:], reads=[b_xt[xi]], writes=[bdst])
            grp += 1


NTK = T + TC


def phase_ssd(g):
    with ExitStack() as pes:
        g.pes = pes
        ssd_in(g)
        g.k.barrier()
    with ExitStack() as pes:
        g.pes = pes
        ssd_scan(g)
        g.k.barrier()


def ssd_in(g):
    k, nc = g.k, g.nc
    w = sbt(g, "s_w", [128, 8, 5184], BF16)
    b_w = Buf()
    for kk in range(8):
        k.dma("pool", w[:, kk, :], g.gin("ssd_w_in")[kk * 128:(kk + 1) * 128, :], writes=[b_w])
    cw = sbt(g, "s_cw", [128, 24, 6], F32)
    b_cw = Buf()
    k.dma("sp", cw[:], g.gin("convT").rearrange("(c p) k -> p c k", p=128), writes=[b_cw])
    dtb = sbt(g, "s_dtb", [128, 64], F32)
    abc = sbt(g, "s_abc", [128, 64], F32)
    b_dtb, b_abc = Buf(), Buf()
    k.dma("sp", dtb[:], bcast_rows(g.gin("ssd_dt_bias")[0:1, :], 128), writes=[b_dtb])
    k.dma("sp", abc[:], bcast_rows(g.gin("ssd_a_log")[0:1, :], 128), writes=[b_abc])
    k.op("act", lambda e: e.activation(out=abc[:], in_=abc[:], func=AF.Exp), reads=[b_abc], writes=[b_abc])
    k.op("dve", lambda e: e.tensor_scalar(out=abc[:], in0=abc[:], scalar1=-1.0, scalar2=None, op0=ALU.mult),
         reads=[b_abc], writes=[b_abc])
    ns = NormSet(g, "s_")
    hT = sbt(g, "s_hT", [128, 8, NTK], BF16)
    b_hT = Buf()
    xt = [sbt(g, "s_xt%d" % i, [128, D], F32) for i in range(2)]
    b_xt = [Buf(), Buf()]
    pre = [sbt(g, "s_pre%d" % i, [128, 2312], F32) for i in range(2)]
    b_pre = [Buf(), Buf()]
    for p_ in pre:
        k.op("pool", lambda e, p_=p_: e.memset(p_[:], 0.0), writes=[b_pre[0], b_pre[1]])
    accv = sbt(g, "s_acc", [128, NTK], F32)
    b_accv = Buf()
    xo = [sbt(g, "s_xo%d" % i, [128, NTK], BF16) for i in range(2)]
    b_xo = [Buf(), Buf()]
    zt = [sbt(g, "s_zt%d" % i, [128, 2048], BF16) for i in range(2)]
    b_zt = [Buf(), Buf()]
    ddt = [sbt(g, "s_dd%d" % i, [128, 192], F32) for i in range(2)]
    b_ddt = [Buf(), Buf()]
    mods = ModBC(g, "s", 0, (0, 1))
    xi = 0
    pi = 0
    for b in range(NB):
        for (src, cond, ntok, off, bsrc) in ((g.ctx[b], 2, TC, 0, g.bufs["ctx"]), (g.x[b], b, T, TC, g.bufs["x"])):
            (shbc, gbc), b_mod = mods.get(cond)
            for j in range(ntok // 128):
                x_ = xt[xi % 2]
                bx = b_xt[xi % 2]
                k.dma("sp", x_[:], src[j * 128:(j + 1) * 128, :], reads=[bsrc], writes=[bx])
                norm_mod_T(g, ns, x_[:], bx, gbc[:], shbc[:], b_mod,
                           hT[:, :, off + j * 128:off + (j + 1) * 128], b_hT, g.ps[6 + xi % 2], g.psb[6 + xi % 2])
                xi += 1
        for ct in range(24):
            pr, bpr = pre[ct % 2], b_pre[ct % 2]
            for (c0, c1) in ((0, 256), (256, 768), (768, 1280), (1280, 1792), (1792, 2304)):
                ps, pb = g.ps[pi % 4], g.psb[pi % 4]
                pi += 1
                n = c1 - c0
                for kk in range(8):
                    k.op("pe", lambda e, kk=kk: e.matmul(ps[:, 0:n], lhsT=w[:, kk, 2048 + ct * 128:2048 + (ct + 1) * 128],
                                                         rhs=hT[:, kk, c0:c1], start=(kk == 0), stop=(kk == 7)),
                         reads=[b_w, b_hT], writes=[pb], inc=(kk == 7))
                po = 2 + c0 if c0 < 256 else c0 + 6
                k.op("act", lambda e: e.copy(out=pr[:, po:po + n], in_=ps[:, 0:n]), reads=[pb], writes=[bpr])
            for (a0, n, p0) in ((0, 256, 0), (256, 2048, 260)):
                k.op("dve", lambda e: e.tensor_scalar(out=accv[:, a0:a0 + n], in0=pr[:, p0:p0 + n],
                                                       scalar1=cw[:, ct, 0:1], scalar2=None, op0=ALU.mult),
                     reads=[bpr, b_cw], writes=[b_accv])
                for q in range(1, 5):
                    k.op("dve", lambda e, q=q: e.scalar_tensor_tensor(out=accv[:, a0:a0 + n], in0=pr[:, p0 + q:p0 + q + n],
                                                                       scalar=cw[:, ct, q:q + 1], in1=accv[:, a0:a0 + n],
                                                                       op0=ALU.mult, op1=ALU.add),
                         reads=[bpr, b_cw, b_accv], writes=[b_accv])
            o_, bo = xo[ct % 2], b_xo[ct % 2]
            k.op("act", lambda e: e.activation(out=o_[:], in_=accv[:], func=AF.Silu, bias=cw[:, ct, 5:6]),
                 reads=[b_accv, b_cw], writes=[bo])
            k.dma("sp", g.xbcT[b, ct * 128:(ct + 1) * 128, :], o_[:], reads=[bo], writes=[g.bufs["xbcT"]])
        for j in range(NTK // 128):
            z_, bz = zt[j % 2], b_zt[j % 2]
            for n4 in range(4):
                ps, pb = g.ps[pi % 4], g.psb[pi % 4]
                pi += 1
                for kk in range(8):
                    k.op("pe", lambda e, kk=kk: e.matmul(ps[:, :], lhsT=hT[:, kk, j * 128:(j + 1) * 128],
                                                         rhs=w[:, kk, n4 * 512:(n4 + 1) * 512], start=(kk == 0), stop=(kk == 7)),
                         reads=[b_w, b_hT], writes=[pb], inc=(kk == 7))
                k.op("act", lambda e: e.activation(out=z_[:, n4 * 512:(n4 + 1) * 512], in_=ps[:, :], func=AF.Silu),
                     reads=[pb], writes=[bz])
            k.dma("sp", g.zs[b, j * 128:(j + 1) * 128, :], z_[:], reads=[bz], writes=[g.bufs["zs"]])
            ps, pb = g.ps[pi % 4], g.psb[pi % 4]
            pi += 1
            for kk in range(8):
                k.op("pe", lambda e, kk=kk: e.matmul(ps[:, 0:64], lhsT=hT[:, kk, j * 128:(j + 1) * 128],
                                                     rhs=w[:, kk, 5120:5184], start=(kk == 0), stop=(kk == 7)),
                     reads=[b_w, b_hT], writes=[pb], inc=(kk == 7))
            d_, bd = ddt[j % 2], b_ddt[j % 2]
            k.op("dve", lambda e: e.tensor_tensor(out=d_[:, 128:192], in0=ps[:, 0:64], in1=dtb[:], op=ALU.add),
                 reads=[pb, b_dtb], writes=[bd])
            k.op("act", lambda e: e.activation(out=d_[:, 128:192], in_=d_[:, 128:192], func=AF.Exp), reads=[bd], writes=[bd])
            k.op("act", lambda e: e.activation(out=d_[:, 0:64], in_=d_[:, 128:192], func=AF.Ln, bias=1.0), reads=[bd], writes=[bd])
            k.op("dve", lambda e: e.tensor_tensor(out=d_[:, 64:128], in0=d_[:, 0:64], in1=abc[:], op=ALU.mult),
                 reads=[bd, b_abc], writes=[bd])
            k.dma("sp", g.dd[b, j * 128:(j + 1) * 128, :], d_[:, 0:128], reads=[bd], writes=[g.bufs["dd"]])


def bc3(ap2, n_in, n_rep):
    a = ap2.ap
    return AP(ap2.tensor, ap2.offset, [list(a[0]), [a[1][0], n_in], [0, n_rep]])


def ssd_scan(g):
    k, nc = g.k, g.nc
    cm = sbt(g, "q_cm", [128, 5, 128], F32)
    b_cm = Buf()
    k.dma("sp", cm[:], g.gin("cmat").rearrange("m p c -> p m c"), writes=[b_cm])
    identb = sbt(g, "q_identb", [128, 128], BF16)
    b_id = Buf()
    k.dma("pool", identb[:], g.gin("ident")[:, :], writes=[b_id])
    wout = sbt(g, "q_wout", [128, 16, D], BF16)
    b_wout = Buf()
    for c in range(16):
        k.dma("pool", wout[:, c, :], g.gin("ssd_w_out")[c * 128:(c + 1) * 128, :], writes=[b_wout])
    nwbc = sbt(g, "q_nw", [128, 2048], F32)
    dsk = sbt(g, "q_dsk", [128, 32], F32)
    b_nw = Buf()
    k.dma("sp", nwbc[:], bcast_rows(g.gin("ssd_norm")[0:1, :], 128), writes=[b_nw])
    k.dma("sp", dsk[:], bcast_rows(g.gin("ssd_d")[0:1, :], 128), writes=[b_nw])
    gate = {}
    S_all = [sbt(g, "q_S%d" % i, [128, 4, 512], F32) for i in range(NB)]
    Sb_all = [sbt(g, "q_Sb%d" % i, [128, 4, 512], BF16) for i in range(NB)]
    b_S_all = [[Buf() for _ in range(4)] for _ in range(NB)]
    b_Sb_all = [[Buf() for _ in range(4)] for _ in range(NB)]
    FM = [sbt(g, "q_FM%d" % i, [128, 24, 128], BF16) for i in range(2)]
    b_FM = [Buf(), Buf()]
    ddT = [sbt(g, "q_dd%d" % i, [128, 128], F32) for i in range(2)]
    b_dd = [Buf(), Buf()]
    Xtok = [sbt(g, "q_X%d" % i, [128, 2048], BF16) for i in range(2)]
    b_X = [Buf(), Buf()]
    Btok = [sbt(g, "q_B%d" % i, [128, 512], BF16) for i in range(2)]
    b_B = [Buf(), Buf()]
    cue = [sbt(g, "q_cue%d" % i, [128, 128], F32) for i in range(2)]
    b_cue = [Buf(), Buf()]
    xdt = sbt(g, "q_xdt", [128, 2048], BF16)
    xdtu = sbt(g, "q_xdtu", [128, 2048], BF16)
    b_xdt, b_xdtu = Buf(), Buf()
    sm = [sbt(g, "q_sm%d" % i, [128, 128], BF16) for i in range(2)]
    b_sm = [Buf(), Buf()]
    lh = [sbt(g, "q_lh%d" % i, [128, 128], F32) for i in range(8)]
    b_lh = [Buf() for _ in range(8)]
    Em = [sbt(g, "q_E%d" % i, [128, 128], BF16) for i in range(8)]
    b_E = [Buf() for _ in range(8)]
    Lm = [sbt(g, "q_L%d" % i, [128, 128], BF16) for i in range(8)]
    b_L = [Buf() for _ in range(8)]
    BURST = 8
    t1 = [sbt(g, "q_t1%d" % i, [128, 512], F32) for i in range(2)]
    b_t1 = [Buf(), Buf()]
    yblk = [sbt(g, "q_y%d" % i, [128, 2048], F32) for i in range(2)]
    b_y = [Buf(), Buf()]
    yfl = sbt(g, "q_yfl", [128, 2048], F32)
    b_yfl = Buf()
    ztl = sbt(g, "q_zt", [128, 2048], BF16)
    b_ztl = Buf()
    ygn = sbt(g, "q_ygn", [128, 2048], BF16)
    b_ygn = Buf()
    ygT = sbt(g, "q_ygT", [128, 16, 128], BF16)
    b_ygT = Buf()
    xt = [sbt(g, "q_xt%d" % i, [128, D], F32) for i in range(2)]
    b_xt = [Buf(), Buf()]
    ot = [sbt(g, "q_ot%d" % i, [128, 512], F32) for i in range(2)]
    b_ot = [Buf(), Buf()]
    gbc_all = [[sbt(g, "q_g%d_%d" % (b_, i), [128, D], F32) for i in range(2)] for b_ in range(NB)]
    b_g = Buf()
    ns = NormSet(g, "q_", nbuf=1)
    sc_slots = [(g.ps[0][:, i * 128:(i + 1) * 128], g.psb[0]) for i in range(3)]
    cum_ps, b_cum = g.ps[0][:, 384:480], g.psb[0]
    d_slots = [(g.ps[1 + i % 2][:, (i // 2) * 128:(i // 2 + 1) * 128], g.psb[1 + i % 2]) for i in range(8)]
    tr_b = [g.psb[6], g.psb[7]]
    cnt = dict(blk=0, sc=0, d=0, h=0, t1=0, o=0)

    for b in range(NB):
        k.dma("sp", gbc_all[b][0][:], bcast_rows(g.mod[0, 2:3, 2 * D:3 * D], 128), reads=[g.bufs["mod"]], writes=[b_g])
        k.dma("sp", gbc_all[b][1][:], bcast_rows(g.mod[0, b:b + 1, 2 * D:3 * D], 128), reads=[g.bufs["mod"]], writes=[b_g])
    for dr in range(2):
        order = list(range(18)) if dr == 0 else [1, 0] + list(range(17, 1, -1))
        CUMM, GL, RM = (0, 1, 0) if dr == 0 else (3, 2, 3)
        for b in range(NB):
            for gi in range(4):
                k.op("pool", lambda e, gi=gi, b=b: e.memset(S_all[b][:, gi, :], 0.0), writes=[b_S_all[b][gi]])
                k.op("pool", lambda e, gi=gi, b=b: e.memset(Sb_all[b][:, gi, :], 0.0), writes=[b_Sb_all[b][gi]])
        for blk in order:
            for b in range(NB):
                S, Sb, b_S, b_Sb, gbc = S_all[b], Sb_all[b], b_S_all[b], b_Sb_all[b], gbc_all[b]
                c0 = blk * 128
                bi = cnt["blk"] % 2
                cnt["blk"] += 1
                fm, bfm, dT, bdT = FM[bi], b_FM[bi], ddT[bi], b_dd[bi]
                X, bX, Bt, bB, cu, bcu = Xtok[bi], b_X[bi], Btok[bi], b_B[bi], cue[bi], b_cue[bi]
                yb, byb = yblk[bi], b_y[bi]
                k.dma("sp", fm[:], g.xbcT[b, :, c0:c0 + 128].rearrange("(c p) t -> p c t", p=128),
                      reads=[g.bufs["xbcT"]], writes=[bfm])
                k.dma("sp", dT[:], g.dd[b, c0:c0 + 128, :], reads=[g.bufs["dd"]], writes=[bdT])
                for hh in range(2):
                    psv = g.ps[6 + hh].bitcast(BF16)
                    for c in range(8):
                        k.op("pe", lambda e, c=c: e.transpose(out=psv[:, c * 128:(c + 1) * 128], in_=fm[:, hh * 8 + c, :],
                                                              identity=identb[:]),
                             reads=[bfm, b_id], writes=[tr_b[hh]], inc=(c == 7))
                    k.op("act", lambda e: e.copy(out=X[:, hh * 1024:(hh + 1) * 1024], in_=psv[:, 0:1024]),
                         reads=[tr_b[hh]], writes=[bX])
                psv = g.ps[6].bitcast(BF16)
                for c in range(4):
                    k.op("pe", lambda e, c=c: e.transpose(out=psv[:, c * 128:(c + 1) * 128], in_=fm[:, 16 + c, :],
                                                          identity=identb[:]),
                         reads=[bfm, b_id], writes=[tr_b[0]], inc=(c == 3))
                k.op("act", lambda e: e.copy(out=Bt[:], in_=psv[:, 0:512]), reads=[tr_b[0]], writes=[bB])
                da = dT[:, 64 + 32 * dr:96 + 32 * dr]
                dtc = dT[:, 32 * dr:32 * dr + 32]
                for q, mi in enumerate((CUMM, GL, 4)):
                    k.op("pe", lambda e, q=q, mi=mi: e.matmul(cum_ps[:, q * 32:(q + 1) * 32], lhsT=cm[:, mi, :], rhs=da,
                                                              start=True, stop=True),
                         reads=[b_cm, bdT], writes=[b_cum], inc=(q == 2))
                k.op("act", lambda e: e.activation(out=cu[:, 0:96], in_=cum_ps, func=AF.Exp), reads=[b_cum], writes=[bcu])
                k.op("dve", lambda e: e.tensor_tensor(out=cu[:, 96:128], in0=cu[:, 32:64], in1=dtc, op=ALU.mult),
                     reads=[bcu, bdT], writes=[bcu])
                X3 = X[:].rearrange("p (h q) -> p h q", q=64)
                k.op("dve", lambda e: e.tensor_tensor(out=xdt[:].rearrange("p (h q) -> p h q", q=64), in0=X3,
                                                       in1=bc3(dtc, 32, 64), op=ALU.mult),
                     reads=[bX, bdT], writes=[b_xdt])
                k.op("dve", lambda e: e.tensor_tensor(out=xdtu[:].rearrange("p (h q) -> p h q", q=64), in0=X3,
                                                       in1=bc3(cu[:, 96:128], 32, 64), op=ALU.mult),
                     reads=[bX, bcu], writes=[b_xdtu])
                for gi in range(4):
                    ps_s, b_ps_s = sc_slots[cnt["sc"] % 3]
                    smi = cnt["sc"] % 2
                    cnt["sc"] += 1
                    k.op("pe", lambda e: e.matmul(ps_s, lhsT=fm[:, 16 + gi, :], rhs=fm[:, 20 + gi, :], start=True, stop=True),
                         reads=[bfm], writes=[b_ps_s])
                    k.op("dve", lambda e: e.tensor_tensor(out=sm[smi][:], in0=ps_s, in1=cm[:, RM, :], op=ALU.mult),
                         reads=[b_ps_s, b_cm], writes=[b_sm[smi]])
                    yd, b_yd = g.ps[3], g.psb[3]
                    for hb0 in range(0, 8, BURST):
                        hs_ = list(range(hb0, hb0 + BURST))
                        slots = {}
                        for h8 in hs_:
                            slots[h8] = d_slots[cnt["d"] % 8]
                            cnt["d"] += 1
                        for h8 in hs_:
                            h = gi * 8 + h8
                            k.op("act", lambda e, h=h, h8=h8: e.activation(out=lh[h8][:], in_=cm[:, GL, :], func=AF.Copy, scale=da[:, h:h + 1]),
                                 reads=[b_cm, bdT], writes=[b_lh[h8]])
                        for h8 in hs_:
                            psD, b_psD = slots[h8]
                            k.op("pe", lambda e, h8=h8, psD=psD: e.matmul(psD, lhsT=lh[h8][:], rhs=cm[:, RM, :], start=True, stop=True),
                                 reads=[b_lh[h8], b_cm], writes=[b_psD])
                        for h8 in hs_:
                            psD, b_psD = slots[h8]
                            k.op("act", lambda e, h8=h8, psD=psD: e.activation(out=Em[h8][:], in_=psD, func=AF.Exp), reads=[b_psD], writes=[b_E[h8]])
                        for h8 in hs_:
                            k.op("dve", lambda e, h8=h8: e.tensor_tensor(out=Lm[h8][:], in0=Em[h8][:], in1=sm[smi][:], op=ALU.mult),
                                 reads=[b_E[h8], b_sm[smi]], writes=[b_L[h8]])
                        for h8 in hs_:
                            h = gi * 8 + h8
                            k.op("pe", lambda e, h=h, h8=h8: e.matmul(yd[:, h8 * 64:(h8 + 1) * 64], lhsT=Lm[h8][:], rhs=xdt[:, h * 64:(h + 1) * 64],
                                                                      start=True, stop=True),
                                 reads=[b_L[h8], b_xdt], writes=[b_yd])
                    yo, b_yo = g.ps[4], g.psb[4]
                    k.op("pe", lambda e: e.matmul(yo[:, :], lhsT=fm[:, 20 + gi, :], rhs=Sb[:, gi, :], start=True, stop=True),
                         reads=[bfm, b_Sb[gi]], writes=[b_yo])
                    ti = cnt["t1"] % 2
                    cnt["t1"] += 1
                    k.op("dve", lambda e: e.tensor_tensor(out=t1[ti][:].rearrange("p (h q) -> p h q", q=64),
                                                           in0=yo[:, :].rearrange("p (h q) -> p h q", q=64),
                                                           in1=bc3(cu[:, gi * 8:gi * 8 + 8], 8, 64), op=ALU.mult),
                         reads=[b_yo, bcu], writes=[b_t1[ti]])
                    k.op("dve", lambda e: e.tensor_tensor(out=yb[:, gi * 512:(gi + 1) * 512], in0=t1[ti][:], in1=yd[:, :], op=ALU.add),
                         reads=[b_t1[ti], b_yd], writes=[byb])
                    cs, b_cs = g.ps[5], g.psb[5]
                    k.op("pe", lambda e: e.matmul(cs[:, :], lhsT=Bt[:, gi * 128:(gi + 1) * 128], rhs=xdtu[:, gi * 512:(gi + 1) * 512],
                                                  start=True, stop=True),
                         reads=[bB, b_xdtu], writes=[b_cs])
                    S3 = S[:, gi, :].rearrange("p (h q) -> p h q", q=64)
                    k.op("dve", lambda e: e.tensor_tensor(out=S3, in0=S3, in1=bc3(cu[:, 64 + gi * 8:64 + gi * 8 + 8], 8, 64), op=ALU.mult),
                         reads=[b_S[gi], bcu], writes=[b_S[gi]])
                    k.op("dve", lambda e: e.tensor_tensor(out=S[:, gi, :], in0=S[:, gi, :], in1=cs[:, :], op=ALU.add),
                         reads=[b_S[gi], b_cs], writes=[b_S[gi]])
                    k.op("act", lambda e: e.copy(out=Sb[:, gi, :], in_=S[:, gi, :]), reads=[b_S[gi]], writes=[b_Sb[gi]])
                if dr == 0:
                    k.dma("sp", g.yf[b, c0:c0 + 128, :], yb[:], reads=[byb], writes=[g.bufs["yf"]])
                    continue
                k.dma("sp", yfl[:], g.yf[b, c0:c0 + 128, :], reads=[g.bufs["yf"]], writes=[b_yfl])
                k.dma("sp", ztl[:], g.zs[b, c0:c0 + 128, :], reads=[g.bufs["zs"]], writes=[b_ztl])
                is_ctx = blk < 2
                xsrc = g.ctx[b, c0:c0 + 128, :] if is_ctx else g.x[b, c0 - TC:c0 - TC + 128, :]
                xdst = g.c1[b, c0:c0 + 128, :] if is_ctx else g.x1[b, c0 - TC:c0 - TC + 128, :]
                bdst = g.bufs["c1"] if is_ctx else g.bufs["x1"]
                x_, bx = xt[bi], b_xt[bi]
                k.dma("sp", x_[:], xsrc, reads=[g.bufs["ctx" if is_ctx else "x"]], writes=[bx])
                k.op("pool", lambda e: e.tensor_tensor(out=yb[:], in0=yb[:], in1=yfl[:], op=ALU.add),
                     reads=[byb, b_yfl], writes=[byb])
                k.op("dve", lambda e: e.tensor_tensor(out=yfl[:].rearrange("p (h q) -> p h q", q=64), in0=X3,
                                                       in1=bc3(dsk[:], 32, 64), op=ALU.mult),
                     reads=[bX, b_nw, b_yfl], writes=[b_yfl])
                k.op("pool", lambda e: e.tensor_tensor(out=yb[:], in0=yb[:], in1=yfl[:], op=ALU.add),
                     reads=[byb, b_yfl], writes=[byb])
                k.op("dve", lambda e: e.tensor_tensor(out=yb[:], in0=yb[:], in1=ztl[:], op=ALU.mult),
                     reads=[byb, b_ztl], writes=[byb])
                rstd, b_st = rstd_of(g, ns, 0, yb[:], byb, dim=2048)
                k.op("dve", lambda e: e.tensor_tensor(out=ygn[:], in0=yb[:], in1=nwbc[:], op=ALU.mult),
                     reads=[byb, b_nw], writes=[b_ygn])
                for hh in range(2):
                    psv = g.ps[6 + hh].bitcast(BF16)
                    for c in range(8):
                        k.op("pe", lambda e, c=c: e.transpose(out=psv[:, c * 128:(c + 1) * 128],
                                                              in_=ygn[:, (hh * 8 + c) * 128:(hh * 8 + c + 1) * 128], identity=identb[:]),
                             reads=[b_ygn, b_id], writes=[tr_b[hh]], inc=(c == 7))
                    k.op("act", lambda e: e.copy(out=ygT[:, hh * 8:(hh + 1) * 8, :],
                                                 in_=psv[:, 0:1024].rearrange("p (c t) -> p c t", t=128)),
                         reads=[tr_b[hh]], writes=[b_ygT])
                gt = gbc[0] if is_ctx else gbc[1]
                for half in range(2):
                    hs = slice(half * 512, (half + 1) * 512)
                    po, b_po = g.ps[3 + half], g.psb[3 + half]
                    for c in range(16):
                        k.op("pe", lambda e, c=c: e.matmul(po[:, :], lhsT=ygT[:, c, :], rhs=wout[:, c, hs],
                                                           start=(c == 0), stop=(c == 15)),
                             reads=[b_ygT, b_wout], writes=[b_po], inc=(c == 15))
                    o_, bo = ot[cnt["o"] % 2], b_ot[cnt["o"] % 2]
                    cnt["o"] += 1
                    k.op("dve", lambda e: e.scalar_tensor_tensor(out=o_[:], in0=po[:, :], scalar=rstd, in1=gt[:, hs],
                                                                  op0=ALU.mult, op1=ALU.mult),
                         reads=[b_po, b_st, b_g], writes=[bo])
                    k.op("pool", lambda e: e.tensor_tensor(out=x_[:, hs], in0=x_[:, hs], in1=o_[:], op=ALU.add),
                         reads=[bo, bx], writes=[bx])
                k.dma("sp", xdst, x_[:], reads=[bx], writes=[bdst])


MAGIC = 12582912.0
TWO_PI_HI = 6.28125
TWO_PI_LO = 0.0019353071795864769


def s5_prep(g):
    k, nc = g.k, g.nc
    V = lambda n, sh=(128, 32): sbt(g, "p_" + n, list(sh), F32)
    ident = sbt(g, "p_identb", [128, 128], BF16)
    b_id = Buf()
    k.dma("pool", ident[:], g.gin("ident")[:, :], writes=[b_id])
    m8 = sbt(g, "p_m8", [128, 2, 128], F32)
    b_m8 = Buf()
    k.dma("sp", m8[:], g.gin("m8").rearrange("m p c -> p m c"), writes=[b_m8])
    bb = Buf()

    def dve(fn, eng="dve"):
        k.op(eng, fn, reads=[bb], writes=[bb])

    def tt(out, a, b_, op):
        dve(lambda e: e.tensor_tensor(out=out, in0=a, in1=b_, op=op))

    def ts(out, a, s1, op0, s2=None, op1=None):
        if op1 is None:
            dve(lambda e: e.tensor_scalar(out=out, in0=a, scalar1=s1, scalar2=None, op0=op0))
        else:
            dve(lambda e: e.tensor_scalar(out=out, in0=a, scalar1=s1, scalar2=s2, op0=op0, op1=op1))

    lre, lim, lst = V("lre"), V("lim"), V("lst")
    step, lr, th, kf, r_, t8, t2 = V("step"), V("lr"), V("th"), V("kf"), V("r"), V("t8"), V("t2")
    sn, cs_, ta, tb, rho1 = V("sn"), V("cs"), V("ta"), V("tb"), V("rho1")
    ar, ai, den, cr, ci = V("ar"), V("ai"), V("den"), V("cr"), V("ci")
    pm, pc, ps_ = V("pm", (128, 32, 9)), V("pc", (128, 32, 9)), V("ps", (128, 32, 9))
    apr, api = V("apr", (128, 32, 9)), V("api", (128, 32, 9))
    anr, ani, imv = V("anr", (128, 32, 9)), V("ani", (128, 32, 9)), V("imv", (128, 32, 9))
    Pc, Ps = V("Pc"), V("Ps")
    bre, bim = V("bre", (128, 32, 16)), V("bim", (128, 32, 16))
    bbr, bbi = V("bbr", (128, 32, 16)), V("bbi", (128, 32, 16))
    cre, cim = V("cre", (128, 32, 16)), V("cim", (128, 32, 16))
    w1, w2 = V("w1", (128, 32, 16)), V("w2", (128, 32, 16))
    WZr = sbt(g, "p_WZr", [128, 32, 8, 16], BF16)
    WZi = sbt(g, "p_WZi", [128, 32, 8, 16], BF16)
    Pr = sbt(g, "p_Pr", [128, 32, 8, 16], BF16)
    Pi = sbt(g, "p_Pi", [128, 32, 8, 16], BF16)
    CAr = sbt(g, "p_CAr", [128, 32, 9, 16], BF16)
    CAi = sbt(g, "p_CAi", [128, 32, 9, 16], BF16)
    cosT = sbt(g, "p_cosT", [128, 32, 288], F32)
    sinT = sbt(g, "p_sinT", [128, 32, 288], F32)
    e1 = sbt(g, "p_e1", [128, 32, 128], F32)
    e2 = sbt(g, "p_e2", [128, 32, 128], F32)
    wst = [sbt(g, "p_wst%d" % i, [128, 6, 128], BF16) for i in range(2)]
    b_wst = [Buf(), Buf()]

    def b16(t, n):
        return bc3(t[:], 32, n)

    def col(t3, j):
        return t3[:, :, j]

    for d in range(2):
        k.dma("sp", lre[:], g.gin("s5_lamT")[d, 0], reads=[bb], writes=[bb])
        k.dma("sp", lim[:], g.gin("s5_lamT")[d, 1], reads=[bb], writes=[bb])
        k.dma("sp", lst[:], g.gin("s5_lamT")[d, 2], reads=[bb], writes=[bb])
        k.dma("sp", bre[:], g.gin("s5_b")[d, 0].rearrange("(gp q) c -> q gp c", q=128), reads=[bb], writes=[bb])
        k.dma("sp", bim[:], g.gin("s5_b")[d, 1].rearrange("(gp q) c -> q gp c", q=128), reads=[bb], writes=[bb])
        k.dma("sp", cre[:], g.gin("s5_cT")[d, 0], reads=[bb], writes=[bb])
        k.dma("sp", cim[:], g.gin("s5_cT")[d, 1], reads=[bb], writes=[bb])
        dve(lambda e: e.activation(out=step[:], in_=lst[:], func=AF.Exp), "act")
        tt(lr[:], lre[:], step[:], ALU.mult)
        tt(th[:], lim[:], step[:], ALU.mult)
        ts(ta[:], lr[:], 1.0 / 5, ALU.mult, 1.0, ALU.add)
        for c_ in (1.0 / 4, 1.0 / 3, 1.0 / 2, 1.0):
            tt(ta[:], ta[:], lr[:], ALU.mult)
            ts(ta[:], ta[:], c_, ALU.mult, 1.0, ALU.add)
        dve(lambda e: e.tensor_copy(out=rho1[:], in_=ta[:]))
        ts(kf[:], th[:], 1.0 / (2 * np.pi), ALU.mult, MAGIC, ALU.add)
        ts(kf[:], kf[:], -MAGIC, ALU.add)
        dve(lambda e: e.scalar_tensor_tensor(out=r_[:], in0=kf[:], scalar=-TWO_PI_HI, in1=th[:], op0=ALU.mult, op1=ALU.add))
        dve(lambda e: e.scalar_tensor_tensor(out=r_[:], in0=kf[:], scalar=-TWO_PI_LO, in1=r_[:], op0=ALU.mult, op1=ALU.add))
        ts(t8[:], r_[:], 0.125, ALU.mult)
        tt(t2[:], t8[:], t8[:], ALU.mult)
        ts(ta[:], t2[:], -1.0 / 5040, ALU.mult, 1.0 / 120, ALU.add)
        tt(ta[:], ta[:], t2[:], ALU.mult)
        ts(ta[:], ta[:], -1.0 / 6, ALU.add)
        tt(ta[:], ta[:], t2[:], ALU.mult)
        ts(ta[:], ta[:], 1.0, ALU.add)
        tt(sn[:], ta[:], t8[:], ALU.mult)
        ts(ta[:], t2[:], 1.0 / 40320, ALU.mult, -1.0 / 720, ALU.add)
        tt(ta[:], ta[:], t2[:], ALU.mult)
        ts(ta[:], ta[:], 1.0 / 24, ALU.add)
        tt(ta[:], ta[:], t2[:], ALU.mult)
        ts(ta[:], ta[:], -0.5, ALU.add)
        tt(ta[:], ta[:], t2[:], ALU.mult)
        ts(cs_[:], ta[:], 1.0, ALU.add)
        for _ in range(3):
            tt(ta[:], cs_[:], cs_[:], ALU.mult)
            tt(tb[:], sn[:], sn[:], ALU.mult)
            tt(sn[:], sn[:], cs_[:], ALU.mult)
            ts(sn[:], sn[:], 2.0, ALU.mult)
            tt(cs_[:], ta[:], tb[:], ALU.subtract)
        tt(ar[:], rho1[:], cs_[:], ALU.mult)
        tt(ai[:], rho1[:], sn[:], ALU.mult)
        ts(ta[:], ar[:], -1.0, ALU.add)
        tt(den[:], lre[:], lre[:], ALU.mult)
        tt(tb[:], lim[:], lim[:], ALU.mult)
        tt(den[:], den[:], tb[:], ALU.add)
        dve(lambda e: e.reciprocal(out=den[:], in_=den[:]))
        tt(cr[:], ta[:], lre[:], ALU.mult)
        tt(tb[:], ai[:], lim[:], ALU.mult)
        tt(cr[:], cr[:], tb[:], ALU.add)
        tt(cr[:], cr[:], den[:], ALU.mult)
        tt(ci[:], ai[:], lre[:], ALU.mult)
        tt(tb[:], ta[:], lim[:], ALU.mult)
        tt(ci[:], ci[:], tb[:], ALU.subtract)
        tt(ci[:], ci[:], den[:], ALU.mult)
        tt(w1[:], bre[:], b16(cr, 16), ALU.mult)
        tt(w2[:], bim[:], b16(ci, 16), ALU.mult)
        tt(bbr[:], w1[:], w2[:], ALU.subtract)
        tt(w1[:], bim[:], b16(cr, 16), ALU.mult)
        tt(w2[:], bre[:], b16(ci, 16), ALU.mult)
        tt(bbi[:], w1[:], w2[:], ALU.add)
        dve(lambda e: e.memset(col(pm, 0), 1.0))
        dve(lambda e: e.memset(col(pc, 0), 1.0))
        dve(lambda e: e.memset(col(ps_, 0), 0.0))
        for j in range(1, 9):
            tt(col(pm, j), col(pm, j - 1), rho1[:], ALU.mult)
            tt(ta[:], col(pc, j - 1), cs_[:], ALU.mult)
            tt(tb[:], col(ps_, j - 1), sn[:], ALU.mult)
            tt(col(pc, j), ta[:], tb[:], ALU.subtract)
            tt(ta[:], col(ps_, j - 1), cs_[:], ALU.mult)
            tt(tb[:], col(pc, j - 1), sn[:], ALU.mult)
            tt(col(ps_, j), ta[:], tb[:], ALU.add)
        dve(lambda e: e.reciprocal(out=imv[:], in_=pm[:]))
        tt(apr[:], pm[:], pc[:], ALU.mult)
        tt(api[:], pm[:], ps_[:], ALU.mult)
        tt(anr[:], imv[:], pc[:], ALU.mult)
        tt(ani[:], imv[:], ps_[:], ALU.mult)
        ts(ani[:], ani[:], -1.0, ALU.mult)
        dve(lambda e: e.tensor_copy(out=ta[:], in_=col(pm, 8)))
        k.dma("sp", g.rho[d], ta[:], reads=[bb], writes=[g.bufs["rho"]])
        for s_ in range(8):
            so = s_ if d == 0 else 7 - s_
            for (dst, pr_, pi_) in ((WZr, col(apr, 7 - s_), col(api, 7 - s_)), (Pr, col(anr, s_), col(ani, s_))):
                dsti = WZi if dst is WZr else Pi
                tt(w1[:], bbr[:], bc3(pr_, 32, 16), ALU.mult)
                tt(w2[:], bbi[:], bc3(pi_, 32, 16), ALU.mult)
                tt(dst[:, :, so, :], w1[:], w2[:], ALU.subtract)
                tt(w1[:], bbi[:], bc3(pr_, 32, 16), ALU.mult)
                tt(w2[:], bbr[:], bc3(pi_, 32, 16), ALU.mult)
                tt(dsti[:, :, so, :], w1[:], w2[:], ALU.add)
        for j in range(9):
            jo = j if d == 0 else 8 - j
            tt(w1[:], cre[:], bc3(col(apr, j), 32, 16), ALU.mult)
            tt(w2[:], cim[:], bc3(col(api, j), 32, 16), ALU.mult)
            tt(CAr[:, :, jo, :], w1[:], w2[:], ALU.subtract)
            tt(w1[:], cim[:], bc3(col(apr, j), 32, 16), ALU.mult)
            tt(w2[:], cre[:], bc3(col(api, j), 32, 16), ALU.mult)
            tt(w1[:], w1[:], w2[:], ALU.add)
            ts(CAi[:, :, jo, :], w1[:], -1.0, ALU.mult)
        q0, y0 = (0, 1) if d == 0 else (1, 0)
        dve(lambda e: e.tensor_copy(out=Pc[:], in_=col(pc, 8)))
        dve(lambda e: e.tensor_copy(out=Ps[:], in_=col(ps_, 8)))
        dve(lambda e: e.memset(cosT[:, :, 0:1], 1.0))
        dve(lambda e: e.memset(sinT[:, :, 0:1], 0.0))
        wdt = 1
        while wdt < 288:
            n = min(wdt, 288 - wdt)
            tt(e1[:, :, 0:n], cosT[:, :, 0:n], b16(Pc, n), ALU.mult)
            tt(e2[:, :, 0:n], sinT[:, :, 0:n], b16(Ps, n), ALU.mult)
            tt(cosT[:, :, wdt:wdt + n], e1[:, :, 0:n], e2[:, :, 0:n], ALU.subtract)
            tt(e1[:, :, 0:n], sinT[:, :, 0:n], b16(Pc, n), ALU.mult)
            tt(e2[:, :, 0:n], cosT[:, :, 0:n], b16(Ps, n), ALU.mult)
            tt(sinT[:, :, wdt:wdt + n], e1[:, :, 0:n], e2[:, :, 0:n], ALU.add)
            tt(ta[:], Pc[:], Pc[:], ALU.mult)
            tt(tb[:], Ps[:], Ps[:], ALU.mult)
            tt(Ps[:], Ps[:], Pc[:], ALU.mult)
            ts(Ps[:], Ps[:], 2.0, ALU.mult)
            tt(Pc[:], ta[:], tb[:], ALU.subtract)
            wdt *= 2
        k.dma("sp", g.etab[d, 0], cosT[:], reads=[bb], writes=[g.bufs["etab"]])
        k.dma("sp", g.etab[d, 1], sinT[:], reads=[bb], writes=[g.bufs["etab"]])
        for gp in range(32):
            wt, bwt = wst[gp % 2], b_wst[gp % 2]
            for g2 in range(2):
                L = slice(g2 * 64, (g2 + 1) * 64)
                pM, bM = g.ps[g2], g.psb[g2]
                k.op("pe", lambda e: e.matmul(pM[:, 0:128], lhsT=Pr[L, gp, :, :], rhs=CAr[L, gp, q0:q0 + 8, :], start=True, stop=False),
                     reads=[bb], writes=[bM], inc=False)
                k.op("pe", lambda e: e.matmul(pM[:, 0:128], lhsT=Pi[L, gp, :, :], rhs=CAi[L, gp, q0:q0 + 8, :], start=False, stop=True),
                     reads=[bb], writes=[bM])
                k.op("dve", lambda e: e.tensor_tensor(out=wt[:, g2, :], in0=pM[:, 0:128], in1=m8[:, d, :], op=ALU.mult),
                     reads=[bM, b_m8], writes=[bwt])
            for ri, src_ in enumerate((WZr, WZi)):
                pT, bT = g.ps[2 + ri], g.psb[2 + ri]
                pTv = pT.bitcast(BF16)
                k.op("pe", lambda e: e.transpose(out=pTv[:, 0:128], in_=src_[:, gp, :, :], identity=ident[:]),
                     reads=[bb, b_id], writes=[bT])
                k.op("act", lambda e: e.copy(out=wt[:, 2 + ri, :], in_=pTv[:, 0:128]), reads=[bT], writes=[bwt])
            k.op("act", lambda e: e.copy(out=wt[:, 4, :], in_=CAr[:, gp, y0:y0 + 8, :]), reads=[bb], writes=[bwt])
            k.op("act", lambda e: e.copy(out=wt[:, 5, :], in_=CAi[:, gp, y0:y0 + 8, :]), reads=[bb], writes=[bwt])
            k.dma("sp", g.s5w[d, gp], wt[:], reads=[bwt], writes=[g.bufs["s5w"]])


def phase_s5(g):
    with ExitStack() as pes:
        g.pes = pes
        s5_prep(g)
        g.k.barrier()
    if g.kinds.get("_s5_prep_only"):
        return
    with ExitStack() as pes:
        g.pes = pes
        s5_main(g)
        g.k.barrier()


def bcf(col_ap, n):
    a = col_ap.ap
    return AP(col_ap.tensor, col_ap.offset, [list(a[0]), [0, n]])


def s5_main(g):
    k, nc = g.k, g.nc
    identb = sbt(g, "v_identb", [128, 128], BF16)
    b_id = Buf()
    k.dma("pool", identb[:], g.gin("ident")[:, :], writes=[b_id])
    bglu = sbt(g, "v_bglu", [128, 2 * D], F32)
    dsk = sbt(g, "v_dsk", [128, D], F32)
    rho = sbt(g, "v_rho", [128, 2, 32], F32)
    b_c = Buf()
    k.dma("sp", bglu[:], bcast_rows(g.gin("s5_b_glu")[0:1, :], 128), writes=[b_c])
    k.dma("sp", dsk[:], bcast_rows(g.gin("s5_d")[0:1, :], 128), writes=[b_c])
    k.dma("sp", rho[:], g.rho.rearrange("d q gp -> q d gp"), reads=[g.bufs["rho"]], writes=[b_c])
    mods = ModBC(g, "v", 1, (0, 1, 2))
    ns = NormSet(g, "v_")
    Ubig = sbt(g, "v_U", [128, 64 * 320], BF16)
    U = Ubig[:, :].rearrange("p (g c) -> p g c", c=320)
    wglu = Ubig[:, 0:8 * 2048].rearrange("p (k n) -> p k n", n=2048)
    b_U = Buf()
    hcx = sbt(g, "v_hcx", [128, 64, 8, 16], BF16)
    b_hcx = Buf()
    hxc = sbt(g, "v_hxc", [128, 2, 64, 8, 16], BF16)
    b_hxc = Buf()
    xt = [sbt(g, "v_xt%d" % i, [128, D], F32) for i in range(2)]
    b_xt = [Buf(), Buf()]
    tab = [sbt(g, "v_tab%d" % i, [128, 2, 288], F32) for i in range(2)]
    b_tab = [Buf(), Buf()]
    wk = [[sbt(g, "v_wk%d_%d" % (i, j), [128, 288], F32) for j in range(6)] for i in range(2)]
    b_wk = [[Buf() for _ in range(6)] for _ in range(2)]
    spv = [sbt(g, "v_spv%d" % i, [128, 2, 2, 256], BF16) for i in range(2)]
    b_spv = [Buf(), Buf()]
    wts = [sbt(g, "v_wts%d" % i, [128, 2, 6, 128], BF16) for i in range(2)]
    b_wts = [Buf(), Buf()]
    ysb = [sbt(g, "v_ysb%d" % i, [128, 256], BF16) for i in range(2)]
    b_ysb = [Buf(), Buf()]
    gt_ = [sbt(g, "v_gt%d" % i, [128, D], F32) for i in range(3)]
    b_gt = [Buf() for _ in range(3)]
    geb = sbt(g, "v_geb", [128, D], BF16)
    b_geb = Buf()
    geT = sbt(g, "v_geT", [128, 8, 128], BF16)
    b_geT = Buf()
    av = [sbt(g, "v_av%d" % i, [128, 512], F32) for i in range(2)]
    b_av = [Buf(), Buf()]
    gv = [sbt(g, "v_gv%d" % i, [128, 512], F32) for i in range(2)]
    b_gv = [Buf(), Buf()]
    cnt = dict(x=0, it=0, y=0, o=0)

    def load_x_tile(dst, bdst, src_b, tile, l):
        for r4 in range(4):
            t0 = (r4 * 8 + l) * 64 + tile * 32
            k.dma("sp", dst[r4 * 32:(r4 + 1) * 32, :], src_b[t0:t0 + 32, :], reads=[g.bufs["x2"]], writes=[bdst])

    for b in range(NB):
        (shbc, gbc, gatebc), b_mod = mods.get(2)
        cv = g.c2[b].rearrange("(c l) d -> l c d", l=8)
        for l in range(8):
            x_, bx = xt[cnt["x"] % 2], b_xt[cnt["x"] % 2]
            cnt["x"] += 1
            k.dma("sp", x_[0:32, :], cv[l], reads=[g.bufs["c2"]], writes=[bx])
            rstd, b_st = rstd_of(g, ns, 0, x_[0:32, :], bx)
            st = ns.st[0]
            k.op("dve", lambda e: e.scalar_tensor_tensor(out=ns.tmp[0][0:32, :], in0=x_[0:32, :], scalar=st[0:32, 3:4], in1=gbc[0:32, :],
                                                          op0=ALU.mult, op1=ALU.mult),
                 reads=[bx, b_st, b_mod], writes=[ns.b_tmp[0]])
            k.op("pool", lambda e: e.tensor_tensor(out=hcx[0:32, :, l, :], in0=ns.tmp[0][0:32, :].rearrange("p (g c) -> p g c", c=16),
                                                   in1=shbc[0:32, :].rearrange("p (g c) -> p g c", c=16), op=ALU.add),
                 reads=[ns.b_tmp[0], b_mod], writes=[b_hcx])
        (shbc, gbc, gatebc), b_mod = mods.get(b)
        for tile in range(2):
            for l in range(8):
                x_, bx = xt[cnt["x"] % 2], b_xt[cnt["x"] % 2]
                cnt["x"] += 1
                load_x_tile(x_, bx, g.x2[b], tile, l)
                rstd, b_st = rstd_of(g, ns, 0, x_[:], bx)
                k.op("dve", lambda e: e.scalar_tensor_tensor(out=ns.tmp[0][:], in0=x_[:], scalar=rstd, in1=gbc[:],
                                                              op0=ALU.mult, op1=ALU.mult),
                     reads=[bx, b_st, b_mod], writes=[ns.b_tmp[0]])
                k.op("pool", lambda e: e.tensor_tensor(out=hxc[:, tile, :, l, :], in0=ns.tmp[0][:].rearrange("p (g c) -> p g c", c=16),
                                                       in1=shbc[:].rearrange("p (g c) -> p g c", c=16), op=ALU.add),
                     reads=[ns.b_tmp[0], b_mod], writes=[b_hxc])
        for gg in range(64):
            ps, pb = g.ps[6 + gg % 2], g.psb[6 + gg % 2]
            psv = ps.bitcast(BF16)
            k.op("pe", lambda e: e.transpose(out=psv[:, 0:32], in_=hcx[0:32, gg, :, :], identity=identb[0:32, 0:32]),
                 reads=[b_hcx, b_id], writes=[pb], inc=False)
            for tile in range(2):
                k.op("pe", lambda e, tile=tile: e.transpose(out=psv[:, 32 + tile * 128:32 + (tile + 1) * 128],
                                                            in_=hxc[:, tile, gg, :, :], identity=identb[:]),
                     reads=[b_hxc, b_id], writes=[pb], inc=(tile == 1))
            k.op("act", lambda e: e.copy(out=U[:, gg, 0:32], in_=psv[:, 0:32]), reads=[pb], writes=[b_U])
            k.op("act", lambda e: e.copy(out=U[:, gg, 288:320], in_=psv[:, 0:32]), reads=[pb], writes=[b_U])
            k.op("act", lambda e: e.copy(out=U[:, gg, 32:288].rearrange("p (t c r) -> p t r c", t=2, c=32, r=4),
                                         in_=psv[:, 32:288].rearrange("p (t r c) -> p t r c", t=2, r=4, c=32)),
                 reads=[pb], writes=[b_U])
        for tile in range(2):
            for l in range(8):
                k.op("dve", lambda e, tile=tile, l=l: e.tensor_tensor(out=hxc[:, tile, :, l, :], in0=hxc[:, tile, :, l, :],
                                                                      in1=dsk[:].rearrange("p (g c) -> p g c", c=16), op=ALU.mult),
                     reads=[b_hxc, b_c], writes=[b_hxc])
        for gp in range(32):
            it = cnt["it"] % 2
            cnt["it"] += 1
            wt, bwt, sp_, bsp = wts[it], b_wts[it], spv[it], b_spv[it]
            for d in range(2):
                tb_, btb = tab[d], b_tab[d]
                W, bW = wk[d], b_wk[d]
                k.dma("sp", wt[:, d], g.s5w[d, gp], reads=[g.bufs["s5w"]], writes=[bwt])
                k.dma("sp", tb_[:, 0, :], g.etab[d, 0, :, gp, :], reads=[g.bufs["etab"]], writes=[btb])
                k.dma("sp", tb_[:, 1, :], g.etab[d, 1, :, gp, :], reads=[g.bufs["etab"]], writes=[btb])
                cols = slice(0, 288) if d == 0 else slice(32, 320)
                pz = [(g.ps[2 * d], g.psb[2 * d]), (g.ps[2 * d + 1], g.psb[2 * d + 1])]
                for ri in range(2):
                    pZ, bZ = pz[ri]
                    for g2 in range(2):
                        k.op("pe", lambda e, g2=g2: e.matmul(pZ[g2 * 64:(g2 + 1) * 64, 0:288], lhsT=wt[:, d, 2 + ri, g2 * 64:(g2 + 1) * 64],
                                                             rhs=U[:, 2 * gp + g2, cols], start=True, stop=True),
                             reads=[bwt, b_U], writes=[bZ], inc=(g2 == 1))
                cosv, sinv = tb_[:, 0, :], tb_[:, 1, :]
                if d == 1:
                    cosv, sinv = rev_ap(cosv, 288), rev_ap(sinv, 288)
                Zr, bZr = pz[0][0][:, 0:288], pz[0][1]
                Zi, bZi = pz[1][0][:, 0:288], pz[1][1]
                za, zb, ztr, zti, sr, si = [w_[:] for w_ in W]
                bza, bzb, bztr, bzti, bsr, bsi = bW
                TT = lambda o, a_, b_, op, rd, wr: k.op("dve", lambda e: e.tensor_tensor(out=o, in0=a_, in1=b_, op=op), reads=rd, writes=wr)
                TT(za, Zr, cosv, ALU.mult, [bZr, btb], [bza])
                TT(zb, Zi, sinv, ALU.mult, [bZi, btb], [bzb])
                TT(ztr, za, zb, ALU.add, [bza, bzb], [bztr])
                TT(za, Zi, cosv, ALU.mult, [bZi, btb], [bza])
                TT(zb, Zr, sinv, ALU.mult, [bZr, btb], [bzb])
                TT(zti, za, zb, ALU.subtract, [bza, bzb], [bzti])
                rbc = bcf(rho[:, d, gp:gp + 1], 288)
                for (o_, i_, bo_, bi_) in ((sr, ztr, bsr, bztr), (si, zti, bsi, bzti)):
                    oo, ii = (o_, i_) if d == 0 else (rev_ap(o_, 288), rev_ap(i_, 288))
                    k.op("dve", lambda e, oo=oo, ii=ii: e.tensor_tensor_scan(out=oo, data0=rbc, data1=ii, initial=0.0,
                                                                             op0=ALU.mult, op1=ALU.add),
                         reads=[bi_, b_c], writes=[bo_])
                sl = slice(31, 287) if d == 0 else slice(1, 257)
                TT(za[:, 0:256], sr[:, sl], cosv[:, sl], ALU.mult, [bsr, btb], [bza])
                TT(zb[:, 0:256], si[:, sl], sinv[:, sl], ALU.mult, [bsi, btb], [bzb])
                TT(sp_[:, d, 0, :], za[:, 0:256], zb[:, 0:256], ALU.subtract, [bza, bzb], [bsp])
                TT(za[:, 0:256], si[:, sl], cosv[:, sl], ALU.mult, [bsi, btb], [bza])
                TT(zb[:, 0:256], sr[:, sl], sinv[:, sl], ALU.mult, [bsr, btb], [bzb])
                TT(sp_[:, d, 1, :], za[:, 0:256], zb[:, 0:256], ALU.add, [bza, bzb], [bsp])
            for g2 in range(2):
                L = slice(g2 * 64, (g2 + 1) * 64)
                gg = 2 * gp + g2
                yi = cnt["y"] % 2
                cnt["y"] += 1
                pY, bY = g.ps[4 + yi], g.psb[4 + yi]
                ops = [(wt[:, 0, g2, :], U[:, gg, 32:288]), (wt[:, 1, g2, :], U[:, gg, 32:288])]
                for d in range(2):
                    ops.append((wt[L, d, 4, :], sp_[L, d, 0, :]))
                    ops.append((wt[L, d, 5, :], sp_[L, d, 1, :]))
                for oi, (lw, rh) in enumerate(ops):
                    k.op("pe", lambda e, lw=lw, rh=rh, oi=oi: e.matmul(pY[:, 0:256], lhsT=lw, rhs=rh, start=(oi == 0), stop=(oi == 5)),
                         reads=[bwt, b_U, bsp], writes=[bY], inc=(oi == 5))
                ys, bys = ysb[yi], b_ysb[yi]
                k.op("act", lambda e: e.copy(out=ys[:].rearrange("p (t r c) -> p t c r", t=2, r=4, c=32),
                                             in_=pY[:, 0:256].rearrange("p (t c r) -> p t c r", t=2, c=32, r=4)),
                     reads=[bY], writes=[bys])
                pT, bT = g.ps[6 + yi], g.psb[6 + yi]
                pTv = pT.bitcast(BF16)
                for tile in range(2):
                    k.op("pe", lambda e, tile=tile: e.transpose(out=pTv[:, tile * 128:(tile + 1) * 128],
                                                                in_=ys[:, tile * 128:(tile + 1) * 128], identity=identb[:]),
                         reads=[bys, b_id], writes=[bT], inc=(tile == 1))
                tyv = hxc[:, :, gg, :, :]
                k.op("dve", lambda e: e.tensor_tensor(out=tyv, in0=tyv, in1=pTv[:, 0:256].rearrange("p (t l c) -> p t l c", t=2, l=8, c=16),
                                                       op=ALU.add),
                     reads=[bT, b_hxc], writes=[b_hxc])
        for kk in range(8):
            k.dma("pool", wglu[:, kk, :], g.gin("s5_w_glu")[kk * 128:(kk + 1) * 128, :], writes=[b_U])
        for tile in range(2):
            for l in range(8):
                yv = hxc[:, tile, :, l, :]
                g0, g1, g2_ = [t_[:].rearrange("p (g c) -> p g c", c=16) for t_ in gt_]
                k.op("act", lambda e: e.activation(out=g0, in_=yv, func=AF.Square), reads=[b_hxc], writes=[b_gt[0]])
                k.op("dve", lambda e: e.tensor_scalar(out=g0, in0=g0, scalar1=0.044715, scalar2=1.0, op0=ALU.mult, op1=ALU.add),
                     reads=[b_gt[0]], writes=[b_gt[0]])
                k.op("dve", lambda e: e.tensor_tensor(out=g1, in0=g0, in1=yv, op=ALU.mult), reads=[b_gt[0], b_hxc], writes=[b_gt[1]])
                k.op("act", lambda e: e.activation(out=g2_, in_=g1, func=AF.Sigmoid, scale=1.5957691216057308),
                     reads=[b_gt[1]], writes=[b_gt[2]])
                k.op("dve", lambda e: e.tensor_tensor(out=geb[:].rearrange("p (g c) -> p g c", c=16), in0=g2_, in1=yv, op=ALU.mult), reads=[b_gt[2], b_hxc], writes=[b_geb])
                ps, pb = g.ps[6 + l % 2], g.psb[6 + l % 2]
                psv = ps.bitcast(BF16)
                for kk in range(8):
                    k.op("pe", lambda e, kk=kk: e.transpose(out=psv[:, kk * 128:(kk + 1) * 128], in_=geb[:, kk * 128:(kk + 1) * 128],
                                                            identity=identb[:]),
                         reads=[b_geb, b_id], writes=[pb], inc=(kk == 7))
                k.op("act", lambda e: e.copy(out=geT[:], in_=psv[:, 0:1024].rearrange("p (k t) -> p k t", t=128)),
                     reads=[pb], writes=[b_geT])
                x_, bx = xt[cnt["x"] % 2], b_xt[cnt["x"] % 2]
                cnt["x"] += 1
                load_x_tile(x_, bx, g.x2[b], tile, l)
                for half in range(2):
                    oi = cnt["o"] % 2
                    cnt["o"] += 1
                    pa, ba = g.ps[oi * 2], g.psb[oi * 2]
                    pg_, bg_ = g.ps[oi * 2 + 1], g.psb[oi * 2 + 1]
                    for (pp, bp, n0) in ((pa, ba, half * 512), (pg_, bg_, D + half * 512)):
                        for kk in range(8):
                            k.op("pe", lambda e, kk=kk, pp=pp, n0=n0: e.matmul(pp[:, :], lhsT=geT[:, kk, :], rhs=wglu[:, kk, n0:n0 + 512],
                                                                               start=(kk == 0), stop=(kk == 7)),
                                 reads=[b_geT, b_U], writes=[bp], inc=(kk == 7))
                    hs = slice(half * 512, (half + 1) * 512)
                    a_, ba_, g_, bg2 = av[oi], b_av[oi], gv[oi], b_gv[oi]
                    k.op("dve", lambda e: e.tensor_tensor(out=a_[:], in0=pa[:, :], in1=bglu[:, hs], op=ALU.add),
                         reads=[ba, b_c], writes=[ba_])
                    k.op("dve", lambda e: e.tensor_tensor(out=g_[:], in0=pg_[:, :], in1=bglu[:, D + half * 512:D + (half + 1) * 512], op=ALU.add),
                         reads=[bg_, b_c], writes=[bg2])
                    k.op("act", lambda e: e.activation(out=g_[:], in_=g_[:], func=AF.Sigmoid), reads=[bg2], writes=[bg2])
                    k.op("dve", lambda e: e.tensor_tensor(out=a_[:], in0=a_[:], in1=g_[:], op=ALU.mult), reads=[ba_, bg2], writes=[ba_])
                    k.op("pool", lambda e: e.tensor_tensor(out=a_[:], in0=a_[:], in1=gatebc[:, hs], op=ALU.mult),
                         reads=[ba_, b_mod], writes=[ba_])
                    k.op("pool", lambda e: e.tensor_tensor(out=x_[:, hs], in0=x_[:, hs], in1=a_[:], op=ALU.add),
                         reads=[ba_, bx], writes=[bx])
                for r4 in range(4):
                    t0 = (r4 * 8 + l) * 64 + tile * 32
                    k.dma("sp", g.x3[b, t0:t0 + 32, :], x_[r4 * 32:(r4 + 1) * 32, :], reads=[bx], writes=[g.bufs["x3"]])


def phase_moe(g):
    k, nc = g.k, g.nc
    src = g.x3 if "s5" in g.phases or g.kinds.get("x3") == "in" else g.x2
    b_src = g.bufs["x3"] if "s5" in g.phases or g.kinds.get("x3") == "in" else g.bufs["x2"]
    NTL = T // 128
    NFG = EDIM // 512
    ns = NormSet(g, "m_")
    identf = sbt(g, "m_identf", [128, 128], F32)
    b_identf = Buf()
    k.dma("sp", identf[:], g.gin("ident")[:, :], writes=[b_identf])
    hT = sbt(g, "m_hT", [128, 8, T], BF16)
    b_hT = Buf()
    acc = sbt(g, "m_acc", [128, NTL, D], F32)
    b_acc = [Buf() for _ in range(NTL)]
    aT = sbt(g, "m_aT", [128, 4, T], BF16)
    b_aT = [Buf() for _ in range(4)]
    wg = [sbt(g, "m_wg%d" % i, [128, 8, 512], BF16) for i in range(2)]
    wu = [sbt(g, "m_wu%d" % i, [128, 8, 512], BF16) for i in range(2)]
    wo = [sbt(g, "m_wo%d" % i, [128, 4, D], BF16) for i in range(2)]
    b_w = [Buf(), Buf()]
    comb = sbt(g, "m_comb", [128, NTL, NEXP], F32)
    b_comb = Buf()
    rw = sbt(g, "m_rw", [128, 8, NEXP], F32)
    rb = sbt(g, "m_rb", [128, NEXP], F32)
    b_rw = Buf()
    k.dma("sp", rw[:], g.gin("moe_router_w").rearrange("(k p) e -> p k e", p=128), writes=[b_rw])
    k.dma("sp", rb[:], bcast_rows(g.gin("moe_router_b")[0:1, :], 128), writes=[b_rw])
    nfin = sbt(g, "m_nfin", [128, D], F32)
    b_nfin = Buf()
    k.dma("sp", nfin[:], bcast_rows(g.gin("norm_final")[0:1, :], 128), writes=[b_nfin])
    hTf = sbt(g, "m_hTf", [128, 8, 128], F32)
    b_hTf = Buf()
    lg = [sbt(g, "m_lg%d" % i, [128, 40], F32) for i in range(2)]
    b_lg = [Buf(), Buf()]
    sg = [sbt(g, "m_sg%d" % i, [128, 512], F32) for i in range(2)]
    b_sg = [Buf(), Buf()]
    tmpo = [sbt(g, "m_to%d" % i, [128, 512], F32) for i in range(2)]
    b_to = [Buf(), Buf()]
    mods = ModBC(g, "m", 1, (3, 4, 5))
    wi = 0
    si = 0
    oi = 0
    pi = 0
    for b in range(NB):
        (shbc, gbc, gatebc), b_mod = mods.get(b)
        for j in range(NTL):
            xt = acc[:, j, :]
            k.dma("sp", xt, src[b, j * 128:(j + 1) * 128, :], reads=[b_src], writes=[b_acc[j]])
            i = ns.i % ns.nbuf
            ns.i += 1
            rstd, b_st = rstd_of(g, ns, i, xt, b_acc[j])
            tmp, b_tmp, hb, b_hb = ns.tmp[i], ns.b_tmp[i], ns.hb[i], ns.b_hb[i]
            k.op("dve", lambda e: e.scalar_tensor_tensor(out=tmp[:], in0=xt, scalar=rstd, in1=gbc[:],
                                                          op0=ALU.mult, op1=ALU.mult),
                 reads=[b_acc[j], b_st, b_mod], writes=[b_tmp])
            k.op("pool", lambda e: e.tensor_tensor(out=tmp[:], in0=tmp[:], in1=shbc[:], op=ALU.add),
                 reads=[b_tmp, b_mod], writes=[b_tmp])
            k.op("act", lambda e: e.copy(out=hb[:], in_=tmp[:]), reads=[b_tmp], writes=[b_hb])
            ps, pb = g.ps[4 + (j % 2)], g.psb[4 + (j % 2)]
            psv = ps.bitcast(BF16)
            for kk in range(8):
                k.op("pe", lambda e, kk=kk: e.transpose(out=psv[:, kk * 128:(kk + 1) * 128],
                                                        in_=hb[:, kk * 128:(kk + 1) * 128], identity=ns.ident[:]),
                     reads=[b_hb, ns.b_ident], writes=[pb], inc=(kk == 7))
            k.op("act", lambda e: e.copy(out=hT[:, :, j * 128:(j + 1) * 128],
                                         in_=psv[:, 0:1024].rearrange("p (k t) -> p k t", t=128)),
                 reads=[pb], writes=[b_hT])
            for hh in range(2):
                psf, pbf = g.ps[6 + hh], g.psb[6 + hh]
                for kk in range(4):
                    kf = hh * 4 + kk
                    k.op("pe", lambda e, kk=kk, kf=kf: e.transpose(out=psf[:, kk * 128:(kk + 1) * 128],
                                                                   in_=tmp[:, kf * 128:(kf + 1) * 128], identity=identf[:]),
                         reads=[b_tmp, b_identf], writes=[pbf], inc=(kk == 3))
                k.op("act", lambda e, hh=hh: e.copy(out=hTf[:, hh * 4:(hh + 1) * 4, :],
                                                    in_=psf[:, :].rearrange("p (k t) -> p k t", t=128)),
                     reads=[pbf], writes=[b_hTf])
            pl, pbl = g.ps[4 + (j % 2)], g.psb[4 + (j % 2)]
            for kk in range(8):
                k.op("pe", lambda e, kk=kk: e.matmul(pl[:, 0:NEXP], lhsT=hTf[:, kk, :], rhs=rw[:, kk, :],
                                                     start=(kk == 0), stop=(kk == 7)),
                     reads=[b_hTf, b_rw], writes=[pbl], inc=(kk == 7))
            L = lg[j % 2]
            bL = b_lg[j % 2]
            k.op("dve", lambda e: e.tensor_tensor(out=L[:, 0:8], in0=pl[:, 0:NEXP], in1=rb[:], op=ALU.add),
                 reads=[pbl, b_rw], writes=[bL])
            k.op("dve", lambda e: e.max(out=L[:, 8:16], in_=L[:, 0:8]), reads=[bL], writes=[bL])
            k.op("dve", lambda e: e.tensor_scalar(out=L[:, 16:17], in0=L[:, 8:9], scalar1=-1.0, scalar2=None,
                                                   op0=ALU.mult), reads=[bL], writes=[bL])
            k.op("act", lambda e: e.activation(out=L[:, 17:18], in_=L[:, 9:10], func=AF.Exp, bias=L[:, 16:17]),
                 reads=[bL], writes=[bL])
            k.op("dve", lambda e: e.tensor_scalar(out=L[:, 20:21], in0=L[:, 17:18], scalar1=1.0, scalar2=None,
                                                   op0=ALU.add), reads=[bL], writes=[bL])
            k.op("dve", lambda e: e.reciprocal(out=L[:, 18:19], in_=L[:, 20:21]), reads=[bL], writes=[bL])
            k.op("dve", lambda e: e.tensor_tensor(out=L[:, 19:20], in0=L[:, 17:18], in1=L[:, 18:19], op=ALU.mult),
                 reads=[bL], writes=[bL])
            k.op("dve", lambda e: e.tensor_scalar(out=L[:, 24:32], in0=L[:, 0:8], scalar1=L[:, 8:9], scalar2=L[:, 18:19],
                                                   op0=ALU.is_equal, op1=ALU.mult), reads=[bL], writes=[bL])
            k.op("dve", lambda e: e.tensor_scalar(out=L[:, 32:40], in0=L[:, 0:8], scalar1=L[:, 9:10], scalar2=L[:, 19:20],
                                                   op0=ALU.is_equal, op1=ALU.mult), reads=[bL], writes=[bL])
            k.op("dve", lambda e: e.tensor_tensor(out=comb[:, j, :], in0=L[:, 24:32], in1=L[:, 32:40], op=ALU.add),
                 reads=[bL], writes=[b_comb])
        for ex in range(NEXP):
            for fg in range(NFG):
                w_i = wi % 2
                wi += 1
                bw = b_w[w_i]
                fs = slice(fg * 512, (fg + 1) * 512)
                k.dma("pool", wg[w_i][:], g.gin("moe_w_in")[ex, :, fg * 512:(fg + 1) * 512].rearrange("(k p) n -> p k n", p=128),
                      writes=[bw])
                k.dma("pool", wu[w_i][:], g.gin("moe_w_in")[ex, :, EDIM + fg * 512:EDIM + (fg + 1) * 512].rearrange("(k p) n -> p k n", p=128),
                      writes=[bw])
                k.dma("pool", wo[w_i][:], g.gin("moe_w_out")[ex, fg * 512:(fg + 1) * 512, :].rearrange("(c p) d -> p c d", p=128),
                      writes=[bw])
                for c4 in range(4):
                    k.op("pool", lambda e, c4=c4: e.tensor_tensor(out=wo[w_i][:, c4, :], in0=wo[w_i][:, c4, :], in1=gatebc[:], op=ALU.mult),
                         reads=[bw, b_mod], writes=[bw])
                for tg in range(4):
                    ts_ = slice(tg * 512, (tg + 1) * 512)
                    for fc in range(4):
                        pg, pu = g.ps[(pi % 2) * 2], g.ps[(pi % 2) * 2 + 1]
                        bg, bu = g.psb[(pi % 2) * 2], g.psb[(pi % 2) * 2 + 1]
                        pi += 1
                        for kk in range(8):
                            k.op("pe", lambda e, kk=kk: e.matmul(pg[:, :], lhsT=wg[w_i][:, kk, fc * 128:(fc + 1) * 128],
                                                                 rhs=hT[:, kk, ts_], start=(kk == 0), stop=(kk == 7)),
                                 reads=[bw, b_hT], writes=[bg], inc=(kk == 7))
                        for kk in range(8):
                            k.op("pe", lambda e, kk=kk: e.matmul(pu[:, :], lhsT=wu[w_i][:, kk, fc * 128:(fc + 1) * 128],
                                                                 rhs=hT[:, kk, ts_], start=(kk == 0), stop=(kk == 7)),
                                 reads=[bw, b_hT], writes=[bu], inc=(kk == 7))
                        s_i = si % 2
                        si += 1
                        k.op("act", lambda e: e.activation(out=sg[s_i][:], in_=pg[:, :], func=AF.Silu),
                             reads=[bg], writes=[b_sg[s_i]])
                        k.op("dve", lambda e: e.tensor_tensor(out=aT[:, fc, ts_], in0=sg[s_i][:], in1=pu[:, :], op=ALU.mult),
                             reads=[b_sg[s_i], bu], writes=[b_aT[tg]])
                for j in range(NTL):
                    for half in range(2):
                        hs = slice(half * 512, (half + 1) * 512)
                        po, pbo = g.ps[4 + (oi % 4)], g.psb[4 + (oi % 4)]
                        to, bto = tmpo[oi % 2], b_to[oi % 2]
                        oi += 1
                        for fc in range(4):
                            k.op("pe", lambda e, fc=fc: e.matmul(po[:, :], lhsT=aT[:, fc, j * 128:(j + 1) * 128],
                                                                 rhs=wo[w_i][:, fc, hs], start=(fc == 0), stop=(fc == 3)),
                                 reads=[b_aT[j // 4], bw], writes=[pbo], inc=(fc == 3))
                        k.op("dve", lambda e: e.scalar_tensor_tensor(out=acc[:, j, hs], in0=po[:, :], scalar=comb[:, j, ex:ex + 1],
                                                                      in1=acc[:, j, hs], op0=ALU.mult, op1=ALU.add),
                             reads=[pbo, b_comb, b_acc[j]], writes=[b_acc[j]])
        for j in range(NTL):
            xt = acc[:, j, :]
            i = ns.i % ns.nbuf
            ns.i += 1
            rstd, b_st = rstd_of(g, ns, i, xt, b_acc[j])
            k.op("dve", lambda e: e.scalar_tensor_tensor(out=xt, in0=xt, scalar=rstd, in1=nfin[:],
                                                          op0=ALU.mult, op1=ALU.mult),
                 reads=[b_acc[j], b_st, b_nfin], writes=[b_acc[j]])
            k.dma("sp", g.out[b, j * 128:(j + 1) * 128, :], xt, reads=[b_acc[j]], writes=[g.bufs["out"]])


_CACHE = {}


def host_consts():
    r = np.arange(128)[:, None]
    c = np.arange(128)[None, :]
    cmat = np.stack([(r <= c), (r > c), (r < c), (r >= c), np.ones((128, 128), bool)]).astype(np.float32)
    rr = np.arange(128)[:, None] // 16
    cc = np.arange(128)[None, :] // 16
    m8 = np.stack([(rr <= cc), (rr >= cc)]).astype(np.float32)
    return {"ident": np.eye(128, dtype=np.float32), "cmat": cmat, "m8": m8}


def s5_lane_tables(inp):
    out = np.empty((2, 3, 128, 32), np.float32)
    for d in range(2):
        for i, a in enumerate((inp["s5_lam_re"][0, d], inp["s5_lam_im"][0, d])):
            out[d, i] = a.reshape(32, 2, 64).transpose(1, 2, 0).reshape(128, 32)
        ls = np.repeat(inp["s5_log_step"][0, d][:, None], 64, axis=1)
        out[d, 2] = ls.reshape(32, 2, 64).transpose(1, 2, 0).reshape(128, 32)
    return out


def s5_c_lanes(inp):
    out = np.empty((2, 2, 128, 32, 16), np.float32)
    for d in range(2):
        for i, a in enumerate((inp["s5_c_re"][0, d], inp["s5_c_im"][0, d])):
            out[d, i] = a.reshape(32, 2, 16, 64).transpose(1, 3, 0, 2).reshape(128, 32, 16)
    return out


def core_inputs(inp, core):
    b0 = core * NB
    cT = np.ascontiguousarray(np.stack([inp["c"][b0], inp["c"][b0 + 1], inp["c_ctx"]], axis=1))
    m = {
        "x": np.ascontiguousarray(inp["x"][b0:b0 + NB]),
        "ctx": np.ascontiguousarray(inp["ctx"][b0:b0 + NB]),
        "cT": cT,
        "ada_w": inp["ada_w"], "ada_b": inp["ada_b"],
        "norm_mix": inp["norm_mix"], "norm_ffn": inp["norm_ffn"],
        "ffn_w_in": inp["ffn_w_in"][0], "ffn_w_out": inp["ffn_w_out"][0],
        "moe_router_w": inp["moe_router_w"][0], "moe_router_b": inp["moe_router_b"].reshape(1, NEXP),
        "moe_w_in": inp["moe_w_in"][0], "moe_w_out": inp["moe_w_out"][0],
        "norm_final": inp["norm_final"].reshape(1, D),
        "ssd_w_in": inp["ssd_w_in"][0],
        "convT": np.ascontiguousarray(np.concatenate([inp["ssd_conv_w"][0], inp["ssd_conv_b"]], axis=0).T),
        "ssd_dt_bias": inp["ssd_dt_bias"].reshape(1, 64), "ssd_a_log": inp["ssd_a_log"].reshape(1, 64),
        "ssd_d": inp["ssd_d"].reshape(1, 32), "ssd_norm": inp["ssd_norm"].reshape(1, 2048),
        "ssd_w_out": inp["ssd_w_out"][0],
        "s5_lamT": s5_lane_tables(inp),
        "s5_b": np.ascontiguousarray(np.stack([inp["s5_b_re"][0], inp["s5_b_im"][0]], axis=1).reshape(2, 2, 4096, 16)),
        "s5_cT": s5_c_lanes(inp),
        "s5_d": inp["s5_d"].reshape(1, D), "s5_w_glu": inp["s5_w_glu"][0], "s5_b_glu": inp["s5_b_glu"].reshape(1, 2 * D),
    }
    m.update(host_consts())
    return m


def kernel(**inp):
    inp = {kk: np.asarray(v) for kk, v in inp.items()}
    if "prog" not in _CACHE:
        _CACHE["prog"] = build_program(("ada", "ssd", "ffn", "s5", "moe"), {})
    nc, g = _CACHE["prog"]
    in_maps = []
    for core in range(8):
        m = core_inputs(inp, core)
        in_maps.append({kk: np.ascontiguousarray(v) for kk, v in m.items() if kk in g.inputs})
    res = run_bass_kernel_spmd(nc, in_maps, core_ids=list(range(8)))
    return np.concatenate([r["out"] for r in res.results], axis=0).astype(np.float32)
```

```python
import numpy as np
from contextlib import ExitStack
import concourse.bass as bass
import concourse.mybir as mybir
from concourse.bass_utils import run_bass_kernel_spmd

F32 = mybir.dt.float32
BF16 = mybir.dt.bfloat16
I32 = mybir.dt.int32
AF = mybir.ActivationFunctionType
ALU = mybir.AluOpType
AX = mybir.AxisListType
AP = bass.AP

D = 1024
NB = 2
T = 2048
TC = 256
EPS = 1e-6
FFN = 2816
NEXP = 8
EDIM = 3584


class Buf:
    __slots__ = ("name", "w", "r", "ps")

    def __init__(self, name="", ps=False):
        self.name = name
        self.w = {}
        self.r = {}
        self.ps = ps


class KB:
    RING = 8

    def __init__(self, nc, es):
        self.nc = nc
        self.es = es
        self.E = dict(pe=nc.tensor, act=nc.scalar, dve=nc.vector, pool=nc.gpsimd, sp=nc.sync)
        self.sem = {}
        self.cnt = {}
        for e in self.E:
            self.sem[e] = es.enter_context(nc.semaphore("s_" + e))
            self.cnt[e] = 0
        self.ring = {}
        self.ring_i = {}
        for q in ("sp", "pool", "act"):
            self.ring[q] = []
            for i in range(self.RING):
                key = ("d", q, i)
                self.sem[key] = es.enter_context(nc.semaphore("d_%s%d" % (q, i)))
                self.cnt[key] = 0
                self.ring[q].append(key)
            self.ring_i[q] = 0
        self.known = {e: {} for e in self.E}
        self.n_wait = 0

    def _deps(self, eng, reads, writes):
        need = {}

        def add(sk, v):
            if v > need.get(sk, 0):
                need[sk] = v

        for b in reads:
            for sk, v in b.w.items():
                if sk == eng and eng == "pe":
                    continue
                add(sk, v)
        for b in writes:
            for sk, v in b.r.items():
                if sk == eng and eng == "pe":
                    continue
                add(sk, v)
            for sk, v in b.w.items():
                if sk == eng and eng == "pe":
                    continue
                add(sk, v)
        kn = self.known[eng]
        out = []
        for sk, v in need.items():
            if kn.get(sk, 0) >= v:
                continue
            kn[sk] = v
            out.append((sk, v))
        return out

    def _emit_waits(self, eng, waits):
        E = self.E[eng]
        for sk, v in waits:
            E.wait_ge(self.sem[sk], v)
            self.n_wait += 1

    def _record(self, ev, reads, writes):
        sk, v = ev
        for b in reads:
            if v > b.r.get(sk, 0):
                b.r[sk] = v
        for b in writes:
            if b.r:
                b.w = {sk: v}
                b.r = {}
            else:
                if v > b.w.get(sk, 0):
                    b.w[sk] = v

    def op(self, eng, fn, reads=(), writes=(), inc=True):
        if any(b.ps for b in reads):
            writes = list(writes) + [b for b in reads if b.ps]
            reads = [b for b in reads if not b.ps]
        self._emit_waits(eng, self._deps(eng, reads, writes))
        ins = fn(self.E[eng])
        if inc:
            self.cnt[eng] += 1
            ins.then_inc(self.sem[eng], 1)
            ev = (eng, self.cnt[eng])
        else:
            ev = (eng, self.cnt[eng] + 1)
        self._record(ev, reads, writes)
        return ins

    def dma(self, q, out, in_, reads=(), writes=()):
        key = self.ring[q][self.ring_i[q] % self.RING]
        self.ring_i[q] += 1
        waits = self._deps(q, reads, writes)
        prev = self.cnt[key]
        if prev > 0 and self.known[q].get(key, 0) < prev:
            self.known[q][key] = prev
            waits.append((key, prev))
        self._emit_waits(q, waits)
        ins = self.E[q].dma_start(out=out, in_=in_)
        self.cnt[key] = prev + 16
        ins.then_inc(self.sem[key], 16)
        self._record((key, prev + 16), reads, writes)
        return ins

    def barrier(self):
        for e in self.E:
            waits = []
            for sk, v in self.cnt.items():
                if v == 0 or sk == e and e == "pe":
                    continue
                if self.known[e].get(sk, 0) >= v:
                    continue
                self.known[e][sk] = v
                waits.append((sk, v))
            self._emit_waits(e, waits)

    def final_wait(self):
        waits = []
        for sk, v in self.cnt.items():
            if v and self.known["sp"].get(sk, 0) < v:
                self.known["sp"][sk] = v
                waits.append((sk, v))
        self._emit_waits("sp", waits)


def rev_ap(ap, n):
    a = ap.ap
    assert len(a) == 2 and a[1][1] == n
    return AP(ap.tensor, ap.offset + (n - 1) * a[1][0], [list(a[0]), [-a[1][0], n]])


class Ctx:
    pass


def build_program(phases, kinds):
    nc = bass.Bass("TRN2", target_bir_lowering=False)
    g = Ctx()
    g.nc = nc
    g.phases = phases
    g.kinds = kinds

    def din(name, shape, dt=F32):
        return nc.dram_tensor(name, list(shape), dt, kind="ExternalInput").ap()

    def dmid(name, shape, dt=F32):
        kind = {"in": "ExternalInput", "out": "ExternalOutput", "int": "Internal"}[kinds.get(name, "int")]
        return nc.dram_tensor(name, list(shape), dt, kind=kind).ap()

    g.inputs = {}
    SH = dict(x=[NB, T, D], ctx=[NB, TC, D], cT=[D, 3], ada_w=[2, D, 6 * D], ada_b=[2, 6 * D],
              norm_mix=[2, D], norm_ffn=[2, D], ffn_w_in=[D, 2 * FFN], ffn_w_out=[FFN, D],
              moe_router_w=[D, NEXP], moe_router_b=[1, NEXP], moe_w_in=[NEXP, D, 2 * EDIM],
              moe_w_out=[NEXP, EDIM, D], norm_final=[1, D], ident=[128, 128],
              ssd_w_in=[D, 5184], convT=[3072, 6], ssd_dt_bias=[1, 64], ssd_a_log=[1, 64], ssd_d=[1, 32],
              ssd_norm=[1, 2048], ssd_w_out=[2048, D], cmat=[5, 128, 128],
              s5_lamT=[2, 3, 128, 32], s5_b=[2, 2, 4096, 16], s5_cT=[2, 2, 128, 32, 16], m8=[2, 128, 128],
              s5_d=[1, D], s5_w_glu=[D, 2 * D], s5_b_glu=[1, 2 * D])
    g.SH = SH

    def gin(name):
        if name not in g.inputs:
            g.inputs[name] = din(name, SH[name])
        return g.inputs[name]
    g.gin = gin
    g.x = gin("x")
    g.ctx = gin("ctx")
    g.mod = dmid("mod", [2, 3, 6 * D])
    g.x1 = dmid("x1", [NB, T, D])
    g.c1 = dmid("c1", [NB, TC, D])
    g.x2 = dmid("x2", [NB, T, D])
    g.c2 = dmid("c2", [NB, TC, D])
    g.x3 = dmid("x3", [NB, T, D])
    NTK = T + TC
    g.xbcT = dmid("xbcT", [NB, 3072, NTK], BF16)
    g.zs = dmid("zs", [NB, NTK, 2048], BF16)
    g.dd = dmid("dd", [NB, NTK, 128])
    g.yf = dmid("yf", [NB, NTK, 2048])
    g.s5w = dmid("s5w", [2, 32, 128, 6, 128], BF16)
    g.etab = dmid("etab", [2, 2, 128, 32, 288])
    g.rho = dmid("rho", [2, 128, 32])
    g.out = nc.dram_tensor("out", [NB, T, D], F32, kind="ExternalOutput").ap()
    g.bufs = {n: Buf(n) for n in ("x", "ctx", "mod", "x1", "c1", "x2", "c2", "x3", "out", "xbcT", "zs", "dd", "yf", "s5w", "etab", "rho")}

    with ExitStack() as es:
        k = KB(nc, es)
        g.k = k
        g.ps = [es.enter_context(nc.psum_tensor("ps%d" % i, [128, 512], F32)) for i in range(8)]
        g.psb = [Buf("ps%d" % i, ps=True) for i in range(8)]
        for ph in phases:
            with ExitStack() as pes:
                g.pes = pes
                {"ada": phase_ada, "ffn": phase_ffn, "moe": phase_moe, "ssd": phase_ssd, "s5": phase_s5}[ph](g)
                k.barrier()
        k.final_wait()
    g.kinds = kinds
    return nc, g


def sbt(g, name, shape, dt):
    return g.pes.enter_context(g.nc.sbuf_tensor(name, list(shape), dt))


def bcast_rows(ap_row, nparts):
    a = ap_row.ap
    return AP(ap_row.tensor, ap_row.offset, [[0, nparts]] + [list(x) for x in a[1:]])


def phase_ada(g):
    k, nc = g.k, g.nc
    cT = sbt(g, "a_cT", [128, 8, 3], F32)
    cs = sbt(g, "a_cs", [128, 8, 3], BF16)
    row = sbt(g, "a_row", [3, 6 * D], F32)
    bias = sbt(g, "a_bias", [3, 6 * D], F32)
    nrm = sbt(g, "a_nrm", [3, D], F32)
    wts = [sbt(g, "a_w%d" % i, [128, 8, 512], BF16) for i in range(2)]
    b_cT, b_cs, b_row, b_bias, b_nrm = Buf(), Buf(), Buf(), Buf(), Buf()
    b_w = [Buf(), Buf()]
    k.dma("sp", cT[:], g.gin("cT").rearrange("(k p) m -> p k m", p=128), writes=[b_cT])
    k.op("act", lambda e: e.activation(out=cs[:], in_=cT[:], func=AF.Silu), reads=[b_cT], writes=[b_cs])
    it = 0
    for layer in range(2):
        k.dma("sp", bias[:], bcast_rows(g.gin("ada_b")[layer:layer + 1, :], 3), writes=[b_bias])
        for j in range(12):
            w = wts[it % 2]
            bw = b_w[it % 2]
            it += 1
            k.dma("pool", w[:], g.gin("ada_w")[layer, :, j * 512:(j + 1) * 512].rearrange("(k p) n -> p k n", p=128), writes=[bw])
            ps = g.ps[it % 2]
            pb = g.psb[it % 2]
            for kk in range(8):
                k.op("pe", lambda e, kk=kk: e.matmul(ps[0:3, :], lhsT=cs[:, kk, :], rhs=w[:, kk, :],
                                                     start=(kk == 0), stop=(kk == 7)),
                     reads=[b_cs, bw], writes=[pb], inc=(kk == 7))
            k.op("dve", lambda e: e.tensor_tensor(out=row[:, j * 512:(j + 1) * 512], in0=ps[0:3, :],
                                                   in1=bias[:, j * 512:(j + 1) * 512], op=ALU.add),
                 reads=[pb, b_bias], writes=[b_row])
        for slot, nw in ((1, g.gin("norm_mix")), (4, g.gin("norm_ffn"))):
            k.dma("sp", nrm[:], bcast_rows(nw[layer:layer + 1, :], 3), writes=[b_nrm])
            k.op("dve", lambda e, slot=slot: e.scalar_tensor_tensor(
                out=row[:, slot * D:(slot + 1) * D], in0=row[:, slot * D:(slot + 1) * D], scalar=1.0,
                in1=nrm[:], op0=ALU.add, op1=ALU.mult), reads=[b_row, b_nrm], writes=[b_row])
        k.dma("sp", g.mod[layer], row[:], reads=[b_row], writes=[g.bufs["mod"]])


class NormSet:
    def __init__(self, g, pfx, nbuf=2):
        self.g = g
        self.junk = sbt(g, pfx + "junk", [128, 2 * D], BF16)
        self.b_junk = Buf()
        self.st = [sbt(g, pfx + "st%d" % i, [128, 4], F32) for i in range(nbuf)]
        self.b_st = [Buf() for _ in range(nbuf)]
        self.tmp = [sbt(g, pfx + "tmp0", [128, D], F32)] * nbuf
        self.b_tmp = [Buf()] * nbuf
        self.hb = [sbt(g, pfx + "hb%d" % i, [128, D], BF16) for i in range(nbuf)]
        self.b_hb = [Buf() for _ in range(nbuf)]
        self.ident = sbt(g, pfx + "ident", [128, 128], BF16)
        self.b_ident = Buf()
        g.k.dma("pool", self.ident[:], g.gin("ident")[:, :], writes=[self.b_ident])
        self.i = 0
        self.nbuf = nbuf


def rstd_of(g, ns, i, xt, b_xt, dim=D):
    k = g.k
    P = xt.ap[0][1]
    st, b_st = ns.st[i], ns.b_st[i]
    k.op("act", lambda e: e.activation(out=ns.junk[0:P, 0:dim], in_=xt, func=AF.Square, accum_out=st[0:P, 0:1]),
         reads=[b_xt], writes=[ns.b_junk, b_st])
    k.op("dve", lambda e: e.tensor_scalar(out=st[0:P, 1:2], in0=st[0:P, 0:1], scalar1=1.0 / dim, scalar2=EPS,
                                           op0=ALU.mult, op1=ALU.add), reads=[b_st], writes=[b_st])
    k.op("act", lambda e: e.sqrt(out=st[0:P, 2:3], in_=st[0:P, 1:2]), reads=[b_st], writes=[b_st])
    k.op("dve", lambda e: e.reciprocal(out=st[0:P, 3:4], in_=st[0:P, 2:3]), reads=[b_st], writes=[b_st])
    return st[0:P, 3:4], b_st


def norm_mod_T(g, ns, xt, b_xt, gbc, shbc, b_mod, hT_dst, b_hT, ps, pb):
    k = g.k
    i = ns.i % ns.nbuf
    ns.i += 1
    rstd, b_st = rstd_of(g, ns, i, xt, b_xt)
    tmp, b_tmp, hb, b_hb = ns.tmp[i], ns.b_tmp[i], ns.hb[i], ns.b_hb[i]
    k.op("dve", lambda e: e.scalar_tensor_tensor(out=tmp[:], in0=xt, scalar=rstd, in1=gbc,
                                                  op0=ALU.mult, op1=ALU.mult),
         reads=[b_xt, b_st, b_mod], writes=[b_tmp])
    k.op("pool", lambda e: e.tensor_tensor(out=hb[:], in0=tmp[:], in1=shbc, op=ALU.add),
         reads=[b_tmp, b_mod], writes=[b_hb])
    psv = ps.bitcast(BF16)
    for kk in range(8):
        k.op("pe", lambda e, kk=kk: e.transpose(out=psv[:, kk * 128:(kk + 1) * 128],
                                                in_=hb[:, kk * 128:(kk + 1) * 128], identity=ns.ident[:]),
             reads=[b_hb, ns.b_ident], writes=[pb], inc=(kk == 7))
    k.op("act", lambda e: e.copy(out=hT_dst, in_=psv[:, 0:1024].rearrange("p (k t) -> p k t", t=128)),
         reads=[pb], writes=[b_hT])
    return hb, b_hb


class ModBC:
    def __init__(self, g, pfx, layer, slots):
        self.g, self.layer, self.slots = g, layer, slots
        self.t = [sbt(g, "%s_m%d" % (pfx, s), [128, D], F32) for s in slots]
        self.b = Buf()
        self.cond = None

    def get(self, cond):
        g = self.g
        if cond != self.cond:
            self.cond = cond
            for t, s in zip(self.t, self.slots):
                g.k.dma("sp", t[:], bcast_rows(g.mod[self.layer, cond:cond + 1, s * D:(s + 1) * D], 128),
                        reads=[g.bufs["mod"]], writes=[self.b])
        return self.t, self.b


def seq_list(g, xsrc, csrc, xdst, cdst, with_ctx=True):
    L = []
    for b in range(NB):
        if with_ctx:
            L.append((g.__dict__[csrc][b], g.__dict__[cdst][b], 2, TC, g.bufs.get(csrc), g.bufs.get(cdst)))
        L.append((g.__dict__[xsrc][b], g.__dict__[xdst][b], b, T, g.bufs.get(xsrc), g.bufs.get(xdst)))
    return L


def phase_ffn(g):
    k, nc = g.k, g.nc
    src_x, src_c = ("x1", "c1") if "ssd" in g.phases else ("x", "ctx")
    NT = 256
    NF = FFN // 128
    w1 = sbt(g, "f_w1", [128, 8, 2 * FFN], BF16)
    w2 = sbt(g, "f_w2", [128, NF, D], BF16)
    b_w1, b_w2 = Buf(), Buf()
    for kk in range(8):
        k.dma("pool", w1[:, kk, :], g.gin("ffn_w_in")[kk * 128:(kk + 1) * 128, :], writes=[b_w1])
    for f in range(NF):
        k.dma("pool", w2[:, f, :], g.gin("ffn_w_out")[f * 128:(f + 1) * 128, :], writes=[b_w2])
    ns = NormSet(g, "f_")
    hT = [sbt(g, "f_hT%d" % i, [128, 8, NT], BF16) for i in range(2)]
    b_hT = [Buf(), Buf()]
    aT = [sbt(g, "f_aT0", [128, NF, NT], BF16)] * 2
    b_aT = [Buf()] * 2
    xt = [sbt(g, "f_xt%d" % i, [128, D], F32) for i in range(4)]
    b_xt = [Buf() for _ in range(4)]
    sg = [sbt(g, "f_sg%d" % i, [128, NT], F32) for i in range(2)]
    b_sg = [Buf(), Buf()]
    sg_o = [sbt(g, "f_o%d" % i, [128, 512], F32) for i in range(2)]
    b_sgo = [Buf(), Buf()]
    mods = ModBC(g, "f", 0, (3, 4, 5))
    grp = 0
    oi = 0
    fi = 0
    LIM, STAGE = 1000, 9
    for (src, dst, cond, ntok, bsrc, bdst) in seq_list(g, src_x, src_c, "x2", "c2")[:LIM]:
        (shbc, gbc, gatebc), b_mod = mods.get(cond)
        for t0 in range(0, ntok, NT):
            hi = grp % 2
            for j in range(NT // 128):
                xi = (grp % 2) * 2 + j
                k.dma("sp", xt[xi][:], src[t0 + j * 128:t0 + (j + 1) * 128, :], reads=[bsrc], writes=[b_xt[xi]])
                norm_mod_T(g, ns, xt[xi][:], b_xt[xi], gbc[:], shbc[:], b_mod,
                           hT[hi][:, :, j * 128:(j + 1) * 128], b_hT[hi], g.ps[4 + j], g.psb[4 + j])
            for f in range(NF if STAGE >= 2 else 0):
                pg, pu = g.ps[(fi % 2) * 2], g.ps[(fi % 2) * 2 + 1]
                bg, bu = g.psb[(fi % 2) * 2], g.psb[(fi % 2) * 2 + 1]
                si = fi % 2
                fi += 1
                for kk in range(8):
                    k.op("pe", lambda e, kk=kk: e.matmul(pg[:, 0:NT], lhsT=w1[:, kk, f * 128:(f + 1) * 128],
                                                         rhs=hT[hi][:, kk, :], start=(kk == 0), stop=(kk == 7)),
                         reads=[b_w1, b_hT[hi]], writes=[bg], inc=(kk == 7))
                for kk in range(8):
                    k.op("pe", lambda e, kk=kk: e.matmul(pu[:, 0:NT], lhsT=w1[:, kk, FFN + f * 128:FFN + (f + 1) * 128],
                                                         rhs=hT[hi][:, kk, :], start=(kk == 0), stop=(kk == 7)),
                         reads=[b_w1, b_hT[hi]], writes=[bu], inc=(kk == 7))
                k.op("act", lambda e: e.activation(out=sg[si][:], in_=pg[:, 0:NT], func=AF.Silu),
                     reads=[bg], writes=[b_sg[si]])
                k.op("dve", lambda e: e.tensor_tensor(out=aT[hi][:, f, :], in0=sg[si][:], in1=pu[:, 0:NT], op=ALU.mult),
                     reads=[b_sg[si], bu], writes=[b_aT[hi]])
            for j in range(NT // 128):
                xi = (grp % 2) * 2 + j
                o = sg_o[oi % 2]
                bo = b_sgo[oi % 2]
                oi += 1
                for half in range(2):
                    po, pbo = g.ps[6 + half], g.psb[6 + half]
                    for f in range(NF):
                        k.op("pe", lambda e, f=f: e.matmul(po[:, :], lhsT=aT[hi][:, f, j * 128:(j + 1) * 128],
                                                           rhs=w2[:, f, half * 512:(half + 1) * 512],
                                                           start=(f == 0), stop=(f == NF - 1)),
                             reads=[b_aT[hi], b_w2], writes=[pbo], inc=(f == NF - 1))
                    hs = slice(half * 512, (half + 1) * 512)
                    o = sg_o[oi % 2]
                    bo = b_sgo[oi % 2]
                    oi += 1
                    k.op("dve", lambda e: e.tensor_tensor(out=o[:], in0=po[:, :], in1=gatebc[:, hs], op=ALU.mult),
                         reads=[pbo, b_mod], writes=[bo])
                    k.op("pool", lambda e: e.tensor_tensor(out=xt[xi][:, hs], in0=o[:], in1=xt[xi][:, hs], op=ALU.add),
                         reads=[bo, b_xt[xi]], writes=[b_xt[xi]])
                k.dma("sp", dst[t0 + j * 128:t0 + (j + 1) * 128, :], xt[xi][:], reads=[b_xt[xi]], writes=[bdst])
            grp += 1


NTK = T + TC


def phase_ssd(g):
    with ExitStack() as pes:
        g.pes = pes
        ssd_in(g)
        g.k.barrier()
    with ExitStack() as pes:
        g.pes = pes
        ssd_scan(g)
        g.k.barrier()


def ssd_in(g):
    k, nc = g.k, g.nc
    w = sbt(g, "s_w", [128, 8, 5184], BF16)
    b_w = Buf()
    for kk in range(8):
        k.dma("pool", w[:, kk, :], g.gin("ssd_w_in")[kk * 128:(kk + 1) * 128, :], writes=[b_w])
    cw = sbt(g, "s_cw", [128, 24, 6], F32)
    b_cw = Buf()
    k.dma("sp", cw[:], g.gin("convT").rearrange("(c p) k -> p c k", p=128), writes=[b_cw])
    dtb = sbt(g, "s_dtb", [128, 64], F32)
    abc = sbt(g, "s_abc", [128, 64], F32)
    b_dtb, b_abc = Buf(), Buf()
    k.dma("sp", dtb[:], bcast_rows(g.gin("ssd_dt_bias")[0:1, :], 128), writes=[b_dtb])
    k.dma("sp", abc[:], bcast_rows(g.gin("ssd_a_log")[0:1, :], 128), writes=[b_abc])
    k.op("act", lambda e: e.activation(out=abc[:], in_=abc[:], func=AF.Exp), reads=[b_abc], writes=[b_abc])
    k.op("dve", lambda e: e.tensor_scalar(out=abc[:], in0=abc[:], scalar1=-1.0, scalar2=None, op0=ALU.mult),
         reads=[b_abc], writes=[b_abc])
    ns = NormSet(g, "s_")
    hT = sbt(g, "s_hT", [128, 8, NTK], BF16)
    b_hT = Buf()
    xt = [sbt(g, "s_xt%d" % i, [128, D], F32) for i in range(2)]
    b_xt = [Buf(), Buf()]
    pre = [sbt(g, "s_pre%d" % i, [128, 2312], F32) for i in range(2)]
    b_pre = [Buf(), Buf()]
    for p_ in pre:
        k.op("pool", lambda e, p_=p_: e.memset(p_[:], 0.0), writes=[b_pre[0], b_pre[1]])
    accv = sbt(g, "s_acc", [128, NTK], F32)
    b_accv = Buf()
    xo = [sbt(g, "s_xo%d" % i, [128, NTK], BF16) for i in range(2)]
    b_xo = [Buf(), Buf()]
    zt = [sbt(g, "s_zt%d" % i, [128, 2048], BF16) for i in range(2)]
    b_zt = [Buf(), Buf()]
    ddt = [sbt(g, "s_dd%d" % i, [128, 192], F32) for i in range(2)]
    b_ddt = [Buf(), Buf()]
    mods = ModBC(g, "s", 0, (0, 1))
    xi = 0
    pi = 0
    for b in range(NB):
        for (src, cond, ntok, off, bsrc) in ((g.ctx[b], 2, TC, 0, g.bufs["ctx"]), (g.x[b], b, T, TC, g.bufs["x"])):
            (shbc, gbc), b_mod = mods.get(cond)
            for j in range(ntok // 128):
                x_ = xt[xi % 2]
                bx = b_xt[xi % 2]
                k.dma("sp", x_[:], src[j * 128:(j + 1) * 128, :], reads=[bsrc], writes=[bx])
                norm_mod_T(g, ns, x_[:], bx, gbc[:], shbc[:], b_mod,
                           hT[:, :, off + j * 128:off + (j + 1) * 128], b_hT, g.ps[6 + xi % 2], g.psb[6 + xi % 2])
                xi += 1
        for ct in range(24):
            pr, bpr = pre[ct % 2], b_pre[ct % 2]
            for (c0, c1) in ((0, 256), (256, 768), (768, 1280), (1280, 1792), (1792, 2304)):
                ps, pb = g.ps[pi % 4], g.psb[pi % 4]
                pi += 1
                n = c1 - c0
                for kk in range(8):
                    k.op("pe", lambda e, kk=kk: e.matmul(ps[:, 0:n], lhsT=w[:, kk, 2048 + ct * 128:2048 + (ct + 1) * 128],
                                                         rhs=hT[:, kk, c0:c1], start=(kk == 0), stop=(kk == 7)),
                         reads=[b_w, b_hT], writes=[pb], inc=(kk == 7))
                po = 2 + c0 if c0 < 256 else c0 + 6
                k.op("act", lambda e: e.copy(out=pr[:, po:po + n], in_=ps[:, 0:n]), reads=[pb], writes=[bpr])
            for (a0, n, p0) in ((0, 256, 0), (256, 2048, 260)):
                k.op("dve", lambda e: e.tensor_scalar(out=accv[:, a0:a0 + n], in0=pr[:, p0:p0 + n],
                                                       scalar1=cw[:, ct, 0:1], scalar2=None, op0=ALU.mult),
                     reads=[bpr, b_cw], writes=[b_accv])
                for q in range(1, 5):
                    k.op("dve", lambda e, q=q: e.scalar_tensor_tensor(out=accv[:, a0:a0 + n], in0=pr[:, p0 + q:p0 + q + n],
                                                                       scalar=cw[:, ct, q:q + 1], in1=accv[:, a0:a0 + n],
                                                                       op0=ALU.mult, op1=ALU.add),
                         reads=[bpr, b_cw, b_accv], writes=[b_accv])
            o_, bo = xo[ct % 2], b_xo[ct % 2]
            k.op("act", lambda e: e.activation(out=o_[:], in_=accv[:], func=AF.Silu, bias=cw[:, ct, 5:6]),
                 reads=[b_accv, b_cw], writes=[bo])
            k.dma("sp", g.xbcT[b, ct * 128:(ct + 1) * 128, :], o_[:], reads=[bo], writes=[g.bufs["xbcT"]])
        for j in range(NTK // 128):
            z_, bz = zt[j % 2], b_zt[j % 2]
            for n4 in range(4):
                ps, pb = g.ps[pi % 4], g.psb[pi % 4]
                pi += 1
                for kk in range(8):
                    k.op("pe", lambda e, kk=kk: e.matmul(ps[:, :], lhsT=hT[:, kk, j * 128:(j + 1) * 128],
                                                         rhs=w[:, kk, n4 * 512:(n4 + 1) * 512], start=(kk == 0), stop=(kk == 7)),
                         reads=[b_w, b_hT], writes=[pb], inc=(kk == 7))
                k.op("act", lambda e: e.activation(out=z_[:, n4 * 512:(n4 + 1) * 512], in_=ps[:, :], func=AF.Silu),
                     reads=[pb], writes=[bz])
            k.dma("sp", g.zs[b, j * 128:(j + 1) * 128, :], z_[:], reads=[bz], writes=[g.bufs["zs"]])
            ps, pb = g.ps[pi % 4], g.psb[pi % 4]
            pi += 1
            for kk in range(8):
                k.op("pe", lambda e, kk=kk: e.matmul(ps[:, 0:64], lhsT=hT[:, kk, j * 128:(j + 1) * 128],
                                                     rhs=w[:, kk, 5120:5184], start=(kk == 0), stop=(kk == 7)),
                     reads=[b_w, b_hT], writes=[pb], inc=(kk == 7))
            d_, bd = ddt[j % 2], b_ddt[j % 2]
            k.op("dve", lambda e: e.tensor_tensor(out=d_[:, 128:192], in0=ps[:, 0:64], in1=dtb[:], op=ALU.add),
                 reads=[pb, b_dtb], writes=[bd])
            k.op("act", lambda e: e.activation(out=d_[:, 128:192], in_=d_[:, 128:192], func=AF.Exp), reads=[bd], writes=[bd])
            k.op("act", lambda e: e.activation(out=d_[:, 0:64], in_=d_[:, 128:192], func=AF.Ln, bias=1.0), reads=[bd], writes=[bd])
            k.op("dve", lambda e: e.tensor_tensor(out=d_[:, 64:128], in0=d_[:, 0:64], in1=abc[:], op=ALU.mult),
                 reads=[bd, b_abc], writes=[bd])
            k.dma("sp", g.dd[b, j * 128:(j + 1) * 128, :], d_[:, 0:128], reads=[bd], writes=[g.bufs["dd"]])


def bc3(ap2, n_in, n_rep):
    a = ap2.ap
    return AP(ap2.tensor, ap2.offset, [list(a[0]), [a[1][0], n_in], [0, n_rep]])


def ssd_scan(g):
    k, nc = g.k, g.nc
    cm = sbt(g, "q_cm", [128, 5, 128], F32)
    b_cm = Buf()
    k.dma("sp", cm[:], g.gin("cmat").rearrange("m p c -> p m c"), writes=[b_cm])
    identb = sbt(g, "q_identb", [128, 128], BF16)
    b_id = Buf()
    k.dma("pool", identb[:], g.gin("ident")[:, :], writes=[b_id])
    wout = sbt(g, "q_wout", [128, 16, D], BF16)
    b_wout = Buf()
    for c in range(16):
        k.dma("pool", wout[:, c, :], g.gin("ssd_w_out")[c * 128:(c + 1) * 128, :], writes=[b_wout])
    nwbc = sbt(g, "q_nw", [128, 2048], F32)
    dsk = sbt(g, "q_dsk", [128, 32], F32)
    b_nw = Buf()
    k.dma("sp", nwbc[:], bcast_rows(g.gin("ssd_norm")[0:1, :], 128), writes=[b_nw])
    k.dma("sp", dsk[:], bcast_rows(g.gin("ssd_d")[0:1, :], 128), writes=[b_nw])
    gate = {}
    S_all = [sbt(g, "q_S%d" % i, [128, 4, 512], F32) for i in range(NB)]
    Sb_all = [sbt(g, "q_Sb%d" % i, [128, 4, 512], BF16) for i in range(NB)]
    b_S_all = [[Buf() for _ in range(4)] for _ in range(NB)]
    b_Sb_all = [[Buf() for _ in range(4)] for _ in range(NB)]
    FM = [sbt(g, "q_FM%d" % i, [128, 24, 128], BF16) for i in range(2)]
    b_FM = [Buf(), Buf()]
    ddT = [sbt(g, "q_dd%d" % i, [128, 128], F32) for i in range(2)]
    b_dd = [Buf(), Buf()]
    Xtok = [sbt(g, "q_X%d" % i, [128, 2048], BF16) for i in range(2)]
    b_X = [Buf(), Buf()]
    Btok = [sbt(g, "q_B%d" % i, [128, 512], BF16) for i in range(2)]
    b_B = [Buf(), Buf()]
    cue = [sbt(g, "q_cue%d" % i, [128, 128], F32) for i in range(2)]
    b_cue = [Buf(), Buf()]
    xdt = sbt(g, "q_xdt", [128, 2048], BF16)
    xdtu = sbt(g, "q_xdtu", [128, 2048], BF16)
    b_xdt, b_xdtu = Buf(), Buf()
    sm = [sbt(g, "q_sm%d" % i, [128, 128], BF16) for i in range(2)]
    b_sm = [Buf(), Buf()]
    lh = [sbt(g, "q_lh%d" % i, [128, 128], F32) for i in range(8)]
    b_lh = [Buf() for _ in range(8)]
    Em = [sbt(g, "q_E%d" % i, [128, 128], BF16) for i in range(8)]
    b_E = [Buf() for _ in range(8)]
    Lm = [sbt(g, "q_L%d" % i, [128, 128], BF16) for i in range(8)]
    b_L = [Buf() for _ in range(8)]
    BURST = 8
    t1 = [sbt(g, "q_t1%d" % i, [128, 512], F32) for i in range(2)]
    b_t1 = [Buf(), Buf()]
    yblk = [sbt(g, "q_y%d" % i, [128, 2048], F32) for i in range(2)]
    b_y = [Buf(), Buf()]
    yfl = sbt(g, "q_yfl", [128, 2048], F32)
    b_yfl = Buf()
    ztl = sbt(g, "q_zt", [128, 2048], BF16)
    b_ztl = Buf()
    ygn = sbt(g, "q_ygn", [128, 2048], BF16)
    b_ygn = Buf()
    ygT = sbt(g, "q_ygT", [128, 16, 128], BF16)
    b_ygT = Buf()
    xt = [sbt(g, "q_xt%d" % i, [128, D], F32) for i in range(2)]
    b_xt = [Buf(), Buf()]
    ot = [sbt(g, "q_ot%d" % i, [128, 512], F32) for i in range(2)]
    b_ot = [Buf(), Buf()]
    gbc_all = [[sbt(g, "q_g%d_%d" % (b_, i), [128, D], F32) for i in range(2)] for b_ in range(NB)]
    b_g = Buf()
    ns = NormSet(g, "q_", nbuf=1)
    sc_slots = [(g.ps[0][:, i * 128:(i + 1) * 128], g.psb[0]) for i in range(3)]
    cum_ps, b_cum = g.ps[0][:, 384:480], g.psb[0]
    d_slots = [(g.ps[1 + i % 2][:, (i // 2) * 128:(i // 2 + 1) * 128], g.psb[1 + i % 2]) for i in range(8)]
    tr_b = [g.psb[6], g.psb[7]]
    cnt = dict(blk=0, sc=0, d=0, h=0, t1=0, o=0)

    for b in range(NB):
        k.dma("sp", gbc_all[b][0][:], bcast_rows(g.mod[0, 2:3, 2 * D:3 * D], 128), reads=[g.bufs["mod"]], writes=[b_g])
        k.dma("sp", gbc_all[b][1][:], bcast_rows(g.mod[0, b:b + 1, 2 * D:3 * D], 128), reads=[g.bufs["mod"]], writes=[b_g])
    for dr in range(2):
        order = list(range(18)) if dr == 0 else [1, 0] + list(range(17, 1, -1))
        CUMM, GL, RM = (0, 1, 0) if dr == 0 else (3, 2, 3)
        for b in range(NB):
            for gi in range(4):
                k.op("pool", lambda e, gi=gi, b=b: e.memset(S_all[b][:, gi, :], 0.0), writes=[b_S_all[b][gi]])
                k.op("pool", lambda e, gi=gi, b=b: e.memset(Sb_all[b][:, gi, :], 0.0), writes=[b_Sb_all[b][gi]])
        for blk in order:
            for b in range(NB):
                S, Sb, b_S, b_Sb, gbc = S_all[b], Sb_all[b], b_S_all[b], b_Sb_all[b], gbc_all[b]
                c0 = blk * 128
                bi = cnt["blk"] % 2
                cnt["blk"] += 1
                fm, bfm, dT, bdT = FM[bi], b_FM[bi], ddT[bi], b_dd[bi]
                X, bX, Bt, bB, cu, bcu = Xtok[bi], b_X[bi], Btok[bi], b_B[bi], cue[bi], b_cue[bi]
                yb, byb = yblk[bi], b_y[bi]
                k.dma("sp", fm[:], g.xbcT[b, :, c0:c0 + 128].rearrange("(c p) t -> p c t", p=128),
                      reads=[g.bufs["xbcT"]], writes=[bfm])
                k.dma("sp", dT[:], g.dd[b, c0:c0 + 128, :], reads=[g.bufs["dd"]], writes=[bdT])
                for hh in range(2):
                    psv = g.ps[6 + hh].bitcast(BF16)
                    for c in range(8):
                        k.op("pe", lambda e, c=c: e.transpose(out=psv[:, c * 128:(c + 1) * 128], in_=fm[:, hh * 8 + c, :],
                                                              identity=identb[:]),
                             reads=[bfm, b_id], writes=[tr_b[hh]], inc=(c == 7))
                    k.op("act", lambda e: e.copy(out=X[:, hh * 1024:(hh + 1) * 1024], in_=psv[:, 0:1024]),
                         reads=[tr_b[hh]], writes=[bX])
                psv = g.ps[6].bitcast(BF16)
                for c in range(4):
                    k.op("pe", lambda e, c=c: e.transpose(out=psv[:, c * 128:(c + 1) * 128], in_=fm[:, 16 + c, :],
                                                          identity=identb[:]),
                         reads=[bfm, b_id], writes=[tr_b[0]], inc=(c == 3))
                k.op("act", lambda e: e.copy(out=Bt[:], in_=psv[:, 0:512]), reads=[tr_b[0]], writes=[bB])
                da = dT[:, 64 + 32 * dr:96 + 32 * dr]
                dtc = dT[:, 32 * dr:32 * dr + 32]
                for q, mi in enumerate((CUMM, GL, 4)):
                    k.op("pe", lambda e, q=q, mi=mi: e.matmul(cum_ps[:, q * 32:(q + 1) * 32], lhsT=cm[:, mi, :], rhs=da,
                                                              start=True, stop=True),
                         reads=[b_cm, bdT], writes=[b_cum], inc=(q == 2))
                k.op("act", lambda e: e.activation(out=cu[:, 0:96], in_=cum_ps, func=AF.Exp), reads=[b_cum], writes=[bcu])
                k.op("dve", lambda e: e.tensor_tensor(out=cu[:, 96:128], in0=cu[:, 32:64], in1=dtc, op=ALU.mult),
                     reads=[bcu, bdT], writes=[bcu])
                X3 = X[:].rearrange("p (h q) -> p h q", q=64)
                k.op("dve", lambda e: e.tensor_tensor(out=xdt[:].rearrange("p (h q) -> p h q", q=64), in0=X3,
                                                       in1=bc3(dtc, 32, 64), op=ALU.mult),
                     reads=[bX, bdT], writes=[b_xdt])
                k.op("dve", lambda e: e.tensor_tensor(out=xdtu[:].rearrange("p (h q) -> p h q", q=64), in0=X3,
                                                       in1=bc3(cu[:, 96:128], 32, 64), op=ALU.mult),
                     reads=[bX, bcu], writes=[b_xdtu])
                for gi in range(4):
                    ps_s, b_ps_s = sc_slots[cnt["sc"] % 3]
                    smi = cnt["sc"] % 2
                    cnt["sc"] += 1
                    k.op("pe", lambda e: e.matmul(ps_s, lhsT=fm[:, 16 + gi, :], rhs=fm[:, 20 + gi, :], start=True, stop=True),
                         reads=[bfm], writes=[b_ps_s])
                    k.op("dve", lambda e: e.tensor_tensor(out=sm[smi][:], in0=ps_s, in1=cm[:, RM, :], op=ALU.mult),
                         reads=[b_ps_s, b_cm], writes=[b_sm[smi]])
                    yd, b_yd = g.ps[3], g.psb[3]
                    for hb0 in range(0, 8, BURST):
                        hs_ = list(range(hb0, hb0 + BURST))
                        slots = {}
                        for h8 in hs_:
                            slots[h8] = d_slots[cnt["d"] % 8]
                            cnt["d"] += 1
                        for h8 in hs_:
                            h = gi * 8 + h8
                            k.op("act", lambda e, h=h, h8=h8: e.activation(out=lh[h8][:], in_=cm[:, GL, :], func=AF.Copy, scale=da[:, h:h + 1]),
                                 reads=[b_cm, bdT], writes=[b_lh[h8]])
                        for h8 in hs_:
                            psD, b_psD = slots[h8]
                            k.op("pe", lambda e, h8=h8, psD=psD: e.matmul(psD, lhsT=lh[h8][:], rhs=cm[:, RM, :], start=True, stop=True),
                                 reads=[b_lh[h8], b_cm], writes=[b_psD])
                        for h8 in hs_:
                            psD, b_psD = slots[h8]
                            k.op("act", lambda e, h8=h8, psD=psD: e.activation(out=Em[h8][:], in_=psD, func=AF.Exp), reads=[b_psD], writes=[b_E[h8]])
                        for h8 in hs_:
                            k.op("dve", lambda e, h8=h8: e.tensor_tensor(out=Lm[h8][:], in0=Em[h8][:], in1=sm[smi][:], op=ALU.mult),
                                 reads=[b_E[h8], b_sm[smi]], writes=[b_L[h8]])
                        for h8 in hs_:
                            h = gi * 8 + h8
                            k.op("pe", lambda e, h=h, h8=h8: e.matmul(yd[:, h8 * 64:(h8 + 1) * 64], lhsT=Lm[h8][:], rhs=xdt[:, h * 64:(h + 1) * 64],
                                                                      start=True, stop=True),
                                 reads=[b_L[h8], b_xdt], writes=[b_yd])
                    yo, b_yo = g.ps[4], g.psb[4]
                    k.op("pe", lambda e: e.matmul(yo[:, :], lhsT=fm[:, 20 + gi, :], rhs=Sb[:, gi, :], start=True, stop=True),
                         reads=[bfm, b_Sb[gi]], writes=[b_yo])
                    ti = cnt["t1"] % 2
                    cnt["t1"] += 1
                    k.op("dve", lambda e: e.tensor_tensor(out=t1[ti][:].rearrange("p (h q) -> p h q", q=64),
                                                           in0=yo[:, :].rearrange("p (h q) -> p h q", q=64),
                                                           in1=bc3(cu[:, gi * 8:gi * 8 + 8], 8, 64), op=ALU.mult),
                         reads=[b_yo, bcu], writes=[b_t1[ti]])
                    k.op("dve", lambda e: e.tensor_tensor(out=yb[:, gi * 512:(gi + 1) * 512], in0=t1[ti][:], in1=yd[:, :], op=ALU.add),
                         reads=[b_t1[ti], b_yd], writes=[byb])
                    cs, b_cs = g.ps[5], g.psb[5]
                    k.op("pe", lambda e: e.matmul(cs[:, :], lhsT=Bt[:, gi * 128:(gi + 1) * 128], rhs=xdtu[:, gi * 512:(gi + 1) * 512],
                                                  start=True, stop=True),
                         reads=[bB, b_xdtu], writes=[b_cs])
                    S3 = S[:, gi, :].rearrange("p (h q) -> p h q", q=64)
                    k.op("dve", lambda e: e.tensor_tensor(out=S3, in0=S3, in1=bc3(cu[:, 64 + gi * 8:64 + gi * 8 + 8], 8, 64), op=ALU.mult),
                         reads=[b_S[gi], bcu], writes=[b_S[gi]])
                    k.op("dve", lambda e: e.tensor_tensor(out=S[:, gi, :], in0=S[:, gi, :], in1=cs[:, :], op=ALU.add),
                         reads=[b_S[gi], b_cs], writes=[b_S[gi]])
                    k.op("act", lambda e: e.copy(out=Sb[:, gi, :], in_=S[:, gi, :]), reads=[b_S[gi]], writes=[b_Sb[gi]])
                if dr == 0:
                    k.dma("sp", g.yf[b, c0:c0 + 128, :], yb[:], reads=[byb], writes=[g.bufs["yf"]])
                    continue
                k.dma("sp", yfl[:], g.yf[b, c0:c0 + 128, :], reads=[g.bufs["yf"]], writes=[b_yfl])
                k.dma("sp", ztl[:], g.zs[b, c0:c0 + 128, :], reads=[g.bufs["zs"]], writes=[b_ztl])
                is_ctx = blk < 2
                xsrc = g.ctx[b, c0:c0 + 128, :] if is_ctx else g.x[b, c0 - TC:c0 - TC + 128, :]
                xdst = g.c1[b, c0:c0 + 128, :] if is_ctx else g.x1[b, c0 - TC:c0 - TC + 128, :]
                bdst = g.bufs["c1"] if is_ctx else g.bufs["x1"]
                x_, bx = xt[bi], b_xt[bi]
                k.dma("sp", x_[:], xsrc, reads=[g.bufs["ctx" if is_ctx else "x"]], writes=[bx])
                k.op("pool", lambda e: e.tensor_tensor(out=yb[:], in0=yb[:], in1=yfl[:], op=ALU.add),
                     reads=[byb, b_yfl], writes=[byb])
                k.op("dve", lambda e: e.tensor_tensor(out=yfl[:].rearrange("p (h q) -> p h q", q=64), in0=X3,
                                                       in1=bc3(dsk[:], 32, 64), op=ALU.mult),
                     reads=[bX, b_nw, b_yfl], writes=[b_yfl])
                k.op("pool", lambda e: e.tensor_tensor(out=yb[:], in0=yb[:], in1=yfl[:], op=ALU.add),
                     reads=[byb, b_yfl], writes=[byb])
                k.op("dve", lambda e: e.tensor_tensor(out=yb[:], in0=yb[:], in1=ztl[:], op=ALU.mult),
                     reads=[byb, b_ztl], writes=[byb])
                rstd, b_st = rstd_of(g, ns, 0, yb[:], byb, dim=2048)
                k.op("dve", lambda e: e.tensor_tensor(out=ygn[:], in0=yb[:], in1=nwbc[:], op=ALU.mult),
                     reads=[byb, b_nw], writes=[b_ygn])
                for hh in range(2):
                    psv = g.ps[6 + hh].bitcast(BF16)
                    for c in range(8):
                        k.op("pe", lambda e, c=c: e.transpose(out=psv[:, c * 128:(c + 1) * 128],
                                                              in_=ygn[:, (hh * 8 + c) * 128:(hh * 8 + c + 1) * 128], identity=identb[:]),
                             reads=[b_ygn, b_id], writes=[tr_b[hh]], inc=(c == 7))
                    k.op("act", lambda e: e.copy(out=ygT[:, hh * 8:(hh + 1) * 8, :],
                                                 in_=psv[:, 0:1024].rearrange("p (c t) -> p c t", t=128)),
                         reads=[tr_b[hh]], writes=[b_ygT])
                gt = gbc[0] if is_ctx else gbc[1]
                for half in range(2):
                    hs = slice(half * 512, (half + 1) * 512)
                    po, b_po = g.ps[3 + half], g.psb[3 + half]
                    for c in range(16):
                        k.op("pe", lambda e, c=c: e.matmul(po[:, :], lhsT=ygT[:, c, :], rhs=wout[:, c, hs],
                                                           start=(c == 0), stop=(c == 15)),
                             reads=[b_ygT, b_wout], writes=[b_po], inc=(c == 15))
                    o_, bo = ot[cnt["o"] % 2], b_ot[cnt["o"] % 2]
                    cnt["o"] += 1
                    k.op("dve", lambda e: e.scalar_tensor_tensor(out=o_[:], in0=po[:, :], scalar=rstd, in1=gt[:, hs],
                                                                  op0=ALU.mult, op1=ALU.mult),
                         reads=[b_po, b_st, b_g], writes=[bo])
                    k.op("pool", lambda e: e.tensor_tensor(out=x_[:, hs], in0=x_[:, hs], in1=o_[:], op=ALU.add),
                         reads=[bo, bx], writes=[bx])
                k.dma("sp", xdst, x_[:], reads=[bx], writes=[bdst])


MAGIC = 12582912.0
TWO_PI_HI = 6.28125
TWO_PI_LO = 0.0019353071795864769


def s5_prep(g):
    k, nc = g.k, g.nc
    V = lambda n, sh=(128, 32): sbt(g, "p_" + n, list(sh), F32)
    ident = sbt(g, "p_identb", [128, 128], BF16)
    b_id = Buf()
    k.dma("pool", ident[:], g.gin("ident")[:, :], writes=[b_id])
    m8 = sbt(g, "p_m8", [128, 2, 128], F32)
    b_m8 = Buf()
    k.dma("sp", m8[:], g.gin("m8").rearrange("m p c -> p m c"), writes=[b_m8])
    bb = Buf()

    def dve(fn, eng="dve"):
        k.op(eng, fn, reads=[bb], writes=[bb])

    def tt(out, a, b_, op):
        dve(lambda e: e.tensor_tensor(out=out, in0=a, in1=b_, op=op))

    def ts(out, a, s1, op0, s2=None, op1=None):
        if op1 is None:
            dve(lambda e: e.tensor_scalar(out=out, in0=a, scalar1=s1, scalar2=None, op0=op0))
        else:
            dve(lambda e: e.tensor_scalar(out=out, in0=a, scalar1=s1, scalar2=s2, op0=op0, op1=op1))

    lre, lim, lst = V("lre"), V("lim"), V("lst")
    step, lr, th, kf, r_, t8, t2 = V("step"), V("lr"), V("th"), V("kf"), V("r"), V("t8"), V("t2")
    sn, cs_, ta, tb, rho1 = V("sn"), V("cs"), V("ta"), V("tb"), V("rho1")
    ar, ai, den, cr, ci = V("ar"), V("ai"), V("den"), V("cr"), V("ci")
    pm, pc, ps_ = V("pm", (128, 32, 9)), V("pc", (128, 32, 9)), V("ps", (128, 32, 9))
    apr, api = V("apr", (128, 32, 9)), V("api", (128, 32, 9))
    anr, ani, imv = V("anr", (128, 32, 9)), V("ani", (128, 32, 9)), V("imv", (128, 32, 9))
    Pc, Ps = V("Pc"), V("Ps")
    bre, bim = V("bre", (128, 32, 16)), V("bim", (128, 32, 16))
    bbr, bbi = V("bbr", (128, 32, 16)), V("bbi", (128, 32, 16))
    cre, cim = V("cre", (128, 32, 16)), V("cim", (128, 32, 16))
    w1, w2 = V("w1", (128, 32, 16)), V("w2", (128, 32, 16))
    WZr = sbt(g, "p_WZr", [128, 32, 8, 16], BF16)
    WZi = sbt(g, "p_WZi", [128, 32, 8, 16], BF16)
    Pr = sbt(g, "p_Pr", [128, 32, 8, 16], BF16)
    Pi = sbt(g, "p_Pi", [128, 32, 8, 16], BF16)
    CAr = sbt(g, "p_CAr", [128, 32, 9, 16], BF16)
    CAi = sbt(g, "p_CAi", [128, 32, 9, 16], BF16)
    cosT = sbt(g, "p_cosT", [128, 32, 288], F32)
    sinT = sbt(g, "p_sinT", [128, 32, 288], F32)
    e1 = sbt(g, "p_e1", [128, 32, 128], F32)
    e2 = sbt(g, "p_e2", [128, 32, 128], F32)
    wst = [sbt(g, "p_wst%d" % i, [128, 6, 128], BF16) for i in range(2)]
    b_wst = [Buf(), Buf()]

    def b16(t, n):
        return bc3(t[:], 32, n)

    def col(t3, j):
        return t3[:, :, j]

    for d in range(2):
        k.dma("sp", lre[:], g.gin("s5_lamT")[d, 0], reads=[bb], writes=[bb])
        k.dma("sp", lim[:], g.gin("s5_lamT")[d, 1], reads=[bb], writes=[bb])
        k.dma("sp", lst[:], g.gin("s5_lamT")[d, 2], reads=[bb], writes=[bb])
        k.dma("sp", bre[:], g.gin("s5_b")[d, 0].rearrange("(gp q) c -> q gp c", q=128), reads=[bb], writes=[bb])
        k.dma("sp", bim[:], g.gin("s5_b")[d, 1].rearrange("(gp q) c -> q gp c", q=128), reads=[bb], writes=[bb])
        k.dma("sp", cre[:], g.gin("s5_cT")[d, 0], reads=[bb], writes=[bb])
        k.dma("sp", cim[:], g.gin("s5_cT")[d, 1], reads=[bb], writes=[bb])
        dve(lambda e: e.activation(out=step[:], in_=lst[:], func=AF.Exp), "act")
        tt(lr[:], lre[:], step[:], ALU.mult)
        tt(th[:], lim[:], step[:], ALU.mult)
        ts(ta[:], lr[:], 1.0 / 5, ALU.mult, 1.0, ALU.add)
        for c_ in (1.0 / 4, 1.0 / 3, 1.0 / 2, 1.0):
            tt(ta[:], ta[:], lr[:], ALU.mult)
            ts(ta[:], ta[:], c_, ALU.mult, 1.0, ALU.add)
        dve(lambda e: e.tensor_copy(out=rho1[:], in_=ta[:]))
        ts(kf[:], th[:], 1.0 / (2 * np.pi), ALU.mult, MAGIC, ALU.add)
        ts(kf[:], kf[:], -MAGIC, ALU.add)
        dve(lambda e: e.scalar_tensor_tensor(out=r_[:], in0=kf[:], scalar=-TWO_PI_HI, in1=th[:], op0=ALU.mult, op1=ALU.add))
        dve(lambda e: e.scalar_tensor_tensor(out=r_[:], in0=kf[:], scalar=-TWO_PI_LO, in1=r_[:], op0=ALU.mult, op1=ALU.add))
        ts(t8[:], r_[:], 0.125, ALU.mult)
        tt(t2[:], t8[:], t8[:], ALU.mult)
        ts(ta[:], t2[:], -1.0 / 5040, ALU.mult, 1.0 / 120, ALU.add)
        tt(ta[:], ta[:], t2[:], ALU.mult)
        ts(ta[:], ta[:], -1.0 / 6, ALU.add)
        tt(ta[:], ta[:], t2[:], ALU.mult)
        ts(ta[:], ta[:], 1.0, ALU.add)
        tt(sn[:], ta[:], t8[:], ALU.mult)
        ts(ta[:], t2[:], 1.0 / 40320, ALU.mult, -1.0 / 720, ALU.add)
        tt(ta[:], ta[:], t2[:], ALU.mult)
        ts(ta[:], ta[:], 1.0 / 24, ALU.add)
        tt(ta[:], ta[:], t2[:], ALU.mult)
        ts(ta[:], ta[:], -0.5, ALU.add)
        tt(ta[:], ta[:], t2[:], ALU.mult)
        ts(cs_[:], ta[:], 1.0, ALU.add)
        for _ in range(3):
            tt(ta[:], cs_[:], cs_[:], ALU.mult)
            tt(tb[:], sn[:], sn[:], ALU.mult)
            tt(sn[:], sn[:], cs_[:], ALU.mult)
            ts(sn[:], sn[:], 2.0, ALU.mult)
            tt(cs_[:], ta[:], tb[:], ALU.subtract)
        tt(ar[:], rho1[:], cs_[:], ALU.mult)
        tt(ai[:], rho1[:], sn[:], ALU.mult)
        ts(ta[:], ar[:], -1.0, ALU.add)
        tt(den[:], lre[:], lre[:], ALU.mult)
        tt(tb[:], lim[:], lim[:], ALU.mult)
        tt(den[:], den[:], tb[:], ALU.add)
        dve(lambda e: e.reciprocal(out=den[:], in_=den[:]))
        tt(cr[:], ta[:], lre[:], ALU.mult)
        tt(tb[:], ai[:], lim[:], ALU.mult)
        tt(cr[:], cr[:], tb[:], ALU.add)
        tt(cr[:], cr[:], den[:], ALU.mult)
        tt(ci[:], ai[:], lre[:], ALU.mult)
        tt(tb[:], ta[:], lim[:], ALU.mult)
        tt(ci[:], ci[:], tb[:], ALU.subtract)
        tt(ci[:], ci[:], den[:], ALU.mult)
        tt(w1[:], bre[:], b16(cr, 16), ALU.mult)
        tt(w2[:], bim[:], b16(ci, 16), ALU.mult)
        tt(bbr[:], w1[:], w2[:], ALU.subtract)
        tt(w1[:], bim[:], b16(cr, 16), ALU.mult)
        tt(w2[:], bre[:], b16(ci, 16), ALU.mult)
        tt(bbi[:], w1[:], w2[:], ALU.add)
        dve(lambda e: e.memset(col(pm, 0), 1.0))
        dve(lambda e: e.memset(col(pc, 0), 1.0))
        dve(lambda e: e.memset(col(ps_, 0), 0.0))
        for j in range(1, 9):
            tt(col(pm, j), col(pm, j - 1), rho1[:], ALU.mult)
            tt(ta[:], col(pc, j - 1), cs_[:], ALU.mult)
            tt(tb[:], col(ps_, j - 1), sn[:], ALU.mult)
            tt(col(pc, j), ta[:], tb[:], ALU.subtract)
            tt(ta[:], col(ps_, j - 1), cs_[:], ALU.mult)
            tt(tb[:], col(pc, j - 1), sn[:], ALU.mult)
            tt(col(ps_, j), ta[:], tb[:], ALU.add)
        dve(lambda e: e.reciprocal(out=imv[:], in_=pm[:]))
        tt(apr[:], pm[:], pc[:], ALU.mult)
        tt(api[:], pm[:], ps_[:], ALU.mult)
        tt(anr[:], imv[:], pc[:], ALU.mult)
        tt(ani[:], imv[:], ps_[:], ALU.mult)
        ts(ani[:], ani[:], -1.0, ALU.mult)
        dve(lambda e: e.tensor_copy(out=ta[:], in_=col(pm, 8)))
        k.dma("sp", g.rho[d], ta[:], reads=[bb], writes=[g.bufs["rho"]])
        for s_ in range(8):
            so = s_ if d == 0 else 7 - s_
            for (dst, pr_, pi_) in ((WZr, col(apr, 7 - s_), col(api, 7 - s_)), (Pr, col(anr, s_), col(ani, s_))):
                dsti = WZi if dst is WZr else Pi
                tt(w1[:], bbr[:], bc3(pr_, 32, 16), ALU.mult)
                tt(w2[:], bbi[:], bc3(pi_, 32, 16), ALU.mult)
                tt(dst[:, :, so, :], w1[:], w2[:], ALU.subtract)
                tt(w1[:], bbi[:], bc3(pr_, 32, 16), ALU.mult)
                tt(w2[:], bbr[:], bc3(pi_, 32, 16), ALU.mult)
                tt(dsti[:, :, so, :], w1[:], w2[:], ALU.add)
        for j in range(9):
            jo = j if d == 0 else 8 - j
            tt(w1[:], cre[:], bc3(col(apr, j), 32, 16), ALU.mult)
            tt(w2[:], cim[:], bc3(col(api, j), 32, 16), ALU.mult)
            tt(CAr[:, :, jo, :], w1[:], w2[:], ALU.subtract)
            tt(w1[:], cim[:], bc3(col(apr, j), 32, 16), ALU.mult)
            tt(w2[:], cre[:], bc3(col(api, j), 32, 16), ALU.mult)
            tt(w1[:], w1[:], w2[:], ALU.add)
            ts(CAi[:, :, jo, :], w1[:], -1.0, ALU.mult)
        q0, y0 = (0, 1) if d == 0 else (1, 0)
        dve(lambda e: e.tensor_copy(out=Pc[:], in_=col(pc, 8)))
        dve(lambda e: e.tensor_copy(out=Ps[:], in_=col(ps_, 8)))
        dve(lambda e: e.memset(cosT[:, :, 0:1], 1.0))
        dve(lambda e: e.memset(sinT[:, :, 0:1], 0.0))
        wdt = 1
        while wdt < 288:
            n = min(wdt, 288 - wdt)
            tt(e1[:, :, 0:n], cosT[:, :, 0:n], b16(Pc, n), ALU.mult)
            tt(e2[:, :, 0:n], sinT[:, :, 0:n], b16(Ps, n), ALU.mult)
            tt(cosT[:, :, wdt:wdt + n], e1[:, :, 0:n], e2[:, :, 0:n], ALU.subtract)
            tt(e1[:, :, 0:n], sinT[:, :, 0:n], b16(Pc, n), ALU.mult)
            tt(e2[:, :, 0:n], cosT[:, :, 0:n], b16(Ps, n), ALU.mult)
            tt(sinT[:, :, wdt:wdt + n], e1[:, :, 0:n], e2[:, :, 0:n], ALU.add)
            tt(ta[:], Pc[:], Pc[:], ALU.mult)
            tt(tb[:], Ps[:], Ps[:], ALU.mult)
            tt(Ps[:], Ps[:], Pc[:], ALU.mult)
            ts(Ps[:], Ps[:], 2.0, ALU.mult)
            tt(Pc[:], ta[:], tb[:], ALU.subtract)
            wdt *= 2
        k.dma("sp", g.etab[d, 0], cosT[:], reads=[bb], writes=[g.bufs["etab"]])
        k.dma("sp", g.etab[d, 1], sinT[:], reads=[bb], writes=[g.bufs["etab"]])
        for gp in range(32):
            wt, bwt = wst[gp % 2], b_wst[gp % 2]
            for g2 in range(2):
                L = slice(g2 * 64, (g2 + 1) * 64)
                pM, bM = g.ps[g2], g.psb[g2]
                k.op("pe", lambda e: e.matmul(pM[:, 0:128], lhsT=Pr[L, gp, :, :], rhs=CAr[L, gp, q0:q0 + 8, :], start=True, stop=False),
                     reads=[bb], writes=[bM], inc=False)
                k.op("pe", lambda e: e.matmul(pM[:, 0:128], lhsT=Pi[L, gp, :, :], rhs=CAi[L, gp, q0:q0 + 8, :], start=False, stop=True),
                     reads=[bb], writes=[bM])
                k.op("dve", lambda e: e.tensor_tensor(out=wt[:, g2, :], in0=pM[:, 0:128], in1=m8[:, d, :], op=ALU.mult),
                     reads=[bM, b_m8], writes=[bwt])
            for ri, src_ in enumerate((WZr, WZi)):
                pT, bT = g.ps[2 + ri], g.psb[2 + ri]
                pTv = pT.bitcast(BF16)
                k.op("pe", lambda e: e.transpose(out=pTv[:, 0:128], in_=src_[:, gp, :, :], identity=ident[:]),
                     reads=[bb, b_id], writes=[bT])
                k.op("act", lambda e: e.copy(out=wt[:, 2 + ri, :], in_=pTv[:, 0:128]), reads=[bT], writes=[bwt])
            k.op("act", lambda e: e.copy(out=wt[:, 4, :], in_=CAr[:, gp, y0:y0 + 8, :]), reads=[bb], writes=[bwt])
            k.op("act", lambda e: e.copy(out=wt[:, 5, :], in_=CAi[:, gp, y0:y0 + 8, :]), reads=[bb], writes=[bwt])
            k.dma("sp", g.s5w[d, gp], wt[:], reads=[bwt], writes=[g.bufs["s5w"]])


def phase_s5(g):
    with ExitStack() as pes:
        g.pes = pes
        s5_prep(g)
        g.k.barrier()
    if g.kinds.get("_s5_prep_only"):
        return
    with ExitStack() as pes:
        g.pes = pes
        s5_main(g)
        g.k.barrier()


def bcf(col_ap, n):
    a = col_ap.ap
    return AP(col_ap.tensor, col_ap.offset, [list(a[0]), [0, n]])


def s5_main(g):
    k, nc = g.k, g.nc
    identb = sbt(g, "v_identb", [128, 128], BF16)
    b_id = Buf()
    k.dma("pool", identb[:], g.gin("ident")[:, :], writes=[b_id])
    bglu = sbt(g, "v_bglu", [128, 2 * D], F32)
    dsk = sbt(g, "v_dsk", [128, D], F32)
    rho = sbt(g, "v_rho", [128, 2, 32], F32)
    b_c = Buf()
    k.dma("sp", bglu[:], bcast_rows(g.gin("s5_b_glu")[0:1, :], 128), writes=[b_c])
    k.dma("sp", dsk[:], bcast_rows(g.gin("s5_d")[0:1, :], 128), writes=[b_c])
    k.dma("sp", rho[:], g.rho.rearrange("d q gp -> q d gp"), reads=[g.bufs["rho"]], writes=[b_c])
    mods = ModBC(g, "v", 1, (0, 1, 2))
    ns = NormSet(g, "v_")
    Ubig = sbt(g, "v_U", [128, 64 * 320], BF16)
    U = Ubig[:, :].rearrange("p (g c) -> p g c", c=320)
    wglu = Ubig[:, 0:8 * 2048].rearrange("p (k n) -> p k n", n=2048)
    b_U = Buf()
    hcx = sbt(g, "v_hcx", [128, 64, 8, 16], BF16)
    b_hcx = Buf()
    hxc = sbt(g, "v_hxc", [128, 2, 64, 8, 16], BF16)
    b_hxc = Buf()
    xt = [sbt(g, "v_xt%d" % i, [128, D], F32) for i in range(2)]
    b_xt = [Buf(), Buf()]
    tab = [sbt(g, "v_tab%d" % i, [128, 2, 288], F32) for i in range(2)]
    b_tab = [Buf(), Buf()]
    wk = [[sbt(g, "v_wk%d_%d" % (i, j), [128, 288], F32) for j in range(6)] for i in range(2)]
    b_wk = [[Buf() for _ in range(6)] for _ in range(2)]
    spv = [sbt(g, "v_spv%d" % i, [128, 2, 2, 256], BF16) for i in range(2)]
    b_spv = [Buf(), Buf()]
    wts = [sbt(g, "v_wts%d" % i, [128, 2, 6, 128], BF16) for i in range(2)]
    b_wts = [Buf(), Buf()]
    ysb = [sbt(g, "v_ysb%d" % i, [128, 256], BF16) for i in range(2)]
    b_ysb = [Buf(), Buf()]
    gt_ = [sbt(g, "v_gt%d" % i, [128, D], F32) for i in range(3)]
    b_gt = [Buf() for _ in range(3)]
    geb = sbt(g, "v_geb", [128, D], BF16)
    b_geb = Buf()
    geT = sbt(g, "v_geT", [128, 8, 128], BF16)
    b_geT = Buf()
    av = [sbt(g, "v_av%d" % i, [128, 512], F32) for i in range(2)]
    b_av = [Buf(), Buf()]
    gv = [sbt(g, "v_gv%d" % i, [128, 512], F32) for i in range(2)]
    b_gv = [Buf(), Buf()]
    cnt = dict(x=0, it=0, y=0, o=0)

    def load_x_tile(dst, bdst, src_b, tile, l):
        for r4 in range(4):
            t0 = (r4 * 8 + l) * 64 + tile * 32
            k.dma("sp", dst[r4 * 32:(r4 + 1) * 32, :], src_b[t0:t0 + 32, :], reads=[g.bufs["x2"]], writes=[bdst])

    for b in range(NB):
        (shbc, gbc, gatebc), b_mod = mods.get(2)
        cv = g.c2[b].rearrange("(c l) d -> l c d", l=8)
        for l in range(8):
            x_, bx = xt[cnt["x"] % 2], b_xt[cnt["x"] % 2]
            cnt["x"] += 1
            k.dma("sp", x_[0:32, :], cv[l], reads=[g.bufs["c2"]], writes=[bx])
            rstd, b_st = rstd_of(g, ns, 0, x_[0:32, :], bx)
            st = ns.st[0]
            k.op("dve", lambda e: e.scalar_tensor_tensor(out=ns.tmp[0][0:32, :], in0=x_[0:32, :], scalar=st[0:32, 3:4], in1=gbc[0:32, :],
                                                          op0=ALU.mult, op1=ALU.mult),
                 reads=[bx, b_st, b_mod], writes=[ns.b_tmp[0]])
            k.op("pool", lambda e: e.tensor_tensor(out=hcx[0:32, :, l, :], in0=ns.tmp[0][0:32, :].rearrange("p (g c) -> p g c", c=16),
                                                   in1=shbc[0:32, :].rearrange("p (g c) -> p g c", c=16), op=ALU.add),
                 reads=[ns.b_tmp[0], b_mod], writes=[b_hcx])
        (shbc, gbc, gatebc), b_mod = mods.get(b)
        for tile in range(2):
            for l in range(8):
                x_, bx = xt[cnt["x"] % 2], b_xt[cnt["x"] % 2]
                cnt["x"] += 1
                load_x_tile(x_, bx, g.x2[b], tile, l)
                rstd, b_st = rstd_of(g, ns, 0, x_[:], bx)
                k.op("dve", lambda e: e.scalar_tensor_tensor(out=ns.tmp[0][:], in0=x_[:], scalar=rstd, in1=gbc[:],
                                                              op0=ALU.mult, op1=ALU.mult),
                     reads=[bx, b_st, b_mod], writes=[ns.b_tmp[0]])
                k.op("pool", lambda e: e.tensor_tensor(out=hxc[:, tile, :, l, :], in0=ns.tmp[0][:].rearrange("p (g c) -> p g c", c=16),
                                                       in1=shbc[:].rearrange("p (g c) -> p g c", c=16), op=ALU.add),
                     reads=[ns.b_tmp[0], b_mod], writes=[b_hxc])
        for gg in range(64):
            ps, pb = g.ps[6 + gg % 2], g.psb[6 + gg % 2]
            psv = ps.bitcast(BF16)
            k.op("pe", lambda e: e.transpose(out=psv[:, 0:32], in_=hcx[0:32, gg, :, :], identity=identb[0:32, 0:32]),
                 reads=[b_hcx, b_id], writes=[pb], inc=False)
            for tile in range(2):
                k.op("pe", lambda e, tile=tile: e.transpose(out=psv[:, 32 + tile * 128:32 + (tile + 1) * 128],
                                                            in_=hxc[:, tile, gg, :, :], identity=identb[:]),
                     reads=[b_hxc, b_id], writes=[pb], inc=(tile == 1))
            k.op("act", lambda e: e.copy(out=U[:, gg, 0:32], in_=psv[:, 0:32]), reads=[pb], writes=[b_U])
            k.op("act", lambda e: e.copy(out=U[:, gg, 288:320], in_=psv[:, 0:32]), reads=[pb], writes=[b_U])
            k.op("act", lambda e: e.copy(out=U[:, gg, 32:288].rearrange("p (t c r) -> p t r c", t=2, c=32, r=4),
                                         in_=psv[:, 32:288].rearrange("p (t r c) -> p t r c", t=2, r=4, c=32)),
                 reads=[pb], writes=[b_U])
        for tile in range(2):
            for l in range(8):
                k.op("dve", lambda e, tile=tile, l=l: e.tensor_tensor(out=hxc[:, tile, :, l, :], in0=hxc[:, tile, :, l, :],
                                                                      in1=dsk[:].rearrange("p (g c) -> p g c", c=16), op=ALU.mult),
                     reads=[b_hxc, b_c], writes=[b_hxc])
        for gp in range(32):
            it = cnt["it"] % 2
            cnt["it"] += 1
            wt, bwt, sp_, bsp = wts[it], b_wts[it], spv[it], b_spv[it]
            for d in range(2):
                tb_, btb = tab[d], b_tab[d]
                W, bW = wk[d], b_wk[d]
                k.dma("sp", wt[:, d], g.s5w[d, gp], reads=[g.bufs["s5w"]], writes=[bwt])
                k.dma("sp", tb_[:, 0, :], g.etab[d, 0, :, gp, :], reads=[g.bufs["etab"]], writes=[btb])
                k.dma("sp", tb_[:, 1, :], g.etab[d, 1, :, gp, :], reads=[g.bufs["etab"]], writes=[btb])
                cols = slice(0, 288) if d == 0 else slice(32, 320)
                pz = [(g.ps[2 * d], g.psb[2 * d]), (g.ps[2 * d + 1], g.psb[2 * d + 1])]
                for ri in range(2):
                    pZ, bZ = pz[ri]
                    for g2 in range(2):
                        k.op("pe", lambda e, g2=g2: e.matmul(pZ[g2 * 64:(g2 + 1) * 64, 0:288], lhsT=wt[:, d, 2 + ri, g2 * 64:(g2 + 1) * 64],
                                                             rhs=U[:, 2 * gp + g2, cols], start=True, stop=True),
                             reads=[bwt, b_U], writes=[bZ], inc=(g2 == 1))
                cosv, sinv = tb_[:, 0, :], tb_[:, 1, :]
                if d == 1:
                    cosv, sinv = rev_ap(cosv, 288), rev_ap(sinv, 288)
                Zr, bZr = pz[0][0][:, 0:288], pz[0][1]
                Zi, bZi = pz[1][0][:, 0:288], pz[1][1]
                za, zb, ztr, zti, sr, si = [w_[:] for w_ in W]
                bza, bzb, bztr, bzti, bsr, bsi = bW
                TT = lambda o, a_, b_, op, rd, wr: k.op("dve", lambda e: e.tensor_tensor(out=o, in0=a_, in1=b_, op=op), reads=rd, writes=wr)
                TT(za, Zr, cosv, ALU.mult, [bZr, btb], [bza])
                TT(zb, Zi, sinv, ALU.mult, [bZi, btb], [bzb])
                TT(ztr, za, zb, ALU.add, [bza, bzb], [bztr])
                TT(za, Zi, cosv, ALU.mult, [bZi, btb], [bza])
                TT(zb, Zr, sinv, ALU.mult, [bZr, btb], [bzb])
                TT(zti, za, zb, ALU.subtract, [bza, bzb], [bzti])
                rbc = bcf(rho[:, d, gp:gp + 1], 288)
                for (o_, i_, bo_, bi_) in ((sr, ztr, bsr, bztr), (si, zti, bsi, bzti)):
                    oo, ii = (o_, i_) if d == 0 else (rev_ap(o_, 288), rev_ap(i_, 288))
                    k.op("dve", lambda e, oo=oo, ii=ii: e.tensor_tensor_scan(out=oo, data0=rbc, data1=ii, initial=0.0,
                                                                             op0=ALU.mult, op1=ALU.add),
                         reads=[bi_, b_c], writes=[bo_])
                sl = slice(31, 287) if d == 0 else slice(1, 257)
                TT(za[:, 0:256], sr[:, sl], cosv[:, sl], ALU.mult, [bsr, btb], [bza])
                TT(zb[:, 0:256], si[:, sl], sinv[:, sl], ALU.mult, [bsi, btb], [bzb])
                TT(sp_[:, d, 0, :], za[:, 0:256], zb[:, 0:256], ALU.subtract, [bza, bzb], [bsp])
                TT(za[:, 0:256], si[:, sl], cosv[:, sl], ALU.mult, [bsi, btb], [bza])
                TT(zb[:, 0:256], sr[:, sl], sinv[:, sl], ALU.mult, [bsr, btb], [bzb])
                TT(sp_[:, d, 1, :], za[:, 0:256], zb[:, 0:256], ALU.add, [bza, bzb], [bsp])
            for g2 in range(2):
                L = slice(g2 * 64, (g2 + 1) * 64)
                gg = 2 * gp + g2
                yi = cnt["y"] % 2
                cnt["y"] += 1
                pY, bY = g.ps[4 + yi], g.psb[4 + yi]
                ops = [(wt[:, 0, g2, :], U[:, gg, 32:288]), (wt[:, 1, g2, :], U[:, gg, 32:288])]
                for d in range(2):
                    ops.append((wt[L, d, 4, :], sp_[L, d, 0, :]))
                    ops.append((wt[L, d, 5, :], sp_[L, d, 1, :]))
                for oi, (lw, rh) in enumerate(ops):
                    k.op("pe", lambda e, lw=lw, rh=rh, oi=oi: e.matmul(pY[:, 0:256], lhsT=lw, rhs=rh, start=(oi == 0), stop=(oi == 5)),
                         reads=[bwt, b_U, bsp], writes=[bY], inc=(oi == 5))
                ys, bys = ysb[yi], b_ysb[yi]
                k.op("act", lambda e: e.copy(out=ys[:].rearrange("p (t r c) -> p t c r", t=2, r=4, c=32),
                                             in_=pY[:, 0:256].rearrange("p (t c r) -> p t c r", t=2, c=32, r=4)),
                     reads=[bY], writes=[bys])
                pT, bT = g.ps[6 + yi], g.psb[6 + yi]
                pTv = pT.bitcast(BF16)
                for tile in range(2):
                    k.op("pe", lambda e, tile=tile: e.transpose(out=pTv[:, tile * 128:(tile + 1) * 128],
                                                                in_=ys[:, tile * 128:(tile + 1) * 128], identity=identb[:]),
                         reads=[bys, b_id], writes=[bT], inc=(tile == 1))
                tyv = hxc[:, :, gg, :, :]
                k.op("dve", lambda e: e.tensor_tensor(out=tyv, in0=tyv, in1=pTv[:, 0:256].rearrange("p (t l c) -> p t l c", t=2, l=8, c=16),
                                                       op=ALU.add),
                     reads=[bT, b_hxc], writes=[b_hxc])
        for kk in range(8):
            k.dma("pool", wglu[:, kk, :], g.gin("s5_w_glu")[kk * 128:(kk + 1) * 128, :], writes=[b_U])
        for tile in range(2):
            for l in range(8):
                yv = hxc[:, tile, :, l, :]
                g0, g1, g2_ = [t_[:].rearrange("p (g c) -> p g c", c=16) for t_ in gt_]
                k.op("act", lambda e: e.activation(out=g0, in_=yv, func=AF.Square), reads=[b_hxc], writes=[b_gt[0]])
                k.op("dve", lambda e: e.tensor_scalar(out=g0, in0=g0, scalar1=0.044715, scalar2=1.0, op0=ALU.mult, op1=ALU.add),
                     reads=[b_gt[0]], writes=[b_gt[0]])
                k.op("dve", lambda e: e.tensor_tensor(out=g1, in0=g0, in1=yv, op=ALU.mult), reads=[b_gt[0], b_hxc], writes=[b_gt[1]])
                k.op("act", lambda e: e.activation(out=g2_, in_=g1, func=AF.Sigmoid, scale=1.5957691216057308),
                     reads=[b_gt[1]], writes=[b_gt[2]])
                k.op("dve", lambda e: e.tensor_tensor(out=geb[:].rearrange("p (g c) -> p g c", c=16), in0=g2_, in1=yv, op=ALU.mult), reads=[b_gt[2], b_hxc], writes=[b_geb])
                ps, pb = g.ps[6 + l % 2], g.psb[6 + l % 2]
                psv = ps.bitcast(BF16)
                for kk in range(8):
                    k.op("pe", lambda e, kk=kk: e.transpose(out=psv[:, kk * 128:(kk + 1) * 128], in_=geb[:, kk * 128:(kk + 1) * 128],
                                                            identity=identb[:]),
                         reads=[b_geb, b_id], writes=[pb], inc=(kk == 7))
                k.op("act", lambda e: e.copy(out=geT[:], in_=psv[:, 0:1024].rearrange("p (k t) -> p k t", t=128)),
                     reads=[pb], writes=[b_geT])
                x_, bx = xt[cnt["x"] % 2], b_xt[cnt["x"] % 2]
                cnt["x"] += 1
                load_x_tile(x_, bx, g.x2[b], tile, l)
                for half in range(2):
                    oi = cnt["o"] % 2
                    cnt["o"] += 1
                    pa, ba = g.ps[oi * 2], g.psb[oi * 2]
                    pg_, bg_ = g.ps[oi * 2 + 1], g.psb[oi * 2 + 1]
                    for (pp, bp, n0) in ((pa, ba, half * 512), (pg_, bg_, D + half * 512)):
                        for kk in range(8):
                            k.op("pe", lambda e, kk=kk, pp=pp, n0=n0: e.matmul(pp[:, :], lhsT=geT[:, kk, :], rhs=wglu[:, kk, n0:n0 + 512],
                                                                               start=(kk == 0), stop=(kk == 7)),
                                 reads=[b_geT, b_U], writes=[bp], inc=(kk == 7))
                    hs = slice(half * 512, (half + 1) * 512)
                    a_, ba_, g_, bg2 = av[oi], b_av[oi], gv[oi], b_gv[oi]
                    k.op("dve", lambda e: e.tensor_tensor(out=a_[:], in0=pa[:, :], in1=bglu[:, hs], op=ALU.add),
                         reads=[ba, b_c], writes=[ba_])
                    k.op("dve", lambda e: e.tensor_tensor(out=g_[:], in0=pg_[:, :], in1=bglu[:, D + half * 512:D + (half + 1) * 512], op=ALU.add),
                         reads=[bg_, b_c], writes=[bg2])
                    k.op("act", lambda e: e.activation(out=g_[:], in_=g_[:], func=AF.Sigmoid), reads=[bg2], writes=[bg2])
                    k.op("dve", lambda e: e.tensor_tensor(out=a_[:], in0=a_[:], in1=g_[:], op=ALU.mult), reads=[ba_, bg2], writes=[ba_])
                    k.op("pool", lambda e: e.tensor_tensor(out=a_[:], in0=a_[:], in1=gatebc[:, hs], op=ALU.mult),
                         reads=[ba_, b_mod], writes=[ba_])
                    k.op("pool", lambda e: e.tensor_tensor(out=x_[:, hs], in0=x_[:, hs], in1=a_[:], op=ALU.add),
                         reads=[ba_, bx], writes=[bx])
                for r4 in range(4):
                    t0 = (r4 * 8 + l) * 64 + tile * 32
                    k.dma("sp", g.x3[b, t0:t0 + 32, :], x_[r4 * 32:(r4 + 1) * 32, :], reads=[bx], writes=[g.bufs["x3"]])


def phase_moe(g):
    k, nc = g.k, g.nc
    src = g.x3 if "s5" in g.phases or g.kinds.get("x3") == "in" else g.x2
    b_src = g.bufs["x3"] if "s5" in g.phases or g.kinds.get("x3") == "in" else g.bufs["x2"]
    NTL = T // 128
    NFG = EDIM // 512
    ns = NormSet(g, "m_")
    identf = sbt(g, "m_identf", [128, 128], F32)
    b_identf = Buf()
    k.dma("sp", identf[:], g.gin("ident")[:, :], writes=[b_identf])
    hT = sbt(g, "m_hT", [128, 8, T], BF16)
    b_hT = Buf()
    acc = sbt(g, "m_acc", [128, NTL, D], F32)
    b_acc = [Buf() for _ in range(NTL)]
    aT = sbt(g, "m_aT", [128, 4, T], BF16)
    b_aT = [Buf() for _ in range(4)]
    wg = [sbt(g, "m_wg%d" % i, [128, 8, 512], BF16) for i in range(2)]
    wu = [sbt(g, "m_wu%d" % i, [128, 8, 512], BF16) for i in range(2)]
    wo = [sbt(g, "m_wo%d" % i, [128, 4, D], BF16) for i in range(2)]
    b_w = [Buf(), Buf()]
    comb = sbt(g, "m_comb", [128, NTL, NEXP], F32)
    b_comb = Buf()
    rw = sbt(g, "m_rw", [128, 8, NEXP], F32)
    rb = sbt(g, "m_rb", [128, NEXP], F32)
    b_rw = Buf()
    k.dma("sp", rw[:], g.gin("moe_router_w").rearrange("(k p) e -> p k e", p=128), writes=[b_rw])
    k.dma("sp", rb[:], bcast_rows(g.gin("moe_router_b")[0:1, :], 128), writes=[b_rw])
    nfin = sbt(g, "m_nfin", [128, D], F32)
    b_nfin = Buf()
    k.dma("sp", nfin[:], bcast_rows(g.gin("norm_final")[0:1, :], 128), writes=[b_nfin])
    hTf = sbt(g, "m_hTf", [128, 8, 128], F32)
    b_hTf = Buf()
    lg = [sbt(g, "m_lg%d" % i, [128, 40], F32) for i in range(2)]
    b_lg = [Buf(), Buf()]
    sg = [sbt(g, "m_sg%d" % i, [128, 512], F32) for i in range(2)]
    b_sg = [Buf(), Buf()]
    tmpo = [sbt(g, "m_to%d" % i, [128, 512], F32) for i in range(2)]
    b_to = [Buf(), Buf()]
    mods = ModBC(g, "m", 1, (3, 4, 5))
    wi = 0
    si = 0
    oi = 0
    pi = 0
    for b in range(NB):
        (shbc, gbc, gatebc), b_mod = mods.get(b)
        for j in range(NTL):
            xt = acc[:, j, :]
            k.dma("sp", xt, src[b, j * 128:(j + 1) * 128, :], reads=[b_src], writes=[b_acc[j]])
            i = ns.i % ns.nbuf
            ns.i += 1
            rstd, b_st = rstd_of(g, ns, i, xt, b_acc[j])
            tmp, b_tmp, hb, b_hb = ns.tmp[i], ns.b_tmp[i], ns.hb[i], ns.b_hb[i]
            k.op("dve", lambda e: e.scalar_tensor_tensor(out=tmp[:], in0=xt, scalar=rstd, in1=gbc[:],
                                                          op0=ALU.mult, op1=ALU.mult),
                 reads=[b_acc[j], b_st, b_mod], writes=[b_tmp])
            k.op("pool", lambda e: e.tensor_tensor(out=tmp[:], in0=tmp[:], in1=shbc[:], op=ALU.add),
                 reads=[b_tmp, b_mod], writes=[b_tmp])
            k.op("act", lambda e: e.copy(out=hb[:], in_=tmp[:]), reads=[b_tmp], writes=[b_hb])
            ps, pb = g.ps[4 + (j % 2)], g.psb[4 + (j % 2)]
            psv = ps.bitcast(BF16)
            for kk in range(8):
                k.op("pe", lambda e, kk=kk: e.transpose(out=psv[:, kk * 128:(kk + 1) * 128],
                                                        in_=hb[:, kk * 128:(kk + 1) * 128], identity=ns.ident[:]),
                     reads=[b_hb, ns.b_ident], writes=[pb], inc=(kk == 7))
            k.op("act", lambda e: e.copy(out=hT[:, :, j * 128:(j + 1) * 128],
                                         in_=psv[:, 0:1024].rearrange("p (k t) -> p k t", t=128)),
                 reads=[pb], writes=[b_hT])
            for hh in range(2):
                psf, pbf = g.ps[6 + hh], g.psb[6 + hh]
                for kk in range(4):
                    kf = hh * 4 + kk
                    k.op("pe", lambda e, kk=kk, kf=kf: e.transpose(out=psf[:, kk * 128:(kk + 1) * 128],
                                                                   in_=tmp[:, kf * 128:(kf + 1) * 128], identity=identf[:]),
                         reads=[b_tmp, b_identf], writes=[pbf], inc=(kk == 3))
                k.op("act", lambda e, hh=hh: e.copy(out=hTf[:, hh * 4:(hh + 1) * 4, :],
                                                    in_=psf[:, :].rearrange("p (k t) -> p k t", t=128)),
                     reads=[pbf], writes=[b_hTf])
            pl, pbl = g.ps[4 + (j % 2)], g.psb[4 + (j % 2)]
            for kk in range(8):
                k.op("pe", lambda e, kk=kk: e.matmul(pl[:, 0:NEXP], lhsT=hTf[:, kk, :], rhs=rw[:, kk, :],
                                                     start=(kk == 0), stop=(kk == 7)),
                     reads=[b_hTf, b_rw], writes=[pbl], inc=(kk == 7))
            L = lg[j % 2]
            bL = b_lg[j % 2]
            k.op("dve", lambda e: e.tensor_tensor(out=L[:, 0:8], in0=pl[:, 0:NEXP], in1=rb[:], op=ALU.add),
                 reads=[pbl, b_rw], writes=[bL])
            k.op("dve", lambda e: e.max(out=L[:, 8:16], in_=L[:, 0:8]), reads=[bL], writes=[bL])
            k.op("dve", lambda e: e.tensor_scalar(out=L[:, 16:17], in0=L[:, 8:9], scalar1=-1.0, scalar2=None,
                                                   op0=ALU.mult), reads=[bL], writes=[bL])
            k.op("act", lambda e: e.activation(out=L[:, 17:18], in_=L[:, 9:10], func=AF.Exp, bias=L[:, 16:17]),
                 reads=[bL], writes=[bL])
            k.op("dve", lambda e: e.tensor_scalar(out=L[:, 20:21], in0=L[:, 17:18], scalar1=1.0, scalar2=None,
                                                   op0=ALU.add), reads=[bL], writes=[bL])
            k.op("dve", lambda e: e.reciprocal(out=L[:, 18:19], in_=L[:, 20:21]), reads=[bL], writes=[bL])
            k.op("dve", lambda e: e.tensor_tensor(out=L[:, 19:20], in0=L[:, 17:18], in1=L[:, 18:19], op=ALU.mult),
                 reads=[bL], writes=[bL])
            k.op("dve", lambda e: e.tensor_scalar(out=L[:, 24:32], in0=L[:, 0:8], scalar1=L[:, 8:9], scalar2=L[:, 18:19],
                                                   op0=ALU.is_equal, op1=ALU.mult), reads=[bL], writes=[bL])
            k.op("dve", lambda e: e.tensor_scalar(out=L[:, 32:40], in0=L[:, 0:8], scalar1=L[:, 9:10], scalar2=L[:, 19:20],
                                                   op0=ALU.is_equal, op1=ALU.mult), reads=[bL], writes=[bL])
            k.op("dve", lambda e: e.tensor_tensor(out=comb[:, j, :], in0=L[:, 24:32], in1=L[:, 32:40], op=ALU.add),
                 reads=[bL], writes=[b_comb])
        for ex in range(NEXP):
            for fg in range(NFG):
                w_i = wi % 2
                wi += 1
                bw = b_w[w_i]
                fs = slice(fg * 512, (fg + 1) * 512)
                k.dma("pool", wg[w_i][:], g.gin("moe_w_in")[ex, :, fg * 512:(fg + 1) * 512].rearrange("(k p) n -> p k n", p=128),
                      writes=[bw])
                k.dma("pool", wu[w_i][:], g.gin("moe_w_in")[ex, :, EDIM + fg * 512:EDIM + (fg + 1) * 512].rearrange("(k p) n -> p k n", p=128),
                      writes=[bw])
                k.dma("pool", wo[w_i][:], g.gin("moe_w_out")[ex, fg * 512:(fg + 1) * 512, :].rearrange("(c p) d -> p c d", p=128),
                      writes=[bw])
                for c4 in range(4):
                    k.op("pool", lambda e, c4=c4: e.tensor_tensor(out=wo[w_i][:, c4, :], in0=wo[w_i][:, c4, :], in1=gatebc[:], op=ALU.mult),
                         reads=[bw, b_mod], writes=[bw])
                for tg in range(4):
                    ts_ = slice(tg * 512, (tg + 1) * 512)
                    for fc in range(4):
                        pg, pu = g.ps[(pi % 2) * 2], g.ps[(pi % 2) * 2 + 1]
                        bg, bu = g.psb[(pi % 2) * 2], g.psb[(pi % 2) * 2 + 1]
                        pi += 1
                        for kk in range(8):
                            k.op("pe", lambda e, kk=kk: e.matmul(pg[:, :], lhsT=wg[w_i][:, kk, fc * 128:(fc + 1) * 128],
                                                                 rhs=hT[:, kk, ts_], start=(kk == 0), stop=(kk == 7)),
                                 reads=[bw, b_hT], writes=[bg], inc=(kk == 7))
                        for kk in range(8):
                            k.op("pe", lambda e, kk=kk: e.matmul(pu[:, :], lhsT=wu[w_i][:, kk, fc * 128:(fc + 1) * 128],
                                                                 rhs=hT[:, kk, ts_], start=(kk == 0), stop=(kk == 7)),
                                 reads=[bw, b_hT], writes=[bu], inc=(kk == 7))
                        s_i = si % 2
                        si += 1
                        k.op("act", lambda e: e.activation(out=sg[s_i][:], in_=pg[:, :], func=AF.Silu),
                             reads=[bg], writes=[b_sg[s_i]])
                        k.op("dve", lambda e: e.tensor_tensor(out=aT[:, fc, ts_], in0=sg[s_i][:], in1=pu[:, :], op=ALU.mult),
                             reads=[b_sg[s_i], bu], writes=[b_aT[tg]])
                for j in range(NTL):
                    for half in range(2):
                        hs = slice(half * 512, (half + 1) * 512)
                        po, pbo = g.ps[4 + (oi % 4)], g.psb[4 + (oi % 4)]
                        to, bto = tmpo[oi % 2], b_to[oi % 2]
                        oi += 1
                        for fc in range(4):
                            k.op("pe", lambda e, fc=fc: e.matmul(po[:, :], lhsT=aT[:, fc, j * 128:(j + 1) * 128],
                                                                 rhs=wo[w_i][:, fc, hs], start=(fc == 0), stop=(fc == 3)),
                                 reads=[b_aT[j // 4], bw], writes=[pbo], inc=(fc == 3))
                        k.op("dve", lambda e: e.scalar_tensor_tensor(out=acc[:, j, hs], in0=po[:, :], scalar=comb[:, j, ex:ex + 1],
                                                                      in1=acc[:, j, hs], op0=ALU.mult, op1=ALU.add),
                             reads=[pbo, b_comb, b_acc[j]], writes=[b_acc[j]])
        for j in range(NTL):
            xt = acc[:, j, :]
            i = ns.i % ns.nbuf
            ns.i += 1
            rstd, b_st = rstd_of(g, ns, i, xt, b_acc[j])
            k.op("dve", lambda e: e.scalar_tensor_tensor(out=xt, in0=xt, scalar=rstd, in1=nfin[:],
                                                          op0=ALU.mult, op1=ALU.mult),
                 reads=[b_acc[j], b_st, b_nfin], writes=[b_acc[j]])
            k.dma("sp", g.out[b, j * 128:(j + 1) * 128, :], xt, reads=[b_acc[j]], writes=[g.bufs["out"]])


_CACHE = {}


def host_consts():
    r = np.arange(128)[:, None]
    c = np.arange(128)[None, :]
    cmat = np.stack([(r <= c), (r > c), (r < c), (r >= c), np.ones((128, 128), bool)]).astype(np.float32)
    rr = np.arange(128)[:, None] // 16
    cc = np.arange(128)[None, :] // 16
    m8 = np.stack([(rr <= cc), (rr >= cc)]).astype(np.float32)
    return {"ident": np.eye(128, dtype=np.float32), "cmat": cmat, "m8": m8}


def s5_lane_tables(inp):
    out = np.empty((2, 3, 128, 32), np.float32)
    for d in range(2):
        for i, a in enumerate((inp["s5_lam_re"][0, d], inp["s5_lam_im"][0, d])):
            out[d, i] = a.reshape(32, 2, 64).transpose(1, 2, 0).reshape(128, 32)
        ls = np.repeat(inp["s5_log_step"][0, d][:, None], 64, axis=1)
        out[d, 2] = ls.reshape(32, 2, 64).transpose(1, 2, 0).reshape(128, 32)
    return out


def s5_c_lanes(inp):
    out = np.empty((2, 2, 128, 32, 16), np.float32)
    for d in range(2):
        for i, a in enumerate((inp["s5_c_re"][0, d], inp["s5_c_im"][0, d])):
            out[d, i] = a.reshape(32, 2, 16, 64).transpose(1, 3, 0, 2).reshape(128, 32, 16)
    return out


def core_inputs(inp, core):
    b0 = core * NB
    cT = np.ascontiguousarray(np.stack([inp["c"][b0], inp["c"][b0 + 1], inp["c_ctx"]], axis=1))
    m = {
        "x": np.ascontiguousarray(inp["x"][b0:b0 + NB]),
        "ctx": np.ascontiguousarray(inp["ctx"][b0:b0 + NB]),
        "cT": cT,
        "ada_w": inp["ada_w"], "ada_b": inp["ada_b"],
        "norm_mix": inp["norm_mix"], "norm_ffn": inp["norm_ffn"],
        "ffn_w_in": inp["ffn_w_in"][0], "ffn_w_out": inp["ffn_w_out"][0],
        "moe_router_w": inp["moe_router_w"][0], "moe_router_b": inp["moe_router_b"].reshape(1, NEXP),
        "moe_w_in": inp["moe_w_in"][0], "moe_w_out": inp["moe_w_out"][0],
        "norm_final": inp["norm_final"].reshape(1, D),
        "ssd_w_in": inp["ssd_w_in"][0],
        "convT": np.ascontiguousarray(np.concatenate([inp["ssd_conv_w"][0], inp["ssd_conv_b"]], axis=0).T),
        "ssd_dt_bias": inp["ssd_dt_bias"].reshape(1, 64), "ssd_a_log": inp["ssd_a_log"].reshape(1, 64),
        "ssd_d": inp["ssd_d"].reshape(1, 32), "ssd_norm": inp["ssd_norm"].reshape(1, 2048),
        "ssd_w_out": inp["ssd_w_out"][0],
        "s5_lamT": s5_lane_tables(inp),
        "s5_b": np.ascontiguousarray(np.stack([inp["s5_b_re"][0], inp["s5_b_im"][0]], axis=1).reshape(2, 2, 4096, 16)),
        "s5_cT": s5_c_lanes(inp),
        "s5_d": inp["s5_d"].reshape(1, D), "s5_w_glu": inp["s5_w_glu"][0], "s5_b_glu": inp["s5_b_glu"].reshape(1, 2 * D),
    }
    m.update(host_consts())
    return m


def kernel(**inp):
    inp = {kk: np.asarray(v) for kk, v in inp.items()}
    if "prog" not in _CACHE:
        _CACHE["prog"] = build_program(("ada", "ssd", "ffn", "s5", "moe"), {})
    nc, g = _CACHE["prog"]
    in_maps = []
    for core in range(8):
        m = core_inputs(inp, core)
        in_maps.append({kk: np.ascontiguousarray(v) for kk, v in m.items() if kk in g.inputs})
    res = run_bass_kernel_spmd(nc, in_maps, core_ids=list(range(8)))
    return np.concatenate([r["out"] for r in res.results], axis=0).astype(np.float32)
```

```python
import numpy as np
from contextlib import ExitStack
import concourse.bass as bass
import concourse.mybir as mybir
from concourse.bass_utils import run_bass_kernel_spmd

F32 = mybir.dt.float32
BF16 = mybir.dt.bfloat16
I32 = mybir.dt.int32
AF = mybir.ActivationFunctionType
ALU = mybir.AluOpType
AX = mybir.AxisListType
AP = bass.AP

D = 1024
NB = 2
T = 2048
TC = 256
EPS = 1e-6
FFN = 2816
NEXP = 8
EDIM = 3584


class Buf:
    __slots__ = ("name", "w", "r", "ps")

    def __init__(self, name="", ps=False):
        self.name = name
        self.w = {}
        self.r = {}
        self.ps = ps


class KB:
    RING = 8

    def __init__(self, nc, es):
        self.nc = nc
        self.es = es
        self.E = dict(pe=nc.tensor, act=nc.scalar, dve=nc.vector, pool=nc.gpsimd, sp=nc.sync)
        self.sem = {}
        self.cnt = {}
        for e in self.E:
            self.sem[e] = es.enter_context(nc.semaphore("s_" + e))
            self.cnt[e] = 0
        self.ring = {}
        self.ring_i = {}
        for q in ("sp", "pool", "act"):
            self.ring[q] = []
            for i in range(self.RING):
                key = ("d", q, i)
                self.sem[key] = es.enter_context(nc.semaphore("d_%s%d" % (q, i)))
                self.cnt[key] = 0
                self.ring[q].append(key)
            self.ring_i[q] = 0
        self.known = {e: {} for e in self.E}
        self.n_wait = 0

    def _deps(self, eng, reads, writes):
        need = {}

        def add(sk, v):
            if v > need.get(sk, 0):
                need[sk] = v

        for b in reads:
            for sk, v in b.w.items():
                if sk == eng and eng == "pe":
                    continue
                add(sk, v)
        for b in writes:
            for sk, v in b.r.items():
                if sk == eng and eng == "pe":
                    continue
                add(sk, v)
            for sk, v in b.w.items():
                if sk == eng and eng == "pe":
                    continue
                add(sk, v)
        kn = self.known[eng]
        out = []
        for sk, v in need.items():
            if kn.get(sk, 0) >= v:
                continue
            kn[sk] = v
            out.append((sk, v))
        return out

    def _emit_waits(self, eng, waits):
        E = self.E[eng]
        for sk, v in waits:
            E.wait_ge(self.sem[sk], v)
            self.n_wait += 1

    def _record(self, ev, reads, writes):
        sk, v = ev
        for b in reads:
            if v > b.r.get(sk, 0):
                b.r[sk] = v
        for b in writes:
            if b.r:
                b.w = {sk: v}
                b.r = {}
            else:
                if v > b.w.get(sk, 0):
                    b.w[sk] = v

    def op(self, eng, fn, reads=(), writes=(), inc=True):
        if any(b.ps for b in reads):
            writes = list(writes) + [b for b in reads if b.ps]
            reads = [b for b in reads if not b.ps]
        self._emit_waits(eng, self._deps(eng, reads, writes))
        ins = fn(self.E[eng])
        if inc:
            self.cnt[eng] += 1
            ins.then_inc(self.sem[eng], 1)
            ev = (eng, self.cnt[eng])
        else:
            ev = (eng, self.cnt[eng] + 1)
        self._record(ev, reads, writes)
        return ins

    def dma(self, q, out, in_, reads=(), writes=()):
        key = self.ring[q][self.ring_i[q] % self.RING]
        self.ring_i[q] += 1
        waits = self._deps(q, reads, writes)
        prev = self.cnt[key]
        if prev > 0 and self.known[q].get(key, 0) < prev:
            self.known[q][key] = prev
            waits.append((key, prev))
        self._emit_waits(q, waits)
        ins = self.E[q].dma_start(out=out, in_=in_)
        self.cnt[key] = prev + 16
        ins.then_inc(self.sem[key], 16)
        self._record((key, prev + 16), reads, writes)
        return ins

    def barrier(self):
        for e in self.E:
            waits = []
            for sk, v in self.cnt.items():
                if v == 0 or sk == e and e == "pe":
                    continue
                if self.known[e].get(sk, 0) >= v:
                    continue
                self.known[e][sk] = v
                waits.append((sk, v))
            self._emit_waits(e, waits)

    def final_wait(self):
        waits = []
        for sk, v in self.cnt.items():
            if v and self.known["sp"].get(sk, 0) < v:
                self.known["sp"][sk] = v
                waits.append((sk, v))
        self._emit_waits("sp", waits)


def rev_ap(ap, n):
    a = ap.ap
    assert len(a) == 2 and a[1][1] == n
    return AP(ap.tensor, ap.offset + (n - 1) * a[1][0], [list(a[0]), [-a[1][0], n]])


class Ctx:
    pass


def build_program(phases, kinds):
    nc = bass.Bass("TRN2", target_bir_lowering=False)
    g = Ctx()
    g.nc = nc
    g.phases = phases
    g.kinds = kinds

    def din(name, shape, dt=F32):
        return nc.dram_tensor(name, list(shape), dt, kind="ExternalInput").ap()

    def dmid(name, shape, dt=F32):
        kind = {"in": "ExternalInput", "out": "ExternalOutput", "int": "Internal"}[kinds.get(name, "int")]
        return nc.dram_tensor(name, list(shape), dt, kind=kind).ap()

    g.inputs = {}
    SH = dict(x=[NB, T, D], ctx=[NB, TC, D], cT=[D, 3], ada_w=[2, D, 6 * D], ada_b=[2, 6 * D],
              norm_mix=[2, D], norm_ffn=[2, D], ffn_w_in=[D, 2 * FFN], ffn_w_out=[FFN, D],
              moe_router_w=[D, NEXP], moe_router_b=[1, NEXP], moe_w_in=[NEXP, D, 2 * EDIM],
              moe_w_out=[NEXP, EDIM, D], norm_final=[1, D], ident=[128, 128],
              ssd_w_in=[D, 5184], convT=[3072, 6], ssd_dt_bias=[1, 64], ssd_a_log=[1, 64], ssd_d=[1, 32],
              ssd_norm=[1, 2048], ssd_w_out=[2048, D], cmat=[5, 128, 128],
              s5_lamT=[2, 3, 128, 32], s5_b=[2, 2, 4096, 16], s5_cT=[2, 2, 128, 32, 16], m8=[2, 128, 128],
              s5_d=[1, D], s5_w_glu=[D, 2 * D], s5_b_glu=[1, 2 * D])
    g.SH = SH

    def gin(name):
        if name not in g.inputs:
            g.inputs[name] = din(name, SH[name])
        return g.inputs[name]
    g.gin = gin
    g.x = gin("x")
    g.ctx = gin("ctx")
    g.mod = dmid("mod", [2, 3, 6 * D])
    g.x1 = dmid("x1", [NB, T, D])
    g.c1 = dmid("c1", [NB, TC, D])
    g.x2 = dmid("x2", [NB, T, D])
    g.c2 = dmid("c2", [NB, TC, D])
    g.x3 = dmid("x3", [NB, T, D])
    NTK = T + TC
    g.xbcT = dmid("xbcT", [NB, 3072, NTK], BF16)
    g.zs = dmid("zs", [NB, NTK, 2048], BF16)
    g.dd = dmid("dd", [NB, NTK, 128])
    g.yf = dmid("yf", [NB, NTK, 2048])
    g.s5w = dmid("s5w", [2, 32, 128, 6, 128], BF16)
    g.etab = dmid("etab", [2, 2, 128, 32, 288])
    g.rho = dmid("rho", [2, 128, 32])
    g.out = nc.dram_tensor("out", [NB, T, D], F32, kind="ExternalOutput").ap()
    g.bufs = {n: Buf(n) for n in ("x", "ctx", "mod", "x1", "c1", "x2", "c2", "x3", "out", "xbcT", "zs", "dd", "yf", "s5w", "etab", "rho")}

    with ExitStack() as es:
        k = KB(nc, es)
        g.k = k
        g.ps = [es.enter_context(nc.psum_tensor("ps%d" % i, [128, 512], F32)) for i in range(8)]
        g.psb = [Buf("ps%d" % i, ps=True) for i in range(8)]
        for ph in phases:
            with ExitStack() as pes:
                g.pes = pes
                {"ada": phase_ada, "ffn": phase_ffn, "moe": phase_moe, "ssd": phase_ssd, "s5": phase_s5}[ph](g)
                k.barrier()
        k.final_wait()
    g.kinds = kinds
    return nc, g


def sbt(g, name, shape, dt):
    return g.pes.enter_context(g.nc.sbuf_tensor(name, list(shape), dt))


def bcast_rows(ap_row, nparts):
    a = ap_row.ap
    return AP(ap_row.tensor, ap_row.offset, [[0, nparts]] + [list(x) for x in a[1:]])


def phase_ada(g):
    k, nc = g.k, g.nc
    cT = sbt(g, "a_cT", [128, 8, 3], F32)
    cs = sbt(g, "a_cs", [128, 8, 3], BF16)
    row = sbt(g, "a_row", [3, 6 * D], F32)
    bias = sbt(g, "a_bias", [3, 6 * D], F32)
    nrm = sbt(g, "a_nrm", [3, D], F32)
    wts = [sbt(g, "a_w%d" % i, [128, 8, 512], BF16) for i in range(2)]
    b_cT, b_cs, b_row, b_bias, b_nrm = Buf(), Buf(), Buf(), Buf(), Buf()
    b_w = [Buf(), Buf()]
    k.dma("sp", cT[:], g.gin("cT").rearrange("(k p) m -> p k m", p=128), writes=[b_cT])
    k.op("act", lambda e: e.activation(out=cs[:], in_=cT[:], func=AF.Silu), reads=[b_cT], writes=[b_cs])
    it = 0
    for layer in range(2):
        k.dma("sp", bias[:], bcast_rows(g.gin("ada_b")[layer:layer + 1, :], 3), writes=[b_bias])
        for j in range(12):
            w = wts[it % 2]
            bw = b_w[it % 2]
            it += 1
            k.dma("pool", w[:], g.gin("ada_w")[layer, :, j * 512:(j + 1) * 512].rearrange("(k p) n -> p k n", p=128), writes=[bw])
            ps = g.ps[it % 2]
            pb = g.psb[it % 2]
            for kk in range(8):
                k.op("pe", lambda e, kk=kk: e.matmul(ps[0:3, :], lhsT=cs[:, kk, :], rhs=w[:, kk, :],
                                                     start=(kk == 0), stop=(kk == 7)),
                     reads=[b_cs, bw], writes=[pb], inc=(kk == 7))
            k.op("dve", lambda e: e.tensor_tensor(out=row[:, j * 512:(j + 1) * 512], in0=ps[0:3, :],
                                                   in1=bias[:, j * 512:(j + 1) * 512], op=ALU.add),
                 reads=[pb, b_bias], writes=[b_row])
        for slot, nw in ((1, g.gin("norm_mix")), (4, g.gin("norm_ffn"))):
            k.dma("sp", nrm[:], bcast_rows(nw[layer:layer + 1, :], 3), writes=[b_nrm])
            k.op("dve", lambda e, slot=slot: e.scalar_tensor_tensor(
                out=row[:, slot * D:(slot + 1) * D], in0=row[:, slot * D:(slot + 1) * D], scalar=1.0,
                in1=nrm[:], op0=ALU.add, op1=ALU.mult), reads=[b_row, b_nrm], writes=[b_row])
        k.dma("sp", g.mod[layer], row[:], reads=[b_row], writes=[g.bufs["mod"]])


class NormSet:
    def __init__(self, g, pfx, nbuf=2):
        self.g = g
        self.junk = sbt(g, pfx + "junk", [128, 2 * D], BF16)
        self.b_junk = Buf()
        self.st = [sbt(g, pfx + "st%d" % i, [128, 4], F32) for i in range(nbuf)]
        self.b_st = [Buf() for _ in range(nbuf)]
        self.tmp = [sbt(g, pfx + "tmp0", [128, D], F32)] * nbuf
        self.b_tmp = [Buf()] * nbuf
        self.hb = [sbt(g, pfx + "hb%d" % i, [128, D], BF16) for i in range(nbuf)]
        self.b_hb = [Buf() for _ in range(nbuf)]
        self.ident = sbt(g, pfx + "ident", [128, 128], BF16)
        self.b_ident = Buf()
        g.k.dma("pool", self.ident[:], g.gin("ident")[:, :], writes=[self.b_ident])
        self.i = 0
        self.nbuf = nbuf


def rstd_of(g, ns, i, xt, b_xt, dim=D):
    k = g.k
    P = xt.ap[0][1]
    st, b_st = ns.st[i], ns.b_st[i]
    k.op("act", lambda e: e.activation(out=ns.junk[0:P, 0:dim], in_=xt, func=AF.Square, accum_out=st[0:P, 0:1]),
         reads=[b_xt], writes=[ns.b_junk, b_st])
    k.op("dve", lambda e: e.tensor_scalar(out=st[0:P, 1:2], in0=st[0:P, 0:1], scalar1=1.0 / dim, scalar2=EPS,
                                           op0=ALU.mult, op1=ALU.add), reads=[b_st], writes=[b_st])
    k.op("act", lambda e: e.sqrt(out=st[0:P, 2:3], in_=st[0:P, 1:2]), reads=[b_st], writes=[b_st])
    k.op("dve", lambda e: e.reciprocal(out=st[0:P, 3:4], in_=st[0:P, 2:3]), reads=[b_st], writes=[b_st])
    return st[0:P, 3:4], b_st


def norm_mod_T(g, ns, xt, b_xt, gbc, shbc, b_mod, hT_dst, b_hT, ps, pb):
    k = g.k
    i = ns.i % ns.nbuf
    ns.i += 1
    rstd, b_st = rstd_of(g, ns, i, xt, b_xt)
    tmp, b_tmp, hb, b_hb = ns.tmp[i], ns.b_tmp[i], ns.hb[i], ns.b_hb[i]
    k.op("dve", lambda e: e.scalar_tensor_tensor(out=tmp[:], in0=xt, scalar=rstd, in1=gbc,
                                                  op0=ALU.mult, op1=ALU.mult),
         reads=[b_xt, b_st, b_mod], writes=[b_tmp])
    k.op("pool", lambda e: e.tensor_tensor(out=hb[:], in0=tmp[:], in1=shbc, op=ALU.add),
         reads=[b_tmp, b_mod], writes=[b_hb])
    psv = ps.bitcast(BF16)
    for kk in range(8):
        k.op("pe", lambda e, kk=kk: e.transpose(out=psv[:, kk * 128:(kk + 1) * 128],
                                                in_=hb[:, kk * 128:(kk + 1) * 128], identity=ns.ident[:]),
             reads=[b_hb, ns.b_ident], writes=[pb], inc=(kk == 7))
    k.op("act", lambda e: e.copy(out=hT_dst, in_=psv[:, 0:1024].rearrange("p (k t) -> p k t", t=128)),
         reads=[pb], writes=[b_hT])
    return hb, b_hb


class ModBC:
    def __init__(self, g, pfx, layer, slots):
        self.g, self.layer, self.slots = g, layer, slots
        self.t = [sbt(g, "%s_m%d" % (pfx, s), [128, D], F32) for s in slots]
        self.b = Buf()
        self.cond = None

    def get(self, cond):
        g = self.g
        if cond != self.cond:
            self.cond = cond
            for t, s in zip(self.t, self.slots):
                g.k.dma("sp", t[:], bcast_rows(g.mod[self.layer, cond:cond + 1, s * D:(s + 1) * D], 128),
                        reads=[g.bufs["mod"]], writes=[self.b])
        return self.t, self.b


def seq_list(g, xsrc, csrc, xdst, cdst, with_ctx=True):
    L = []
    for b in range(NB):
        if with_ctx:
            L.append((g.__dict__[csrc][b], g.__dict__[cdst][b], 2, TC, g.bufs.get(csrc), g.bufs.get(cdst)))
        L.append((g.__dict__[xsrc][b], g.__dict__[xdst][b], b, T, g.bufs.get(xsrc), g.bufs.get(xdst)))
    return L


def phase_ffn(g):
    k, nc = g.k, g.nc
    src_x, src_c = ("x1", "c1") if "ssd" in g.phases else ("x", "ctx")
    NT = 256
    NF = FFN // 128
    w1 = sbt(g, "f_w1", [128, 8, 2 * FFN], BF16)
    w2 = sbt(g, "f_w2", [128, NF, D], BF16)
    b_w1, b_w2 = Buf(), Buf()
    for kk in range(8):
        k.dma("pool", w1[:, kk, :], g.gin("ffn_w_in")[kk * 128:(kk + 1) * 128, :], writes=[b_w1])
    for f in range(NF):
        k.dma("pool", w2[:, f, :], g.gin("ffn_w_out")[f * 128:(f + 1) * 128, :], writes=[b_w2])
    ns = NormSet(g, "f_")
    hT = [sbt(g, "f_hT%d" % i, [128, 8, NT], BF16) for i in range(2)]
    b_hT = [Buf(), Buf()]
    aT = [sbt(g, "f_aT0", [128, NF, NT], BF16)] * 2
    b_aT = [Buf()] * 2
    xt = [sbt(g, "f_xt%d" % i, [128, D], F32) for i in range(4)]
    b_xt = [Buf() for _ in range(4)]
    sg = [sbt(g, "f_sg%d" % i, [128, NT], F32) for i in range(2)]
    b_sg = [Buf(), Buf()]
    sg_o = [sbt(g, "f_o%d" % i, [128, 512], F32) for i in range(2)]
    b_sgo = [Buf(), Buf()]
    mods = ModBC(g, "f", 0, (3, 4, 5))
    grp = 0
    oi = 0
    fi = 0
    LIM, STAGE = 1000, 9
    for (src, dst, cond, ntok, bsrc, bdst) in seq_list(g, src_x, src_c, "x2", "c2")[:LIM]:
        (shbc, gbc, gatebc), b_mod = mods.get(cond)
        for t0 in range(0, ntok, NT):
            hi = grp % 2
            for j in range(NT // 128):
                xi = (grp % 2) * 2 + j
                k.dma("sp", xt[xi][:], src[t0 + j * 128:t0 + (j + 1) * 128, :], reads=[bsrc], writes=[b_xt[xi]])
                norm_mod_T(g, ns, xt[xi][:], b_xt[xi], gbc[:], shbc[:], b_mod,
                           hT[hi][:, :, j * 128:(j + 1) * 128], b_hT[hi], g.ps[4 + j], g.psb[4 + j])
            for f in range(NF if STAGE >= 2 else 0):
                pg, pu = g.ps[(fi % 2) * 2], g.ps[(fi % 2) * 2 + 1]
                bg, bu = g.psb[(fi % 2) * 2], g.psb[(fi % 2) * 2 + 1]
                si = fi % 2
                fi += 1
                for kk in range(8):
                    k.op("pe", lambda e, kk=kk: e.matmul(pg[:, 0:NT], lhsT=w1[:, kk, f * 128:(f + 1) * 128],
                                                         rhs=hT[hi][:, kk, :], start=(kk == 0), stop=(kk == 7)),
                         reads=[b_w1, b_hT[hi]], writes=[bg], inc=(kk == 7))
                for kk in range(8):
                    k.op("pe", lambda e, kk=kk: e.matmul(pu[:, 0:NT], lhsT=w1[:, kk, FFN + f * 128:FFN + (f + 1) * 128],
                                                         rhs=hT[hi][:, kk, :], start=(kk == 0), stop=(kk == 7)),
                         reads=[b_w1, b_hT[hi]], writes=[bu], inc=(kk == 7))
                k.op("act", lambda e: e.activation(out=sg[si][:], in_=pg[:, 0:NT], func=AF.Silu),
                     reads=[bg], writes=[b_sg[si]])
                k.op("dve", lambda e: e.tensor_tensor(out=aT[hi][:, f, :], in0=sg[si][:], in1=pu[:, 0:NT], op=ALU.mult),
                     reads=[b_sg[si], bu], writes=[b_aT[hi]])
            for j in range(NT // 128):
                xi = (grp % 2) * 2 + j
                o = sg_o[oi % 2]
                bo = b_sgo[oi % 2]
                oi += 1
                for half in range(2):
                    po, pbo = g.ps[6 + half], g.psb[6 + half]
                    for f in range(NF):
                        k.op("pe", lambda e, f=f: e.matmul(po[:, :], lhsT=aT[hi][:, f, j * 128:(j + 1) * 128],
                                                           rhs=w2[:, f, half * 512:(half + 1) * 512],
                                                           start=(f == 0), stop=(f == NF - 1)),
                             reads=[b_aT[hi], b_w2], writes=[pbo], inc=(f == NF - 1))
                    hs = slice(half * 512, (half + 1) * 512)
                    o = sg_o[oi % 2]
                    bo = b_sgo[oi % 2]
                    oi += 1
                    k.op("dve", lambda e: e.tensor_tensor(out=o[:], in0=po[:, :], in1=gatebc[:, hs], op=ALU.mult),
                         reads=[pbo, b_mod], writes=[bo])
                    k.op("pool", lambda e: e.tensor_tensor(out=xt[xi][:, hs], in0=o[:], in1=xt[xi][:, hs], op=ALU.add),
                         reads=[bo, b_xt[xi]], writes=[b_xt[xi]])
                k.dma("pool", dst[t0 + j * 128:t0 + (j + 1) * 128, :], xt[xi][:], reads=[b_xt[xi]], writes=[bdst])
            grp += 1


NTK = T + TC


def phase_ssd(g):
    with ExitStack() as pes:
        g.pes = pes
        ssd_in(g)
        g.k.barrier()
    with ExitStack() as pes:
        g.pes = pes
        ssd_scan(g)
        g.k.barrier()


def ssd_in(g):
    k, nc = g.k, g.nc
    w = sbt(g, "s_w", [128, 8, 5184], BF16)
    b_w = Buf()
    for kk in range(8):
        k.dma("pool", w[:, kk, :], g.gin("ssd_w_in")[kk * 128:(kk + 1) * 128, :], writes=[b_w])
    cw = sbt(g, "s_cw", [128, 24, 6], F32)
    b_cw = Buf()
    k.dma("sp", cw[:], g.gin("convT").rearrange("(c p) k -> p c k", p=128), writes=[b_cw])
    dtb = sbt(g, "s_dtb", [128, 64], F32)
    abc = sbt(g, "s_abc", [128, 64], F32)
    b_dtb, b_abc = Buf(), Buf()
    k.dma("sp", dtb[:], bcast_rows(g.gin("ssd_dt_bias")[0:1, :], 128), writes=[b_dtb])
    k.dma("sp", abc[:], bcast_rows(g.gin("ssd_a_log")[0:1, :], 128), writes=[b_abc])
    k.op("act", lambda e: e.activation(out=abc[:], in_=abc[:], func=AF.Exp), reads=[b_abc], writes=[b_abc])
    k.op("dve", lambda e: e.tensor_scalar(out=abc[:], in0=abc[:], scalar1=-1.0, scalar2=None, op0=ALU.mult),
         reads=[b_abc], writes=[b_abc])
    ns = NormSet(g, "s_")
    hT = sbt(g, "s_hT", [128, 8, NTK], BF16)
    b_hT = Buf()
    xt = [sbt(g, "s_xt%d" % i, [128, D], F32) for i in range(2)]
    b_xt = [Buf(), Buf()]
    pre = [sbt(g, "s_pre%d" % i, [128, 2312], F32) for i in range(2)]
    b_pre = [Buf(), Buf()]
    for p_ in pre:
        k.op("pool", lambda e, p_=p_: e.memset(p_[:], 0.0), writes=[b_pre[0], b_pre[1]])
    accv = sbt(g, "s_acc", [128, NTK], F32)
    b_accv = Buf()
    xo = [sbt(g, "s_xo%d" % i, [128, NTK], BF16) for i in range(2)]
    b_xo = [Buf(), Buf()]
    zt = [sbt(g, "s_zt%d" % i, [128, 2048], BF16) for i in range(2)]
    b_zt = [Buf(), Buf()]
    ddt = [sbt(g, "s_dd%d" % i, [128, 192], F32) for i in range(2)]
    b_ddt = [Buf(), Buf()]
    mods = ModBC(g, "s", 0, (0, 1))
    xi = 0
    pi = 0
    for b in range(NB):
        for (src, cond, ntok, off, bsrc) in ((g.ctx[b], 2, TC, 0, g.bufs["ctx"]), (g.x[b], b, T, TC, g.bufs["x"])):
            (shbc, gbc), b_mod = mods.get(cond)
            for j in range(ntok // 128):
                x_ = xt[xi % 2]
                bx = b_xt[xi % 2]
                k.dma("sp", x_[:], src[j * 128:(j + 1) * 128, :], reads=[bsrc], writes=[bx])
                norm_mod_T(g, ns, x_[:], bx, gbc[:], shbc[:], b_mod,
                           hT[:, :, off + j * 128:off + (j + 1) * 128], b_hT, g.ps[6 + xi % 2], g.psb[6 + xi % 2])
                xi += 1
        for ct in range(24):
            pr, bpr = pre[ct % 2], b_pre[ct % 2]
            for (c0, c1) in ((0, 256), (256, 768), (768, 1280), (1280, 1792), (1792, 2304)):
                ps, pb = g.ps[pi % 4], g.psb[pi % 4]
                pi += 1
                n = c1 - c0
                for kk in range(8):
                    k.op("pe", lambda e, kk=kk: e.matmul(ps[:, 0:n], lhsT=w[:, kk, 2048 + ct * 128:2048 + (ct + 1) * 128],
                                                         rhs=hT[:, kk, c0:c1], start=(kk == 0), stop=(kk == 7)),
                         reads=[b_w, b_hT], writes=[pb], inc=(kk == 7))
                po = 2 + c0 if c0 < 256 else c0 + 6
                k.op("act", lambda e: e.copy(out=pr[:, po:po + n], in_=ps[:, 0:n]), reads=[pb], writes=[bpr])
            for (a0, n, p0) in ((0, 256, 0), (256, 2048, 260)):
                k.op("dve", lambda e: e.tensor_scalar(out=accv[:, a0:a0 + n], in0=pr[:, p0:p0 + n],
                                                       scalar1=cw[:, ct, 0:1], scalar2=None, op0=ALU.mult),
                     reads=[bpr, b_cw], writes=[b_accv])
                for q in range(1, 5):
                    k.op("dve", lambda e, q=q: e.scalar_tensor_tensor(out=accv[:, a0:a0 + n], in0=pr[:, p0 + q:p0 + q + n],
                                                                       scalar=cw[:, ct, q:q + 1], in1=accv[:, a0:a0 + n],
                                                                       op0=ALU.mult, op1=ALU.add),
                         reads=[bpr, b_cw, b_accv], writes=[b_accv])
            o_, bo = xo[ct % 2], b_xo[ct % 2]
            k.op("act", lambda e: e.activation(out=o_[:], in_=accv[:], func=AF.Silu, bias=cw[:, ct, 5:6]),
                 reads=[b_accv, b_cw], writes=[bo])
            k.dma("pool", g.xbcT[b, ct * 128:(ct + 1) * 128, :], o_[:], reads=[bo], writes=[g.bufs["xbcT"]])
        for j in range(NTK // 128):
            z_, bz = zt[j % 2], b_zt[j % 2]
            for n4 in range(4):
                ps, pb = g.ps[pi % 4], g.psb[pi % 4]
                pi += 1
                for kk in range(8):
                    k.op("pe", lambda e, kk=kk: e.matmul(ps[:, :], lhsT=hT[:, kk, j * 128:(j + 1) * 128],
                                                         rhs=w[:, kk, n4 * 512:(n4 + 1) * 512], start=(kk == 0), stop=(kk == 7)),
                         reads=[b_w, b_hT], writes=[pb], inc=(kk == 7))
                k.op("act", lambda e: e.activation(out=z_[:, n4 * 512:(n4 + 1) * 512], in_=ps[:, :], func=AF.Silu),
                     reads=[pb], writes=[bz])
            k.dma("pool", g.zs[b, j * 128:(j + 1) * 128, :], z_[:], reads=[bz], writes=[g.bufs["zs"]])
            ps, pb = g.ps[pi % 4], g.psb[pi % 4]
            pi += 1
            for kk in range(8):
                k.op("pe", lambda e, kk=kk: e.matmul(ps[:, 0:64], lhsT=hT[:, kk, j * 128:(j + 1) * 128],
                                                     rhs=w[:, kk, 5120:5184], start=(kk == 0), stop=(kk == 7)),
                     reads=[b_w, b_hT], writes=[pb], inc=(kk == 7))
            d_, bd = ddt[j % 2], b_ddt[j % 2]
            k.op("dve", lambda e: e.tensor_tensor(out=d_[:, 128:192], in0=ps[:, 0:64], in1=dtb[:], op=ALU.add),
                 reads=[pb, b_dtb], writes=[bd])
            k.op("act", lambda e: e.activation(out=d_[:, 128:192], in_=d_[:, 128:192], func=AF.Exp), reads=[bd], writes=[bd])
            k.op("act", lambda e: e.activation(out=d_[:, 0:64], in_=d_[:, 128:192], func=AF.Ln, bias=1.0), reads=[bd], writes=[bd])
            k.op("dve", lambda e: e.tensor_tensor(out=d_[:, 64:128], in0=d_[:, 0:64], in1=abc[:], op=ALU.mult),
                 reads=[bd, b_abc], writes=[bd])
            k.dma("pool", g.dd[b, j * 128:(j + 1) * 128, :], d_[:, 0:128], reads=[bd], writes=[g.bufs["dd"]])


def bc3(ap2, n_in, n_rep):
    a = ap2.ap
    return AP(ap2.tensor, ap2.offset, [list(a[0]), [a[1][0], n_in], [0, n_rep]])


def bmid(ap2, n_rep):
    a = ap2.ap
    return AP(ap2.tensor, ap2.offset, [list(a[0]), [0, n_rep], [a[1][0], a[1][1]]])


def ssd_scan(g):
    k, nc = g.k, g.nc
    cm = sbt(g, "q_cm", [128, 5, 128], F32)
    b_cm = Buf()
    k.dma("sp", cm[:], g.gin("cmat").rearrange("m p c -> p m c"), writes=[b_cm])
    cmb = sbt(g, "q_cmb", [128, 5, 128], BF16)
    k.op("dve", lambda e: e.tensor_copy(out=cmb[:], in_=cm[:]), reads=[b_cm], writes=[b_cm])
    identb = sbt(g, "q_identb", [128, 128], BF16)
    b_id = Buf()
    k.dma("pool", identb[:], g.gin("ident")[:, :], writes=[b_id])
    wout = sbt(g, "q_wout", [128, 16, D], BF16)
    b_wout = Buf()
    for c in range(16):
        k.dma("pool", wout[:, c, :], g.gin("ssd_w_out")[c * 128:(c + 1) * 128, :], writes=[b_wout])
    nwbc = sbt(g, "q_nw", [128, 2048], F32)
    dsk = sbt(g, "q_dsk", [128, 32], F32)
    b_nw = Buf()
    k.dma("sp", nwbc[:], bcast_rows(g.gin("ssd_norm")[0:1, :], 128), writes=[b_nw])
    k.dma("sp", dsk[:], bcast_rows(g.gin("ssd_d")[0:1, :], 128), writes=[b_nw])
    gate = {}
    S_all = [sbt(g, "q_S%d" % i, [128, 4, 512], F32) for i in range(NB)]
    Sb_all = [sbt(g, "q_Sb%d" % i, [128, 4, 512], BF16) for i in range(NB)]
    b_S_all = [[Buf() for _ in range(4)] for _ in range(NB)]
    b_Sb_all = [[Buf() for _ in range(4)] for _ in range(NB)]
    FM = [sbt(g, "q_FM%d" % i, [128, 24, 128], BF16) for i in range(2)]
    b_FM = [Buf(), Buf()]
    ddT = [sbt(g, "q_dd%d" % i, [128, 128], F32) for i in range(2)]
    b_dd = [Buf(), Buf()]
    Xtok = [sbt(g, "q_X%d" % i, [128, 2048], BF16) for i in range(2)]
    b_X = [Buf(), Buf()]
    Btok = [sbt(g, "q_B%d" % i, [128, 512], BF16) for i in range(2)]
    b_B = [Buf(), Buf()]
    cue = [sbt(g, "q_cue%d" % i, [128, 128], F32) for i in range(2)]
    b_cue = [Buf(), Buf()]
    xdt = sbt(g, "q_xdt", [128, 2048], BF16)
    xdtu = sbt(g, "q_xdtu", [128, 2048], BF16)
    b_xdt, b_xdtu = Buf(), Buf()
    sm = [sbt(g, "q_sm%d" % i, [128, 128], BF16) for i in range(2)]
    b_sm = [Buf(), Buf()]
    lh4 = [sbt(g, "q_lh%d" % i, [128, 4, 128], BF16) for i in range(2)]
    b_lh4 = [Buf(), Buf()]
    Em4 = [sbt(g, "q_E%d" % i, [128, 512], BF16) for i in range(2)]
    b_E4 = [Buf(), Buf()]
    Lm4 = [sbt(g, "q_L%d" % i, [128, 512], BF16) for i in range(2)]
    b_L4 = [Buf(), Buf()]
    BURST = 8
    t1 = [sbt(g, "q_t1%d" % i, [128, 512], F32) for i in range(2)]
    b_t1 = [Buf(), Buf()]
    yblk = [sbt(g, "q_y%d" % i, [128, 2048], F32) for i in range(2)]
    b_y = [Buf(), Buf()]
    yfl = sbt(g, "q_yfl", [128, 2048], F32)
    b_yfl = Buf()
    ztl = sbt(g, "q_zt", [128, 2048], BF16)
    b_ztl = Buf()
    ygn = sbt(g, "q_ygn", [128, 2048], BF16)
    b_ygn = Buf()
    ygT = sbt(g, "q_ygT", [128, 16, 128], BF16)
    b_ygT = Buf()
    xt = [sbt(g, "q_xt%d" % i, [128, D], F32) for i in range(2)]
    b_xt = [Buf(), Buf()]
    ot = [sbt(g, "q_ot%d" % i, [128, 512], F32) for i in range(2)]
    b_ot = [Buf(), Buf()]
    gbc_all = [[sbt(g, "q_g%d_%d" % (b_, i), [128, D], F32) for i in range(2)] for b_ in range(NB)]
    b_g = Buf()
    ns = NormSet(g, "q_", nbuf=1)
    sc_slots = [(g.ps[0][:, i * 128:(i + 1) * 128], g.psb[0]) for i in range(3)]
    cum_ps, b_cum = g.ps[0][:, 384:480], g.psb[0]
    d_slots = [(g.ps[1 + i // 4][:, (i % 4) * 128:(i % 4 + 1) * 128], g.psb[1 + i // 4]) for i in range(8)]
    tr_b = [g.psb[6], g.psb[7]]
    cnt = dict(blk=0, sc=0, d=0, h=0, t1=0, o=0)

    for b in range(NB):
        k.dma("sp", gbc_all[b][0][:], bcast_rows(g.mod[0, 2:3, 2 * D:3 * D], 128), reads=[g.bufs["mod"]], writes=[b_g])
        k.dma("sp", gbc_all[b][1][:], bcast_rows(g.mod[0, b:b + 1, 2 * D:3 * D], 128), reads=[g.bufs["mod"]], writes=[b_g])
    for dr in range(2):
        order = list(range(18)) if dr == 0 else [1, 0] + list(range(17, 1, -1))
        CUMM, GL, RM = (0, 1, 0) if dr == 0 else (3, 2, 3)
        for b in range(NB):
            for gi in range(4):
                k.op("pool", lambda e, gi=gi, b=b: e.memset(S_all[b][:, gi, :], 0.0), writes=[b_S_all[b][gi]])
                k.op("pool", lambda e, gi=gi, b=b: e.memset(Sb_all[b][:, gi, :], 0.0), writes=[b_Sb_all[b][gi]])
        for blk in order:
            for b in range(NB):
                S, Sb, b_S, b_Sb, gbc = S_all[b], Sb_all[b], b_S_all[b], b_Sb_all[b], gbc_all[b]
                c0 = blk * 128
                bi = cnt["blk"] % 2
                cnt["blk"] += 1
                fm, bfm, dT, bdT = FM[bi], b_FM[bi], ddT[bi], b_dd[bi]
                X, bX, Bt, bB, cu, bcu = Xtok[bi], b_X[bi], Btok[bi], b_B[bi], cue[bi], b_cue[bi]
                yb, byb = yblk[bi], b_y[bi]
                k.dma("sp", fm[:], g.xbcT[b, :, c0:c0 + 128].rearrange("(c p) t -> p c t", p=128),
                      reads=[g.bufs["xbcT"]], writes=[bfm])
                k.dma("sp", dT[:], g.dd[b, c0:c0 + 128, :], reads=[g.bufs["dd"]], writes=[bdT])
                for hh in range(2):
                    psv = g.ps[6 + hh].bitcast(BF16)
                    for c in range(8):
                        k.op("pe", lambda e, c=c: e.transpose(out=psv[:, c * 128:(c + 1) * 128], in_=fm[:, hh * 8 + c, :],
                                                              identity=identb[:]),
                             reads=[bfm, b_id], writes=[tr_b[hh]], inc=(c == 7))
                    k.op("act", lambda e: e.copy(out=X[:, hh * 1024:(hh + 1) * 1024], in_=psv[:, 0:1024]),
                         reads=[tr_b[hh]], writes=[bX])
                psv = g.ps[6].bitcast(BF16)
                for c in range(4):
                    k.op("pe", lambda e, c=c: e.transpose(out=psv[:, c * 128:(c + 1) * 128], in_=fm[:, 16 + c, :],
                                                          identity=identb[:]),
                         reads=[bfm, b_id], writes=[tr_b[0]], inc=(c == 3))
                k.op("act", lambda e: e.copy(out=Bt[:], in_=psv[:, 0:512]), reads=[tr_b[0]], writes=[bB])
                da = dT[:, 64 + 32 * dr:96 + 32 * dr]
                dtc = dT[:, 32 * dr:32 * dr + 32]
                for q, mi in enumerate((CUMM, GL, 4)):
                    k.op("pe", lambda e, q=q, mi=mi: e.matmul(cum_ps[:, q * 32:(q + 1) * 32], lhsT=cm[:, mi, :], rhs=da,
                                                              start=True, stop=True),
                         reads=[b_cm, bdT], writes=[b_cum], inc=(q == 2))
                k.op("act", lambda e: e.activation(out=cu[:, 0:96], in_=cum_ps, func=AF.Exp), reads=[b_cum], writes=[bcu])
                k.op("dve", lambda e: e.tensor_tensor(out=cu[:, 96:128], in0=cu[:, 32:64], in1=dtc, op=ALU.mult),
                     reads=[bcu, bdT], writes=[bcu])
                X3 = X[:].rearrange("p (h q) -> p h q", q=64)
                k.op("pool", lambda e: e.tensor_tensor(out=xdt[:].rearrange("p (h q) -> p h q", q=64), in0=X3,
                                                        in1=bc3(dtc, 32, 64), op=ALU.mult),
                     reads=[bX, bdT], writes=[b_xdt])
                k.op("pool", lambda e: e.tensor_tensor(out=xdtu[:].rearrange("p (h q) -> p h q", q=64), in0=X3,
                                                        in1=bc3(cu[:, 96:128], 32, 64), op=ALU.mult),
                     reads=[bX, bcu], writes=[b_xdtu])
                smis = {}

                def group_pre(gi):
                    ps_s, b_ps_s = sc_slots[cnt["sc"] % 3]
                    smi = cnt["sc"] % 2
                    cnt["sc"] += 1
                    smis[gi] = smi
                    k.op("pe", lambda e: e.matmul(ps_s, lhsT=fm[:, 16 + gi, :], rhs=fm[:, 20 + gi, :], start=True, stop=True),
                         reads=[bfm], writes=[b_ps_s])
                    k.op("dve", lambda e: e.tensor_tensor(out=sm[smi][:], in0=ps_s, in1=cm[:, RM, :], op=ALU.mult),
                         reads=[b_ps_s, b_cm], writes=[b_sm[smi]])

                def front(i):
                    gi, half = divmod(i, 2)
                    par = i % 2
                    h0 = gi * 8 + half * 4
                    k.op("dve", lambda e: e.tensor_tensor(out=lh4[par][:], in0=bmid(cm[:, GL, :], 4), in1=bc3(da[:, h0:h0 + 4], 4, 128),
                                                           op=ALU.mult),
                         reads=[b_cm, bdT], writes=[b_lh4[par]])
                    psD4, b_psD = g.ps[1 + par], g.psb[1 + par]
                    for j in range(4):
                        k.op("pe", lambda e, j=j: e.matmul(psD4[:, j * 128:(j + 1) * 128], lhsT=lh4[par][:, j, :], rhs=cmb[:, RM, :],
                                                           start=True, stop=True),
                             reads=[b_lh4[par], b_cm], writes=[b_psD], inc=(j == 3))

                def back(i):
                    gi, half = divmod(i, 2)
                    par = i % 2
                    smi = smis[gi]
                    yd, b_yd = g.ps[3], g.psb[3]
                    psD4, b_psD = g.ps[1 + par], g.psb[1 + par]
                    k.op("act", lambda e: e.activation(out=Em4[par][:], in_=psD4[:, :], func=AF.Exp), reads=[b_psD], writes=[b_E4[par]])
                    k.op("dve", lambda e: e.tensor_tensor(out=Lm4[par][:].rearrange("p (h c) -> p h c", c=128),
                                                           in0=Em4[par][:].rearrange("p (h c) -> p h c", c=128),
                                                           in1=bmid(sm[smi][:], 4), op=ALU.mult),
                         reads=[b_E4[par], b_sm[smi]], writes=[b_L4[par]])
                    for j in range(4):
                        h8 = half * 4 + j
                        h = gi * 8 + h8
                        k.op("pe", lambda e, h=h, h8=h8, j=j: e.matmul(yd[:, h8 * 64:(h8 + 1) * 64], lhsT=Lm4[par][:, j * 128:(j + 1) * 128],
                                                                      rhs=xdt[:, h * 64:(h + 1) * 64], start=True, stop=True),
                             reads=[b_L4[par], b_xdt], writes=[b_yd], inc=(j == 3))

                def group_tail(gi):
                    yd, b_yd = g.ps[3], g.psb[3]
                    yo, b_yo = g.ps[4], g.psb[4]
                    k.op("pe", lambda e: e.matmul(yo[:, :], lhsT=fm[:, 20 + gi, :], rhs=Sb[:, gi, :], start=True, stop=True),
                         reads=[bfm, b_Sb[gi]], writes=[b_yo])
                    ti = cnt["t1"] % 2
                    cnt["t1"] += 1
                    k.op("dve", lambda e: e.tensor_tensor(out=t1[ti][:].rearrange("p (h q) -> p h q", q=64),
                                                           in0=yo[:, :].rearrange("p (h q) -> p h q", q=64),
                                                           in1=bc3(cu[:, gi * 8:gi * 8 + 8], 8, 64), op=ALU.mult),
                         reads=[b_yo, bcu], writes=[b_t1[ti]])
                    k.op("dve", lambda e: e.tensor_tensor(out=yb[:, gi * 512:(gi + 1) * 512], in0=t1[ti][:], in1=yd[:, :], op=ALU.add),
                         reads=[b_t1[ti], b_yd], writes=[byb])
                    cs, b_cs = g.ps[5], g.psb[5]
                    k.op("pe", lambda e: e.matmul(cs[:, :], lhsT=Bt[:, gi * 128:(gi + 1) * 128], rhs=xdtu[:, gi * 512:(gi + 1) * 512],
                                                  start=True, stop=True),
                         reads=[bB, b_xdtu], writes=[b_cs])
                    S3 = S[:, gi, :].rearrange("p (h q) -> p h q", q=64)
                    k.op("pool", lambda e: e.tensor_tensor(out=S3, in0=S3, in1=bc3(cu[:, 64 + gi * 8:64 + gi * 8 + 8], 8, 64), op=ALU.mult),
                         reads=[b_S[gi], bcu], writes=[b_S[gi]])
                    k.op("dve", lambda e: e.tensor_tensor(out=S[:, gi, :], in0=S[:, gi, :], in1=cs[:, :], op=ALU.add),
                         reads=[b_S[gi], b_cs], writes=[b_S[gi]])
                    k.op("act", lambda e: e.copy(out=Sb[:, gi, :], in_=S[:, gi, :]), reads=[b_S[gi]], writes=[b_Sb[gi]])
                group_pre(0)
                front(0)
                for i in range(8):
                    if i + 1 < 8:
                        if (i + 1) % 2 == 0:
                            group_pre((i + 1) // 2)
                        front(i + 1)
                    back(i)
                    if i % 2 == 1:
                        group_tail(i // 2)
                if dr == 0:
                    k.dma("pool", g.yf[b, c0:c0 + 128, :], yb[:], reads=[byb], writes=[g.bufs["yf"]])
                    continue
                k.dma("sp", yfl[:], g.yf[b, c0:c0 + 128, :], reads=[g.bufs["yf"]], writes=[b_yfl])
                k.dma("sp", ztl[:], g.zs[b, c0:c0 + 128, :], reads=[g.bufs["zs"]], writes=[b_ztl])
                is_ctx = blk < 2
                xsrc = g.ctx[b, c0:c0 + 128, :] if is_ctx else g.x[b, c0 - TC:c0 - TC + 128, :]
                xdst = g.c1[b, c0:c0 + 128, :] if is_ctx else g.x1[b, c0 - TC:c0 - TC + 128, :]
                bdst = g.bufs["c1"] if is_ctx else g.bufs["x1"]
                x_, bx = xt[bi], b_xt[bi]
                k.dma("sp", x_[:], xsrc, reads=[g.bufs["ctx" if is_ctx else "x"]], writes=[bx])
                k.op("pool", lambda e: e.tensor_tensor(out=yb[:], in0=yb[:], in1=yfl[:], op=ALU.add),
                     reads=[byb, b_yfl], writes=[byb])
                k.op("dve", lambda e: e.tensor_tensor(out=yfl[:].rearrange("p (h q) -> p h q", q=64), in0=X3,
                                                       in1=bc3(dsk[:], 32, 64), op=ALU.mult),
                     reads=[bX, b_nw, b_yfl], writes=[b_yfl])
                k.op("pool", lambda e: e.tensor_tensor(out=yb[:], in0=yb[:], in1=yfl[:], op=ALU.add),
                     reads=[byb, b_yfl], writes=[byb])
                k.op("dve", lambda e: e.tensor_tensor(out=yb[:], in0=yb[:], in1=ztl[:], op=ALU.mult),
                     reads=[byb, b_ztl], writes=[byb])
                rstd, b_st = rstd_of(g, ns, 0, yb[:], byb, dim=2048)
                k.op("dve", lambda e: e.tensor_tensor(out=ygn[:], in0=yb[:], in1=nwbc[:], op=ALU.mult),
                     reads=[byb, b_nw], writes=[b_ygn])
                for hh in range(2):
                    psv = g.ps[6 + hh].bitcast(BF16)
                    for c in range(8):
                        k.op("pe", lambda e, c=c: e.transpose(out=psv[:, c * 128:(c + 1) * 128],
                                                              in_=ygn[:, (hh * 8 + c) * 128:(hh * 8 + c + 1) * 128], identity=identb[:]),
                             reads=[b_ygn, b_id], writes=[tr_b[hh]], inc=(c == 7))
                    k.op("act", lambda e: e.copy(out=ygT[:, hh * 8:(hh + 1) * 8, :],
                                                 in_=psv[:, 0:1024].rearrange("p (c t) -> p c t", t=128)),
                         reads=[tr_b[hh]], writes=[b_ygT])
                gt = gbc[0] if is_ctx else gbc[1]
                for half in range(2):
                    hs = slice(half * 512, (half + 1) * 512)
                    po, b_po = g.ps[3 + half], g.psb[3 + half]
                    for c in range(16):
                        k.op("pe", lambda e, c=c: e.matmul(po[:, :], lhsT=ygT[:, c, :], rhs=wout[:, c, hs],
                                                           start=(c == 0), stop=(c == 15)),
                             reads=[b_ygT, b_wout], writes=[b_po], inc=(c == 15))
                    o_, bo = ot[cnt["o"] % 2], b_ot[cnt["o"] % 2]
                    cnt["o"] += 1
                    k.op("dve", lambda e: e.scalar_tensor_tensor(out=o_[:], in0=po[:, :], scalar=rstd, in1=gt[:, hs],
                                                                  op0=ALU.mult, op1=ALU.mult),
                         reads=[b_po, b_st, b_g], writes=[bo])
                    k.op("pool", lambda e: e.tensor_tensor(out=x_[:, hs], in0=x_[:, hs], in1=o_[:], op=ALU.add),
                         reads=[bo, bx], writes=[bx])
                k.dma("pool", xdst, x_[:], reads=[bx], writes=[bdst])


MAGIC = 12582912.0
TWO_PI_HI = 6.28125
TWO_PI_LO = 0.0019353071795864769


def s5_prep(g):
    k, nc = g.k, g.nc
    V = lambda n, sh=(128, 32): sbt(g, "p_" + n, list(sh), F32)
    ident = sbt(g, "p_identb", [128, 128], BF16)
    b_id = Buf()
    k.dma("pool", ident[:], g.gin("ident")[:, :], writes=[b_id])
    m8 = sbt(g, "p_m8", [128, 2, 128], F32)
    b_m8 = Buf()
    k.dma("sp", m8[:], g.gin("m8").rearrange("m p c -> p m c"), writes=[b_m8])
    bb = Buf()

    def dve(fn, eng="dve"):
        k.op(eng, fn, reads=[bb], writes=[bb])

    def tt(out, a, b_, op):
        dve(lambda e: e.tensor_tensor(out=out, in0=a, in1=b_, op=op))

    def ts(out, a, s1, op0, s2=None, op1=None):
        if op1 is None:
            dve(lambda e: e.tensor_scalar(out=out, in0=a, scalar1=s1, scalar2=None, op0=op0))
        else:
            dve(lambda e: e.tensor_scalar(out=out, in0=a, scalar1=s1, scalar2=s2, op0=op0, op1=op1))

    lre, lim, lst = V("lre"), V("lim"), V("lst")
    step, lr, th, kf, r_, t8, t2 = V("step"), V("lr"), V("th"), V("kf"), V("r"), V("t8"), V("t2")
    sn, cs_, ta, tb, rho1 = V("sn"), V("cs"), V("ta"), V("tb"), V("rho1")
    ar, ai, den, cr, ci = V("ar"), V("ai"), V("den"), V("cr"), V("ci")
    pm, pc, ps_ = V("pm", (128, 32, 9)), V("pc", (128, 32, 9)), V("ps", (128, 32, 9))
    apr, api = V("apr", (128, 32, 9)), V("api", (128, 32, 9))
    anr, ani, imv = V("anr", (128, 32, 9)), V("ani", (128, 32, 9)), V("imv", (128, 32, 9))
    Pc, Ps = V("Pc"), V("Ps")
    bre, bim = V("bre", (128, 32, 16)), V("bim", (128, 32, 16))
    bbr, bbi = V("bbr", (128, 32, 16)), V("bbi", (128, 32, 16))
    cre, cim = V("cre", (128, 32, 16)), V("cim", (128, 32, 16))
    w1, w2 = V("w1", (128, 32, 16)), V("w2", (128, 32, 16))
    WZr = sbt(g, "p_WZr", [128, 32, 8, 16], BF16)
    WZi = sbt(g, "p_WZi", [128, 32, 8, 16], BF16)
    Pr = sbt(g, "p_Pr", [128, 32, 8, 16], BF16)
    Pi = sbt(g, "p_Pi", [128, 32, 8, 16], BF16)
    CAr = sbt(g, "p_CAr", [128, 32, 9, 16], BF16)
    CAi = sbt(g, "p_CAi", [128, 32, 9, 16], BF16)
    cosT = sbt(g, "p_cosT", [128, 32, 288], F32)
    sinT = sbt(g, "p_sinT", [128, 32, 288], F32)
    e1 = sbt(g, "p_e1", [128, 32, 128], F32)
    e2 = sbt(g, "p_e2", [128, 32, 128], F32)
    wst = [sbt(g, "p_wst%d" % i, [128, 6, 128], BF16) for i in range(2)]
    b_wst = [Buf(), Buf()]

    def b16(t, n):
        return bc3(t[:], 32, n)

    def col(t3, j):
        return t3[:, :, j]

    for d in range(2):
        k.dma("sp", lre[:], g.gin("s5_lamT")[d, 0], reads=[bb], writes=[bb])
        k.dma("sp", lim[:], g.gin("s5_lamT")[d, 1], reads=[bb], writes=[bb])
        k.dma("sp", lst[:], g.gin("s5_lamT")[d, 2], reads=[bb], writes=[bb])
        k.dma("sp", bre[:], g.gin("s5_b")[d, 0].rearrange("(gp q) c -> q gp c", q=128), reads=[bb], writes=[bb])
        k.dma("sp", bim[:], g.gin("s5_b")[d, 1].rearrange("(gp q) c -> q gp c", q=128), reads=[bb], writes=[bb])
        k.dma("sp", cre[:], g.gin("s5_cT")[d, 0], reads=[bb], writes=[bb])
        k.dma("sp", cim[:], g.gin("s5_cT")[d, 1], reads=[bb], writes=[bb])
        dve(lambda e: e.activation(out=step[:], in_=lst[:], func=AF.Exp), "act")
        tt(lr[:], lre[:], step[:], ALU.mult)
        tt(th[:], lim[:], step[:], ALU.mult)
        ts(ta[:], lr[:], 1.0 / 5, ALU.mult, 1.0, ALU.add)
        for c_ in (1.0 / 4, 1.0 / 3, 1.0 / 2, 1.0):
            tt(ta[:], ta[:], lr[:], ALU.mult)
            ts(ta[:], ta[:], c_, ALU.mult, 1.0, ALU.add)
        dve(lambda e: e.tensor_copy(out=rho1[:], in_=ta[:]))
        ts(kf[:], th[:], 1.0 / (2 * np.pi), ALU.mult, MAGIC, ALU.add)
        ts(kf[:], kf[:], -MAGIC, ALU.add)
        dve(lambda e: e.scalar_tensor_tensor(out=r_[:], in0=kf[:], scalar=-TWO_PI_HI, in1=th[:], op0=ALU.mult, op1=ALU.add))
        dve(lambda e: e.scalar_tensor_tensor(out=r_[:], in0=kf[:], scalar=-TWO_PI_LO, in1=r_[:], op0=ALU.mult, op1=ALU.add))
        ts(t8[:], r_[:], 0.125, ALU.mult)
        tt(t2[:], t8[:], t8[:], ALU.mult)
        ts(ta[:], t2[:], -1.0 / 5040, ALU.mult, 1.0 / 120, ALU.add)
        tt(ta[:], ta[:], t2[:], ALU.mult)
        ts(ta[:], ta[:], -1.0 / 6, ALU.add)
        tt(ta[:], ta[:], t2[:], ALU.mult)
        ts(ta[:], ta[:], 1.0, ALU.add)
        tt(sn[:], ta[:], t8[:], ALU.mult)
        ts(ta[:], t2[:], 1.0 / 40320, ALU.mult, -1.0 / 720, ALU.add)
        tt(ta[:], ta[:], t2[:], ALU.mult)
        ts(ta[:], ta[:], 1.0 / 24, ALU.add)
        tt(ta[:], ta[:], t2[:], ALU.mult)
        ts(ta[:], ta[:], -0.5, ALU.add)
        tt(ta[:], ta[:], t2[:], ALU.mult)
        ts(cs_[:], ta[:], 1.0, ALU.add)
        for _ in range(3):
            tt(ta[:], cs_[:], cs_[:], ALU.mult)
            tt(tb[:], sn[:], sn[:], ALU.mult)
            tt(sn[:], sn[:], cs_[:], ALU.mult)
            ts(sn[:], sn[:], 2.0, ALU.mult)
            tt(cs_[:], ta[:], tb[:], ALU.subtract)
        tt(ar[:], rho1[:], cs_[:], ALU.mult)
        tt(ai[:], rho1[:], sn[:], ALU.mult)
        ts(ta[:], ar[:], -1.0, ALU.add)
        tt(den[:], lre[:], lre[:], ALU.mult)
        tt(tb[:], lim[:], lim[:], ALU.mult)
        tt(den[:], den[:], tb[:], ALU.add)
        dve(lambda e: e.reciprocal(out=den[:], in_=den[:]))
        tt(cr[:], ta[:], lre[:], ALU.mult)
        tt(tb[:], ai[:], lim[:], ALU.mult)
        tt(cr[:], cr[:], tb[:], ALU.add)
        tt(cr[:], cr[:], den[:], ALU.mult)
        tt(ci[:], ai[:], lre[:], ALU.mult)
        tt(tb[:], ta[:], lim[:], ALU.mult)
        tt(ci[:], ci[:], tb[:], ALU.subtract)
        tt(ci[:], ci[:], den[:], ALU.mult)
        tt(w1[:], bre[:], b16(cr, 16), ALU.mult)
        tt(w2[:], bim[:], b16(ci, 16), ALU.mult)
        tt(bbr[:], w1[:], w2[:], ALU.subtract)
        tt(w1[:], bim[:], b16(cr, 16), ALU.mult)
        tt(w2[:], bre[:], b16(ci, 16), ALU.mult)
        tt(bbi[:], w1[:], w2[:], ALU.add)
        dve(lambda e: e.memset(col(pm, 0), 1.0))
        dve(lambda e: e.memset(col(pc, 0), 1.0))
        dve(lambda e: e.memset(col(ps_, 0), 0.0))
        for j in range(1, 9):
            tt(col(pm, j), col(pm, j - 1), rho1[:], ALU.mult)
            tt(ta[:], col(pc, j - 1), cs_[:], ALU.mult)
            tt(tb[:], col(ps_, j - 1), sn[:], ALU.mult)
            tt(col(pc, j), ta[:], tb[:], ALU.subtract)
            tt(ta[:], col(ps_, j - 1), cs_[:], ALU.mult)
            tt(tb[:], col(pc, j - 1), sn[:], ALU.mult)
            tt(col(ps_, j), ta[:], tb[:], ALU.add)
        dve(lambda e: e.reciprocal(out=imv[:], in_=pm[:]))
        tt(apr[:], pm[:], pc[:], ALU.mult)
        tt(api[:], pm[:], ps_[:], ALU.mult)
        tt(anr[:], imv[:], pc[:], ALU.mult)
        tt(ani[:], imv[:], ps_[:], ALU.mult)
        ts(ani[:], ani[:], -1.0, ALU.mult)
        dve(lambda e: e.tensor_copy(out=ta[:], in_=col(pm, 8)))
        k.dma("sp", g.rho[d], ta[:], reads=[bb], writes=[g.bufs["rho"]])
        for s_ in range(8):
            so = s_ if d == 0 else 7 - s_
            for (dst, pr_, pi_) in ((WZr, col(apr, 7 - s_), col(api, 7 - s_)), (Pr, col(anr, s_), col(ani, s_))):
                dsti = WZi if dst is WZr else Pi
                tt(w1[:], bbr[:], bc3(pr_, 32, 16), ALU.mult)
                tt(w2[:], bbi[:], bc3(pi_, 32, 16), ALU.mult)
                tt(dst[:, :, so, :], w1[:], w2[:], ALU.subtract)
                tt(w1[:], bbi[:], bc3(pr_, 32, 16), ALU.mult)
                tt(w2[:], bbr[:], bc3(pi_, 32, 16), ALU.mult)
                tt(dsti[:, :, so, :], w1[:], w2[:], ALU.add)
        for j in range(9):
            jo = j if d == 0 else 8 - j
            tt(w1[:], cre[:], bc3(col(apr, j), 32, 16), ALU.mult)
            tt(w2[:], cim[:], bc3(col(api, j), 32, 16), ALU.mult)
            tt(CAr[:, :, jo, :], w1[:], w2[:], ALU.subtract)
            tt(w1[:], cim[:], bc3(col(apr, j), 32, 16), ALU.mult)
            tt(w2[:], cre[:], bc3(col(api, j), 32, 16), ALU.mult)
            tt(w1[:], w1[:], w2[:], ALU.add)
            ts(CAi[:, :, jo, :], w1[:], -1.0, ALU.mult)
        q0, y0 = (0, 1) if d == 0 else (1, 0)
        dve(lambda e: e.tensor_copy(out=Pc[:], in_=col(pc, 8)))
        dve(lambda e: e.tensor_copy(out=Ps[:], in_=col(ps_, 8)))
        dve(lambda e: e.memset(cosT[:, :, 0:1], 1.0))
        dve(lambda e: e.memset(sinT[:, :, 0:1], 0.0))
        wdt = 1
        while wdt < 288:
            n = min(wdt, 288 - wdt)
            tt(e1[:, :, 0:n], cosT[:, :, 0:n], b16(Pc, n), ALU.mult)
            tt(e2[:, :, 0:n], sinT[:, :, 0:n], b16(Ps, n), ALU.mult)
            tt(cosT[:, :, wdt:wdt + n], e1[:, :, 0:n], e2[:, :, 0:n], ALU.subtract)
            tt(e1[:, :, 0:n], sinT[:, :, 0:n], b16(Pc, n), ALU.mult)
            tt(e2[:, :, 0:n], cosT[:, :, 0:n], b16(Ps, n), ALU.mult)
            tt(sinT[:, :, wdt:wdt + n], e1[:, :, 0:n], e2[:, :, 0:n], ALU.add)
            tt(ta[:], Pc[:], Pc[:], ALU.mult)
            tt(tb[:], Ps[:], Ps[:], ALU.mult)
            tt(Ps[:], Ps[:], Pc[:], ALU.mult)
            ts(Ps[:], Ps[:], 2.0, ALU.mult)
            tt(Pc[:], ta[:], tb[:], ALU.subtract)
            wdt *= 2
        k.dma("sp", g.etab[d, 0], cosT[:], reads=[bb], writes=[g.bufs["etab"]])
        k.dma("sp", g.etab[d, 1], sinT[:], reads=[bb], writes=[g.bufs["etab"]])
        for gp in range(32):
            wt, bwt = wst[gp % 2], b_wst[gp % 2]
            for g2 in range(2):
                L = slice(g2 * 64, (g2 + 1) * 64)
                pM, bM = g.ps[g2], g.psb[g2]
                k.op("pe", lambda e: e.matmul(pM[:, 0:128], lhsT=Pr[L, gp, :, :], rhs=CAr[L, gp, q0:q0 + 8, :], start=True, stop=False),
                     reads=[bb], writes=[bM], inc=False)
                k.op("pe", lambda e: e.matmul(pM[:, 0:128], lhsT=Pi[L, gp, :, :], rhs=CAi[L, gp, q0:q0 + 8, :], start=False, stop=True),
                     reads=[bb], writes=[bM])
                k.op("dve", lambda e: e.tensor_tensor(out=wt[:, g2, :], in0=pM[:, 0:128], in1=m8[:, d, :], op=ALU.mult),
                     reads=[bM, b_m8], writes=[bwt])
            for ri, src_ in enumerate((WZr, WZi)):
                pT, bT = g.ps[2 + ri], g.psb[2 + ri]
                pTv = pT.bitcast(BF16)
                k.op("pe", lambda e: e.transpose(out=pTv[:, 0:128], in_=src_[:, gp, :, :], identity=ident[:]),
                     reads=[bb, b_id], writes=[bT])
                k.op("act", lambda e: e.copy(out=wt[:, 2 + ri, :], in_=pTv[:, 0:128]), reads=[bT], writes=[bwt])
            k.op("act", lambda e: e.copy(out=wt[:, 4, :], in_=CAr[:, gp, y0:y0 + 8, :]), reads=[bb], writes=[bwt])
            k.op("act", lambda e: e.copy(out=wt[:, 5, :], in_=CAi[:, gp, y0:y0 + 8, :]), reads=[bb], writes=[bwt])
            k.dma("sp", g.s5w[d, gp], wt[:], reads=[bwt], writes=[g.bufs["s5w"]])


def phase_s5(g):
    with ExitStack() as pes:
        g.pes = pes
        s5_prep(g)
        g.k.barrier()
    if g.kinds.get("_s5_prep_only"):
        return
    with ExitStack() as pes:
        g.pes = pes
        s5_main(g)
        g.k.barrier()


def bcf(col_ap, n):
    a = col_ap.ap
    return AP(col_ap.tensor, col_ap.offset, [list(a[0]), [0, n]])


def s5_main(g):
    k, nc = g.k, g.nc
    identb = sbt(g, "v_identb", [128, 128], BF16)
    b_id = Buf()
    k.dma("pool", identb[:], g.gin("ident")[:, :], writes=[b_id])
    bglu = sbt(g, "v_bglu", [128, 2 * D], F32)
    dsk = sbt(g, "v_dsk", [128, D], F32)
    rho = sbt(g, "v_rho", [128, 2, 32], F32)
    b_c = Buf()
    k.dma("sp", bglu[:], bcast_rows(g.gin("s5_b_glu")[0:1, :], 128), writes=[b_c])
    k.dma("sp", dsk[:], bcast_rows(g.gin("s5_d")[0:1, :], 128), writes=[b_c])
    k.dma("sp", rho[:], g.rho.rearrange("d q gp -> q d gp"), reads=[g.bufs["rho"]], writes=[b_c])
    mods = ModBC(g, "v", 1, (0, 1, 2))
    ns = NormSet(g, "v_")
    Ubig = sbt(g, "v_U", [128, 64 * 320], BF16)
    U = Ubig[:, :].rearrange("p (g c) -> p g c", c=320)
    wglu = Ubig[:, 0:8 * 2048].rearrange("p (k n) -> p k n", n=2048)
    b_U = Buf()
    hcx = sbt(g, "v_hcx", [128, 64, 8, 16], BF16)
    b_hcx = Buf()
    hxc = sbt(g, "v_hxc", [128, 2, 64, 8, 16], BF16)
    b_hxc = Buf()
    xt = [sbt(g, "v_xt%d" % i, [128, D], F32) for i in range(2)]
    b_xt = [Buf(), Buf()]
    tab = [sbt(g, "v_tab%d" % i, [128, 2, 288], F32) for i in range(2)]
    b_tab = [Buf(), Buf()]
    wk = [[sbt(g, "v_wk%d_%d" % (i, j), [128, 288], F32) for j in range(6)] for i in range(2)]
    b_wk = [[Buf() for _ in range(6)] for _ in range(2)]
    spv = [sbt(g, "v_spv%d" % i, [128, 2, 2, 256], BF16) for i in range(2)]
    b_spv = [Buf(), Buf()]
    wts = [sbt(g, "v_wts%d" % i, [128, 2, 6, 128], BF16) for i in range(2)]
    b_wts = [Buf(), Buf()]
    ysb = [sbt(g, "v_ysb%d" % i, [128, 256], BF16) for i in range(2)]
    b_ysb = [Buf(), Buf()]
    gt_ = [sbt(g, "v_gt%d" % i, [128, D], F32) for i in range(3)]
    b_gt = [Buf() for _ in range(3)]
    geb = sbt(g, "v_geb", [128, D], BF16)
    b_geb = Buf()
    geT = sbt(g, "v_geT", [128, 8, 128], BF16)
    b_geT = Buf()
    av = [sbt(g, "v_av%d" % i, [128, 512], F32) for i in range(2)]
    b_av = [Buf(), Buf()]
    gv = [sbt(g, "v_gv%d" % i, [128, 512], F32) for i in range(2)]
    b_gv = [Buf(), Buf()]
    cnt = dict(x=0, it=0, y=0, o=0)

    def load_x_tile(dst, bdst, src_b, tile, l):
        for r4 in range(4):
            t0 = (r4 * 8 + l) * 64 + tile * 32
            k.dma("sp", dst[r4 * 32:(r4 + 1) * 32, :], src_b[t0:t0 + 32, :], reads=[g.bufs["x2"]], writes=[bdst])

    for b in range(NB):
        (shbc, gbc, gatebc), b_mod = mods.get(2)
        cv = g.c2[b].rearrange("(c l) d -> l c d", l=8)
        for l in range(8):
            x_, bx = xt[cnt["x"] % 2], b_xt[cnt["x"] % 2]
            cnt["x"] += 1
            k.dma("sp", x_[0:32, :], cv[l], reads=[g.bufs["c2"]], writes=[bx])
            rstd, b_st = rstd_of(g, ns, 0, x_[0:32, :], bx)
            st = ns.st[0]
            k.op("dve", lambda e: e.scalar_tensor_tensor(out=ns.tmp[0][0:32, :], in0=x_[0:32, :], scalar=st[0:32, 3:4], in1=gbc[0:32, :],
                                                          op0=ALU.mult, op1=ALU.mult),
                 reads=[bx, b_st, b_mod], writes=[ns.b_tmp[0]])
            k.op("pool", lambda e: e.tensor_tensor(out=hcx[0:32, :, l, :], in0=ns.tmp[0][0:32, :].rearrange("p (g c) -> p g c", c=16),
                                                   in1=shbc[0:32, :].rearrange("p (g c) -> p g c", c=16), op=ALU.add),
                 reads=[ns.b_tmp[0], b_mod], writes=[b_hcx])
        (shbc, gbc, gatebc), b_mod = mods.get(b)
        for tile in range(2):
            for l in range(8):
                x_, bx = xt[cnt["x"] % 2], b_xt[cnt["x"] % 2]
                cnt["x"] += 1
                load_x_tile(x_, bx, g.x2[b], tile, l)
                rstd, b_st = rstd_of(g, ns, 0, x_[:], bx)
                k.op("dve", lambda e: e.scalar_tensor_tensor(out=ns.tmp[0][:], in0=x_[:], scalar=rstd, in1=gbc[:],
                                                              op0=ALU.mult, op1=ALU.mult),
                     reads=[bx, b_st, b_mod], writes=[ns.b_tmp[0]])
                k.op("pool", lambda e: e.tensor_tensor(out=hxc[:, tile, :, l, :], in0=ns.tmp[0][:].rearrange("p (g c) -> p g c", c=16),
                                                       in1=shbc[:].rearrange("p (g c) -> p g c", c=16), op=ALU.add),
                     reads=[ns.b_tmp[0], b_mod], writes=[b_hxc])
        for gg in range(64):
            ps, pb = g.ps[6 + gg % 2], g.psb[6 + gg % 2]
            psv = ps.bitcast(BF16)
            k.op("pe", lambda e: e.transpose(out=psv[:, 0:32], in_=hcx[0:32, gg, :, :], identity=identb[0:32, 0:32]),
                 reads=[b_hcx, b_id], writes=[pb], inc=False)
            for tile in range(2):
                k.op("pe", lambda e, tile=tile: e.transpose(out=psv[:, 32 + tile * 128:32 + (tile + 1) * 128],
                                                            in_=hxc[:, tile, gg, :, :], identity=identb[:]),
                     reads=[b_hxc, b_id], writes=[pb], inc=(tile == 1))
            k.op("act", lambda e: e.copy(out=U[:, gg, 0:32], in_=psv[:, 0:32]), reads=[pb], writes=[b_U])
            k.op("act", lambda e: e.copy(out=U[:, gg, 288:320], in_=psv[:, 0:32]), reads=[pb], writes=[b_U])
            k.op("act", lambda e: e.copy(out=U[:, gg, 32:288].rearrange("p (t c r) -> p t r c", t=2, c=32, r=4),
                                         in_=psv[:, 32:288].rearrange("p (t r c) -> p t r c", t=2, r=4, c=32)),
                 reads=[pb], writes=[b_U])
        for tile in range(2):
            for l in range(8):
                k.op("dve", lambda e, tile=tile, l=l: e.tensor_tensor(out=hxc[:, tile, :, l, :], in0=hxc[:, tile, :, l, :],
                                                                      in1=dsk[:].rearrange("p (g c) -> p g c", c=16), op=ALU.mult),
                     reads=[b_hxc, b_c], writes=[b_hxc])
        for gp in range(32):
            it = cnt["it"] % 2
            cnt["it"] += 1
            wt, bwt, sp_, bsp = wts[it], b_wts[it], spv[it], b_spv[it]
            for d in range(2):
                tb_, btb = tab[d], b_tab[d]
                W, bW = wk[d], b_wk[d]
                k.dma("sp", wt[:, d], g.s5w[d, gp], reads=[g.bufs["s5w"]], writes=[bwt])
                k.dma("sp", tb_[:, 0, :], g.etab[d, 0, :, gp, :], reads=[g.bufs["etab"]], writes=[btb])
                k.dma("sp", tb_[:, 1, :], g.etab[d, 1, :, gp, :], reads=[g.bufs["etab"]], writes=[btb])
                cols = slice(0, 288) if d == 0 else slice(32, 320)
                pz = [(g.ps[2 * d], g.psb[2 * d]), (g.ps[2 * d + 1], g.psb[2 * d + 1])]
                for ri in range(2):
                    pZ, bZ = pz[ri]
                    for g2 in range(2):
                        k.op("pe", lambda e, g2=g2: e.matmul(pZ[g2 * 64:(g2 + 1) * 64, 0:288], lhsT=wt[:, d, 2 + ri, g2 * 64:(g2 + 1) * 64],
                                                             rhs=U[:, 2 * gp + g2, cols], start=True, stop=True),
                             reads=[bwt, b_U], writes=[bZ], inc=(g2 == 1))
                cosv, sinv = tb_[:, 0, :], tb_[:, 1, :]
                if d == 1:
                    cosv, sinv = rev_ap(cosv, 288), rev_ap(sinv, 288)
                Zr, bZr = pz[0][0][:, 0:288], pz[0][1]
                Zi, bZi = pz[1][0][:, 0:288], pz[1][1]
                za, zb, ztr, zti, sr, si = [w_[:] for w_ in W]
                bza, bzb, bztr, bzti, bsr, bsi = bW
                TT = lambda o, a_, b_, op, rd, wr: k.op("dve", lambda e: e.tensor_tensor(out=o, in0=a_, in1=b_, op=op), reads=rd, writes=wr)
                TT(za, Zr, cosv, ALU.mult, [bZr, btb], [bza])
                TT(zb, Zi, sinv, ALU.mult, [bZi, btb], [bzb])
                TT(ztr, za, zb, ALU.add, [bza, bzb], [bztr])
                TT(za, Zi, cosv, ALU.mult, [bZi, btb], [bza])
                TT(zb, Zr, sinv, ALU.mult, [bZr, btb], [bzb])
                TT(zti, za, zb, ALU.subtract, [bza, bzb], [bzti])
                rbc = bcf(rho[:, d, gp:gp + 1], 288)
                for (o_, i_, bo_, bi_) in ((sr, ztr, bsr, bztr), (si, zti, bsi, bzti)):
                    oo, ii = (o_, i_) if d == 0 else (rev_ap(o_, 288), rev_ap(i_, 288))
                    k.op("dve", lambda e, oo=oo, ii=ii: e.tensor_tensor_scan(out=oo, data0=rbc, data1=ii, initial=0.0,
                                                                             op0=ALU.mult, op1=ALU.add),
                         reads=[bi_, b_c], writes=[bo_])
                sl = slice(31, 287) if d == 0 else slice(1, 257)
                TT(za[:, 0:256], sr[:, sl], cosv[:, sl], ALU.mult, [bsr, btb], [bza])
                TT(zb[:, 0:256], si[:, sl], sinv[:, sl], ALU.mult, [bsi, btb], [bzb])
                TT(sp_[:, d, 0, :], za[:, 0:256], zb[:, 0:256], ALU.subtract, [bza, bzb], [bsp])
                TT(za[:, 0:256], si[:, sl], cosv[:, sl], ALU.mult, [bsi, btb], [bza])
                TT(zb[:, 0:256], sr[:, sl], sinv[:, sl], ALU.mult, [bsr, btb], [bzb])
                TT(sp_[:, d, 1, :], za[:, 0:256], zb[:, 0:256], ALU.add, [bza, bzb], [bsp])
            for g2 in range(2):
                L = slice(g2 * 64, (g2 + 1) * 64)
                gg = 2 * gp + g2
                yi = cnt["y"] % 2
                cnt["y"] += 1
                pY, bY = g.ps[4 + yi], g.psb[4 + yi]
                ops = [(wt[:, 0, g2, :], U[:, gg, 32:288]), (wt[:, 1, g2, :], U[:, gg, 32:288])]
                for d in range(2):
                    ops.append((wt[L, d, 4, :], sp_[L, d, 0, :]))
                    ops.append((wt[L, d, 5, :], sp_[L, d, 1, :]))
                for oi, (lw, rh) in enumerate(ops):
                    k.op("pe", lambda e, lw=lw, rh=rh, oi=oi: e.matmul(pY[:, 0:256], lhsT=lw, rhs=rh, start=(oi == 0), stop=(oi == 5)),
                         reads=[bwt, b_U, bsp], writes=[bY], inc=(oi == 5))
                ys, bys = ysb[yi], b_ysb[yi]
                k.op("act", lambda e: e.copy(out=ys[:].rearrange("p (t r c) -> p t c r", t=2, r=4, c=32),
                                             in_=pY[:, 0:256].rearrange("p (t c r) -> p t c r", t=2, c=32, r=4)),
                     reads=[bY], writes=[bys])
                pT, bT = g.ps[6 + yi], g.psb[6 + yi]
                pTv = pT.bitcast(BF16)
                for tile in range(2):
                    k.op("pe", lambda e, tile=tile: e.transpose(out=pTv[:, tile * 128:(tile + 1) * 128],
                                                                in_=ys[:, tile * 128:(tile + 1) * 128], identity=identb[:]),
                         reads=[bys, b_id], writes=[bT], inc=(tile == 1))
                tyv = hxc[:, :, gg, :, :]
                k.op("dve", lambda e: e.tensor_tensor(out=tyv, in0=tyv, in1=pTv[:, 0:256].rearrange("p (t l c) -> p t l c", t=2, l=8, c=16),
                                                       op=ALU.add),
                     reads=[bT, b_hxc], writes=[b_hxc])
        for kk in range(8):
            k.dma("pool", wglu[:, kk, :], g.gin("s5_w_glu")[kk * 128:(kk + 1) * 128, :], writes=[b_U])
        for tile in range(2):
            for l in range(8):
                yv = hxc[:, tile, :, l, :]
                g0, g1, g2_ = [t_[:].rearrange("p (g c) -> p g c", c=16) for t_ in gt_]
                k.op("act", lambda e: e.activation(out=g0, in_=yv, func=AF.Square), reads=[b_hxc], writes=[b_gt[0]])
                k.op("dve", lambda e: e.tensor_scalar(out=g0, in0=g0, scalar1=0.044715, scalar2=1.0, op0=ALU.mult, op1=ALU.add),
                     reads=[b_gt[0]], writes=[b_gt[0]])
                k.op("dve", lambda e: e.tensor_tensor(out=g1, in0=g0, in1=yv, op=ALU.mult), reads=[b_gt[0], b_hxc], writes=[b_gt[1]])
                k.op("act", lambda e: e.activation(out=g2_, in_=g1, func=AF.Sigmoid, scale=1.5957691216057308),
                     reads=[b_gt[1]], writes=[b_gt[2]])
                k.op("dve", lambda e: e.tensor_tensor(out=geb[:].rearrange("p (g c) -> p g c", c=16), in0=g2_, in1=yv, op=ALU.mult), reads=[b_gt[2], b_hxc], writes=[b_geb])
                ps, pb = g.ps[6 + l % 2], g.psb[6 + l % 2]
                psv = ps.bitcast(BF16)
                for kk in range(8):
                    k.op("pe", lambda e, kk=kk: e.transpose(out=psv[:, kk * 128:(kk + 1) * 128], in_=geb[:, kk * 128:(kk + 1) * 128],
                                                            identity=identb[:]),
                         reads=[b_geb, b_id], writes=[pb], inc=(kk == 7))
                k.op("act", lambda e: e.copy(out=geT[:], in_=psv[:, 0:1024].rearrange("p (k t) -> p k t", t=128)),
                     reads=[pb], writes=[b_geT])
                x_, bx = xt[cnt["x"] % 2], b_xt[cnt["x"] % 2]
                cnt["x"] += 1
                load_x_tile(x_, bx, g.x2[b], tile, l)
                for half in range(2):
                    oi = cnt["o"] % 2
                    cnt["o"] += 1
                    pa, ba = g.ps[oi * 2], g.psb[oi * 2]
                    pg_, bg_ = g.ps[oi * 2 + 1], g.psb[oi * 2 + 1]
                    for (pp, bp, n0) in ((pa, ba, half * 512), (pg_, bg_, D + half * 512)):
                        for kk in range(8):
                            k.op("pe", lambda e, kk=kk, pp=pp, n0=n0: e.matmul(pp[:, :], lhsT=geT[:, kk, :], rhs=wglu[:, kk, n0:n0 + 512],
                                                                               start=(kk == 0), stop=(kk == 7)),
                                 reads=[b_geT, b_U], writes=[bp], inc=(kk == 7))
                    hs = slice(half * 512, (half + 1) * 512)
                    a_, ba_, g_, bg2 = av[oi], b_av[oi], gv[oi], b_gv[oi]
                    k.op("dve", lambda e: e.tensor_tensor(out=a_[:], in0=pa[:, :], in1=bglu[:, hs], op=ALU.add),
                         reads=[ba, b_c], writes=[ba_])
                    k.op("dve", lambda e: e.tensor_tensor(out=g_[:], in0=pg_[:, :], in1=bglu[:, D + half * 512:D + (half + 1) * 512], op=ALU.add),
                         reads=[bg_, b_c], writes=[bg2])
                    k.op("act", lambda e: e.activation(out=g_[:], in_=g_[:], func=AF.Sigmoid), reads=[bg2], writes=[bg2])
                    k.op("dve", lambda e: e.tensor_tensor(out=a_[:], in0=a_[:], in1=g_[:], op=ALU.mult), reads=[ba_, bg2], writes=[ba_])
                    k.op("pool", lambda e: e.tensor_tensor(out=a_[:], in0=a_[:], in1=gatebc[:, hs], op=ALU.mult),
                         reads=[ba_, b_mod], writes=[ba_])
                    k.op("pool", lambda e: e.tensor_tensor(out=x_[:, hs], in0=x_[:, hs], in1=a_[:], op=ALU.add),
                         reads=[ba_, bx], writes=[bx])
                for r4 in range(4):
                    t0 = (r4 * 8 + l) * 64 + tile * 32
                    k.dma("pool", g.x3[b, t0:t0 + 32, :], x_[r4 * 32:(r4 + 1) * 32, :], reads=[bx], writes=[g.bufs["x3"]])


def phase_moe(g):
    k, nc = g.k, g.nc
    src = g.x3 if "s5" in g.phases or g.kinds.get("x3") == "in" else g.x2
    b_src = g.bufs["x3"] if "s5" in g.phases or g.kinds.get("x3") == "in" else g.bufs["x2"]
    NTL = T // 128
    NFG = EDIM // 512
    ns = NormSet(g, "m_")
    identf = sbt(g, "m_identf", [128, 128], F32)
    b_identf = Buf()
    k.dma("sp", identf[:], g.gin("ident")[:, :], writes=[b_identf])
    hT = sbt(g, "m_hT", [128, 8, T], BF16)
    b_hT = Buf()
    acc = sbt(g, "m_acc", [128, NTL, D], F32)
    b_acc = [Buf() for _ in range(NTL)]
    aT = sbt(g, "m_aT", [128, 4, T], BF16)
    b_aT = [Buf() for _ in range(4)]
    wg = [sbt(g, "m_wg%d" % i, [128, 8, 512], BF16) for i in range(2)]
    wu = [sbt(g, "m_wu%d" % i, [128, 8, 512], BF16) for i in range(2)]
    wo = [sbt(g, "m_wo%d" % i, [128, 4, D], BF16) for i in range(2)]
    b_w = [Buf(), Buf()]
    comb = sbt(g, "m_comb", [128, NTL, NEXP], F32)
    b_comb = Buf()
    rw = sbt(g, "m_rw", [128, 8, NEXP], F32)
    rb = sbt(g, "m_rb", [128, NEXP], F32)
    b_rw = Buf()
    k.dma("sp", rw[:], g.gin("moe_router_w").rearrange("(k p) e -> p k e", p=128), writes=[b_rw])
    k.dma("sp", rb[:], bcast_rows(g.gin("moe_router_b")[0:1, :], 128), writes=[b_rw])
    nfin = sbt(g, "m_nfin", [128, D], F32)
    b_nfin = Buf()
    k.dma("sp", nfin[:], bcast_rows(g.gin("norm_final")[0:1, :], 128), writes=[b_nfin])
    hTf = sbt(g, "m_hTf", [128, 8, 128], F32)
    b_hTf = Buf()
    lg = [sbt(g, "m_lg%d" % i, [128, 40], F32) for i in range(2)]
    b_lg = [Buf(), Buf()]
    sg = [sbt(g, "m_sg%d" % i, [128, 512], F32) for i in range(2)]
    b_sg = [Buf(), Buf()]
    tmpo = [sbt(g, "m_to%d" % i, [128, 512], F32) for i in range(2)]
    b_to = [Buf(), Buf()]
    mods = ModBC(g, "m", 1, (3, 4, 5))
    wi = 0
    si = 0
    oi = 0
    pi = 0
    for b in range(NB):
        (shbc, gbc, gatebc), b_mod = mods.get(b)
        for j in range(NTL):
            xt = acc[:, j, :]
            k.dma("sp", xt, src[b, j * 128:(j + 1) * 128, :], reads=[b_src], writes=[b_acc[j]])
            i = ns.i % ns.nbuf
            ns.i += 1
            rstd, b_st = rstd_of(g, ns, i, xt, b_acc[j])
            tmp, b_tmp, hb, b_hb = ns.tmp[i], ns.b_tmp[i], ns.hb[i], ns.b_hb[i]
            k.op("dve", lambda e: e.scalar_tensor_tensor(out=tmp[:], in0=xt, scalar=rstd, in1=gbc[:],
                                                          op0=ALU.mult, op1=ALU.mult),
                 reads=[b_acc[j], b_st, b_mod], writes=[b_tmp])
            k.op("pool", lambda e: e.tensor_tensor(out=tmp[:], in0=tmp[:], in1=shbc[:], op=ALU.add),
                 reads=[b_tmp, b_mod], writes=[b_tmp])
            k.op("act", lambda e: e.copy(out=hb[:], in_=tmp[:]), reads=[b_tmp], writes=[b_hb])
            ps, pb = g.ps[4 + (j % 2)], g.psb[4 + (j % 2)]
            psv = ps.bitcast(BF16)
            for kk in range(8):
                k.op("pe", lambda e, kk=kk: e.transpose(out=psv[:, kk * 128:(kk + 1) * 128],
                                                        in_=hb[:, kk * 128:(kk + 1) * 128], identity=ns.ident[:]),
                     reads=[b_hb, ns.b_ident], writes=[pb], inc=(kk == 7))
            k.op("act", lambda e: e.copy(out=hT[:, :, j * 128:(j + 1) * 128],
                                         in_=psv[:, 0:1024].rearrange("p (k t) -> p k t", t=128)),
                 reads=[pb], writes=[b_hT])
            for hh in range(2):
                psf, pbf = g.ps[6 + hh], g.psb[6 + hh]
                for kk in range(4):
                    kf = hh * 4 + kk
                    k.op("pe", lambda e, kk=kk, kf=kf: e.transpose(out=psf[:, kk * 128:(kk + 1) * 128],
                                                                   in_=tmp[:, kf * 128:(kf + 1) * 128], identity=identf[:]),
                         reads=[b_tmp, b_identf], writes=[pbf], inc=(kk == 3))
                k.op("act", lambda e, hh=hh: e.copy(out=hTf[:, hh * 4:(hh + 1) * 4, :],
                                                    in_=psf[:, :].rearrange("p (k t) -> p k t", t=128)),
                     reads=[pbf], writes=[b_hTf])
            pl, pbl = g.ps[4 + (j % 2)], g.psb[4 + (j % 2)]
            for kk in range(8):
                k.op("pe", lambda e, kk=kk: e.matmul(pl[:, 0:NEXP], lhsT=hTf[:, kk, :], rhs=rw[:, kk, :],
                                                     start=(kk == 0), stop=(kk == 7)),
                     reads=[b_hTf, b_rw], writes=[pbl], inc=(kk == 7))
            L = lg[j % 2]
            bL = b_lg[j % 2]
            k.op("dve", lambda e: e.tensor_tensor(out=L[:, 0:8], in0=pl[:, 0:NEXP], in1=rb[:], op=ALU.add),
                 reads=[pbl, b_rw], writes=[bL])
            k.op("dve", lambda e: e.max(out=L[:, 8:16], in_=L[:, 0:8]), reads=[bL], writes=[bL])
            k.op("dve", lambda e: e.tensor_scalar(out=L[:, 16:17], in0=L[:, 8:9], scalar1=-1.0, scalar2=None,
                                                   op0=ALU.mult), reads=[bL], writes=[bL])
            k.op("act", lambda e: e.activation(out=L[:, 17:18], in_=L[:, 9:10], func=AF.Exp, bias=L[:, 16:17]),
                 reads=[bL], writes=[bL])
            k.op("dve", lambda e: e.tensor_scalar(out=L[:, 20:21], in0=L[:, 17:18], scalar1=1.0, scalar2=None,
                                                   op0=ALU.add), reads=[bL], writes=[bL])
            k.op("dve", lambda e: e.reciprocal(out=L[:, 18:19], in_=L[:, 20:21]), reads=[bL], writes=[bL])
            k.op("dve", lambda e: e.tensor_tensor(out=L[:, 19:20], in0=L[:, 17:18], in1=L[:, 18:19], op=ALU.mult),
                 reads=[bL], writes=[bL])
            k.op("dve", lambda e: e.tensor_scalar(out=L[:, 24:32], in0=L[:, 0:8], scalar1=L[:, 8:9], scalar2=L[:, 18:19],
                                                   op0=ALU.is_equal, op1=ALU.mult), reads=[bL], writes=[bL])
            k.op("dve", lambda e: e.tensor_scalar(out=L[:, 32:40], in0=L[:, 0:8], scalar1=L[:, 9:10], scalar2=L[:, 19:20],
                                                   op0=ALU.is_equal, op1=ALU.mult), reads=[bL], writes=[bL])
            k.op("dve", lambda e: e.tensor_tensor(out=comb[:, j, :], in0=L[:, 24:32], in1=L[:, 32:40], op=ALU.add),
                 reads=[bL], writes=[b_comb])
        for ex in range(NEXP):
            for fg in range(NFG):
                w_i = wi % 2
                wi += 1
                bw = b_w[w_i]
                fs = slice(fg * 512, (fg + 1) * 512)
                k.dma("pool", wg[w_i][:], g.gin("moe_w_in")[ex, :, fg * 512:(fg + 1) * 512].rearrange("(k p) n -> p k n", p=128),
                      writes=[bw])
                k.dma("pool", wu[w_i][:], g.gin("moe_w_in")[ex, :, EDIM + fg * 512:EDIM + (fg + 1) * 512].rearrange("(k p) n -> p k n", p=128),
                      writes=[bw])
                k.dma("pool", wo[w_i][:], g.gin("moe_w_out")[ex, fg * 512:(fg + 1) * 512, :].rearrange("(c p) d -> p c d", p=128),
                      writes=[bw])
                for c4 in range(4):
                    k.op("pool", lambda e, c4=c4: e.tensor_tensor(out=wo[w_i][:, c4, :], in0=wo[w_i][:, c4, :], in1=gatebc[:], op=ALU.mult),
                         reads=[bw, b_mod], writes=[bw])
                for tg in range(4):
                    ts_ = slice(tg * 512, (tg + 1) * 512)
                    for fc in range(4):
                        pg, pu = g.ps[(pi % 2) * 2], g.ps[(pi % 2) * 2 + 1]
                        bg, bu = g.psb[(pi % 2) * 2], g.psb[(pi % 2) * 2 + 1]
                        pi += 1
                        for kk in range(8):
                            k.op("pe", lambda e, kk=kk: e.matmul(pg[:, :], lhsT=wg[w_i][:, kk, fc * 128:(fc + 1) * 128],
                                                                 rhs=hT[:, kk, ts_], start=(kk == 0), stop=(kk == 7)),
                                 reads=[bw, b_hT], writes=[bg], inc=(kk == 7))
                        for kk in range(8):
                            k.op("pe", lambda e, kk=kk: e.matmul(pu[:, :], lhsT=wu[w_i][:, kk, fc * 128:(fc + 1) * 128],
                                                                 rhs=hT[:, kk, ts_], start=(kk == 0), stop=(kk == 7)),
                                 reads=[bw, b_hT], writes=[bu], inc=(kk == 7))
                        s_i = si % 2
                        si += 1
                        k.op("act", lambda e: e.activation(out=sg[s_i][:], in_=pg[:, :], func=AF.Silu),
                             reads=[bg], writes=[b_sg[s_i]])
                        k.op("dve", lambda e: e.tensor_tensor(out=aT[:, fc, ts_], in0=sg[s_i][:], in1=pu[:, :], op=ALU.mult),
                             reads=[b_sg[s_i], bu], writes=[b_aT[tg]])
                for j in range(NTL):
                    for half in range(2):
                        hs = slice(half * 512, (half + 1) * 512)
                        po, pbo = g.ps[4 + (oi % 4)], g.psb[4 + (oi % 4)]
                        to, bto = tmpo[oi % 2], b_to[oi % 2]
                        oi += 1
                        for fc in range(4):
                            k.op("pe", lambda e, fc=fc: e.matmul(po[:, :], lhsT=aT[:, fc, j * 128:(j + 1) * 128],
                                                                 rhs=wo[w_i][:, fc, hs], start=(fc == 0), stop=(fc == 3)),
                                 reads=[b_aT[j // 4], bw], writes=[pbo], inc=(fc == 3))
                        k.op("dve", lambda e: e.scalar_tensor_tensor(out=acc[:, j, hs], in0=po[:, :], scalar=comb[:, j, ex:ex + 1],
                                                                      in1=acc[:, j, hs], op0=ALU.mult, op1=ALU.add),
                             reads=[pbo, b_comb, b_acc[j]], writes=[b_acc[j]])
        for j in range(NTL):
            xt = acc[:, j, :]
            i = ns.i % ns.nbuf
            ns.i += 1
            rstd, b_st = rstd_of(g, ns, i, xt, b_acc[j])
            k.op("dve", lambda e: e.scalar_tensor_tensor(out=xt, in0=xt, scalar=rstd, in1=nfin[:],
                                                          op0=ALU.mult, op1=ALU.mult),
                 reads=[b_acc[j], b_st, b_nfin], writes=[b_acc[j]])
            k.dma("pool", g.out[b, j * 128:(j + 1) * 128, :], xt, reads=[b_acc[j]], writes=[g.bufs["out"]])


_CACHE = {}


def host_consts():
    r = np.arange(128)[:, None]
    c = np.arange(128)[None, :]
    cmat = np.stack([(r <= c), (r > c), (r < c), (r >= c), np.ones((128, 128), bool)]).astype(np.float32)
    rr = np.arange(128)[:, None] // 16
    cc = np.arange(128)[None, :] // 16
    m8 = np.stack([(rr <= cc), (rr >= cc)]).astype(np.float32)
    return {"ident": np.eye(128, dtype=np.float32), "cmat": cmat, "m8": m8}


def s5_lane_tables(inp):
    out = np.empty((2, 3, 128, 32), np.float32)
    for d in range(2):
        for i, a in enumerate((inp["s5_lam_re"][0, d], inp["s5_lam_im"][0, d])):
            out[d, i] = a.reshape(32, 2, 64).transpose(1, 2, 0).reshape(128, 32)
        ls = np.repeat(inp["s5_log_step"][0, d][:, None], 64, axis=1)
        out[d, 2] = ls.reshape(32, 2, 64).transpose(1, 2, 0).reshape(128, 32)
    return out


def s5_c_lanes(inp):
    out = np.empty((2, 2, 128, 32, 16), np.float32)
    for d in range(2):
        for i, a in enumerate((inp["s5_c_re"][0, d], inp["s5_c_im"][0, d])):
            out[d, i] = a.reshape(32, 2, 16, 64).transpose(1, 3, 0, 2).reshape(128, 32, 16)
    return out


def core_inputs(inp, core):
    b0 = core * NB
    cT = np.ascontiguousarray(np.stack([inp["c"][b0], inp["c"][b0 + 1], inp["c_ctx"]], axis=1))
    m = {
        "x": np.ascontiguousarray(inp["x"][b0:b0 + NB]),
        "ctx": np.ascontiguousarray(inp["ctx"][b0:b0 + NB]),
        "cT": cT,
        "ada_w": inp["ada_w"], "ada_b": inp["ada_b"],
        "norm_mix": inp["norm_mix"], "norm_ffn": inp["norm_ffn"],
        "ffn_w_in": inp["ffn_w_in"][0], "ffn_w_out": inp["ffn_w_out"][0],
        "moe_router_w": inp["moe_router_w"][0], "moe_router_b": inp["moe_router_b"].reshape(1, NEXP),
        "moe_w_in": inp["moe_w_in"][0], "moe_w_out": inp["moe_w_out"][0],
        "norm_final": inp["norm_final"].reshape(1, D),
        "ssd_w_in": inp["ssd_w_in"][0],
        "convT": np.ascontiguousarray(np.concatenate([inp["ssd_conv_w"][0], inp["ssd_conv_b"]], axis=0).T),
        "ssd_dt_bias": inp["ssd_dt_bias"].reshape(1, 64), "ssd_a_log": inp["ssd_a_log"].reshape(1, 64),
        "ssd_d": inp["ssd_d"].reshape(1, 32), "ssd_norm": inp["ssd_norm"].reshape(1, 2048),
        "ssd_w_out": inp["ssd_w_out"][0],
        "s5_lamT": s5_lane_tables(inp),
        "s5_b": np.ascontiguousarray(np.stack([inp["s5_b_re"][0], inp["s5_b_im"][0]], axis=1).reshape(2, 2, 4096, 16)),
        "s5_cT": s5_c_lanes(inp),
        "s5_d": inp["s5_d"].reshape(1, D), "s5_w_glu": inp["s5_w_glu"][0], "s5_b_glu": inp["s5_b_glu"].reshape(1, 2 * D),
    }
    m.update(host_consts())
    return m


def kernel(**inp):
    inp = {kk: np.asarray(v) for kk, v in inp.items()}
    if "prog" not in _CACHE:
        _CACHE["prog"] = build_program(("ada", "ssd", "ffn", "s5", "moe"), {})
    nc, g = _CACHE["prog"]
    in_maps = []
    for core in range(8):
        m = core_inputs(inp, core)
        in_maps.append({kk: np.ascontiguousarray(v) for kk, v in m.items() if kk in g.inputs})
    res = run_bass_kernel_spmd(nc, in_maps, core_ids=list(range(8)))
    return np.concatenate([r["out"] for r in res.results], axis=0).astype(np.float32)
```

```python
import numpy as np
from contextlib import ExitStack
import concourse.bass as bass
import concourse.mybir as mybir
from concourse.bass_utils import run_bass_kernel_spmd

F32 = mybir.dt.float32
BF16 = mybir.dt.bfloat16
I32 = mybir.dt.int32
AF = mybir.ActivationFunctionType
ALU = mybir.AluOpType
AX = mybir.AxisListType
AP = bass.AP

D = 1024
NB = 2
T = 2048
TC = 256
EPS = 1e-6
FFN = 2816
NEXP = 8
EDIM = 3584


class Buf:
    __slots__ = ("name", "w", "r", "ps")

    def __init__(self, name="", ps=False):
        self.name = name
        self.w = {}
        self.r = {}
        self.ps = ps


class KB:
    RING = 8

    def __init__(self, nc, es):
        self.nc = nc
        self.es = es
        self.E = dict(pe=nc.tensor, act=nc.scalar, dve=nc.vector, pool=nc.gpsimd, sp=nc.sync)
        self.sem = {}
        self.cnt = {}
        for e in self.E:
            self.sem[e] = es.enter_context(nc.semaphore("s_" + e))
            self.cnt[e] = 0
        self.ring = {}
        self.ring_i = {}
        for q in ("sp", "pool", "act"):
            self.ring[q] = []
            for i in range(self.RING):
                key = ("d", q, i)
                self.sem[key] = es.enter_context(nc.semaphore("d_%s%d" % (q, i)))
                self.cnt[key] = 0
                self.ring[q].append(key)
            self.ring_i[q] = 0
        self.known = {e: {} for e in self.E}
        self.n_wait = 0

    def _deps(self, eng, reads, writes):
        need = {}

        def add(sk, v):
            if v > need.get(sk, 0):
                need[sk] = v

        for b in reads:
            for sk, v in b.w.items():
                if sk == eng and eng == "pe":
                    continue
                add(sk, v)
        for b in writes:
            for sk, v in b.r.items():
                if sk == eng and eng == "pe":
                    continue
                add(sk, v)
            for sk, v in b.w.items():
                if sk == eng and eng == "pe":
                    continue
                add(sk, v)
        kn = self.known[eng]
        out = []
        for sk, v in need.items():
            if kn.get(sk, 0) >= v:
                continue
            kn[sk] = v
            out.append((sk, v))
        return out

    def _emit_waits(self, eng, waits):
        E = self.E[eng]
        for sk, v in waits:
            E.wait_ge(self.sem[sk], v)
            self.n_wait += 1

    def _record(self, ev, reads, writes):
        sk, v = ev
        for b in reads:
            if v > b.r.get(sk, 0):
                b.r[sk] = v
        for b in writes:
            if b.r:
                b.w = {sk: v}
                b.r = {}
            else:
                if v > b.w.get(sk, 0):
                    b.w[sk] = v

    def op(self, eng, fn, reads=(), writes=(), inc=True):
        if any(b.ps for b in reads):
            writes = list(writes) + [b for b in reads if b.ps]
            reads = [b for b in reads if not b.ps]
        self._emit_waits(eng, self._deps(eng, reads, writes))
        ins = fn(self.E[eng])
        if inc:
            self.cnt[eng] += 1
            ins.then_inc(self.sem[eng], 1)
            ev = (eng, self.cnt[eng])
        else:
            ev = (eng, self.cnt[eng] + 1)
        self._record(ev, reads, writes)
        return ins

    def dma(self, q, out, in_, reads=(), writes=()):
        key = self.ring[q][self.ring_i[q] % self.RING]
        self.ring_i[q] += 1
        waits = self._deps(q, reads, writes)
        prev = self.cnt[key]
        if prev > 0 and self.known[q].get(key, 0) < prev:
            self.known[q][key] = prev
            waits.append((key, prev))
        self._emit_waits(q, waits)
        ins = self.E[q].dma_start(out=out, in_=in_)
        self.cnt[key] = prev + 16
        ins.then_inc(self.sem[key], 16)
        self._record((key, prev + 16), reads, writes)
        return ins

    def barrier(self):
        for e in self.E:
            waits = []
            for sk, v in self.cnt.items():
                if v == 0 or sk == e and e == "pe":
                    continue
                if self.known[e].get(sk, 0) >= v:
                    continue
                self.known[e][sk] = v
                waits.append((sk, v))
            self._emit_waits(e, waits)

    def final_wait(self):
        waits = []
        for sk, v in self.cnt.items():
            if v and self.known["sp"].get(sk, 0) < v:
                self.known["sp"][sk] = v
                waits.append((sk, v))
        self._emit_waits("sp", waits)


def rev_ap(ap, n):
    a = ap.ap
    assert len(a) == 2 and a[1][1] == n
    return AP(ap.tensor, ap.offset + (n - 1) * a[1][0], [list(a[0]), [-a[1][0], n]])


class Ctx:
    pass


def build_program(phases, kinds):
    nc = bass.Bass("TRN2", target_bir_lowering=False)
    g = Ctx()
    g.nc = nc
    g.phases = phases
    g.kinds = kinds

    def din(name, shape, dt=F32):
        return nc.dram_tensor(name, list(shape), dt, kind="ExternalInput").ap()

    def dmid(name, shape, dt=F32):
        kind = {"in": "ExternalInput", "out": "ExternalOutput", "int": "Internal"}[kinds.get(name, "int")]
        return nc.dram_tensor(name, list(shape), dt, kind=kind).ap()

    g.inputs = {}
    SH = dict(x=[NB, T, D], ctx=[NB, TC, D], cT=[D, 3], ada_w=[2, D, 6 * D], ada_b=[2, 6 * D],
              norm_mix=[2, D], norm_ffn=[2, D], ffn_w_in=[D, 2 * FFN], ffn_w_out=[FFN, D],
              moe_router_w=[D, NEXP], moe_router_b=[1, NEXP], moe_w_in=[NEXP, D, 2 * EDIM],
              moe_w_out=[NEXP, EDIM, D], norm_final=[1, D], ident=[128, 128],
              ssd_w_in=[D, 5184], convT=[3072, 6], ssd_dt_bias=[1, 64], ssd_a_log=[1, 64], ssd_d=[1, 32],
              ssd_norm=[1, 2048], ssd_w_out=[2048, D], cmat=[5, 128, 128],
              s5_lamT=[2, 3, 128, 32], s5_b=[2, 2, 4096, 16], s5_cT=[2, 2, 128, 32, 16], m8=[2, 128, 128],
              s5_d=[1, D], s5_w_glu=[D, 2 * D], s5_b_glu=[1, 2 * D])
    g.SH = SH

    def gin(name):
        if name not in g.inputs:
            g.inputs[name] = din(name, SH[name])
        return g.inputs[name]
    g.gin = gin
    g.x = gin("x")
    g.ctx = gin("ctx")
    g.mod = dmid("mod", [2, 3, 6 * D])
    g.x1 = dmid("x1", [NB, T, D])
    g.c1 = dmid("c1", [NB, TC, D])
    g.x2 = dmid("x2", [NB, T, D])
    g.c2 = dmid("c2", [NB, TC, D])
    g.x3 = dmid("x3", [NB, T, D])
    NTK = T + TC
    g.xbcT = dmid("xbcT", [NB, 3072, NTK], BF16)
    g.zs = dmid("zs", [NB, NTK, 2048], BF16)
    g.dd = dmid("dd", [NB, NTK, 128])
    g.yf = dmid("yf", [NB, NTK, 2048])
    g.s5w = dmid("s5w", [2, 32, 128, 6, 128], BF16)
    g.etab = dmid("etab", [2, 2, 128, 32, 288])
    g.rho = dmid("rho", [2, 128, 32])
    g.out = nc.dram_tensor("out", [NB, T, D], F32, kind="ExternalOutput").ap()
    g.bufs = {n: Buf(n) for n in ("x", "ctx", "mod", "x1", "c1", "x2", "c2", "x3", "out", "xbcT", "zs", "dd", "yf", "s5w", "etab", "rho")}

    with ExitStack() as es:
        k = KB(nc, es)
        g.k = k
        g.ps = [es.enter_context(nc.psum_tensor("ps%d" % i, [128, 512], F32)) for i in range(8)]
        g.psb = [Buf("ps%d" % i, ps=True) for i in range(8)]
        for ph in phases:
            with ExitStack() as pes:
                g.pes = pes
                {"ada": phase_ada, "ffn": phase_ffn, "moe": phase_moe, "ssd": phase_ssd, "s5": phase_s5}[ph](g)
                k.barrier()
        k.final_wait()
    g.kinds = kinds
    return nc, g


def sbt(g, name, shape, dt):
    return g.pes.enter_context(g.nc.sbuf_tensor(name, list(shape), dt))


def bcast_rows(ap_row, nparts):
    a = ap_row.ap
    return AP(ap_row.tensor, ap_row.offset, [[0, nparts]] + [list(x) for x in a[1:]])


def phase_ada(g):
    k, nc = g.k, g.nc
    cT = sbt(g, "a_cT", [128, 8, 3], F32)
    cs = sbt(g, "a_cs", [128, 8, 3], BF16)
    row = sbt(g, "a_row", [3, 6 * D], F32)
    bias = sbt(g, "a_bias", [3, 6 * D], F32)
    nrm = sbt(g, "a_nrm", [3, D], F32)
    wts = [sbt(g, "a_w%d" % i, [128, 8, 512], BF16) for i in range(2)]
    b_cT, b_cs, b_row, b_bias, b_nrm = Buf(), Buf(), Buf(), Buf(), Buf()
    b_w = [Buf(), Buf()]
    k.dma("sp", cT[:], g.gin("cT").rearrange("(k p) m -> p k m", p=128), writes=[b_cT])
    k.op("act", lambda e: e.activation(out=cs[:], in_=cT[:], func=AF.Silu), reads=[b_cT], writes=[b_cs])
    it = 0
    for layer in range(2):
        k.dma("sp", bias[:], bcast_rows(g.gin("ada_b")[layer:layer + 1, :], 3), writes=[b_bias])
        for j in range(12):
            w = wts[it % 2]
            bw = b_w[it % 2]
            it += 1
            k.dma("pool", w[:], g.gin("ada_w")[layer, :, j * 512:(j + 1) * 512].rearrange("(k p) n -> p k n", p=128), writes=[bw])
            ps = g.ps[it % 2]
            pb = g.psb[it % 2]
            for kk in range(8):
                k.op("pe", lambda e, kk=kk: e.matmul(ps[0:3, :], lhsT=cs[:, kk, :], rhs=w[:, kk, :],
                                                     start=(kk == 0), stop=(kk == 7)),
                     reads=[b_cs, bw], writes=[pb], inc=(kk == 7))
            k.op("dve", lambda e: e.tensor_tensor(out=row[:, j * 512:(j + 1) * 512], in0=ps[0:3, :],
                                                   in1=bias[:, j * 512:(j + 1) * 512], op=ALU.add),
                 reads=[pb, b_bias], writes=[b_row])
        for slot, nw in ((1, g.gin("norm_mix")), (4, g.gin("norm_ffn"))):
            k.dma("sp", nrm[:], bcast_rows(nw[layer:layer + 1, :], 3), writes=[b_nrm])
            k.op("dve", lambda e, slot=slot: e.scalar_tensor_tensor(
                out=row[:, slot * D:(slot + 1) * D], in0=row[:, slot * D:(slot + 1) * D], scalar=1.0,
                in1=nrm[:], op0=ALU.add, op1=ALU.mult), reads=[b_row, b_nrm], writes=[b_row])
        k.dma("sp", g.mod[layer], row[:], reads=[b_row], writes=[g.bufs["mod"]])


class NormSet:
    def __init__(self, g, pfx, nbuf=2):
        self.g = g
        self.junk = sbt(g, pfx + "junk", [128, 2 * D], BF16)
        self.b_junk = Buf()
        self.st = [sbt(g, pfx + "st%d" % i, [128, 4], F32) for i in range(nbuf)]
        self.b_st = [Buf() for _ in range(nbuf)]
        self.tmp = [sbt(g, pfx + "tmp0", [128, D], F32)] * nbuf
        self.b_tmp = [Buf()] * nbuf
        self.hb = [sbt(g, pfx + "hb%d" % i, [128, D], BF16) for i in range(nbuf)]
        self.b_hb = [Buf() for _ in range(nbuf)]
        self.ident = sbt(g, pfx + "ident", [128, 128], BF16)
        self.b_ident = Buf()
        g.k.dma("pool", self.ident[:], g.gin("ident")[:, :], writes=[self.b_ident])
        self.i = 0
        self.nbuf = nbuf


def rstd_of(g, ns, i, xt, b_xt, dim=D):
    k = g.k
    P = xt.ap[0][1]
    st, b_st = ns.st[i], ns.b_st[i]
    k.op("act", lambda e: e.activation(out=ns.junk[0:P, 0:dim], in_=xt, func=AF.Square, accum_out=st[0:P, 0:1]),
         reads=[b_xt], writes=[ns.b_junk, b_st])
    k.op("dve", lambda e: e.tensor_scalar(out=st[0:P, 1:2], in0=st[0:P, 0:1], scalar1=1.0 / dim, scalar2=EPS,
                                           op0=ALU.mult, op1=ALU.add), reads=[b_st], writes=[b_st])
    k.op("act", lambda e: e.sqrt(out=st[0:P, 2:3], in_=st[0:P, 1:2]), reads=[b_st], writes=[b_st])
    k.op("dve", lambda e: e.reciprocal(out=st[0:P, 3:4], in_=st[0:P, 2:3]), reads=[b_st], writes=[b_st])
    return st[0:P, 3:4], b_st


def norm_mod_T(g, ns, xt, b_xt, gbc, shbc, b_mod, hT_dst, b_hT, ps, pb):
    k = g.k
    i = ns.i % ns.nbuf
    ns.i += 1
    rstd, b_st = rstd_of(g, ns, i, xt, b_xt)
    tmp, b_tmp, hb, b_hb = ns.tmp[i], ns.b_tmp[i], ns.hb[i], ns.b_hb[i]
    k.op("dve", lambda e: e.scalar_tensor_tensor(out=tmp[:], in0=xt, scalar=rstd, in1=gbc,
                                                  op0=ALU.mult, op1=ALU.mult),
         reads=[b_xt, b_st, b_mod], writes=[b_tmp])
    k.op("pool", lambda e: e.tensor_tensor(out=hb[:], in0=tmp[:], in1=shbc, op=ALU.add),
         reads=[b_tmp, b_mod], writes=[b_hb])
    psv = ps.bitcast(BF16)
    for kk in range(8):
        k.op("pe", lambda e, kk=kk: e.transpose(out=psv[:, kk * 128:(kk + 1) * 128],
                                                in_=hb[:, kk * 128:(kk + 1) * 128], identity=ns.ident[:]),
             reads=[b_hb, ns.b_ident], writes=[pb], inc=(kk == 7))
    k.op("act", lambda e: e.copy(out=hT_dst, in_=psv[:, 0:1024].rearrange("p (k t) -> p k t", t=128)),
         reads=[pb], writes=[b_hT])
    return hb, b_hb


class ModBC:
    def __init__(self, g, pfx, layer, slots):
        self.g, self.layer, self.slots = g, layer, slots
        self.t = [sbt(g, "%s_m%d" % (pfx, s), [128, D], F32) for s in slots]
        self.b = Buf()
        self.cond = None

    def get(self, cond):
        g = self.g
        if cond != self.cond:
            self.cond = cond
            for t, s in zip(self.t, self.slots):
                g.k.dma("sp", t[:], bcast_rows(g.mod[self.layer, cond:cond + 1, s * D:(s + 1) * D], 128),
                        reads=[g.bufs["mod"]], writes=[self.b])
        return self.t, self.b


def seq_list(g, xsrc, csrc, xdst, cdst, with_ctx=True):
    L = []
    for b in range(NB):
        if with_ctx:
            L.append((g.__dict__[csrc][b], g.__dict__[cdst][b], 2, TC, g.bufs.get(csrc), g.bufs.get(cdst)))
        L.append((g.__dict__[xsrc][b], g.__dict__[xdst][b], b, T, g.bufs.get(xsrc), g.bufs.get(xdst)))
    return L


def phase_ffn(g):
    k, nc = g.k, g.nc
    src_x, src_c = ("x1", "c1") if "ssd" in g.phases else ("x", "ctx")
    NT = 256
    NF = FFN // 128
    w1 = sbt(g, "f_w1", [128, 8, 2 * FFN], BF16)
    w2 = sbt(g, "f_w2", [128, NF, D], BF16)
    b_w1, b_w2 = Buf(), Buf()
    for kk in range(8):
        k.dma("pool", w1[:, kk, :], g.gin("ffn_w_in")[kk * 128:(kk + 1) * 128, :], writes=[b_w1])
    for f in range(NF):
        k.dma("pool", w2[:, f, :], g.gin("ffn_w_out")[f * 128:(f + 1) * 128, :], writes=[b_w2])
    ns = NormSet(g, "f_")
    hT = [sbt(g, "f_hT%d" % i, [128, 8, NT], BF16) for i in range(2)]
    b_hT = [Buf(), Buf()]
    aT = [sbt(g, "f_aT0", [128, NF, NT], BF16)] * 2
    b_aT = [Buf()] * 2
    xt = [sbt(g, "f_xt%d" % i, [128, D], F32) for i in range(4)]
    b_xt = [Buf() for _ in range(4)]
    sg = [sbt(g, "f_sg%d" % i, [128, NT], F32) for i in range(2)]
    b_sg = [Buf(), Buf()]
    sg_o = [sbt(g, "f_o%d" % i, [128, 512], F32) for i in range(2)]
    b_sgo = [Buf(), Buf()]
    mods = ModBC(g, "f", 0, (3, 4, 5))
    grp = 0
    oi = 0
    fi = 0
    LIM, STAGE = 1000, 9
    for (src, dst, cond, ntok, bsrc, bdst) in seq_list(g, src_x, src_c, "x2", "c2")[:LIM]:
        (shbc, gbc, gatebc), b_mod = mods.get(cond)
        for t0 in range(0, ntok, NT):
            hi = grp % 2
            for j in range(NT // 128):
                xi = (grp % 2) * 2 + j
                k.dma("sp", xt[xi][:], src[t0 + j * 128:t0 + (j + 1) * 128, :], reads=[bsrc], writes=[b_xt[xi]])
                norm_mod_T(g, ns, xt[xi][:], b_xt[xi], gbc[:], shbc[:], b_mod,
                           hT[hi][:, :, j * 128:(j + 1) * 128], b_hT[hi], g.ps[4 + j], g.psb[4 + j])
            for f in range(NF if STAGE >= 2 else 0):
                pg, pu = g.ps[(fi % 2) * 2], g.ps[(fi % 2) * 2 + 1]
                bg, bu = g.psb[(fi % 2) * 2], g.psb[(fi % 2) * 2 + 1]
                si = fi % 2
                fi += 1
                for kk in range(8):
                    k.op("pe", lambda e, kk=kk: e.matmul(pg[:, 0:NT], lhsT=w1[:, kk, f * 128:(f + 1) * 128],
                                                         rhs=hT[hi][:, kk, :], start=(kk == 0), stop=(kk == 7)),
                         reads=[b_w1, b_hT[hi]], writes=[bg], inc=(kk == 7))
                for kk in range(8):
                    k.op("pe", lambda e, kk=kk: e.matmul(pu[:, 0:NT], lhsT=w1[:, kk, FFN + f * 128:FFN + (f + 1) * 128],
                                                         rhs=hT[hi][:, kk, :], start=(kk == 0), stop=(kk == 7)),
                         reads=[b_w1, b_hT[hi]], writes=[bu], inc=(kk == 7))
                k.op("act", lambda e: e.activation(out=sg[si][:], in_=pg[:, 0:NT], func=AF.Silu),
                     reads=[bg], writes=[b_sg[si]])
                k.op("dve", lambda e: e.tensor_tensor(out=aT[hi][:, f, :], in0=sg[si][:], in1=pu[:, 0:NT], op=ALU.mult),
                     reads=[b_sg[si], bu], writes=[b_aT[hi]])
            for j in range(NT // 128):
                xi = (grp % 2) * 2 + j
                o = sg_o[oi % 2]
                bo = b_sgo[oi % 2]
                oi += 1
                for half in range(2):
                    po, pbo = g.ps[6 + half], g.psb[6 + half]
                    for f in range(NF):
                        k.op("pe", lambda e, f=f: e.matmul(po[:, :], lhsT=aT[hi][:, f, j * 128:(j + 1) * 128],
                                                           rhs=w2[:, f, half * 512:(half + 1) * 512],
                                                           start=(f == 0), stop=(f == NF - 1)),
                             reads=[b_aT[hi], b_w2], writes=[pbo], inc=(f == NF - 1))
                    hs = slice(half * 512, (half + 1) * 512)
                    o = sg_o[oi % 2]
                    bo = b_sgo[oi % 2]
                    oi += 1
                    k.op("dve", lambda e: e.tensor_tensor(out=o[:], in0=po[:, :], in1=gatebc[:, hs], op=ALU.mult),
                         reads=[pbo, b_mod], writes=[bo])
                    k.op("pool", lambda e: e.tensor_tensor(out=xt[xi][:, hs], in0=o[:], in1=xt[xi][:, hs], op=ALU.add),
                         reads=[bo, b_xt[xi]], writes=[b_xt[xi]])
                k.dma("pool", dst[t0 + j * 128:t0 + (j + 1) * 128, :], xt[xi][:], reads=[b_xt[xi]], writes=[bdst])
            grp += 1


NTK = T + TC


def phase_ssd(g):
    with ExitStack() as pes:
        g.pes = pes
        ssd_in(g)
        g.k.barrier()
    with ExitStack() as pes:
        g.pes = pes
        ssd_scan(g)
        g.k.barrier()


def ssd_in(g):
    k, nc = g.k, g.nc
    w = sbt(g, "s_w", [128, 8, 5184], BF16)
    b_w = Buf()
    for kk in range(8):
        k.dma("pool", w[:, kk, :], g.gin("ssd_w_in")[kk * 128:(kk + 1) * 128, :], writes=[b_w])
    cw = sbt(g, "s_cw", [128, 24, 6], F32)
    b_cw = Buf()
    k.dma("sp", cw[:], g.gin("convT").rearrange("(c p) k -> p c k", p=128), writes=[b_cw])
    dtb = sbt(g, "s_dtb", [128, 64], F32)
    abc = sbt(g, "s_abc", [128, 64], F32)
    b_dtb, b_abc = Buf(), Buf()
    k.dma("sp", dtb[:], bcast_rows(g.gin("ssd_dt_bias")[0:1, :], 128), writes=[b_dtb])
    k.dma("sp", abc[:], bcast_rows(g.gin("ssd_a_log")[0:1, :], 128), writes=[b_abc])
    k.op("act", lambda e: e.activation(out=abc[:], in_=abc[:], func=AF.Exp), reads=[b_abc], writes=[b_abc])
    k.op("dve", lambda e: e.tensor_scalar(out=abc[:], in0=abc[:], scalar1=-1.0, scalar2=None, op0=ALU.mult),
         reads=[b_abc], writes=[b_abc])
    ns = NormSet(g, "s_")
    hT = sbt(g, "s_hT", [128, 8, NTK], BF16)
    b_hT = Buf()
    xt = [sbt(g, "s_xt%d" % i, [128, D], F32) for i in range(2)]
    b_xt = [Buf(), Buf()]
    pre = [sbt(g, "s_pre%d" % i, [128, 2312], F32) for i in range(2)]
    b_pre = [Buf(), Buf()]
    for p_ in pre:
        k.op("pool", lambda e, p_=p_: e.memset(p_[:], 0.0), writes=[b_pre[0], b_pre[1]])
    accv = sbt(g, "s_acc", [128, NTK], F32)
    b_accv = Buf()
    xo = [sbt(g, "s_xo%d" % i, [128, NTK], BF16) for i in range(2)]
    b_xo = [Buf(), Buf()]
    zt = [sbt(g, "s_zt%d" % i, [128, 2048], BF16) for i in range(2)]
    b_zt = [Buf(), Buf()]
    ddt = [sbt(g, "s_dd%d" % i, [128, 192], F32) for i in range(2)]
    b_ddt = [Buf(), Buf()]
    mods = ModBC(g, "s", 0, (0, 1))
    xi = 0
    pi = 0
    for b in range(NB):
        for (src, cond, ntok, off, bsrc) in ((g.ctx[b], 2, TC, 0, g.bufs["ctx"]), (g.x[b], b, T, TC, g.bufs["x"])):
            (shbc, gbc), b_mod = mods.get(cond)
            for j in range(ntok // 128):
                x_ = xt[xi % 2]
                bx = b_xt[xi % 2]
                k.dma("sp", x_[:], src[j * 128:(j + 1) * 128, :], reads=[bsrc], writes=[bx])
                norm_mod_T(g, ns, x_[:], bx, gbc[:], shbc[:], b_mod,
                           hT[:, :, off + j * 128:off + (j + 1) * 128], b_hT, g.ps[6 + xi % 2], g.psb[6 + xi % 2])
                xi += 1
        for ct in range(24):
            pr, bpr = pre[ct % 2], b_pre[ct % 2]
            for (c0, c1) in ((0, 256), (256, 768), (768, 1280), (1280, 1792), (1792, 2304)):
                ps, pb = g.ps[pi % 4], g.psb[pi % 4]
                pi += 1
                n = c1 - c0
                for kk in range(8):
                    k.op("pe", lambda e, kk=kk: e.matmul(ps[:, 0:n], lhsT=w[:, kk, 2048 + ct * 128:2048 + (ct + 1) * 128],
                                                         rhs=hT[:, kk, c0:c1], start=(kk == 0), stop=(kk == 7)),
                         reads=[b_w, b_hT], writes=[pb], inc=(kk == 7))
                po = 2 + c0 if c0 < 256 else c0 + 6
                k.op("act", lambda e: e.copy(out=pr[:, po:po + n], in_=ps[:, 0:n]), reads=[pb], writes=[bpr])
            for (a0, n, p0) in ((0, 256, 0), (256, 2048, 260)):
                k.op("dve", lambda e: e.tensor_scalar(out=accv[:, a0:a0 + n], in0=pr[:, p0:p0 + n],
                                                       scalar1=cw[:, ct, 0:1], scalar2=None, op0=ALU.mult),
                     reads=[bpr, b_cw], writes=[b_accv])
                for q in range(1, 5):
                    k.op("dve", lambda e, q=q: e.scalar_tensor_tensor(out=accv[:, a0:a0 + n], in0=pr[:, p0 + q:p0 + q + n],
                                                                       scalar=cw[:, ct, q:q + 1], in1=accv[:, a0:a0 + n],
                                                                       op0=ALU.mult, op1=ALU.add),
                         reads=[bpr, b_cw, b_accv], writes=[b_accv])
            o_, bo = xo[ct % 2], b_xo[ct % 2]
            k.op("act", lambda e: e.activation(out=o_[:], in_=accv[:], func=AF.Silu, bias=cw[:, ct, 5:6]),
                 reads=[b_accv, b_cw], writes=[bo])
            k.dma("pool", g.xbcT[b, ct * 128:(ct + 1) * 128, :], o_[:], reads=[bo], writes=[g.bufs["xbcT"]])
        for j in range(NTK // 128):
            z_, bz = zt[j % 2], b_zt[j % 2]
            for n4 in range(4):
                ps, pb = g.ps[pi % 4], g.psb[pi % 4]
                pi += 1
                for kk in range(8):
                    k.op("pe", lambda e, kk=kk: e.matmul(ps[:, :], lhsT=hT[:, kk, j * 128:(j + 1) * 128],
                                                         rhs=w[:, kk, n4 * 512:(n4 + 1) * 512], start=(kk == 0), stop=(kk == 7)),
                         reads=[b_w, b_hT], writes=[pb], inc=(kk == 7))
                k.op("act", lambda e: e.activation(out=z_[:, n4 * 512:(n4 + 1) * 512], in_=ps[:, :], func=AF.Silu),
                     reads=[pb], writes=[bz])
            k.dma("pool", g.zs[b, j * 128:(j + 1) * 128, :], z_[:], reads=[bz], writes=[g.bufs["zs"]])
            ps, pb = g.ps[pi % 4], g.psb[pi % 4]
            pi += 1
            for kk in range(8):
                k.op("pe", lambda e, kk=kk: e.matmul(ps[:, 0:64], lhsT=hT[:, kk, j * 128:(j + 1) * 128],
                                                     rhs=w[:, kk, 5120:5184], start=(kk == 0), stop=(kk == 7)),
                     reads=[b_w, b_hT], writes=[pb], inc=(kk == 7))
            d_, bd = ddt[j % 2], b_ddt[j % 2]
            k.op("dve", lambda e: e.tensor_tensor(out=d_[:, 128:192], in0=ps[:, 0:64], in1=dtb[:], op=ALU.add),
                 reads=[pb, b_dtb], writes=[bd])
            k.op("act", lambda e: e.activation(out=d_[:, 128:192], in_=d_[:, 128:192], func=AF.Exp), reads=[bd], writes=[bd])
            k.op("act", lambda e: e.activation(out=d_[:, 0:64], in_=d_[:, 128:192], func=AF.Ln, bias=1.0), reads=[bd], writes=[bd])
            k.op("dve", lambda e: e.tensor_tensor(out=d_[:, 64:128], in0=d_[:, 0:64], in1=abc[:], op=ALU.mult),
                 reads=[bd, b_abc], writes=[bd])
            k.dma("pool", g.dd[b, j * 128:(j + 1) * 128, :], d_[:, 0:128], reads=[bd], writes=[g.bufs["dd"]])


def bc3(ap2, n_in, n_rep):
    a = ap2.ap
    return AP(ap2.tensor, ap2.offset, [list(a[0]), [a[1][0], n_in], [0, n_rep]])


def bmid(ap2, n_rep):
    a = ap2.ap
    return AP(ap2.tensor, ap2.offset, [list(a[0]), [0, n_rep], [a[1][0], a[1][1]]])


def ssd_scan(g):
    k, nc = g.k, g.nc
    cm = sbt(g, "q_cm", [128, 5, 128], F32)
    b_cm = Buf()
    k.dma("sp", cm[:], g.gin("cmat").rearrange("m p c -> p m c"), writes=[b_cm])
    cmb = sbt(g, "q_cmb", [128, 5, 128], BF16)
    k.op("dve", lambda e: e.tensor_copy(out=cmb[:], in_=cm[:]), reads=[b_cm], writes=[b_cm])
    identb = sbt(g, "q_identb", [128, 128], BF16)
    b_id = Buf()
    k.dma("pool", identb[:], g.gin("ident")[:, :], writes=[b_id])
    wout = sbt(g, "q_wout", [128, 16, D], BF16)
    b_wout = Buf()
    for c in range(16):
        k.dma("pool", wout[:, c, :], g.gin("ssd_w_out")[c * 128:(c + 1) * 128, :], writes=[b_wout])
    nwbc = sbt(g, "q_nw", [128, 2048], F32)
    dsk = sbt(g, "q_dsk", [128, 32], F32)
    b_nw = Buf()
    k.dma("sp", nwbc[:], bcast_rows(g.gin("ssd_norm")[0:1, :], 128), writes=[b_nw])
    k.dma("sp", dsk[:], bcast_rows(g.gin("ssd_d")[0:1, :], 128), writes=[b_nw])
    gate = {}
    S_all = [sbt(g, "q_S%d" % i, [128, 4, 512], F32) for i in range(NB)]
    Sb_all = [sbt(g, "q_Sb%d" % i, [128, 4, 512], BF16) for i in range(NB)]
    b_S_all = [[Buf() for _ in range(4)] for _ in range(NB)]
    b_Sb_all = [[Buf() for _ in range(4)] for _ in range(NB)]
    FM = [sbt(g, "q_FM%d" % i, [128, 24, 128], BF16) for i in range(2)]
    b_FM = [Buf(), Buf()]
    ddT = [sbt(g, "q_dd%d" % i, [128, 128], F32) for i in range(2)]
    b_dd = [Buf(), Buf()]
    Xtok = [sbt(g, "q_X%d" % i, [128, 2048], BF16) for i in range(2)]
    b_X = [Buf(), Buf()]
    Btok = [sbt(g, "q_B%d" % i, [128, 512], BF16) for i in range(2)]
    b_B = [Buf(), Buf()]
    cue = [sbt(g, "q_cue%d" % i, [128, 128], F32) for i in range(2)]
    b_cue = [Buf(), Buf()]
    xdt = sbt(g, "q_xdt", [128, 2048], BF16)
    xdtu = sbt(g, "q_xdtu", [128, 2048], BF16)
    b_xdt, b_xdtu = Buf(), Buf()
    sm = [sbt(g, "q_sm%d" % i, [128, 128], BF16) for i in range(2)]
    b_sm = [Buf(), Buf()]
    lh4 = [sbt(g, "q_lh%d" % i, [128, 4, 128], BF16) for i in range(2)]
    b_lh4 = [Buf(), Buf()]
    Em4 = [sbt(g, "q_E%d" % i, [128, 512], BF16) for i in range(2)]
    b_E4 = [Buf(), Buf()]
    Lm4 = [sbt(g, "q_L%d" % i, [128, 512], BF16) for i in range(2)]
    b_L4 = [Buf(), Buf()]
    BURST = 8
    t1 = [sbt(g, "q_t1%d" % i, [128, 512], F32) for i in range(2)]
    b_t1 = [Buf(), Buf()]
    yblk = [sbt(g, "q_y%d" % i, [128, 2048], F32) for i in range(2)]
    b_y = [Buf(), Buf()]
    yfl = sbt(g, "q_yfl", [128, 2048], F32)
    b_yfl = Buf()
    ztl = sbt(g, "q_zt", [128, 2048], BF16)
    b_ztl = Buf()
    ygn = sbt(g, "q_ygn", [128, 2048], BF16)
    b_ygn = Buf()
    ygT = sbt(g, "q_ygT", [128, 16, 128], BF16)
    b_ygT = Buf()
    xt = [sbt(g, "q_xt%d" % i, [128, D], F32) for i in range(2)]
    b_xt = [Buf(), Buf()]
    ot = [sbt(g, "q_ot%d" % i, [128, 512], F32) for i in range(2)]
    b_ot = [Buf(), Buf()]
    gbc_all = [[sbt(g, "q_g%d_%d" % (b_, i), [128, D], F32) for i in range(2)] for b_ in range(NB)]
    b_g = Buf()
    ns = NormSet(g, "q_", nbuf=1)
    sc_slots = [(g.ps[0][:, i * 128:(i + 1) * 128], g.psb[0]) for i in range(3)]
    cum_ps, b_cum = g.ps[0][:, 384:480], g.psb[0]
    d_slots = [(g.ps[1 + i // 4][:, (i % 4) * 128:(i % 4 + 1) * 128], g.psb[1 + i // 4]) for i in range(8)]
    tr_b = [g.psb[6], g.psb[7]]
    cnt = dict(blk=0, sc=0, d=0, h=0, t1=0, o=0)

    for b in range(NB):
        k.dma("sp", gbc_all[b][0][:], bcast_rows(g.mod[0, 2:3, 2 * D:3 * D], 128), reads=[g.bufs["mod"]], writes=[b_g])
        k.dma("sp", gbc_all[b][1][:], bcast_rows(g.mod[0, b:b + 1, 2 * D:3 * D], 128), reads=[g.bufs["mod"]], writes=[b_g])
    for dr in range(2):
        order = list(range(18)) if dr == 0 else [1, 0] + list(range(17, 1, -1))
        CUMM, GL, RM = (0, 1, 0) if dr == 0 else (3, 2, 3)
        for b in range(NB):
            for gi in range(4):
                k.op("pool", lambda e, gi=gi, b=b: e.memset(S_all[b][:, gi, :], 0.0), writes=[b_S_all[b][gi]])
                k.op("pool", lambda e, gi=gi, b=b: e.memset(Sb_all[b][:, gi, :], 0.0), writes=[b_Sb_all[b][gi]])
        for blk in order:
            for b in range(NB):
                S, Sb, b_S, b_Sb, gbc = S_all[b], Sb_all[b], b_S_all[b], b_Sb_all[b], gbc_all[b]
                c0 = blk * 128
                bi = cnt["blk"] % 2
                cnt["blk"] += 1
                fm, bfm, dT, bdT = FM[bi], b_FM[bi], ddT[bi], b_dd[bi]
                X, bX, Bt, bB, cu, bcu = Xtok[bi], b_X[bi], Btok[bi], b_B[bi], cue[bi], b_cue[bi]
                yb, byb = yblk[bi], b_y[bi]
                k.dma("sp", fm[:], g.xbcT[b, :, c0:c0 + 128].rearrange("(c p) t -> p c t", p=128),
                      reads=[g.bufs["xbcT"]], writes=[bfm])
                k.dma("sp", dT[:], g.dd[b, c0:c0 + 128, :], reads=[g.bufs["dd"]], writes=[bdT])
                for hh in range(2):
                    psv = g.ps[6 + hh].bitcast(BF16)
                    for c in range(8):
                        k.op("pe", lambda e, c=c: e.transpose(out=psv[:, c * 128:(c + 1) * 128], in_=fm[:, hh * 8 + c, :],
                                                              identity=identb[:]),
                             reads=[bfm, b_id], writes=[tr_b[hh]], inc=(c == 7))
                    k.op("act", lambda e: e.copy(out=X[:, hh * 1024:(hh + 1) * 1024], in_=psv[:, 0:1024]),
                         reads=[tr_b[hh]], writes=[bX])
                psv = g.ps[6].bitcast(BF16)
                for c in range(4):
                    k.op("pe", lambda e, c=c: e.transpose(out=psv[:, c * 128:(c + 1) * 128], in_=fm[:, 16 + c, :],
                                                          identity=identb[:]),
                         reads=[bfm, b_id], writes=[tr_b[0]], inc=(c == 3))
                k.op("act", lambda e: e.copy(out=Bt[:], in_=psv[:, 0:512]), reads=[tr_b[0]], writes=[bB])
                da = dT[:, 64 + 32 * dr:96 + 32 * dr]
                dtc = dT[:, 32 * dr:32 * dr + 32]
                for q, mi in enumerate((CUMM, GL, 4)):
                    k.op("pe", lambda e, q=q, mi=mi: e.matmul(cum_ps[:, q * 32:(q + 1) * 32], lhsT=cm[:, mi, :], rhs=da,
                                                              start=True, stop=True),
                         reads=[b_cm, bdT], writes=[b_cum], inc=(q == 2))
                k.op("act", lambda e: e.activation(out=cu[:, 0:96], in_=cum_ps, func=AF.Exp), reads=[b_cum], writes=[bcu])
                k.op("dve", lambda e: e.tensor_tensor(out=cu[:, 96:128], in0=cu[:, 32:64], in1=dtc, op=ALU.mult),
                     reads=[bcu, bdT], writes=[bcu])
                X3 = X[:].rearrange("p (h q) -> p h q", q=64)
                k.op("pool", lambda e: e.tensor_tensor(out=xdt[:].rearrange("p (h q) -> p h q", q=64), in0=X3,
                                                        in1=bc3(dtc, 32, 64), op=ALU.mult),
                     reads=[bX, bdT], writes=[b_xdt])
                k.op("pool", lambda e: e.tensor_tensor(out=xdtu[:].rearrange("p (h q) -> p h q", q=64), in0=X3,
                                                        in1=bc3(cu[:, 96:128], 32, 64), op=ALU.mult),
                     reads=[bX, bcu], writes=[b_xdtu])
                smis = {}

                def group_pre(gi):
                    ps_s, b_ps_s = sc_slots[cnt["sc"] % 3]
                    smi = cnt["sc"] % 2
                    cnt["sc"] += 1
                    smis[gi] = smi
                    k.op("pe", lambda e: e.matmul(ps_s, lhsT=fm[:, 16 + gi, :], rhs=fm[:, 20 + gi, :], start=True, stop=True),
                         reads=[bfm], writes=[b_ps_s])
                    k.op("dve", lambda e: e.tensor_tensor(out=sm[smi][:], in0=ps_s, in1=cm[:, RM, :], op=ALU.mult),
                         reads=[b_ps_s, b_cm], writes=[b_sm[smi]])

                def front(i):
                    gi, half = divmod(i, 2)
                    par = i % 2
                    h0 = gi * 8 + half * 4
                    k.op("dve", lambda e: e.tensor_tensor(out=lh4[par][:], in0=bmid(cm[:, GL, :], 4), in1=bc3(da[:, h0:h0 + 4], 4, 128),
                                                           op=ALU.mult),
                         reads=[b_cm, bdT], writes=[b_lh4[par]])
                    psD4, b_psD = g.ps[1 + par], g.psb[1 + par]
                    for j in range(4):
                        k.op("pe", lambda e, j=j: e.matmul(psD4[:, j * 128:(j + 1) * 128], lhsT=lh4[par][:, j, :], rhs=cmb[:, RM, :],
                                                           start=True, stop=True),
                             reads=[b_lh4[par], b_cm], writes=[b_psD], inc=(j == 3))

                def back(i):
                    gi, half = divmod(i, 2)
                    par = i % 2
                    smi = smis[gi]
                    yd, b_yd = g.ps[3], g.psb[3]
                    psD4, b_psD = g.ps[1 + par], g.psb[1 + par]
                    k.op("act", lambda e: e.activation(out=Em4[par][:], in_=psD4[:, :], func=AF.Exp), reads=[b_psD], writes=[b_E4[par]])
                    k.op("dve", lambda e: e.tensor_tensor(out=Lm4[par][:].rearrange("p (h c) -> p h c", c=128),
                                                           in0=Em4[par][:].rearrange("p (h c) -> p h c", c=128),
                                                           in1=bmid(sm[smi][:], 4), op=ALU.mult),
                         reads=[b_E4[par], b_sm[smi]], writes=[b_L4[par]])
                    for j in range(4):
                        h8 = half * 4 + j
                        h = gi * 8 + h8
                        k.op("pe", lambda e, h=h, h8=h8, j=j: e.matmul(yd[:, h8 * 64:(h8 + 1) * 64], lhsT=Lm4[par][:, j * 128:(j + 1) * 128],
                                                                      rhs=xdt[:, h * 64:(h + 1) * 64], start=True, stop=True),
                             reads=[b_L4[par], b_xdt], writes=[b_yd], inc=(j == 3))

                def group_tail(gi):
                    yd, b_yd = g.ps[3], g.psb[3]
                    yo, b_yo = g.ps[4], g.psb[4]
                    k.op("pe", lambda e: e.matmul(yo[:, :], lhsT=fm[:, 20 + gi, :], rhs=Sb[:, gi, :], start=True, stop=True),
                         reads=[bfm, b_Sb[gi]], writes=[b_yo])
                    ti = cnt["t1"] % 2
                    cnt["t1"] += 1
                    k.op("dve", lambda e: e.tensor_tensor(out=t1[ti][:].rearrange("p (h q) -> p h q", q=64),
                                                           in0=yo[:, :].rearrange("p (h q) -> p h q", q=64),
                                                           in1=bc3(cu[:, gi * 8:gi * 8 + 8], 8, 64), op=ALU.mult),
                         reads=[b_yo, bcu], writes=[b_t1[ti]])
                    k.op("dve", lambda e: e.tensor_tensor(out=yb[:, gi * 512:(gi + 1) * 512], in0=t1[ti][:], in1=yd[:, :], op=ALU.add),
                         reads=[b_t1[ti], b_yd], writes=[byb])
                    cs, b_cs = g.ps[5], g.psb[5]
                    k.op("pe", lambda e: e.matmul(cs[:, :], lhsT=Bt[:, gi * 128:(gi + 1) * 128], rhs=xdtu[:, gi * 512:(gi + 1) * 512],
                                                  start=True, stop=True),
                         reads=[bB, b_xdtu], writes=[b_cs])
                    S3 = S[:, gi, :].rearrange("p (h q) -> p h q", q=64)
                    k.op("pool", lambda e: e.tensor_tensor(out=S3, in0=S3, in1=bc3(cu[:, 64 + gi * 8:64 + gi * 8 + 8], 8, 64), op=ALU.mult),
                         reads=[b_S[gi], bcu], writes=[b_S[gi]])
                    k.op("dve", lambda e: e.tensor_tensor(out=S[:, gi, :], in0=S[:, gi, :], in1=cs[:, :], op=ALU.add),
                         reads=[b_S[gi], b_cs], writes=[b_S[gi]])
                    k.op("act", lambda e: e.copy(out=Sb[:, gi, :], in_=S[:, gi, :]), reads=[b_S[gi]], writes=[b_Sb[gi]])
                group_pre(0)
                front(0)
                for i in range(8):
                    if i + 1 < 8:
                        if (i + 1) % 2 == 0:
                            group_pre((i + 1) // 2)
                        front(i + 1)
                    back(i)
                    if i % 2 == 1:
                        group_tail(i // 2)
                if dr == 0:
                    k.dma("pool", g.yf[b, c0:c0 + 128, :], yb[:], reads=[byb], writes=[g.bufs["yf"]])
                    continue
                k.dma("sp", yfl[:], g.yf[b, c0:c0 + 128, :], reads=[g.bufs["yf"]], writes=[b_yfl])
                k.dma("sp", ztl[:], g.zs[b, c0:c0 + 128, :], reads=[g.bufs["zs"]], writes=[b_ztl])
                is_ctx = blk < 2
                xsrc = g.ctx[b, c0:c0 + 128, :] if is_ctx else g.x[b, c0 - TC:c0 - TC + 128, :]
                xdst = g.c1[b, c0:c0 + 128, :] if is_ctx else g.x1[b, c0 - TC:c0 - TC + 128, :]
                bdst = g.bufs["c1"] if is_ctx else g.bufs["x1"]
                x_, bx = xt[bi], b_xt[bi]
                k.dma("sp", x_[:], xsrc, reads=[g.bufs["ctx" if is_ctx else "x"]], writes=[bx])
                k.op("pool", lambda e: e.tensor_tensor(out=yb[:], in0=yb[:], in1=yfl[:], op=ALU.add),
                     reads=[byb, b_yfl], writes=[byb])
                k.op("dve", lambda e: e.tensor_tensor(out=yfl[:].rearrange("p (h q) -> p h q", q=64), in0=X3,
                                                       in1=bc3(dsk[:], 32, 64), op=ALU.mult),
                     reads=[bX, b_nw, b_yfl], writes=[b_yfl])
                k.op("pool", lambda e: e.tensor_tensor(out=yb[:], in0=yb[:], in1=yfl[:], op=ALU.add),
                     reads=[byb, b_yfl], writes=[byb])
                k.op("dve", lambda e: e.tensor_tensor(out=yb[:], in0=yb[:], in1=ztl[:], op=ALU.mult),
                     reads=[byb, b_ztl], writes=[byb])
                rstd, b_st = rstd_of(g, ns, 0, yb[:], byb, dim=2048)
                k.op("dve", lambda e: e.tensor_tensor(out=ygn[:], in0=yb[:], in1=nwbc[:], op=ALU.mult),
                     reads=[byb, b_nw], writes=[b_ygn])
                for hh in range(2):
                    psv = g.ps[6 + hh].bitcast(BF16)
                    for c in range(8):
                        k.op("pe", lambda e, c=c: e.transpose(out=psv[:, c * 128:(c + 1) * 128],
                                                              in_=ygn[:, (hh * 8 + c) * 128:(hh * 8 + c + 1) * 128], identity=identb[:]),
                             reads=[b_ygn, b_id], writes=[tr_b[hh]], inc=(c == 7))
                    k.op("act", lambda e: e.copy(out=ygT[:, hh * 8:(hh + 1) * 8, :],
                                                 in_=psv[:, 0:1024].rearrange("p (c t) -> p c t", t=128)),
                         reads=[tr_b[hh]], writes=[b_ygT])
                gt = gbc[0] if is_ctx else gbc[1]
                for half in range(2):
                    hs = slice(half * 512, (half + 1) * 512)
                    po, b_po = g.ps[3 + half], g.psb[3 + half]
                    for c in range(16):
                        k.op("pe", lambda e, c=c: e.matmul(po[:, :], lhsT=ygT[:, c, :], rhs=wout[:, c, hs],
                                                           start=(c == 0), stop=(c == 15)),
                             reads=[b_ygT, b_wout], writes=[b_po], inc=(c == 15))
                    o_, bo = ot[cnt["o"] % 2], b_ot[cnt["o"] % 2]
                    cnt["o"] += 1
                    k.op("dve", lambda e: e.scalar_tensor_tensor(out=o_[:], in0=po[:, :], scalar=rstd, in1=gt[:, hs],
                                                                  op0=ALU.mult, op1=ALU.mult),
                         reads=[b_po, b_st, b_g], writes=[bo])
                    k.op("pool", lambda e: e.tensor_tensor(out=x_[:, hs], in0=x_[:, hs], in1=o_[:], op=ALU.add),
                         reads=[bo, bx], writes=[bx])
                k.dma("pool", xdst, x_[:], reads=[bx], writes=[bdst])


MAGIC = 12582912.0
TWO_PI_HI = 6.28125
TWO_PI_LO = 0.0019353071795864769


def s5_prep(g):
    k, nc = g.k, g.nc
    V = lambda n, sh=(128, 32): sbt(g, "p_" + n, list(sh), F32)
    ident = sbt(g, "p_identb", [128, 128], BF16)
    b_id = Buf()
    k.dma("pool", ident[:], g.gin("ident")[:, :], writes=[b_id])
    m8 = sbt(g, "p_m8", [128, 2, 128], F32)
    b_m8 = Buf()
    k.dma("sp", m8[:], g.gin("m8").rearrange("m p c -> p m c"), writes=[b_m8])
    bb = Buf()

    def dve(fn, eng="dve"):
        k.op(eng, fn, reads=[bb], writes=[bb])

    def tt(out, a, b_, op):
        dve(lambda e: e.tensor_tensor(out=out, in0=a, in1=b_, op=op))

    def ts(out, a, s1, op0, s2=None, op1=None):
        if op1 is None:
            dve(lambda e: e.tensor_scalar(out=out, in0=a, scalar1=s1, scalar2=None, op0=op0))
        else:
            dve(lambda e: e.tensor_scalar(out=out, in0=a, scalar1=s1, scalar2=s2, op0=op0, op1=op1))

    lre, lim, lst = V("lre"), V("lim"), V("lst")
    step, lr, th, kf, r_, t8, t2 = V("step"), V("lr"), V("th"), V("kf"), V("r"), V("t8"), V("t2")
    sn, cs_, ta, tb, rho1 = V("sn"), V("cs"), V("ta"), V("tb"), V("rho1")
    ar, ai, den, cr, ci = V("ar"), V("ai"), V("den"), V("cr"), V("ci")
    pm, pc, ps_ = V("pm", (128, 32, 9)), V("pc", (128, 32, 9)), V("ps", (128, 32, 9))
    apr, api = V("apr", (128, 32, 9)), V("api", (128, 32, 9))
    anr, ani, imv = V("anr", (128, 32, 9)), V("ani", (128, 32, 9)), V("imv", (128, 32, 9))
    Pc, Ps = V("Pc"), V("Ps")
    bre, bim = V("bre", (128, 32, 16)), V("bim", (128, 32, 16))
    bbr, bbi = V("bbr", (128, 32, 16)), V("bbi", (128, 32, 16))
    cre, cim = V("cre", (128, 32, 16)), V("cim", (128, 32, 16))
    w1, w2 = V("w1", (128, 32, 16)), V("w2", (128, 32, 16))
    WZr = sbt(g, "p_WZr", [128, 32, 8, 16], BF16)
    WZi = sbt(g, "p_WZi", [128, 32, 8, 16], BF16)
    Pr = sbt(g, "p_Pr", [128, 32, 8, 16], BF16)
    Pi = sbt(g, "p_Pi", [128, 32, 8, 16], BF16)
    CAr = sbt(g, "p_CAr", [128, 32, 9, 16], BF16)
    CAi = sbt(g, "p_CAi", [128, 32, 9, 16], BF16)
    cosT = sbt(g, "p_cosT", [128, 32, 288], F32)
    sinT = sbt(g, "p_sinT", [128, 32, 288], F32)
    e1 = sbt(g, "p_e1", [128, 32, 128], F32)
    e2 = sbt(g, "p_e2", [128, 32, 128], F32)
    wst = [sbt(g, "p_wst%d" % i, [128, 6, 128], BF16) for i in range(2)]
    b_wst = [Buf(), Buf()]

    def b16(t, n):
        return bc3(t[:], 32, n)

    def col(t3, j):
        return t3[:, :, j]

    for d in range(2):
        k.dma("sp", lre[:], g.gin("s5_lamT")[d, 0], reads=[bb], writes=[bb])
        k.dma("sp", lim[:], g.gin("s5_lamT")[d, 1], reads=[bb], writes=[bb])
        k.dma("sp", lst[:], g.gin("s5_lamT")[d, 2], reads=[bb], writes=[bb])
        k.dma("sp", bre[:], g.gin("s5_b")[d, 0].rearrange("(gp q) c -> q gp c", q=128), reads=[bb], writes=[bb])
        k.dma("sp", bim[:], g.gin("s5_b")[d, 1].rearrange("(gp q) c -> q gp c", q=128), reads=[bb], writes=[bb])
        k.dma("sp", cre[:], g.gin("s5_cT")[d, 0], reads=[bb], writes=[bb])
        k.dma("sp", cim[:], g.gin("s5_cT")[d, 1], reads=[bb], writes=[bb])
        dve(lambda e: e.activation(out=step[:], in_=lst[:], func=AF.Exp), "act")
        tt(lr[:], lre[:], step[:], ALU.mult)
        tt(th[:], lim[:], step[:], ALU.mult)
        ts(ta[:], lr[:], 1.0 / 5, ALU.mult, 1.0, ALU.add)
        for c_ in (1.0 / 4, 1.0 / 3, 1.0 / 2, 1.0):
            tt(ta[:], ta[:], lr[:], ALU.mult)
            ts(ta[:], ta[:], c_, ALU.mult, 1.0, ALU.add)
        dve(lambda e: e.tensor_copy(out=rho1[:], in_=ta[:]))
        ts(kf[:], th[:], 1.0 / (2 * np.pi), ALU.mult, MAGIC, ALU.add)
        ts(kf[:], kf[:], -MAGIC, ALU.add)
        dve(lambda e: e.scalar_tensor_tensor(out=r_[:], in0=kf[:], scalar=-TWO_PI_HI, in1=th[:], op0=ALU.mult, op1=ALU.add))
        dve(lambda e: e.scalar_tensor_tensor(out=r_[:], in0=kf[:], scalar=-TWO_PI_LO, in1=r_[:], op0=ALU.mult, op1=ALU.add))
        ts(t8[:], r_[:], 0.125, ALU.mult)
        tt(t2[:], t8[:], t8[:], ALU.mult)
        ts(ta[:], t2[:], -1.0 / 5040, ALU.mult, 1.0 / 120, ALU.add)
        tt(ta[:], ta[:], t2[:], ALU.mult)
        ts(ta[:], ta[:], -1.0 / 6, ALU.add)
        tt(ta[:], ta[:], t2[:], ALU.mult)
        ts(ta[:], ta[:], 1.0, ALU.add)
        tt(sn[:], ta[:], t8[:], ALU.mult)
        ts(ta[:], t2[:], 1.0 / 40320, ALU.mult, -1.0 / 720, ALU.add)
        tt(ta[:], ta[:], t2[:], ALU.mult)
        ts(ta[:], ta[:], 1.0 / 24, ALU.add)
        tt(ta[:], ta[:], t2[:], ALU.mult)
        ts(ta[:], ta[:], -0.5, ALU.add)
        tt(ta[:], ta[:], t2[:], ALU.mult)
        ts(cs_[:], ta[:], 1.0, ALU.add)
        for _ in range(3):
            tt(ta[:], cs_[:], cs_[:], ALU.mult)
            tt(tb[:], sn[:], sn[:], ALU.mult)
            tt(sn[:], sn[:], cs_[:], ALU.mult)
            ts(sn[:], sn[:], 2.0, ALU.mult)
            tt(cs_[:], ta[:], tb[:], ALU.subtract)
        tt(ar[:], rho1[:], cs_[:], ALU.mult)
        tt(ai[:], rho1[:], sn[:], ALU.mult)
        ts(ta[:], ar[:], -1.0, ALU.add)
        tt(den[:], lre[:], lre[:], ALU.mult)
        tt(tb[:], lim[:], lim[:], ALU.mult)
        tt(den[:], den[:], tb[:], ALU.add)
        dve(lambda e: e.reciprocal(out=den[:], in_=den[:]))
        tt(cr[:], ta[:], lre[:], ALU.mult)
        tt(tb[:], ai[:], lim[:], ALU.mult)
        tt(cr[:], cr[:], tb[:], ALU.add)
        tt(cr[:], cr[:], den[:], ALU.mult)
        tt(ci[:], ai[:], lre[:], ALU.mult)
        tt(tb[:], ta[:], lim[:], ALU.mult)
        tt(ci[:], ci[:], tb[:], ALU.subtract)
        tt(ci[:], ci[:], den[:], ALU.mult)
        tt(w1[:], bre[:], b16(cr, 16), ALU.mult)
        tt(w2[:], bim[:], b16(ci, 16), ALU.mult)
        tt(bbr[:], w1[:], w2[:], ALU.subtract)
        tt(w1[:], bim[:], b16(cr, 16), ALU.mult)
        tt(w2[:], bre[:], b16(ci, 16), ALU.mult)
        tt(bbi[:], w1[:], w2[:], ALU.add)
        dve(lambda e: e.memset(col(pm, 0), 1.0))
        dve(lambda e: e.memset(col(pc, 0), 1.0))
        dve(lambda e: e.memset(col(ps_, 0), 0.0))
        for j in range(1, 9):
            tt(col(pm, j), col(pm, j - 1), rho1[:], ALU.mult)
            tt(ta[:], col(pc, j - 1), cs_[:], ALU.mult)
            tt(tb[:], col(ps_, j - 1), sn[:], ALU.mult)
            tt(col(pc, j), ta[:], tb[:], ALU.subtract)
            tt(ta[:], col(ps_, j - 1), cs_[:], ALU.mult)
            tt(tb[:], col(pc, j - 1), sn[:], ALU.mult)
            tt(col(ps_, j), ta[:], tb[:], ALU.add)
        dve(lambda e: e.reciprocal(out=imv[:], in_=pm[:]))
        tt(apr[:], pm[:], pc[:], ALU.mult)
        tt(api[:], pm[:], ps_[:], ALU.mult)
        tt(anr[:], imv[:], pc[:], ALU.mult)
        tt(ani[:], imv[:], ps_[:], ALU.mult)
        ts(ani[:], ani[:], -1.0, ALU.mult)
        dve(lambda e: e.tensor_copy(out=ta[:], in_=col(pm, 8)))
        k.dma("sp", g.rho[d], ta[:], reads=[bb], writes=[g.bufs["rho"]])
        for s_ in range(8):
            so = s_ if d == 0 else 7 - s_
            for (dst, pr_, pi_) in ((WZr, col(apr, 7 - s_), col(api, 7 - s_)), (Pr, col(anr, s_), col(ani, s_))):
                dsti = WZi if dst is WZr else Pi
                tt(w1[:], bbr[:], bc3(pr_, 32, 16), ALU.mult)
                tt(w2[:], bbi[:], bc3(pi_, 32, 16), ALU.mult)
                tt(dst[:, :, so, :], w1[:], w2[:], ALU.subtract)
                tt(w1[:], bbi[:], bc3(pr_, 32, 16), ALU.mult)
                tt(w2[:], bbr[:], bc3(pi_, 32, 16), ALU.mult)
                tt(dsti[:, :, so, :], w1[:], w2[:], ALU.add)
        for j in range(9):
            jo = j if d == 0 else 8 - j
            tt(w1[:], cre[:], bc3(col(apr, j), 32, 16), ALU.mult)
            tt(w2[:], cim[:], bc3(col(api, j), 32, 16), ALU.mult)
            tt(CAr[:, :, jo, :], w1[:], w2[:], ALU.subtract)
            tt(w1[:], cim[:], bc3(col(apr, j), 32, 16), ALU.mult)
            tt(w2[:], cre[:], bc3(col(api, j), 32, 16), ALU.mult)
            tt(w1[:], w1[:], w2[:], ALU.add)
            ts(CAi[:, :, jo, :], w1[:], -1.0, ALU.mult)
        q0, y0 = (0, 1) if d == 0 else (1, 0)
        dve(lambda e: e.tensor_copy(out=Pc[:], in_=col(pc, 8)))
        dve(lambda e: e.tensor_copy(out=Ps[:], in_=col(ps_, 8)))
        dve(lambda e: e.memset(cosT[:, :, 0:1], 1.0))
        dve(lambda e: e.memset(sinT[:, :, 0:1], 0.0))
        wdt = 1
        while wdt < 288:
            n = min(wdt, 288 - wdt)
            tt(e1[:, :, 0:n], cosT[:, :, 0:n], b16(Pc, n), ALU.mult)
            tt(e2[:, :, 0:n], sinT[:, :, 0:n], b16(Ps, n), ALU.mult)
            tt(cosT[:, :, wdt:wdt + n], e1[:, :, 0:n], e2[:, :, 0:n], ALU.subtract)
            tt(e1[:, :, 0:n], sinT[:, :, 0:n], b16(Pc, n), ALU.mult)
            tt(e2[:, :, 0:n], cosT[:, :, 0:n], b16(Ps, n), ALU.mult)
            tt(sinT[:, :, wdt:wdt + n], e1[:, :, 0:n], e2[:, :, 0:n], ALU.add)
            tt(ta[:], Pc[:], Pc[:], ALU.mult)
            tt(tb[:], Ps[:], Ps[:], ALU.mult)
            tt(Ps[:], Ps[:], Pc[:], ALU.mult)
            ts(Ps[:], Ps[:], 2.0, ALU.mult)
            tt(Pc[:], ta[:], tb[:], ALU.subtract)
            wdt *= 2
        k.dma("sp", g.etab[d, 0], cosT[:], reads=[bb], writes=[g.bufs["etab"]])
        k.dma("sp", g.etab[d, 1], sinT[:], reads=[bb], writes=[g.bufs["etab"]])
        for gp in range(32):
            wt, bwt = wst[gp % 2], b_wst[gp % 2]
            for g2 in range(2):
                L = slice(g2 * 64, (g2 + 1) * 64)
                pM, bM = g.ps[g2], g.psb[g2]
                k.op("pe", lambda e: e.matmul(pM[:, 0:128], lhsT=Pr[L, gp, :, :], rhs=CAr[L, gp, q0:q0 + 8, :], start=True, stop=False),
                     reads=[bb], writes=[bM], inc=False)
                k.op("pe", lambda e: e.matmul(pM[:, 0:128], lhsT=Pi[L, gp, :, :], rhs=CAi[L, gp, q0:q0 + 8, :], start=False, stop=True),
                     reads=[bb], writes=[bM])
                k.op("dve", lambda e: e.tensor_tensor(out=wt[:, g2, :], in0=pM[:, 0:128], in1=m8[:, d, :], op=ALU.mult),
                     reads=[bM, b_m8], writes=[bwt])
            for ri, src_ in enumerate((WZr, WZi)):
                pT, bT = g.ps[2 + ri], g.psb[2 + ri]
                pTv = pT.bitcast(BF16)
                k.op("pe", lambda e: e.transpose(out=pTv[:, 0:128], in_=src_[:, gp, :, :], identity=ident[:]),
                     reads=[bb, b_id], writes=[bT])
                k.op("act", lambda e: e.copy(out=wt[:, 2 + ri, :], in_=pTv[:, 0:128]), reads=[bT], writes=[bwt])
            k.op("act", lambda e: e.copy(out=wt[:, 4, :], in_=CAr[:, gp, y0:y0 + 8, :]), reads=[bb], writes=[bwt])
            k.op("act", lambda e: e.copy(out=wt[:, 5, :], in_=CAi[:, gp, y0:y0 + 8, :]), reads=[bb], writes=[bwt])
            k.dma("sp", g.s5w[d, gp], wt[:], reads=[bwt], writes=[g.bufs["s5w"]])


def phase_s5(g):
    with ExitStack() as pes:
        g.pes = pes
        s5_prep(g)
        g.k.barrier()
    if g.kinds.get("_s5_prep_only"):
        return
    with ExitStack() as pes:
        g.pes = pes
        s5_main(g)
        g.k.barrier()


def bcf(col_ap, n):
    a = col_ap.ap
    return AP(col_ap.tensor, col_ap.offset, [list(a[0]), [0, n]])


def s5_main(g):
    k, nc = g.k, g.nc
    identb = sbt(g, "v_identb", [128, 128], BF16)
    b_id = Buf()
    k.dma("pool", identb[:], g.gin("ident")[:, :], writes=[b_id])
    bglu = sbt(g, "v_bglu", [128, 2 * D], F32)
    dsk = sbt(g, "v_dsk", [128, D], F32)
    rho = sbt(g, "v_rho", [128, 2, 32], F32)
    b_c = Buf()
    k.dma("sp", bglu[:], bcast_rows(g.gin("s5_b_glu")[0:1, :], 128), writes=[b_c])
    k.dma("sp", dsk[:], bcast_rows(g.gin("s5_d")[0:1, :], 128), writes=[b_c])
    k.dma("sp", rho[:], g.rho.rearrange("d q gp -> q d gp"), reads=[g.bufs["rho"]], writes=[b_c])
    mods = ModBC(g, "v", 1, (0, 1, 2))
    ns = NormSet(g, "v_")
    Ubig = sbt(g, "v_U", [128, 64 * 320], BF16)
    U = Ubig[:, :].rearrange("p (g c) -> p g c", c=320)
    wglu = Ubig[:, 0:8 * 2048].rearrange("p (k n) -> p k n", n=2048)
    b_U = Buf()
    hcx = sbt(g, "v_hcx", [128, 64, 8, 16], BF16)
    b_hcx = Buf()
    hxc = sbt(g, "v_hxc", [128, 2, 64, 8, 16], BF16)
    b_hxc = Buf()
    xt = [sbt(g, "v_xt%d" % i, [128, D], F32) for i in range(2)]
    b_xt = [Buf(), Buf()]
    tab = [sbt(g, "v_tab%d" % i, [128, 2, 288], F32) for i in range(2)]
    b_tab = [Buf(), Buf()]
    wk = [[sbt(g, "v_wk%d_%d" % (i, j), [128, 288], F32) for j in range(8)] for i in range(2)]
    b_wk = [[Buf() for _ in range(8)] for _ in range(2)]
    spv = [sbt(g, "v_spv%d" % i, [128, 2, 2, 256], BF16) for i in range(2)]
    b_spv = [Buf(), Buf()]
    wts = [sbt(g, "v_wts%d" % i, [128, 2, 6, 128], BF16) for i in range(2)]
    b_wts = [Buf(), Buf()]
    ysb = [sbt(g, "v_ysb%d" % i, [128, 256], BF16) for i in range(2)]
    b_ysb = [Buf(), Buf()]
    gt_ = [sbt(g, "v_gt%d" % i, [128, D], F32) for i in range(3)]
    b_gt = [Buf() for _ in range(3)]
    geb2 = [sbt(g, "v_geb%d" % i, [128, D], BF16) for i in range(2)]
    b_geb2 = [Buf(), Buf()]
    geT2 = [sbt(g, "v_geT%d" % i, [128, 8, 128], BF16) for i in range(2)]
    b_geT2 = [Buf(), Buf()]
    av = [sbt(g, "v_av%d" % i, [128, 512], F32) for i in range(2)]
    b_av = [Buf(), Buf()]
    gv = [sbt(g, "v_gv%d" % i, [128, 512], F32) for i in range(2)]
    b_gv = [Buf(), Buf()]
    cnt = dict(x=0, it=0, y=0, o=0)

    def load_x_tile(dst, bdst, src_b, tile, l):
        for r4 in range(4):
            t0 = (r4 * 8 + l) * 64 + tile * 32
            k.dma("sp", dst[r4 * 32:(r4 + 1) * 32, :], src_b[t0:t0 + 32, :], reads=[g.bufs["x2"]], writes=[bdst])

    for b in range(NB):
        (shbc, gbc, gatebc), b_mod = mods.get(2)
        cv = g.c2[b].rearrange("(c l) d -> l c d", l=8)
        for l in range(8):
            x_, bx = xt[cnt["x"] % 2], b_xt[cnt["x"] % 2]
            cnt["x"] += 1
            k.dma("sp", x_[0:32, :], cv[l], reads=[g.bufs["c2"]], writes=[bx])
            rstd, b_st = rstd_of(g, ns, 0, x_[0:32, :], bx)
            st = ns.st[0]
            k.op("dve", lambda e: e.scalar_tensor_tensor(out=ns.tmp[0][0:32, :], in0=x_[0:32, :], scalar=st[0:32, 3:4], in1=gbc[0:32, :],
                                                          op0=ALU.mult, op1=ALU.mult),
                 reads=[bx, b_st, b_mod], writes=[ns.b_tmp[0]])
            k.op("pool", lambda e: e.tensor_tensor(out=hcx[0:32, :, l, :], in0=ns.tmp[0][0:32, :].rearrange("p (g c) -> p g c", c=16),
                                                   in1=shbc[0:32, :].rearrange("p (g c) -> p g c", c=16), op=ALU.add),
                 reads=[ns.b_tmp[0], b_mod], writes=[b_hcx])
        (shbc, gbc, gatebc), b_mod = mods.get(b)
        for tile in range(2):
            for l in range(8):
                x_, bx = xt[cnt["x"] % 2], b_xt[cnt["x"] % 2]
                cnt["x"] += 1
                load_x_tile(x_, bx, g.x2[b], tile, l)
                rstd, b_st = rstd_of(g, ns, 0, x_[:], bx)
                k.op("dve", lambda e: e.scalar_tensor_tensor(out=ns.tmp[0][:], in0=x_[:], scalar=rstd, in1=gbc[:],
                                                              op0=ALU.mult, op1=ALU.mult),
                     reads=[bx, b_st, b_mod], writes=[ns.b_tmp[0]])
                k.op("pool", lambda e: e.tensor_tensor(out=hxc[:, tile, :, l, :], in0=ns.tmp[0][:].rearrange("p (g c) -> p g c", c=16),
                                                       in1=shbc[:].rearrange("p (g c) -> p g c", c=16), op=ALU.add),
                     reads=[ns.b_tmp[0], b_mod], writes=[b_hxc])
        for gg in range(64):
            ps, pb = g.ps[6 + gg % 2], g.psb[6 + gg % 2]
            psv = ps.bitcast(BF16)
            k.op("pe", lambda e: e.transpose(out=psv[:, 0:32], in_=hcx[0:32, gg, :, :], identity=identb[0:32, 0:32]),
                 reads=[b_hcx, b_id], writes=[pb], inc=False)
            for tile in range(2):
                k.op("pe", lambda e, tile=tile: e.transpose(out=psv[:, 32 + tile * 128:32 + (tile + 1) * 128],
                                                            in_=hxc[:, tile, gg, :, :], identity=identb[:]),
                     reads=[b_hxc, b_id], writes=[pb], inc=(tile == 1))
            k.op("act", lambda e: e.copy(out=U[:, gg, 0:32], in_=psv[:, 0:32]), reads=[pb], writes=[b_U])
            k.op("act", lambda e: e.copy(out=U[:, gg, 288:320], in_=psv[:, 0:32]), reads=[pb], writes=[b_U])
            k.op("act", lambda e: e.copy(out=U[:, gg, 32:288].rearrange("p (t c r) -> p t r c", t=2, c=32, r=4),
                                         in_=psv[:, 32:288].rearrange("p (t r c) -> p t r c", t=2, r=4, c=32)),
                 reads=[pb], writes=[b_U])
        for tile in range(2):
            for l in range(8):
                k.op("dve", lambda e, tile=tile, l=l: e.tensor_tensor(out=hxc[:, tile, :, l, :], in0=hxc[:, tile, :, l, :],
                                                                      in1=dsk[:].rearrange("p (g c) -> p g c", c=16), op=ALU.mult),
                     reads=[b_hxc, b_c], writes=[b_hxc])
        for gp in range(32):
            it = cnt["it"] % 2
            cnt["it"] += 1
            wt, bwt, sp_, bsp = wts[it], b_wts[it], spv[it], b_spv[it]
            for d in range(2):
                tb_, btb = tab[d], b_tab[d]
                W, bW = wk[d], b_wk[d]
                k.dma("sp", wt[:, d], g.s5w[d, gp], reads=[g.bufs["s5w"]], writes=[bwt])
                k.dma("sp", tb_[:, 0, :], g.etab[d, 0, :, gp, :], reads=[g.bufs["etab"]], writes=[btb])
                k.dma("sp", tb_[:, 1, :], g.etab[d, 1, :, gp, :], reads=[g.bufs["etab"]], writes=[btb])
                cols = slice(0, 288) if d == 0 else slice(32, 320)
                pz = [(g.ps[2 * d], g.psb[2 * d]), (g.ps[2 * d + 1], g.psb[2 * d + 1])]
                for ri in range(2):
                    pZ, bZ = pz[ri]
                    for g2 in range(2):
                        k.op("pe", lambda e, g2=g2: e.matmul(pZ[g2 * 64:(g2 + 1) * 64, 0:288], lhsT=wt[:, d, 2 + ri, g2 * 64:(g2 + 1) * 64],
                                                             rhs=U[:, 2 * gp + g2, cols], start=True, stop=True),
                             reads=[bwt, b_U], writes=[bZ], inc=(g2 == 1))
                cosv, sinv = tb_[:, 0, :], tb_[:, 1, :]
                if d == 1:
                    cosv, sinv = rev_ap(cosv, 288), rev_ap(sinv, 288)
                Zr, bZr = pz[0][0][:, 0:288], pz[0][1]
                Zi, bZi = pz[1][0][:, 0:288], pz[1][1]
                za, zb, ztr, zti, sr, si, zc, zd = [w_[:] for w_ in W]
                bza, bzb, bztr, bzti, bsr, bsi, bzc, bzd = bW
                TT = lambda o, a_, b_, op, rd, wr: k.op("dve", lambda e: e.tensor_tensor(out=o, in0=a_, in1=b_, op=op), reads=rd, writes=wr)
                TT(za, Zr, cosv, ALU.mult, [bZr, btb], [bza])
                TT(zb, Zi, sinv, ALU.mult, [bZi, btb], [bzb])
                TT(zc, Zi, cosv, ALU.mult, [bZi, btb], [bzc])
                TT(zd, Zr, sinv, ALU.mult, [bZr, btb], [bzd])
                TT(ztr, za, zb, ALU.add, [bza, bzb], [bztr])
                TT(zti, zc, zd, ALU.subtract, [bzc, bzd], [bzti])
                rbc = bcf(rho[:, d, gp:gp + 1], 288)
                for (o_, i_, bo_, bi_) in ((sr, ztr, bsr, bztr), (si, zti, bsi, bzti)):
                    oo, ii = (o_, i_) if d == 0 else (rev_ap(o_, 288), rev_ap(i_, 288))
                    k.op("dve", lambda e, oo=oo, ii=ii: e.tensor_tensor_scan(out=oo, data0=rbc, data1=ii, initial=0.0,
                                                                             op0=ALU.mult, op1=ALU.add),
                         reads=[bi_, b_c], writes=[bo_])
                sl = slice(31, 287) if d == 0 else slice(1, 257)
                TT(za[:, 0:256], sr[:, sl], cosv[:, sl], ALU.mult, [bsr, btb], [bza])
                TT(zc[:, 0:256], sr[:, sl], sinv[:, sl], ALU.mult, [bsr, btb], [bzc])
                TT(zb[:, 0:256], si[:, sl], sinv[:, sl], ALU.mult, [bsi, btb], [bzb])
                TT(zd[:, 0:256], si[:, sl], cosv[:, sl], ALU.mult, [bsi, btb], [bzd])
                TT(sp_[:, d, 0, :], za[:, 0:256], zb[:, 0:256], ALU.subtract, [bza, bzb], [bsp])
                TT(sp_[:, d, 1, :], zd[:, 0:256], zc[:, 0:256], ALU.add, [bzc, bzd], [bsp])
            for g2 in range(2):
                L = slice(g2 * 64, (g2 + 1) * 64)
                gg = 2 * gp + g2
                yi = cnt["y"] % 2
                cnt["y"] += 1
                pY, bY = g.ps[4 + yi], g.psb[4 + yi]
                ops = [(wt[:, 0, g2, :], U[:, gg, 32:288]), (wt[:, 1, g2, :], U[:, gg, 32:288])]
                for d in range(2):
                    ops.append((wt[L, d, 4, :], sp_[L, d, 0, :]))
                    ops.append((wt[L, d, 5, :], sp_[L, d, 1, :]))
                for oi, (lw, rh) in enumerate(ops):
                    k.op("pe", lambda e, lw=lw, rh=rh, oi=oi: e.matmul(pY[:, 0:256], lhsT=lw, rhs=rh, start=(oi == 0), stop=(oi == 5)),
                         reads=[bwt, b_U, bsp], writes=[bY], inc=(oi == 5))
                ys, bys = ysb[yi], b_ysb[yi]
                k.op("act", lambda e: e.copy(out=ys[:].rearrange("p (t r c) -> p t c r", t=2, r=4, c=32),
                                             in_=pY[:, 0:256].rearrange("p (t c r) -> p t c r", t=2, c=32, r=4)),
                     reads=[bY], writes=[bys])
                pT, bT = g.ps[6 + yi], g.psb[6 + yi]
                pTv = pT.bitcast(BF16)
                for tile in range(2):
                    k.op("pe", lambda e, tile=tile: e.transpose(out=pTv[:, tile * 128:(tile + 1) * 128],
                                                                in_=ys[:, tile * 128:(tile + 1) * 128], identity=identb[:]),
                         reads=[bys, b_id], writes=[bT], inc=(tile == 1))
                tyv = hxc[:, :, gg, :, :]
                k.op("dve", lambda e: e.tensor_tensor(out=tyv, in0=tyv, in1=pTv[:, 0:256].rearrange("p (t l c) -> p t l c", t=2, l=8, c=16),
                                                       op=ALU.add),
                     reads=[bT, b_hxc], writes=[b_hxc])
        for kk in range(8):
            k.dma("pool", wglu[:, kk, :], g.gin("s5_w_glu")[kk * 128:(kk + 1) * 128, :], writes=[b_U])
        def d_pre(si_):
            tile, l = divmod(si_, 8)
            gb, bgb, gT, bgT = geb2[si_ % 2], b_geb2[si_ % 2], geT2[si_ % 2], b_geT2[si_ % 2]
            yv = hxc[:, tile, :, l, :]
            g0, g1, g2_ = [t_[:].rearrange("p (g c) -> p g c", c=16) for t_ in gt_]
            k.op("act", lambda e: e.activation(out=g0, in_=yv, func=AF.Square), reads=[b_hxc], writes=[b_gt[0]])
            k.op("dve", lambda e: e.tensor_scalar(out=g0, in0=g0, scalar1=0.044715, scalar2=1.0, op0=ALU.mult, op1=ALU.add),
                 reads=[b_gt[0]], writes=[b_gt[0]])
            k.op("dve", lambda e: e.tensor_tensor(out=g1, in0=g0, in1=yv, op=ALU.mult), reads=[b_gt[0], b_hxc], writes=[b_gt[1]])
            k.op("act", lambda e: e.activation(out=g2_, in_=g1, func=AF.Sigmoid, scale=1.5957691216057308),
                 reads=[b_gt[1]], writes=[b_gt[2]])
            k.op("dve", lambda e: e.tensor_tensor(out=gb[:].rearrange("p (g c) -> p g c", c=16), in0=g2_, in1=yv, op=ALU.mult),
                 reads=[b_gt[2], b_hxc], writes=[bgb])
            ps, pb = g.ps[6 + si_ % 2], g.psb[6 + si_ % 2]
            psv = ps.bitcast(BF16)
            for kk in range(8):
                k.op("pe", lambda e, kk=kk: e.transpose(out=psv[:, kk * 128:(kk + 1) * 128], in_=gb[:, kk * 128:(kk + 1) * 128],
                                                        identity=identb[:]),
                     reads=[bgb, b_id], writes=[pb], inc=(kk == 7))
            k.op("act", lambda e: e.copy(out=gT[:], in_=psv[:, 0:1024].rearrange("p (k t) -> p k t", t=128)),
                 reads=[pb], writes=[bgT])
            x_, bx = xt[si_ % 2], b_xt[si_ % 2]
            load_x_tile(x_, bx, g.x2[b], tile, l)

        def d_post(si_):
            tile, l = divmod(si_, 8)
            gT, bgT = geT2[si_ % 2], b_geT2[si_ % 2]
            x_, bx = xt[si_ % 2], b_xt[si_ % 2]
            for half in range(2):
                oi = cnt["o"] % 2
                cnt["o"] += 1
                pa, ba = g.ps[oi * 2], g.psb[oi * 2]
                pg_, bg_ = g.ps[oi * 2 + 1], g.psb[oi * 2 + 1]
                for (pp, bp, n0) in ((pa, ba, half * 512), (pg_, bg_, D + half * 512)):
                    for kk in range(8):
                        k.op("pe", lambda e, kk=kk, pp=pp, n0=n0: e.matmul(pp[:, :], lhsT=gT[:, kk, :], rhs=wglu[:, kk, n0:n0 + 512],
                                                                           start=(kk == 0), stop=(kk == 7)),
                             reads=[bgT, b_U], writes=[bp], inc=(kk == 7))
                hs = slice(half * 512, (half + 1) * 512)
                a_, ba_, g_, bg2 = av[oi], b_av[oi], gv[oi], b_gv[oi]
                k.op("dve", lambda e: e.tensor_tensor(out=a_[:], in0=pa[:, :], in1=bglu[:, hs], op=ALU.add),
                     reads=[ba, b_c], writes=[ba_])
                k.op("dve", lambda e: e.tensor_tensor(out=g_[:], in0=pg_[:, :], in1=bglu[:, D + half * 512:D + (half + 1) * 512], op=ALU.add),
                     reads=[bg_, b_c], writes=[bg2])
                k.op("act", lambda e: e.activation(out=g_[:], in_=g_[:], func=AF.Sigmoid), reads=[bg2], writes=[bg2])
                k.op("dve", lambda e: e.tensor_tensor(out=a_[:], in0=a_[:], in1=g_[:], op=ALU.mult), reads=[ba_, bg2], writes=[ba_])
                k.op("pool", lambda e: e.tensor_tensor(out=a_[:], in0=a_[:], in1=gatebc[:, hs], op=ALU.mult),
                     reads=[ba_, b_mod], writes=[ba_])
                k.op("pool", lambda e: e.tensor_tensor(out=x_[:, hs], in0=x_[:, hs], in1=a_[:], op=ALU.add),
                     reads=[ba_, bx], writes=[bx])
            for r4 in range(4):
                t0 = (r4 * 8 + l) * 64 + tile * 32
                k.dma("pool", g.x3[b, t0:t0 + 32, :], x_[r4 * 32:(r4 + 1) * 32, :], reads=[bx], writes=[g.bufs["x3"]])

        d_pre(0)
        for si_ in range(16):
            if si_ + 1 < 16:
                d_pre(si_ + 1)
            d_post(si_)


def phase_moe(g):
    k, nc = g.k, g.nc
    src = g.x3 if "s5" in g.phases or g.kinds.get("x3") == "in" else g.x2
    b_src = g.bufs["x3"] if "s5" in g.phases or g.kinds.get("x3") == "in" else g.bufs["x2"]
    NTL = T // 128
    NFG = EDIM // 512
    ns = NormSet(g, "m_")
    identf = sbt(g, "m_identf", [128, 128], F32)
    b_identf = Buf()
    k.dma("sp", identf[:], g.gin("ident")[:, :], writes=[b_identf])
    hT = sbt(g, "m_hT", [128, 8, T], BF16)
    b_hT = Buf()
    acc = sbt(g, "m_acc", [128, NTL, D], F32)
    b_acc = [Buf() for _ in range(NTL)]
    aT = sbt(g, "m_aT", [128, 4, T], BF16)
    b_aT = [Buf() for _ in range(4)]
    wg = [sbt(g, "m_wg%d" % i, [128, 8, 512], BF16) for i in range(2)]
    wu = [sbt(g, "m_wu%d" % i, [128, 8, 512], BF16) for i in range(2)]
    wo = [sbt(g, "m_wo%d" % i, [128, 4, D], BF16) for i in range(2)]
    b_w = [Buf(), Buf()]
    comb = sbt(g, "m_comb", [128, NTL, NEXP], F32)
    b_comb = Buf()
    rw = sbt(g, "m_rw", [128, 8, NEXP], F32)
    rb = sbt(g, "m_rb", [128, NEXP], F32)
    b_rw = Buf()
    k.dma("sp", rw[:], g.gin("moe_router_w").rearrange("(k p) e -> p k e", p=128), writes=[b_rw])
    k.dma("sp", rb[:], bcast_rows(g.gin("moe_router_b")[0:1, :], 128), writes=[b_rw])
    nfin = sbt(g, "m_nfin", [128, D], F32)
    b_nfin = Buf()
    k.dma("sp", nfin[:], bcast_rows(g.gin("norm_final")[0:1, :], 128), writes=[b_nfin])
    hTf = sbt(g, "m_hTf", [128, 8, 128], F32)
    b_hTf = Buf()
    lg = [sbt(g, "m_lg%d" % i, [128, 40], F32) for i in range(2)]
    b_lg = [Buf(), Buf()]
    sg = [sbt(g, "m_sg%d" % i, [128, 512], F32) for i in range(2)]
    b_sg = [Buf(), Buf()]
    tmpo = [sbt(g, "m_to%d" % i, [128, 512], F32) for i in range(2)]
    b_to = [Buf(), Buf()]
    mods = ModBC(g, "m", 1, (3, 4, 5))
    wi = 0
    si = 0
    oi = 0
    pi = 0
    for b in range(NB):
        (shbc, gbc, gatebc), b_mod = mods.get(b)
        for j in range(NTL):
            xt = acc[:, j, :]
            k.dma("sp", xt, src[b, j * 128:(j + 1) * 128, :], reads=[b_src], writes=[b_acc[j]])
            i = ns.i % ns.nbuf
            ns.i += 1
            rstd, b_st = rstd_of(g, ns, i, xt, b_acc[j])
            tmp, b_tmp, hb, b_hb = ns.tmp[i], ns.b_tmp[i], ns.hb[i], ns.b_hb[i]
            k.op("dve", lambda e: e.scalar_tensor_tensor(out=tmp[:], in0=xt, scalar=rstd, in1=gbc[:],
                                                          op0=ALU.mult, op1=ALU.mult),
                 reads=[b_acc[j], b_st, b_mod], writes=[b_tmp])
            k.op("pool", lambda e: e.tensor_tensor(out=tmp[:], in0=tmp[:], in1=shbc[:], op=ALU.add),
                 reads=[b_tmp, b_mod], writes=[b_tmp])
            k.op("act", lambda e: e.copy(out=hb[:], in_=tmp[:]), reads=[b_tmp], writes=[b_hb])
            ps, pb = g.ps[4 + (j % 2)], g.psb[4 + (j % 2)]
            psv = ps.bitcast(BF16)
            for kk in range(8):
                k.op("pe", lambda e, kk=kk: e.transpose(out=psv[:, kk * 128:(kk + 1) * 128],
                                                        in_=hb[:, kk * 128:(kk + 1) * 128], identity=ns.ident[:]),
                     reads=[b_hb, ns.b_ident], writes=[pb], inc=(kk == 7))
            k.op("act", lambda e: e.copy(out=hT[:, :, j * 128:(j + 1) * 128],
                                         in_=psv[:, 0:1024].rearrange("p (k t) -> p k t", t=128)),
                 reads=[pb], writes=[b_hT])
            for hh in range(2):
                psf, pbf = g.ps[6 + hh], g.psb[6 + hh]
                for kk in range(4):
                    kf = hh * 4 + kk
                    k.op("pe", lambda e, kk=kk, kf=kf: e.transpose(out=psf[:, kk * 128:(kk + 1) * 128],
                                                                   in_=tmp[:, kf * 128:(kf + 1) * 128], identity=identf[:]),
                         reads=[b_tmp, b_identf], writes=[pbf], inc=(kk == 3))
                k.op("act", lambda e, hh=hh: e.copy(out=hTf[:, hh * 4:(hh + 1) * 4, :],
                                                    in_=psf[:, :].rearrange("p (k t) -> p k t", t=128)),
                     reads=[pbf], writes=[b_hTf])
            pl, pbl = g.ps[4 + (j % 2)], g.psb[4 + (j % 2)]
            for kk in range(8):
                k.op("pe", lambda e, kk=kk: e.matmul(pl[:, 0:NEXP], lhsT=hTf[:, kk, :], rhs=rw[:, kk, :],
                                                     start=(kk == 0), stop=(kk == 7)),
                     reads=[b_hTf, b_rw], writes=[pbl], inc=(kk == 7))
            L = lg[j % 2]
            bL = b_lg[j % 2]
            k.op("dve", lambda e: e.tensor_tensor(out=L[:, 0:8], in0=pl[:, 0:NEXP], in1=rb[:], op=ALU.add),
                 reads=[pbl, b_rw], writes=[bL])
            k.op("dve", lambda e: e.max(out=L[:, 8:16], in_=L[:, 0:8]), reads=[bL], writes=[bL])
            k.op("dve", lambda e: e.tensor_scalar(out=L[:, 16:17], in0=L[:, 8:9], scalar1=-1.0, scalar2=None,
                                                   op0=ALU.mult), reads=[bL], writes=[bL])
            k.op("act", lambda e: e.activation(out=L[:, 17:18], in_=L[:, 9:10], func=AF.Exp, bias=L[:, 16:17]),
                 reads=[bL], writes=[bL])
            k.op("dve", lambda e: e.tensor_scalar(out=L[:, 20:21], in0=L[:, 17:18], scalar1=1.0, scalar2=None,
                                                   op0=ALU.add), reads=[bL], writes=[bL])
            k.op("dve", lambda e: e.reciprocal(out=L[:, 18:19], in_=L[:, 20:21]), reads=[bL], writes=[bL])
            k.op("dve", lambda e: e.tensor_tensor(out=L[:, 19:20], in0=L[:, 17:18], in1=L[:, 18:19], op=ALU.mult),
                 reads=[bL], writes=[bL])
            k.op("dve", lambda e: e.tensor_scalar(out=L[:, 24:32], in0=L[:, 0:8], scalar1=L[:, 8:9], scalar2=L[:, 18:19],
                                                   op0=ALU.is_equal, op1=ALU.mult), reads=[bL], writes=[bL])
            k.op("dve", lambda e: e.tensor_scalar(out=L[:, 32:40], in0=L[:, 0:8], scalar1=L[:, 9:10], scalar2=L[:, 19:20],
                                                   op0=ALU.is_equal, op1=ALU.mult), reads=[bL], writes=[bL])
            k.op("dve", lambda e: e.tensor_tensor(out=comb[:, j, :], in0=L[:, 24:32], in1=L[:, 32:40], op=ALU.add),
                 reads=[bL], writes=[b_comb])
        for ex in range(NEXP):
            for fg in range(NFG):
                w_i = wi % 2
                wi += 1
                bw = b_w[w_i]
                fs = slice(fg * 512, (fg + 1) * 512)
                k.dma("pool", wg[w_i][:], g.gin("moe_w_in")[ex, :, fg * 512:(fg + 1) * 512].rearrange("(k p) n -> p k n", p=128),
                      writes=[bw])
                k.dma("pool", wu[w_i][:], g.gin("moe_w_in")[ex, :, EDIM + fg * 512:EDIM + (fg + 1) * 512].rearrange("(k p) n -> p k n", p=128),
                      writes=[bw])
                k.dma("pool", wo[w_i][:], g.gin("moe_w_out")[ex, fg * 512:(fg + 1) * 512, :].rearrange("(c p) d -> p c d", p=128),
                      writes=[bw])
                for c4 in range(4):
                    k.op("pool", lambda e, c4=c4: e.tensor_tensor(out=wo[w_i][:, c4, :], in0=wo[w_i][:, c4, :], in1=gatebc[:], op=ALU.mult),
                         reads=[bw, b_mod], writes=[bw])
                for tg in range(4):
                    ts_ = slice(tg * 512, (tg + 1) * 512)
                    for fc in range(4):
                        pg, pu = g.ps[(pi % 2) * 2], g.ps[(pi % 2) * 2 + 1]
                        bg, bu = g.psb[(pi % 2) * 2], g.psb[(pi % 2) * 2 + 1]
                        pi += 1
                        for kk in range(8):
                            k.op("pe", lambda e, kk=kk: e.matmul(pg[:, :], lhsT=wg[w_i][:, kk, fc * 128:(fc + 1) * 128],
                                                                 rhs=hT[:, kk, ts_], start=(kk == 0), stop=(kk == 7)),
                                 reads=[bw, b_hT], writes=[bg], inc=(kk == 7))
                        for kk in range(8):
                            k.op("pe", lambda e, kk=kk: e.matmul(pu[:, :], lhsT=wu[w_i][:, kk, fc * 128:(fc + 1) * 128],
                                                                 rhs=hT[:, kk, ts_], start=(kk == 0), stop=(kk == 7)),
                                 reads=[bw, b_hT], writes=[bu], inc=(kk == 7))
                        s_i = si % 2
                        si += 1
                        k.op("act", lambda e: e.activation(out=sg[s_i][:], in_=pg[:, :], func=AF.Silu),
                             reads=[bg], writes=[b_sg[s_i]])
                        k.op("dve", lambda e: e.tensor_tensor(out=aT[:, fc, ts_], in0=sg[s_i][:], in1=pu[:, :], op=ALU.mult),
                             reads=[b_sg[s_i], bu], writes=[b_aT[tg]])
                for j in range(NTL):
                    for half in range(2):
                        hs = slice(half * 512, (half + 1) * 512)
                        po, pbo = g.ps[4 + (oi % 4)], g.psb[4 + (oi % 4)]
                        to, bto = tmpo[oi % 2], b_to[oi % 2]
                        oi += 1
                        for fc in range(4):
                            k.op("pe", lambda e, fc=fc: e.matmul(po[:, :], lhsT=aT[:, fc, j * 128:(j + 1) * 128],
                                                                 rhs=wo[w_i][:, fc, hs], start=(fc == 0), stop=(fc == 3)),
                                 reads=[b_aT[j // 4], bw], writes=[pbo], inc=(fc == 3))
                        k.op("dve", lambda e: e.scalar_tensor_tensor(out=acc[:, j, hs], in0=po[:, :], scalar=comb[:, j, ex:ex + 1],
                                                                      in1=acc[:, j, hs], op0=ALU.mult, op1=ALU.add),
                             reads=[pbo, b_comb, b_acc[j]], writes=[b_acc[j]])
        for j in range(NTL):
            xt = acc[:, j, :]
            i = ns.i % ns.nbuf
            ns.i += 1
            rstd, b_st = rstd_of(g, ns, i, xt, b_acc[j])
            k.op("dve", lambda e: e.scalar_tensor_tensor(out=xt, in0=xt, scalar=rstd, in1=nfin[:],
                                                          op0=ALU.mult, op1=ALU.mult),
                 reads=[b_acc[j], b_st, b_nfin], writes=[b_acc[j]])
            k.dma("pool", g.out[b, j * 128:(j + 1) * 128, :], xt, reads=[b_acc[j]], writes=[g.bufs["out"]])


_CACHE = {}


def host_consts():
    r = np.arange(128)[:, None]
    c = np.arange(128)[None, :]
    cmat = np.stack([(r <= c), (r > c), (r < c), (r >= c), np.ones((128, 128), bool)]).astype(np.float32)
    rr = np.arange(128)[:, None] // 16
    cc = np.arange(128)[None, :] // 16
    m8 = np.stack([(rr <= cc), (rr >= cc)]).astype(np.float32)
    return {"ident": np.eye(128, dtype=np.float32), "cmat": cmat, "m8": m8}


def s5_lane_tables(inp):
    out = np.empty((2, 3, 128, 32), np.float32)
    for d in range(2):
        for i, a in enumerate((inp["s5_lam_re"][0, d], inp["s5_lam_im"][0, d])):
            out[d, i] = a.reshape(32, 2, 64).transpose(1, 2, 0).reshape(128, 32)
        ls = np.repeat(inp["s5_log_step"][0, d][:, None], 64, axis=1)
        out[d, 2] = ls.reshape(32, 2, 64).transpose(1, 2, 0).reshape(128, 32)
    return out


def s5_c_lanes(inp):
    out = np.empty((2, 2, 128, 32, 16), np.float32)
    for d in range(2):
        for i, a in enumerate((inp["s5_c_re"][0, d], inp["s5_c_im"][0, d])):
            out[d, i] = a.reshape(32, 2, 16, 64).transpose(1, 3, 0, 2).reshape(128, 32, 16)
    return out


def core_inputs(inp, core):
    b0 = core * NB
    cT = np.ascontiguousarray(np.stack([inp["c"][b0], inp["c"][b0 + 1], inp["c_ctx"]], axis=1))
    m = {
        "x": np.ascontiguousarray(inp["x"][b0:b0 + NB]),
        "ctx": np.ascontiguousarray(inp["ctx"][b0:b0 + NB]),
        "cT": cT,
        "ada_w": inp["ada_w"], "ada_b": inp["ada_b"],
        "norm_mix": inp["norm_mix"], "norm_ffn": inp["norm_ffn"],
        "ffn_w_in": inp["ffn_w_in"][0], "ffn_w_out": inp["ffn_w_out"][0],
        "moe_router_w": inp["moe_router_w"][0], "moe_router_b": inp["moe_router_b"].reshape(1, NEXP),
        "moe_w_in": inp["moe_w_in"][0], "moe_w_out": inp["moe_w_out"][0],
        "norm_final": inp["norm_final"].reshape(1, D),
        "ssd_w_in": inp["ssd_w_in"][0],
        "convT": np.ascontiguousarray(np.concatenate([inp["ssd_conv_w"][0], inp["ssd_conv_b"]], axis=0).T),
        "ssd_dt_bias": inp["ssd_dt_bias"].reshape(1, 64), "ssd_a_log": inp["ssd_a_log"].reshape(1, 64),
        "ssd_d": inp["ssd_d"].reshape(1, 32), "ssd_norm": inp["ssd_norm"].reshape(1, 2048),
        "ssd_w_out": inp["ssd_w_out"][0],
        "s5_lamT": s5_lane_tables(inp),
        "s5_b": np.ascontiguousarray(np.stack([inp["s5_b_re"][0], inp["s5_b_im"][0]], axis=1).reshape(2, 2, 4096, 16)),
        "s5_cT": s5_c_lanes(inp),
        "s5_d": inp["s5_d"].reshape(1, D), "s5_w_glu": inp["s5_w_glu"][0], "s5_b_glu": inp["s5_b_glu"].reshape(1, 2 * D),
    }
    m.update(host_consts())
    return m


def kernel(**inp):
    inp = {kk: np.asarray(v) for kk, v in inp.items()}
    if "prog" not in _CACHE:
        _CACHE["prog"] = build_program(("ada", "ssd", "ffn", "s5", "moe"), {})
    nc, g = _CACHE["prog"]
    in_maps = []
    for core in range(8):
        m = core_inputs(inp, core)
        in_maps.append({kk: np.ascontiguousarray(v) for kk, v in m.items() if kk in g.inputs})
    res = run_bass_kernel_spmd(nc, in_maps, core_ids=list(range(8)))
    return np.concatenate([r["out"] for r in res.results], axis=0).astype(np.float32)
```

```python
import numpy as np
from contextlib import ExitStack
import concourse.bass as bass
import concourse.mybir as mybir
from concourse.bass_utils import run_bass_kernel_spmd

F32 = mybir.dt.float32
BF16 = mybir.dt.bfloat16
I32 = mybir.dt.int32
AF = mybir.ActivationFunctionType
ALU = mybir.AluOpType
AX = mybir.AxisListType
AP = bass.AP

D = 1024
NB = 2
T = 2048
TC = 256
EPS = 1e-6
FFN = 2816
NEXP = 8
EDIM = 3584


class Buf:
    __slots__ = ("name", "w", "r", "ps")

    def __init__(self, name="", ps=False):
        self.name = name
        self.w = {}
        self.r = {}
        self.ps = ps


class KB:
    RING = 8

    def __init__(self, nc, es):
        self.nc = nc
        self.es = es
        self.E = dict(pe=nc.tensor, act=nc.scalar, dve=nc.vector, pool=nc.gpsimd, sp=nc.sync)
        self.sem = {}
        self.cnt = {}
        for e in self.E:
            self.sem[e] = es.enter_context(nc.semaphore("s_" + e))
            self.cnt[e] = 0
        self.ring = {}
        self.ring_i = {}
        for q in ("sp", "pool", "act"):
            self.ring[q] = []
            for i in range(self.RING):
                key = ("d", q, i)
                self.sem[key] = es.enter_context(nc.semaphore("d_%s%d" % (q, i)))
                self.cnt[key] = 0
                self.ring[q].append(key)
            self.ring_i[q] = 0
        self.known = {e: {} for e in self.E}
        self.n_wait = 0

    def _deps(self, eng, reads, writes):
        need = {}

        def add(sk, v):
            if v > need.get(sk, 0):
                need[sk] = v

        for b in reads:
            for sk, v in b.w.items():
                if sk == eng and eng == "pe":
                    continue
                add(sk, v)
        for b in writes:
            for sk, v in b.r.items():
                if sk == eng and eng == "pe":
                    continue
                add(sk, v)
            for sk, v in b.w.items():
                if sk == eng and eng == "pe":
                    continue
                add(sk, v)
        kn = self.known[eng]
        out = []
        for sk, v in need.items():
            if kn.get(sk, 0) >= v:
                continue
            kn[sk] = v
            out.append((sk, v))
        return out

    def _emit_waits(self, eng, waits):
        E = self.E[eng]
        for sk, v in waits:
            E.wait_ge(self.sem[sk], v)
            self.n_wait += 1

    def _record(self, ev, reads, writes):
        sk, v = ev
        for b in reads:
            if v > b.r.get(sk, 0):
                b.r[sk] = v
        for b in writes:
            if b.r:
                b.w = {sk: v}
                b.r = {}
            else:
                if v > b.w.get(sk, 0):
                    b.w[sk] = v

    def op(self, eng, fn, reads=(), writes=(), inc=True):
        if any(b.ps for b in reads):
            writes = list(writes) + [b for b in reads if b.ps]
            reads = [b for b in reads if not b.ps]
        self._emit_waits(eng, self._deps(eng, reads, writes))
        ins = fn(self.E[eng])
        if inc:
            self.cnt[eng] += 1
            ins.then_inc(self.sem[eng], 1)
            ev = (eng, self.cnt[eng])
        else:
            ev = (eng, self.cnt[eng] + 1)
        self._record(ev, reads, writes)
        return ins

    def dma(self, q, out, in_, reads=(), writes=()):
        key = self.ring[q][self.ring_i[q] % self.RING]
        self.ring_i[q] += 1
        waits = self._deps(q, reads, writes)
        prev = self.cnt[key]
        if prev > 0 and self.known[q].get(key, 0) < prev:
            self.known[q][key] = prev
            waits.append((key, prev))
        self._emit_waits(q, waits)
        ins = self.E[q].dma_start(out=out, in_=in_)
        self.cnt[key] = prev + 16
        ins.then_inc(self.sem[key], 16)
        self._record((key, prev + 16), reads, writes)
        return ins

    def barrier(self):
        for e in self.E:
            waits = []
            for sk, v in self.cnt.items():
                if v == 0 or sk == e and e == "pe":
                    continue
                if self.known[e].get(sk, 0) >= v:
                    continue
                self.known[e][sk] = v
                waits.append((sk, v))
            self._emit_waits(e, waits)

    def final_wait(self):
        waits = []
        for sk, v in self.cnt.items():
            if v and self.known["sp"].get(sk, 0) < v:
                self.known["sp"][sk] = v
                waits.append((sk, v))
        self._emit_waits("sp", waits)


def rev_ap(ap, n):
    a = ap.ap
    assert len(a) == 2 and a[1][1] == n
    return AP(ap.tensor, ap.offset + (n - 1) * a[1][0], [list(a[0]), [-a[1][0], n]])


class Ctx:
    pass


def build_program(phases, kinds):
    nc = bass.Bass("TRN2", target_bir_lowering=False)
    g = Ctx()
    g.nc = nc
    g.phases = phases
    g.kinds = kinds

    def din(name, shape, dt=F32):
        return nc.dram_tensor(name, list(shape), dt, kind="ExternalInput").ap()

    def dmid(name, shape, dt=F32):
        kind = {"in": "ExternalInput", "out": "ExternalOutput", "int": "Internal"}[kinds.get(name, "int")]
        return nc.dram_tensor(name, list(shape), dt, kind=kind).ap()

    g.inputs = {}
    SH = dict(x=[NB, T, D], ctx=[NB, TC, D], cT=[D, 3], ada_w=[2, D, 6 * D], ada_b=[2, 6 * D],
              norm_mix=[2, D], norm_ffn=[2, D], ffn_w_in=[D, 2 * FFN], ffn_w_out=[FFN, D],
              moe_router_w=[D, NEXP], moe_router_b=[1, NEXP], moe_w_in=[NEXP, D, 2 * EDIM],
              moe_w_out=[NEXP, EDIM, D], norm_final=[1, D], ident=[128, 128],
              ssd_w_in=[D, 5184], convT=[3072, 6], ssd_dt_bias=[1, 64], ssd_a_log=[1, 64], ssd_d=[1, 32],
              ssd_norm=[1, 2048], ssd_w_out=[2048, D], cmat=[5, 128, 128],
              s5_lamT=[2, 3, 128, 32], s5_b=[2, 2, 4096, 16], s5_cT=[2, 2, 128, 32, 16], m8=[2, 128, 128],
              s5_d=[1, D], s5_w_glu=[D, 2 * D], s5_b_glu=[1, 2 * D])
    g.SH = SH

    def gin(name):
        if name not in g.inputs:
            g.inputs[name] = din(name, SH[name])
        return g.inputs[name]
    g.gin = gin
    g.x = gin("x")
    g.ctx = gin("ctx")
    g.mod = dmid("mod", [2, 3, 6 * D])
    g.x1 = dmid("x1", [NB, T, D])
    g.c1 = dmid("c1", [NB, TC, D])
    g.x2 = dmid("x2", [NB, T, D])
    g.c2 = dmid("c2", [NB, TC, D])
    g.x3 = dmid("x3", [NB, T, D])
    NTK = T + TC
    g.xbcT = dmid("xbcT", [NB, 3072, NTK], BF16)
    g.zs = dmid("zs", [NB, NTK, 2048], BF16)
    g.dd = dmid("dd", [NB, NTK, 128])
    g.yf = dmid("yf", [NB, NTK, 2048])
    g.s5w = dmid("s5w", [2, 32, 128, 6, 128], BF16)
    g.etab = dmid("etab", [2, 2, 128, 32, 288])
    g.rho = dmid("rho", [2, 128, 32])
    g.out = nc.dram_tensor("out", [NB, T, D], F32, kind="ExternalOutput").ap()
    g.bufs = {n: Buf(n) for n in ("x", "ctx", "mod", "x1", "c1", "x2", "c2", "x3", "out", "xbcT", "zs", "dd", "yf", "s5w", "etab", "rho")}

    with ExitStack() as es:
        k = KB(nc, es)
        g.k = k
        g.ps = [es.enter_context(nc.psum_tensor("ps%d" % i, [128, 512], F32)) for i in range(8)]
        g.psb = [Buf("ps%d" % i, ps=True) for i in range(8)]
        for ph in phases:
            with ExitStack() as pes:
                g.pes = pes
                {"ada": phase_ada, "ffn": phase_ffn, "moe": phase_moe, "ssd": phase_ssd, "s5": phase_s5}[ph](g)
                k.barrier()
        k.final_wait()
    g.kinds = kinds
    return nc, g


def sbt(g, name, shape, dt):
    return g.pes.enter_context(g.nc.sbuf_tensor(name, list(shape), dt))


def bcast_rows(ap_row, nparts):
    a = ap_row.ap
    return AP(ap_row.tensor, ap_row.offset, [[0, nparts]] + [list(x) for x in a[1:]])


def phase_ada(g):
    k, nc = g.k, g.nc
    cT = sbt(g, "a_cT", [128, 8, 3], F32)
    cs = sbt(g, "a_cs", [128, 8, 3], BF16)
    row = sbt(g, "a_row", [3, 6 * D], F32)
    bias = sbt(g, "a_bias", [3, 6 * D], F32)
    nrm = sbt(g, "a_nrm", [3, D], F32)
    wts = [sbt(g, "a_w%d" % i, [128, 8, 512], BF16) for i in range(2)]
    b_cT, b_cs, b_row, b_bias, b_nrm = Buf(), Buf(), Buf(), Buf(), Buf()
    b_w = [Buf(), Buf()]
    k.dma("sp", cT[:], g.gin("cT").rearrange("(k p) m -> p k m", p=128), writes=[b_cT])
    k.op("act", lambda e: e.activation(out=cs[:], in_=cT[:], func=AF.Silu), reads=[b_cT], writes=[b_cs])
    it = 0
    for layer in range(2):
        k.dma("sp", bias[:], bcast_rows(g.gin("ada_b")[layer:layer + 1, :], 3), writes=[b_bias])
        for j in range(12):
            w = wts[it % 2]
            bw = b_w[it % 2]
            it += 1
            k.dma("pool", w[:], g.gin("ada_w")[layer, :, j * 512:(j + 1) * 512].rearrange("(k p) n -> p k n", p=128), writes=[bw])
            ps = g.ps[it % 2]
            pb = g.psb[it % 2]
            for kk in range(8):
                k.op("pe", lambda e, kk=kk: e.matmul(ps[0:3, :], lhsT=cs[:, kk, :], rhs=w[:, kk, :],
                                                     start=(kk == 0), stop=(kk == 7)),
                     reads=[b_cs, bw], writes=[pb], inc=(kk == 7))
            k.op("dve", lambda e: e.tensor_tensor(out=row[:, j * 512:(j + 1) * 512], in0=ps[0:3, :],
                                                   in1=bias[:, j * 512:(j + 1) * 512], op=ALU.add),
                 reads=[pb, b_bias], writes=[b_row])
        for slot, nw in ((1, g.gin("norm_mix")), (4, g.gin("norm_ffn"))):
            k.dma("sp", nrm[:], bcast_rows(nw[layer:layer + 1, :], 3), writes=[b_nrm])
            k.op("dve", lambda e, slot=slot: e.scalar_tensor_tensor(
                out=row[:, slot * D:(slot + 1) * D], in0=row[:, slot * D:(slot + 1) * D], scalar=1.0,
                in1=nrm[:], op0=ALU.add, op1=ALU.mult), reads=[b_row, b_nrm], writes=[b_row])
        k.dma("sp", g.mod[layer], row[:], reads=[b_row], writes=[g.bufs["mod"]])


class NormSet:
    def __init__(self, g, pfx, nbuf=2):
        self.g = g
        self.junk = sbt(g, pfx + "junk", [128, 2 * D], BF16)
        self.b_junk = Buf()
        self.st = [sbt(g, pfx + "st%d" % i, [128, 4], F32) for i in range(nbuf)]
        self.b_st = [Buf() for _ in range(nbuf)]
        self.tmp = [sbt(g, pfx + "tmp0", [128, D], F32)] * nbuf
        self.b_tmp = [Buf()] * nbuf
        self.hb = [sbt(g, pfx + "hb%d" % i, [128, D], BF16) for i in range(nbuf)]
        self.b_hb = [Buf() for _ in range(nbuf)]
        self.ident = sbt(g, pfx + "ident", [128, 128], BF16)
        self.b_ident = Buf()
        g.k.dma("pool", self.ident[:], g.gin("ident")[:, :], writes=[self.b_ident])
        self.i = 0
        self.nbuf = nbuf


def rstd_of(g, ns, i, xt, b_xt, dim=D):
    k = g.k
    P = xt.ap[0][1]
    st, b_st = ns.st[i], ns.b_st[i]
    k.op("act", lambda e: e.activation(out=ns.junk[0:P, 0:dim], in_=xt, func=AF.Square, accum_out=st[0:P, 0:1]),
         reads=[b_xt], writes=[ns.b_junk, b_st])
    k.op("dve", lambda e: e.tensor_scalar(out=st[0:P, 1:2], in0=st[0:P, 0:1], scalar1=1.0 / dim, scalar2=EPS,
                                           op0=ALU.mult, op1=ALU.add), reads=[b_st], writes=[b_st])
    k.op("act", lambda e: e.sqrt(out=st[0:P, 2:3], in_=st[0:P, 1:2]), reads=[b_st], writes=[b_st])
    k.op("dve", lambda e: e.reciprocal(out=st[0:P, 3:4], in_=st[0:P, 2:3]), reads=[b_st], writes=[b_st])
    return st[0:P, 3:4], b_st


def norm_mod_T(g, ns, xt, b_xt, gbc, shbc, b_mod, hT_dst, b_hT, ps, pb):
    k = g.k
    i = ns.i % ns.nbuf
    ns.i += 1
    rstd, b_st = rstd_of(g, ns, i, xt, b_xt)
    tmp, b_tmp, hb, b_hb = ns.tmp[i], ns.b_tmp[i], ns.hb[i], ns.b_hb[i]
    k.op("dve", lambda e: e.scalar_tensor_tensor(out=tmp[:], in0=xt, scalar=rstd, in1=gbc,
                                                  op0=ALU.mult, op1=ALU.mult),
         reads=[b_xt, b_st, b_mod], writes=[b_tmp])
    k.op("pool", lambda e: e.tensor_tensor(out=hb[:], in0=tmp[:], in1=shbc, op=ALU.add),
         reads=[b_tmp, b_mod], writes=[b_hb])
    psv = ps.bitcast(BF16)
    for kk in range(8):
        k.op("pe", lambda e, kk=kk: e.transpose(out=psv[:, kk * 128:(kk + 1) * 128],
                                                in_=hb[:, kk * 128:(kk + 1) * 128], identity=ns.ident[:]),
             reads=[b_hb, ns.b_ident], writes=[pb], inc=(kk == 7))
    k.op("act", lambda e: e.copy(out=hT_dst, in_=psv[:, 0:1024].rearrange("p (k t) -> p k t", t=128)),
         reads=[pb], writes=[b_hT])
    return hb, b_hb


class ModBC:
    def __init__(self, g, pfx, layer, slots):
        self.g, self.layer, self.slots = g, layer, slots
        self.t = [sbt(g, "%s_m%d" % (pfx, s), [128, D], F32) for s in slots]
        self.b = Buf()
        self.cond = None

    def get(self, cond):
        g = self.g
        if cond != self.cond:
            self.cond = cond
            for t, s in zip(self.t, self.slots):
                g.k.dma("sp", t[:], bcast_rows(g.mod[self.layer, cond:cond + 1, s * D:(s + 1) * D], 128),
                        reads=[g.bufs["mod"]], writes=[self.b])
        return self.t, self.b


def seq_list(g, xsrc, csrc, xdst, cdst, with_ctx=True):
    L = []
    for b in range(NB):
        if with_ctx:
            L.append((g.__dict__[csrc][b], g.__dict__[cdst][b], 2, TC, g.bufs.get(csrc), g.bufs.get(cdst)))
        L.append((g.__dict__[xsrc][b], g.__dict__[xdst][b], b, T, g.bufs.get(xsrc), g.bufs.get(xdst)))
    return L


def phase_ffn(g):
    k, nc = g.k, g.nc
    src_x, src_c = ("x1", "c1") if "ssd" in g.phases else ("x", "ctx")
    NT = 256
    NF = FFN // 128
    w1 = sbt(g, "f_w1", [128, 8, 2 * FFN], BF16)
    w2 = sbt(g, "f_w2", [128, NF, D], BF16)
    b_w1, b_w2 = Buf(), Buf()
    for kk in range(8):
        k.dma("pool", w1[:, kk, :], g.gin("ffn_w_in")[kk * 128:(kk + 1) * 128, :], writes=[b_w1])
    for f in range(NF):
        k.dma("pool", w2[:, f, :], g.gin("ffn_w_out")[f * 128:(f + 1) * 128, :], writes=[b_w2])
    ns = NormSet(g, "f_")
    hT = [sbt(g, "f_hT%d" % i, [128, 8, NT], BF16) for i in range(2)]
    b_hT = [Buf(), Buf()]
    aT = [sbt(g, "f_aT0", [128, NF, NT], BF16)] * 2
    b_aT = [Buf()] * 2
    xt = [sbt(g, "f_xt%d" % i, [128, D], F32) for i in range(4)]
    b_xt = [Buf() for _ in range(4)]
    sg = [sbt(g, "f_sg%d" % i, [128, NT], F32) for i in range(2)]
    b_sg = [Buf(), Buf()]
    sg_o = [sbt(g, "f_o%d" % i, [128, 512], F32) for i in range(2)]
    b_sgo = [Buf(), Buf()]
    mods = ModBC(g, "f", 0, (3, 4, 5))
    grp = 0
    oi = 0
    fi = 0
    LIM, STAGE = 1000, 9
    for (src, dst, cond, ntok, bsrc, bdst) in seq_list(g, src_x, src_c, "x2", "c2")[:LIM]:
        (shbc, gbc, gatebc), b_mod = mods.get(cond)
        for t0 in range(0, ntok, NT):
            hi = grp % 2
            for j in range(NT // 128):
                xi = (grp % 2) * 2 + j
                k.dma("sp", xt[xi][:], src[t0 + j * 128:t0 + (j + 1) * 128, :], reads=[bsrc], writes=[b_xt[xi]])
                norm_mod_T(g, ns, xt[xi][:], b_xt[xi], gbc[:], shbc[:], b_mod,
                           hT[hi][:, :, j * 128:(j + 1) * 128], b_hT[hi], g.ps[4 + j], g.psb[4 + j])
            for f in range(NF if STAGE >= 2 else 0):
                pg, pu = g.ps[(fi % 2) * 2], g.ps[(fi % 2) * 2 + 1]
                bg, bu = g.psb[(fi % 2) * 2], g.psb[(fi % 2) * 2 + 1]
                si = fi % 2
                fi += 1
                for kk in range(8):
                    k.op("pe", lambda e, kk=kk: e.matmul(pg[:, 0:NT], lhsT=w1[:, kk, f * 128:(f + 1) * 128],
                                                         rhs=hT[hi][:, kk, :], start=(kk == 0), stop=(kk == 7)),
                         reads=[b_w1, b_hT[hi]], writes=[bg], inc=(kk == 7))
                for kk in range(8):
                    k.op("pe", lambda e, kk=kk: e.matmul(pu[:, 0:NT], lhsT=w1[:, kk, FFN + f * 128:FFN + (f + 1) * 128],
                                                         rhs=hT[hi][:, kk, :], start=(kk == 0), stop=(kk == 7)),
                         reads=[b_w1, b_hT[hi]], writes=[bu], inc=(kk == 7))
                k.op("act", lambda e: e.activation(out=sg[si][:], in_=pg[:, 0:NT], func=AF.Silu),
                     reads=[bg], writes=[b_sg[si]])
                k.op("dve", lambda e: e.tensor_tensor(out=aT[hi][:, f, :], in0=sg[si][:], in1=pu[:, 0:NT], op=ALU.mult),
                     reads=[b_sg[si], bu], writes=[b_aT[hi]])
            for j in range(NT // 128):
                xi = (grp % 2) * 2 + j
                o = sg_o[oi % 2]
                bo = b_sgo[oi % 2]
                oi += 1
                for half in range(2):
                    po, pbo = g.ps[6 + half], g.psb[6 + half]
                    for f in range(NF):
                        k.op("pe", lambda e, f=f: e.matmul(po[:, :], lhsT=aT[hi][:, f, j * 128:(j + 1) * 128],
                                                           rhs=w2[:, f, half * 512:(half + 1) * 512],
                                                           start=(f == 0), stop=(f == NF - 1)),
                             reads=[b_aT[hi], b_w2], writes=[pbo], inc=(f == NF - 1))
                    hs = slice(half * 512, (half + 1) * 512)
                    o = sg_o[oi % 2]
                    bo = b_sgo[oi % 2]
                    oi += 1
                    k.op("dve", lambda e: e.tensor_tensor(out=o[:], in0=po[:, :], in1=gatebc[:, hs], op=ALU.mult),
                         reads=[pbo, b_mod], writes=[bo])
                    k.op("pool", lambda e: e.tensor_tensor(out=xt[xi][:, hs], in0=o[:], in1=xt[xi][:, hs], op=ALU.add),
                         reads=[bo, b_xt[xi]], writes=[b_xt[xi]])
                k.dma("pool", dst[t0 + j * 128:t0 + (j + 1) * 128, :], xt[xi][:], reads=[b_xt[xi]], writes=[bdst])
            grp += 1


NTK = T + TC


def phase_ssd(g):
    with ExitStack() as pes:
        g.pes = pes
        ssd_in(g)
        g.k.barrier()
    with ExitStack() as pes:
        g.pes = pes
        ssd_scan(g)
        g.k.barrier()


def ssd_in(g):
    k, nc = g.k, g.nc
    w = sbt(g, "s_w", [128, 8, 5184], BF16)
    b_w = Buf()
    for kk in range(8):
        k.dma("pool", w[:, kk, :], g.gin("ssd_w_in")[kk * 128:(kk + 1) * 128, :], writes=[b_w])
    cw = sbt(g, "s_cw", [128, 24, 6], F32)
    b_cw = Buf()
    k.dma("sp", cw[:], g.gin("convT").rearrange("(c p) k -> p c k", p=128), writes=[b_cw])
    dtb = sbt(g, "s_dtb", [128, 64], F32)
    abc = sbt(g, "s_abc", [128, 64], F32)
    b_dtb, b_abc = Buf(), Buf()
    k.dma("sp", dtb[:], bcast_rows(g.gin("ssd_dt_bias")[0:1, :], 128), writes=[b_dtb])
    k.dma("sp", abc[:], bcast_rows(g.gin("ssd_a_log")[0:1, :], 128), writes=[b_abc])
    k.op("act", lambda e: e.activation(out=abc[:], in_=abc[:], func=AF.Exp), reads=[b_abc], writes=[b_abc])
    k.op("dve", lambda e: e.tensor_scalar(out=abc[:], in0=abc[:], scalar1=-1.0, scalar2=None, op0=ALU.mult),
         reads=[b_abc], writes=[b_abc])
    ns = NormSet(g, "s_")
    hT = sbt(g, "s_hT", [128, 8, NTK], BF16)
    b_hT = Buf()
    xt = [sbt(g, "s_xt%d" % i, [128, D], F32) for i in range(2)]
    b_xt = [Buf(), Buf()]
    pre = [sbt(g, "s_pre%d" % i, [128, 2312], F32) for i in range(2)]
    b_pre = [Buf(), Buf()]
    for p_ in pre:
        k.op("pool", lambda e, p_=p_: e.memset(p_[:], 0.0), writes=[b_pre[0], b_pre[1]])
    accv = sbt(g, "s_acc", [128, NTK], F32)
    b_accv = Buf()
    xo = [sbt(g, "s_xo%d" % i, [128, NTK], BF16) for i in range(2)]
    b_xo = [Buf(), Buf()]
    zt = [sbt(g, "s_zt%d" % i, [128, 2048], BF16) for i in range(2)]
    b_zt = [Buf(), Buf()]
    ddt = [sbt(g, "s_dd%d" % i, [128, 192], F32) for i in range(2)]
    b_ddt = [Buf(), Buf()]
    mods = ModBC(g, "s", 0, (0, 1))
    xi = 0
    pi = 0
    for b in range(NB):
        for (src, cond, ntok, off, bsrc) in ((g.ctx[b], 2, TC, 0, g.bufs["ctx"]), (g.x[b], b, T, TC, g.bufs["x"])):
            (shbc, gbc), b_mod = mods.get(cond)
            for j in range(ntok // 128):
                x_ = xt[xi % 2]
                bx = b_xt[xi % 2]
                k.dma("sp", x_[:], src[j * 128:(j + 1) * 128, :], reads=[bsrc], writes=[bx])
                norm_mod_T(g, ns, x_[:], bx, gbc[:], shbc[:], b_mod,
                           hT[:, :, off + j * 128:off + (j + 1) * 128], b_hT, g.ps[6 + xi % 2], g.psb[6 + xi % 2])
                xi += 1
        for ct in range(24):
            pr, bpr = pre[ct % 2], b_pre[ct % 2]
            for (c0, c1) in ((0, 256), (256, 768), (768, 1280), (1280, 1792), (1792, 2304)):
                ps, pb = g.ps[pi % 4], g.psb[pi % 4]
                pi += 1
                n = c1 - c0
                for kk in range(8):
                    k.op("pe", lambda e, kk=kk: e.matmul(ps[:, 0:n], lhsT=w[:, kk, 2048 + ct * 128:2048 + (ct + 1) * 128],
                                                         rhs=hT[:, kk, c0:c1], start=(kk == 0), stop=(kk == 7)),
                         reads=[b_w, b_hT], writes=[pb], inc=(kk == 7))
                po = 2 + c0 if c0 < 256 else c0 + 6
                k.op("act", lambda e: e.copy(out=pr[:, po:po + n], in_=ps[:, 0:n]), reads=[pb], writes=[bpr])
            for (a0, n, p0) in ((0, 256, 0), (256, 2048, 260)):
                k.op("dve", lambda e: e.tensor_scalar(out=accv[:, a0:a0 + n], in0=pr[:, p0:p0 + n],
                                                       scalar1=cw[:, ct, 0:1], scalar2=None, op0=ALU.mult),
                     reads=[bpr, b_cw], writes=[b_accv])
                for q in range(1, 5):
                    k.op("dve", lambda e, q=q: e.scalar_tensor_tensor(out=accv[:, a0:a0 + n], in0=pr[:, p0 + q:p0 + q + n],
                                                                       scalar=cw[:, ct, q:q + 1], in1=accv[:, a0:a0 + n],
                                                                       op0=ALU.mult, op1=ALU.add),
                         reads=[bpr, b_cw, b_accv], writes=[b_accv])
            o_, bo = xo[ct % 2], b_xo[ct % 2]
            k.op("act", lambda e: e.activation(out=o_[:], in_=accv[:], func=AF.Silu, bias=cw[:, ct, 5:6]),
                 reads=[b_accv, b_cw], writes=[bo])
            k.dma("pool", g.xbcT[b, ct * 128:(ct + 1) * 128, :], o_[:], reads=[bo], writes=[g.bufs["xbcT"]])
        for j in range(NTK // 128):
            z_, bz = zt[j % 2], b_zt[j % 2]
            for n4 in range(4):
                ps, pb = g.ps[pi % 4], g.psb[pi % 4]
                pi += 1
                for kk in range(8):
                    k.op("pe", lambda e, kk=kk: e.matmul(ps[:, :], lhsT=hT[:, kk, j * 128:(j + 1) * 128],
                                                         rhs=w[:, kk, n4 * 512:(n4 + 1) * 512], start=(kk == 0), stop=(kk == 7)),
                         reads=[b_w, b_hT], writes=[pb], inc=(kk == 7))
                k.op("act", lambda e: e.activation(out=z_[:, n4 * 512:(n4 + 1) * 512], in_=ps[:, :], func=AF.Silu),
                     reads=[pb], writes=[bz])
            k.dma("pool", g.zs[b, j * 128:(j + 1) * 128, :], z_[:], reads=[bz], writes=[g.bufs["zs"]])
            ps, pb = g.ps[pi % 4], g.psb[pi % 4]
            pi += 1
            for kk in range(8):
                k.op("pe", lambda e, kk=kk: e.matmul(ps[:, 0:64], lhsT=hT[:, kk, j * 128:(j + 1) * 128],
                                                     rhs=w[:, kk, 5120:5184], start=(kk == 0), stop=(kk == 7)),
                     reads=[b_w, b_hT], writes=[pb], inc=(kk == 7))
            d_, bd = ddt[j % 2], b_ddt[j % 2]
            k.op("dve", lambda e: e.tensor_tensor(out=d_[:, 128:192], in0=ps[:, 0:64], in1=dtb[:], op=ALU.add),
                 reads=[pb, b_dtb], writes=[bd])
            k.op("act", lambda e: e.activation(out=d_[:, 128:192], in_=d_[:, 128:192], func=AF.Exp), reads=[bd], writes=[bd])
            k.op("act", lambda e: e.activation(out=d_[:, 0:64], in_=d_[:, 128:192], func=AF.Ln, bias=1.0), reads=[bd], writes=[bd])
            k.op("dve", lambda e: e.tensor_tensor(out=d_[:, 64:128], in0=d_[:, 0:64], in1=abc[:], op=ALU.mult),
                 reads=[bd, b_abc], writes=[bd])
            k.dma("pool", g.dd[b, j * 128:(j + 1) * 128, :], d_[:, 0:128], reads=[bd], writes=[g.bufs["dd"]])


def bc3(ap2, n_in, n_rep):
    a = ap2.ap
    return AP(ap2.tensor, ap2.offset, [list(a[0]), [a[1][0], n_in], [0, n_rep]])


def bmid(ap2, n_rep):
    a = ap2.ap
    return AP(ap2.tensor, ap2.offset, [list(a[0]), [0, n_rep], [a[1][0], a[1][1]]])


def ssd_scan(g):
    k, nc = g.k, g.nc
    cm = sbt(g, "q_cm", [128, 5, 128], F32)
    b_cm = Buf()
    k.dma("sp", cm[:], g.gin("cmat").rearrange("m p c -> p m c"), writes=[b_cm])
    cmb = sbt(g, "q_cmb", [128, 5, 128], BF16)
    k.op("dve", lambda e: e.tensor_copy(out=cmb[:], in_=cm[:]), reads=[b_cm], writes=[b_cm])
    identb = sbt(g, "q_identb", [128, 128], BF16)
    b_id = Buf()
    k.dma("pool", identb[:], g.gin("ident")[:, :], writes=[b_id])
    wout = sbt(g, "q_wout", [128, 16, D], BF16)
    b_wout = Buf()
    for c in range(16):
        k.dma("pool", wout[:, c, :], g.gin("ssd_w_out")[c * 128:(c + 1) * 128, :], writes=[b_wout])
    nwbc = sbt(g, "q_nw", [128, 2048], F32)
    dsk = sbt(g, "q_dsk", [128, 32], F32)
    b_nw = Buf()
    k.dma("sp", nwbc[:], bcast_rows(g.gin("ssd_norm")[0:1, :], 128), writes=[b_nw])
    k.dma("sp", dsk[:], bcast_rows(g.gin("ssd_d")[0:1, :], 128), writes=[b_nw])
    gate = {}
    S_all = [sbt(g, "q_S%d" % i, [128, 4, 512], F32) for i in range(NB)]
    Sb_all = [sbt(g, "q_Sb%d" % i, [128, 4, 512], BF16) for i in range(NB)]
    b_S_all = [[Buf() for _ in range(4)] for _ in range(NB)]
    b_Sb_all = [[Buf() for _ in range(4)] for _ in range(NB)]
    FM = [sbt(g, "q_FM%d" % i, [128, 24, 128], BF16) for i in range(2)]
    b_FM = [Buf(), Buf()]
    ddT = [sbt(g, "q_dd%d" % i, [128, 128], F32) for i in range(2)]
    b_dd = [Buf(), Buf()]
    Xtok = [sbt(g, "q_X%d" % i, [128, 2048], BF16) for i in range(2)]
    b_X = [Buf(), Buf()]
    Btok = [sbt(g, "q_B%d" % i, [128, 512], BF16) for i in range(2)]
    b_B = [Buf(), Buf()]
    cue = [sbt(g, "q_cue%d" % i, [128, 128], F32) for i in range(2)]
    b_cue = [Buf(), Buf()]
    xdt = sbt(g, "q_xdt", [128, 2048], BF16)
    xdtu = sbt(g, "q_xdtu", [128, 2048], BF16)
    b_xdt, b_xdtu = Buf(), Buf()
    sm = [sbt(g, "q_sm%d" % i, [128, 128], BF16) for i in range(2)]
    b_sm = [Buf(), Buf()]
    lh4 = [sbt(g, "q_lh%d" % i, [128, 4, 128], BF16) for i in range(2)]
    b_lh4 = [Buf(), Buf()]
    Em4 = [sbt(g, "q_E%d" % i, [128, 512], BF16) for i in range(2)]
    b_E4 = [Buf(), Buf()]
    Lm4 = [sbt(g, "q_L%d" % i, [128, 512], BF16) for i in range(2)]
    b_L4 = [Buf(), Buf()]
    BURST = 8
    t1 = [sbt(g, "q_t1%d" % i, [128, 512], F32) for i in range(2)]
    b_t1 = [Buf(), Buf()]
    yblk = [sbt(g, "q_y%d" % i, [128, 2048], F32) for i in range(2)]
    b_y = [Buf(), Buf()]
    yfl = sbt(g, "q_yfl", [128, 2048], F32)
    b_yfl = Buf()
    ztl = sbt(g, "q_zt", [128, 2048], BF16)
    b_ztl = Buf()
    ygn = sbt(g, "q_ygn", [128, 2048], BF16)
    b_ygn = Buf()
    ygT = sbt(g, "q_ygT", [128, 16, 128], BF16)
    b_ygT = Buf()
    xt = [sbt(g, "q_xt%d" % i, [128, D], F32) for i in range(2)]
    b_xt = [Buf(), Buf()]
    ot = [sbt(g, "q_ot%d" % i, [128, 512], F32) for i in range(2)]
    b_ot = [Buf(), Buf()]
    gbc_all = [[sbt(g, "q_g%d_%d" % (b_, i), [128, D], F32) for i in range(2)] for b_ in range(NB)]
    b_g = Buf()
    ns = NormSet(g, "q_", nbuf=1)
    sc_slots = [(g.ps[0][:, i * 128:(i + 1) * 128], g.psb[0]) for i in range(3)]
    cum_ps, b_cum = g.ps[0][:, 384:480], g.psb[0]
    d_slots = [(g.ps[1 + i // 4][:, (i % 4) * 128:(i % 4 + 1) * 128], g.psb[1 + i // 4]) for i in range(8)]
    tr_b = [g.psb[6], g.psb[7]]
    cnt = dict(blk=0, sc=0, d=0, h=0, t1=0, o=0)

    for b in range(NB):
        k.dma("sp", gbc_all[b][0][:], bcast_rows(g.mod[0, 2:3, 2 * D:3 * D], 128), reads=[g.bufs["mod"]], writes=[b_g])
        k.dma("sp", gbc_all[b][1][:], bcast_rows(g.mod[0, b:b + 1, 2 * D:3 * D], 128), reads=[g.bufs["mod"]], writes=[b_g])
    for dr in range(2):
        order = list(range(18)) if dr == 0 else [1, 0] + list(range(17, 1, -1))
        CUMM, GL, RM = (0, 1, 0) if dr == 0 else (3, 2, 3)
        for b in range(NB):
            for gi in range(4):
                k.op("pool", lambda e, gi=gi, b=b: e.memset(S_all[b][:, gi, :], 0.0), writes=[b_S_all[b][gi]])
                k.op("pool", lambda e, gi=gi, b=b: e.memset(Sb_all[b][:, gi, :], 0.0), writes=[b_Sb_all[b][gi]])
        for blk in order:
            for b in range(NB):
                S, Sb, b_S, b_Sb, gbc = S_all[b], Sb_all[b], b_S_all[b], b_Sb_all[b], gbc_all[b]
                c0 = blk * 128
                bi = cnt["blk"] % 2
                cnt["blk"] += 1
                fm, bfm, dT, bdT = FM[bi], b_FM[bi], ddT[bi], b_dd[bi]
                X, bX, Bt, bB, cu, bcu = Xtok[bi], b_X[bi], Btok[bi], b_B[bi], cue[bi], b_cue[bi]
                yb, byb = yblk[bi], b_y[bi]
                k.dma("sp", fm[:], g.xbcT[b, :, c0:c0 + 128].rearrange("(c p) t -> p c t", p=128),
                      reads=[g.bufs["xbcT"]], writes=[bfm])
                k.dma("sp", dT[:], g.dd[b, c0:c0 + 128, :], reads=[g.bufs["dd"]], writes=[bdT])
                for hh in range(2):
                    psv = g.ps[6 + hh].bitcast(BF16)
                    for c in range(8):
                        k.op("pe", lambda e, c=c: e.transpose(out=psv[:, c * 128:(c + 1) * 128], in_=fm[:, hh * 8 + c, :],
                                                              identity=identb[:]),
                             reads=[bfm, b_id], writes=[tr_b[hh]], inc=(c == 7))
                    k.op("act", lambda e: e.copy(out=X[:, hh * 1024:(hh + 1) * 1024], in_=psv[:, 0:1024]),
                         reads=[tr_b[hh]], writes=[bX])
                psv = g.ps[6].bitcast(BF16)
                for c in range(4):
                    k.op("pe", lambda e, c=c: e.transpose(out=psv[:, c * 128:(c + 1) * 128], in_=fm[:, 16 + c, :],
                                                          identity=identb[:]),
                         reads=[bfm, b_id], writes=[tr_b[0]], inc=(c == 3))
                k.op("act", lambda e: e.copy(out=Bt[:], in_=psv[:, 0:512]), reads=[tr_b[0]], writes=[bB])
                da = dT[:, 64 + 32 * dr:96 + 32 * dr]
                dtc = dT[:, 32 * dr:32 * dr + 32]
                for q, mi in enumerate((CUMM, GL, 4)):
                    k.op("pe", lambda e, q=q, mi=mi: e.matmul(cum_ps[:, q * 32:(q + 1) * 32], lhsT=cm[:, mi, :], rhs=da,
                                                              start=True, stop=True),
                         reads=[b_cm, bdT], writes=[b_cum], inc=(q == 2))
                k.op("act", lambda e: e.activation(out=cu[:, 0:96], in_=cum_ps, func=AF.Exp), reads=[b_cum], writes=[bcu])
                k.op("dve", lambda e: e.tensor_tensor(out=cu[:, 96:128], in0=cu[:, 32:64], in1=dtc, op=ALU.mult),
                     reads=[bcu, bdT], writes=[bcu])
                X3 = X[:].rearrange("p (h q) -> p h q", q=64)
                k.op("pool", lambda e: e.tensor_tensor(out=xdt[:].rearrange("p (h q) -> p h q", q=64), in0=X3,
                                                        in1=bc3(dtc, 32, 64), op=ALU.mult),
                     reads=[bX, bdT], writes=[b_xdt])
                k.op("pool", lambda e: e.tensor_tensor(out=xdtu[:].rearrange("p (h q) -> p h q", q=64), in0=X3,
                                                        in1=bc3(cu[:, 96:128], 32, 64), op=ALU.mult),
                     reads=[bX, bcu], writes=[b_xdtu])
                smis = {}

                def group_pre(gi):
                    ps_s, b_ps_s = sc_slots[cnt["sc"] % 3]
                    smi = cnt["sc"] % 2
                    cnt["sc"] += 1
                    smis[gi] = smi
                    k.op("pe", lambda e: e.matmul(ps_s, lhsT=fm[:, 16 + gi, :], rhs=fm[:, 20 + gi, :], start=True, stop=True),
                         reads=[bfm], writes=[b_ps_s])
                    k.op("dve", lambda e: e.tensor_tensor(out=sm[smi][:], in0=ps_s, in1=cm[:, RM, :], op=ALU.mult),
                         reads=[b_ps_s, b_cm], writes=[b_sm[smi]])

                def front(i):
                    gi, half = divmod(i, 2)
                    par = i % 2
                    h0 = gi * 8 + half * 4
                    k.op("dve", lambda e: e.tensor_tensor(out=lh4[par][:], in0=bmid(cm[:, GL, :], 4), in1=bc3(da[:, h0:h0 + 4], 4, 128),
                                                           op=ALU.mult),
                         reads=[b_cm, bdT], writes=[b_lh4[par]])
                    psD4, b_psD = g.ps[1 + par], g.psb[1 + par]
                    for j in range(4):
                        k.op("pe", lambda e, j=j: e.matmul(psD4[:, j * 128:(j + 1) * 128], lhsT=lh4[par][:, j, :], rhs=cmb[:, RM, :],
                                                           start=True, stop=True),
                             reads=[b_lh4[par], b_cm], writes=[b_psD], inc=(j == 3))

                def back(i):
                    gi, half = divmod(i, 2)
                    par = i % 2
                    smi = smis[gi]
                    yd, b_yd = g.ps[3], g.psb[3]
                    psD4, b_psD = g.ps[1 + par], g.psb[1 + par]
                    k.op("act", lambda e: e.activation(out=Em4[par][:], in_=psD4[:, :], func=AF.Exp), reads=[b_psD], writes=[b_E4[par]])
                    k.op("dve", lambda e: e.tensor_tensor(out=Lm4[par][:].rearrange("p (h c) -> p h c", c=128),
                                                           in0=Em4[par][:].rearrange("p (h c) -> p h c", c=128),
                                                           in1=bmid(sm[smi][:], 4), op=ALU.mult),
                         reads=[b_E4[par], b_sm[smi]], writes=[b_L4[par]])
                    for j in range(4):
                        h8 = half * 4 + j
                        h = gi * 8 + h8
                        k.op("pe", lambda e, h=h, h8=h8, j=j: e.matmul(yd[:, h8 * 64:(h8 + 1) * 64], lhsT=Lm4[par][:, j * 128:(j + 1) * 128],
                                                                      rhs=xdt[:, h * 64:(h + 1) * 64], start=True, stop=True),
                             reads=[b_L4[par], b_xdt], writes=[b_yd], inc=(j == 3))

                def group_tail(gi):
                    yd, b_yd = g.ps[3], g.psb[3]
                    yo, b_yo = g.ps[4], g.psb[4]
                    k.op("pe", lambda e: e.matmul(yo[:, :], lhsT=fm[:, 20 + gi, :], rhs=Sb[:, gi, :], start=True, stop=True),
                         reads=[bfm, b_Sb[gi]], writes=[b_yo])
                    ti = cnt["t1"] % 2
                    cnt["t1"] += 1
                    k.op("dve", lambda e: e.tensor_tensor(out=t1[ti][:].rearrange("p (h q) -> p h q", q=64),
                                                           in0=yo[:, :].rearrange("p (h q) -> p h q", q=64),
                                                           in1=bc3(cu[:, gi * 8:gi * 8 + 8], 8, 64), op=ALU.mult),
                         reads=[b_yo, bcu], writes=[b_t1[ti]])
                    k.op("dve", lambda e: e.tensor_tensor(out=yb[:, gi * 512:(gi + 1) * 512], in0=t1[ti][:], in1=yd[:, :], op=ALU.add),
                         reads=[b_t1[ti], b_yd], writes=[byb])
                    cs, b_cs = g.ps[5], g.psb[5]
                    k.op("pe", lambda e: e.matmul(cs[:, :], lhsT=Bt[:, gi * 128:(gi + 1) * 128], rhs=xdtu[:, gi * 512:(gi + 1) * 512],
                                                  start=True, stop=True),
                         reads=[bB, b_xdtu], writes=[b_cs])
                    S3 = S[:, gi, :].rearrange("p (h q) -> p h q", q=64)
                    k.op("pool", lambda e: e.tensor_tensor(out=S3, in0=S3, in1=bc3(cu[:, 64 + gi * 8:64 + gi * 8 + 8], 8, 64), op=ALU.mult),
                         reads=[b_S[gi], bcu], writes=[b_S[gi]])
                    k.op("dve", lambda e: e.tensor_tensor(out=S[:, gi, :], in0=S[:, gi, :], in1=cs[:, :], op=ALU.add),
                         reads=[b_S[gi], b_cs], writes=[b_S[gi]])
                    k.op("act", lambda e: e.copy(out=Sb[:, gi, :], in_=S[:, gi, :]), reads=[b_S[gi]], writes=[b_Sb[gi]])
                group_pre(0)
                front(0)
                for i in range(8):
                    if i + 1 < 8:
                        if (i + 1) % 2 == 0:
                            group_pre((i + 1) // 2)
                        front(i + 1)
                    back(i)
                    if i % 2 == 1:
                        group_tail(i // 2)
                if dr == 0:
                    k.dma("pool", g.yf[b, c0:c0 + 128, :], yb[:], reads=[byb], writes=[g.bufs["yf"]])
                    continue
                k.dma("sp", yfl[:], g.yf[b, c0:c0 + 128, :], reads=[g.bufs["yf"]], writes=[b_yfl])
                k.dma("sp", ztl[:], g.zs[b, c0:c0 + 128, :], reads=[g.bufs["zs"]], writes=[b_ztl])
                is_ctx = blk < 2
                xsrc = g.ctx[b, c0:c0 + 128, :] if is_ctx else g.x[b, c0 - TC:c0 - TC + 128, :]
                xdst = g.c1[b, c0:c0 + 128, :] if is_ctx else g.x1[b, c0 - TC:c0 - TC + 128, :]
                bdst = g.bufs["c1"] if is_ctx else g.bufs["x1"]
                x_, bx = xt[bi], b_xt[bi]
                k.dma("sp", x_[:], xsrc, reads=[g.bufs["ctx" if is_ctx else "x"]], writes=[bx])
                k.op("pool", lambda e: e.tensor_tensor(out=yb[:], in0=yb[:], in1=yfl[:], op=ALU.add),
                     reads=[byb, b_yfl], writes=[byb])
                k.op("dve", lambda e: e.tensor_tensor(out=yfl[:].rearrange("p (h q) -> p h q", q=64), in0=X3,
                                                       in1=bc3(dsk[:], 32, 64), op=ALU.mult),
                     reads=[bX, b_nw, b_yfl], writes=[b_yfl])
                k.op("pool", lambda e: e.tensor_tensor(out=yb[:], in0=yb[:], in1=yfl[:], op=ALU.add),
                     reads=[byb, b_yfl], writes=[byb])
                k.op("dve", lambda e: e.tensor_tensor(out=yb[:], in0=yb[:], in1=ztl[:], op=ALU.mult),
                     reads=[byb, b_ztl], writes=[byb])
                rstd, b_st = rstd_of(g, ns, 0, yb[:], byb, dim=2048)
                k.op("dve", lambda e: e.tensor_tensor(out=ygn[:], in0=yb[:], in1=nwbc[:], op=ALU.mult),
                     reads=[byb, b_nw], writes=[b_ygn])
                for hh in range(2):
                    psv = g.ps[6 + hh].bitcast(BF16)
                    for c in range(8):
                        k.op("pe", lambda e, c=c: e.transpose(out=psv[:, c * 128:(c + 1) * 128],
                                                              in_=ygn[:, (hh * 8 + c) * 128:(hh * 8 + c + 1) * 128], identity=identb[:]),
                             reads=[b_ygn, b_id], writes=[tr_b[hh]], inc=(c == 7))
                    k.op("act", lambda e: e.copy(out=ygT[:, hh * 8:(hh + 1) * 8, :],
                                                 in_=psv[:, 0:1024].rearrange("p (c t) -> p c t", t=128)),
                         reads=[tr_b[hh]], writes=[b_ygT])
                gt = gbc[0] if is_ctx else gbc[1]
                for half in range(2):
                    hs = slice(half * 512, (half + 1) * 512)
                    po, b_po = g.ps[3 + half], g.psb[3 + half]
                    for c in range(16):
                        k.op("pe", lambda e, c=c: e.matmul(po[:, :], lhsT=ygT[:, c, :], rhs=wout[:, c, hs],
                                                           start=(c == 0), stop=(c == 15)),
                             reads=[b_ygT, b_wout], writes=[b_po], inc=(c == 15))
                    o_, bo = ot[cnt["o"] % 2], b_ot[cnt["o"] % 2]
                    cnt["o"] += 1
                    k.op("dve", lambda e: e.scalar_tensor_tensor(out=o_[:], in0=po[:, :], scalar=rstd, in1=gt[:, hs],
                                                                  op0=ALU.mult, op1=ALU.mult),
                         reads=[b_po, b_st, b_g], writes=[bo])
                    k.op("pool", lambda e: e.tensor_tensor(out=x_[:, hs], in0=x_[:, hs], in1=o_[:], op=ALU.add),
                         reads=[bo, bx], writes=[bx])
                k.dma("pool", xdst, x_[:], reads=[bx], writes=[bdst])


MAGIC = 12582912.0
TWO_PI_HI = 6.28125
TWO_PI_LO = 0.0019353071795864769


def s5_prep(g):
    k, nc = g.k, g.nc
    V = lambda n, sh=(128, 32): sbt(g, "p_" + n, list(sh), F32)
    ident = sbt(g, "p_identb", [128, 128], BF16)
    b_id = Buf()
    k.dma("pool", ident[:], g.gin("ident")[:, :], writes=[b_id])
    m8 = sbt(g, "p_m8", [128, 2, 128], F32)
    b_m8 = Buf()
    k.dma("sp", m8[:], g.gin("m8").rearrange("m p c -> p m c"), writes=[b_m8])
    bb = Buf()

    def dve(fn, eng="dve"):
        k.op(eng, fn, reads=[bb], writes=[bb])

    def tt(out, a, b_, op):
        dve(lambda e: e.tensor_tensor(out=out, in0=a, in1=b_, op=op))

    def ts(out, a, s1, op0, s2=None, op1=None):
        if op1 is None:
            dve(lambda e: e.tensor_scalar(out=out, in0=a, scalar1=s1, scalar2=None, op0=op0))
        else:
            dve(lambda e: e.tensor_scalar(out=out, in0=a, scalar1=s1, scalar2=s2, op0=op0, op1=op1))

    lre, lim, lst = V("lre"), V("lim"), V("lst")
    step, lr, th, kf, r_, t8, t2 = V("step"), V("lr"), V("th"), V("kf"), V("r"), V("t8"), V("t2")
    sn, cs_, ta, tb, rho1 = V("sn"), V("cs"), V("ta"), V("tb"), V("rho1")
    ar, ai, den, cr, ci = V("ar"), V("ai"), V("den"), V("cr"), V("ci")
    pm, pc, ps_ = V("pm", (128, 32, 9)), V("pc", (128, 32, 9)), V("ps", (128, 32, 9))
    apr, api = V("apr", (128, 32, 9)), V("api", (128, 32, 9))
    anr, ani, imv = V("anr", (128, 32, 9)), V("ani", (128, 32, 9)), V("imv", (128, 32, 9))
    Pc, Ps = V("Pc"), V("Ps")
    bre, bim = V("bre", (128, 32, 16)), V("bim", (128, 32, 16))
    bbr, bbi = V("bbr", (128, 32, 16)), V("bbi", (128, 32, 16))
    cre, cim = V("cre", (128, 32, 16)), V("cim", (128, 32, 16))
    w1, w2 = V("w1", (128, 32, 16)), V("w2", (128, 32, 16))
    WZr = sbt(g, "p_WZr", [128, 32, 8, 16], BF16)
    WZi = sbt(g, "p_WZi", [128, 32, 8, 16], BF16)
    Pr = sbt(g, "p_Pr", [128, 32, 8, 16], BF16)
    Pi = sbt(g, "p_Pi", [128, 32, 8, 16], BF16)
    CAr = sbt(g, "p_CAr", [128, 32, 9, 16], BF16)
    CAi = sbt(g, "p_CAi", [128, 32, 9, 16], BF16)
    cosT = sbt(g, "p_cosT", [128, 32, 288], F32)
    sinT = sbt(g, "p_sinT", [128, 32, 288], F32)
    e1 = sbt(g, "p_e1", [128, 32, 128], F32)
    e2 = sbt(g, "p_e2", [128, 32, 128], F32)
    wst = [sbt(g, "p_wst%d" % i, [128, 6, 128], BF16) for i in range(2)]
    b_wst = [Buf(), Buf()]

    def b16(t, n):
        return bc3(t[:], 32, n)

    def col(t3, j):
        return t3[:, :, j]

    for d in range(2):
        k.dma("sp", lre[:], g.gin("s5_lamT")[d, 0], reads=[bb], writes=[bb])
        k.dma("sp", lim[:], g.gin("s5_lamT")[d, 1], reads=[bb], writes=[bb])
        k.dma("sp", lst[:], g.gin("s5_lamT")[d, 2], reads=[bb], writes=[bb])
        k.dma("sp", bre[:], g.gin("s5_b")[d, 0].rearrange("(gp q) c -> q gp c", q=128), reads=[bb], writes=[bb])
        k.dma("sp", bim[:], g.gin("s5_b")[d, 1].rearrange("(gp q) c -> q gp c", q=128), reads=[bb], writes=[bb])
        k.dma("sp", cre[:], g.gin("s5_cT")[d, 0], reads=[bb], writes=[bb])
        k.dma("sp", cim[:], g.gin("s5_cT")[d, 1], reads=[bb], writes=[bb])
        dve(lambda e: e.activation(out=step[:], in_=lst[:], func=AF.Exp), "act")
        tt(lr[:], lre[:], step[:], ALU.mult)
        tt(th[:], lim[:], step[:], ALU.mult)
        ts(ta[:], lr[:], 1.0 / 5, ALU.mult, 1.0, ALU.add)
        for c_ in (1.0 / 4, 1.0 / 3, 1.0 / 2, 1.0):
            tt(ta[:], ta[:], lr[:], ALU.mult)
            ts(ta[:], ta[:], c_, ALU.mult, 1.0, ALU.add)
        dve(lambda e: e.tensor_copy(out=rho1[:], in_=ta[:]))
        ts(kf[:], th[:], 1.0 / (2 * np.pi), ALU.mult, MAGIC, ALU.add)
        ts(kf[:], kf[:], -MAGIC, ALU.add)
        dve(lambda e: e.scalar_tensor_tensor(out=r_[:], in0=kf[:], scalar=-TWO_PI_HI, in1=th[:], op0=ALU.mult, op1=ALU.add))
        dve(lambda e: e.scalar_tensor_tensor(out=r_[:], in0=kf[:], scalar=-TWO_PI_LO, in1=r_[:], op0=ALU.mult, op1=ALU.add))
        ts(t8[:], r_[:], 0.125, ALU.mult)
        tt(t2[:], t8[:], t8[:], ALU.mult)
        ts(ta[:], t2[:], -1.0 / 5040, ALU.mult, 1.0 / 120, ALU.add)
        tt(ta[:], ta[:], t2[:], ALU.mult)
        ts(ta[:], ta[:], -1.0 / 6, ALU.add)
        tt(ta[:], ta[:], t2[:], ALU.mult)
        ts(ta[:], ta[:], 1.0, ALU.add)
        tt(sn[:], ta[:], t8[:], ALU.mult)
        ts(ta[:], t2[:], 1.0 / 40320, ALU.mult, -1.0 / 720, ALU.add)
        tt(ta[:], ta[:], t2[:], ALU.mult)
        ts(ta[:], ta[:], 1.0 / 24, ALU.add)
        tt(ta[:], ta[:], t2[:], ALU.mult)
        ts(ta[:], ta[:], -0.5, ALU.add)
        tt(ta[:], ta[:], t2[:], ALU.mult)
        ts(cs_[:], ta[:], 1.0, ALU.add)
        for _ in range(3):
            tt(ta[:], cs_[:], cs_[:], ALU.mult)
            tt(tb[:], sn[:], sn[:], ALU.mult)
            tt(sn[:], sn[:], cs_[:], ALU.mult)
            ts(sn[:], sn[:], 2.0, ALU.mult)
            tt(cs_[:], ta[:], tb[:], ALU.subtract)
        tt(ar[:], rho1[:], cs_[:], ALU.mult)
        tt(ai[:], rho1[:], sn[:], ALU.mult)
        ts(ta[:], ar[:], -1.0, ALU.add)
        tt(den[:], lre[:], lre[:], ALU.mult)
        tt(tb[:], lim[:], lim[:], ALU.mult)
        tt(den[:], den[:], tb[:], ALU.add)
        dve(lambda e: e.reciprocal(out=den[:], in_=den[:]))
        tt(cr[:], ta[:], lre[:], ALU.mult)
        tt(tb[:], ai[:], lim[:], ALU.mult)
        tt(cr[:], cr[:], tb[:], ALU.add)
        tt(cr[:], cr[:], den[:], ALU.mult)
        tt(ci[:], ai[:], lre[:], ALU.mult)
        tt(tb[:], ta[:], lim[:], ALU.mult)
        tt(ci[:], ci[:], tb[:], ALU.subtract)
        tt(ci[:], ci[:], den[:], ALU.mult)
        tt(w1[:], bre[:], b16(cr, 16), ALU.mult)
        tt(w2[:], bim[:], b16(ci, 16), ALU.mult)
        tt(bbr[:], w1[:], w2[:], ALU.subtract)
        tt(w1[:], bim[:], b16(cr, 16), ALU.mult)
        tt(w2[:], bre[:], b16(ci, 16), ALU.mult)
        tt(bbi[:], w1[:], w2[:], ALU.add)
        dve(lambda e: e.memset(col(pm, 0), 1.0))
        dve(lambda e: e.memset(col(pc, 0), 1.0))
        dve(lambda e: e.memset(col(ps_, 0), 0.0))
        for j in range(1, 9):
            tt(col(pm, j), col(pm, j - 1), rho1[:], ALU.mult)
            tt(ta[:], col(pc, j - 1), cs_[:], ALU.mult)
            tt(tb[:], col(ps_, j - 1), sn[:], ALU.mult)
            tt(col(pc, j), ta[:], tb[:], ALU.subtract)
            tt(ta[:], col(ps_, j - 1), cs_[:], ALU.mult)
            tt(tb[:], col(pc, j - 1), sn[:], ALU.mult)
            tt(col(ps_, j), ta[:], tb[:], ALU.add)
        dve(lambda e: e.reciprocal(out=imv[:], in_=pm[:]))
        tt(apr[:], pm[:], pc[:], ALU.mult)
        tt(api[:], pm[:], ps_[:], ALU.mult)
        tt(anr[:], imv[:], pc[:], ALU.mult)
        tt(ani[:], imv[:], ps_[:], ALU.mult)
        ts(ani[:], ani[:], -1.0, ALU.mult)
        dve(lambda e: e.tensor_copy(out=ta[:], in_=col(pm, 8)))
        k.dma("sp", g.rho[d], ta[:], reads=[bb], writes=[g.bufs["rho"]])
        for s_ in range(8):
            so = s_ if d == 0 else 7 - s_
            for (dst, pr_, pi_) in ((WZr, col(apr, 7 - s_), col(api, 7 - s_)), (Pr, col(anr, s_), col(ani, s_))):
                dsti = WZi if dst is WZr else Pi
                tt(w1[:], bbr[:], bc3(pr_, 32, 16), ALU.mult)
                tt(w2[:], bbi[:], bc3(pi_, 32, 16), ALU.mult)
                tt(dst[:, :, so, :], w1[:], w2[:], ALU.subtract)
                tt(w1[:], bbi[:], bc3(pr_, 32, 16), ALU.mult)
                tt(w2[:], bbr[:], bc3(pi_, 32, 16), ALU.mult)
                tt(dsti[:, :, so, :], w1[:], w2[:], ALU.add)
        for j in range(9):
            jo = j if d == 0 else 8 - j
            tt(w1[:], cre[:], bc3(col(apr, j), 32, 16), ALU.mult)
            tt(w2[:], cim[:], bc3(col(api, j), 32, 16), ALU.mult)
            tt(CAr[:, :, jo, :], w1[:], w2[:], ALU.subtract)
            tt(w1[:], cim[:], bc3(col(apr, j), 32, 16), ALU.mult)
            tt(w2[:], cre[:], bc3(col(api, j), 32, 16), ALU.mult)
            tt(w1[:], w1[:], w2[:], ALU.add)
            ts(CAi[:, :, jo, :], w1[:], -1.0, ALU.mult)
        q0, y0 = (0, 1) if d == 0 else (1, 0)
        dve(lambda e: e.tensor_copy(out=Pc[:], in_=col(pc, 8)))
        dve(lambda e: e.tensor_copy(out=Ps[:], in_=col(ps_, 8)))
        dve(lambda e: e.memset(cosT[:, :, 0:1], 1.0))
        dve(lambda e: e.memset(sinT[:, :, 0:1], 0.0))
        wdt = 1
        while wdt < 288:
            n = min(wdt, 288 - wdt)
            tt(e1[:, :, 0:n], cosT[:, :, 0:n], b16(Pc, n), ALU.mult)
            tt(e2[:, :, 0:n], sinT[:, :, 0:n], b16(Ps, n), ALU.mult)
            tt(cosT[:, :, wdt:wdt + n], e1[:, :, 0:n], e2[:, :, 0:n], ALU.subtract)
            tt(e1[:, :, 0:n], sinT[:, :, 0:n], b16(Pc, n), ALU.mult)
            tt(e2[:, :, 0:n], cosT[:, :, 0:n], b16(Ps, n), ALU.mult)
            tt(sinT[:, :, wdt:wdt + n], e1[:, :, 0:n], e2[:, :, 0:n], ALU.add)
            tt(ta[:], Pc[:], Pc[:], ALU.mult)
            tt(tb[:], Ps[:], Ps[:], ALU.mult)
            tt(Ps[:], Ps[:], Pc[:], ALU.mult)
            ts(Ps[:], Ps[:], 2.0, ALU.mult)
            tt(Pc[:], ta[:], tb[:], ALU.subtract)
            wdt *= 2
        k.dma("sp", g.etab[d, 0], cosT[:], reads=[bb], writes=[g.bufs["etab"]])
        k.dma("sp", g.etab[d, 1], sinT[:], reads=[bb], writes=[g.bufs["etab"]])
        for gp in range(32):
            wt, bwt = wst[gp % 2], b_wst[gp % 2]
            for g2 in range(2):
                L = slice(g2 * 64, (g2 + 1) * 64)
                pM, bM = g.ps[g2], g.psb[g2]
                k.op("pe", lambda e: e.matmul(pM[:, 0:128], lhsT=Pr[L, gp, :, :], rhs=CAr[L, gp, q0:q0 + 8, :], start=True, stop=False),
                     reads=[bb], writes=[bM], inc=False)
                k.op("pe", lambda e: e.matmul(pM[:, 0:128], lhsT=Pi[L, gp, :, :], rhs=CAi[L, gp, q0:q0 + 8, :], start=False, stop=True),
                     reads=[bb], writes=[bM])
                k.op("dve", lambda e: e.tensor_tensor(out=wt[:, g2, :], in0=pM[:, 0:128], in1=m8[:, d, :], op=ALU.mult),
                     reads=[bM, b_m8], writes=[bwt])
            for ri, src_ in enumerate((WZr, WZi)):
                pT, bT = g.ps[2 + ri], g.psb[2 + ri]
                pTv = pT.bitcast(BF16)
                k.op("pe", lambda e: e.transpose(out=pTv[:, 0:128], in_=src_[:, gp, :, :], identity=ident[:]),
                     reads=[bb, b_id], writes=[bT])
                k.op("act", lambda e: e.copy(out=wt[:, 2 + ri, :], in_=pTv[:, 0:128]), reads=[bT], writes=[bwt])
            k.op("act", lambda e: e.copy(out=wt[:, 4, :], in_=CAr[:, gp, y0:y0 + 8, :]), reads=[bb], writes=[bwt])
            k.op("act", lambda e: e.copy(out=wt[:, 5, :], in_=CAi[:, gp, y0:y0 + 8, :]), reads=[bb], writes=[bwt])
            k.dma("sp", g.s5w[d, gp], wt[:], reads=[bwt], writes=[g.bufs["s5w"]])


def phase_s5(g):
    with ExitStack() as pes:
        g.pes = pes
        s5_prep(g)
        g.k.barrier()
    if g.kinds.get("_s5_prep_only"):
        return
    with ExitStack() as pes:
        g.pes = pes
        s5_main(g)
        g.k.barrier()


def bcf(col_ap, n):
    a = col_ap.ap
    return AP(col_ap.tensor, col_ap.offset, [list(a[0]), [0, n]])


def s5_main(g):
    k, nc = g.k, g.nc
    identb = sbt(g, "v_identb", [128, 128], BF16)
    b_id = Buf()
    k.dma("pool", identb[:], g.gin("ident")[:, :], writes=[b_id])
    bglu = sbt(g, "v_bglu", [128, 2 * D], F32)
    dsk = sbt(g, "v_dsk", [128, D], F32)
    rho = sbt(g, "v_rho", [128, 2, 32], F32)
    b_c = Buf()
    k.dma("sp", bglu[:], bcast_rows(g.gin("s5_b_glu")[0:1, :], 128), writes=[b_c])
    k.dma("sp", dsk[:], bcast_rows(g.gin("s5_d")[0:1, :], 128), writes=[b_c])
    k.dma("sp", rho[:], g.rho.rearrange("d q gp -> q d gp"), reads=[g.bufs["rho"]], writes=[b_c])
    mods = ModBC(g, "v", 1, (0, 1, 2))
    ns = NormSet(g, "v_")
    Ubig = sbt(g, "v_U", [128, 64 * 320], BF16)
    U = Ubig[:, :].rearrange("p (g c) -> p g c", c=320)
    wglu = Ubig[:, 0:8 * 2048].rearrange("p (k n) -> p k n", n=2048)
    b_U = Buf()
    hcx = sbt(g, "v_hcx", [128, 64, 8, 16], BF16)
    b_hcx = Buf()
    hxc = sbt(g, "v_hxc", [128, 2, 64, 8, 16], BF16)
    b_hxc = Buf()
    xt = [sbt(g, "v_xt%d" % i, [128, D], F32) for i in range(2)]
    b_xt = [Buf(), Buf()]
    tab = [sbt(g, "v_tab%d" % i, [128, 2, 288], F32) for i in range(2)]
    b_tab = [Buf(), Buf()]
    wk = [[sbt(g, "v_wk%d_%d" % (i, j), [128, 288], F32) for j in range(8)] for i in range(2)]
    b_wk = [[Buf() for _ in range(8)] for _ in range(2)]
    spv = [sbt(g, "v_spv%d" % i, [128, 2, 2, 256], BF16) for i in range(2)]
    b_spv = [Buf(), Buf()]
    wts = [sbt(g, "v_wts%d" % i, [128, 2, 6, 128], BF16) for i in range(2)]
    b_wts = [Buf(), Buf()]
    ysb = [sbt(g, "v_ysb%d" % i, [128, 256], BF16) for i in range(2)]
    b_ysb = [Buf(), Buf()]
    gt_ = [sbt(g, "v_gt%d" % i, [128, D], F32) for i in range(3)]
    b_gt = [Buf() for _ in range(3)]
    geb2 = [sbt(g, "v_geb%d" % i, [128, D], BF16) for i in range(2)]
    b_geb2 = [Buf(), Buf()]
    geT2 = [sbt(g, "v_geT%d" % i, [128, 8, 128], BF16) for i in range(2)]
    b_geT2 = [Buf(), Buf()]
    av = [sbt(g, "v_av%d" % i, [128, 512], F32) for i in range(2)]
    b_av = [Buf(), Buf()]
    gv = [sbt(g, "v_gv%d" % i, [128, 512], F32) for i in range(2)]
    b_gv = [Buf(), Buf()]
    cnt = dict(x=0, it=0, y=0, o=0)

    def load_x_tile(dst, bdst, src_b, tile, l):
        for r4 in range(4):
            t0 = (r4 * 8 + l) * 64 + tile * 32
            k.dma("sp", dst[r4 * 32:(r4 + 1) * 32, :], src_b[t0:t0 + 32, :], reads=[g.bufs["x2"]], writes=[bdst])

    for b in range(NB):
        (shbc, gbc, gatebc), b_mod = mods.get(2)
        cv = g.c2[b].rearrange("(c l) d -> l c d", l=8)
        for l in range(8):
            x_, bx = xt[cnt["x"] % 2], b_xt[cnt["x"] % 2]
            cnt["x"] += 1
            k.dma("sp", x_[0:32, :], cv[l], reads=[g.bufs["c2"]], writes=[bx])
            rstd, b_st = rstd_of(g, ns, 0, x_[0:32, :], bx)
            st = ns.st[0]
            k.op("dve", lambda e: e.scalar_tensor_tensor(out=ns.tmp[0][0:32, :], in0=x_[0:32, :], scalar=st[0:32, 3:4], in1=gbc[0:32, :],
                                                          op0=ALU.mult, op1=ALU.mult),
                 reads=[bx, b_st, b_mod], writes=[ns.b_tmp[0]])
            k.op("pool", lambda e: e.tensor_tensor(out=hcx[0:32, :, l, :], in0=ns.tmp[0][0:32, :].rearrange("p (g c) -> p g c", c=16),
                                                   in1=shbc[0:32, :].rearrange("p (g c) -> p g c", c=16), op=ALU.add),
                 reads=[ns.b_tmp[0], b_mod], writes=[b_hcx])
        (shbc, gbc, gatebc), b_mod = mods.get(b)
        for tile in range(2):
            for l in range(8):
                x_, bx = xt[cnt["x"] % 2], b_xt[cnt["x"] % 2]
                cnt["x"] += 1
                load_x_tile(x_, bx, g.x2[b], tile, l)
                rstd, b_st = rstd_of(g, ns, 0, x_[:], bx)
                k.op("dve", lambda e: e.scalar_tensor_tensor(out=ns.tmp[0][:], in0=x_[:], scalar=rstd, in1=gbc[:],
                                                              op0=ALU.mult, op1=ALU.mult),
                     reads=[bx, b_st, b_mod], writes=[ns.b_tmp[0]])
                k.op("pool", lambda e: e.tensor_tensor(out=hxc[:, tile, :, l, :], in0=ns.tmp[0][:].rearrange("p (g c) -> p g c", c=16),
                                                       in1=shbc[:].rearrange("p (g c) -> p g c", c=16), op=ALU.add),
                     reads=[ns.b_tmp[0], b_mod], writes=[b_hxc])
        for gg in range(64):
            ps, pb = g.ps[6 + gg % 2], g.psb[6 + gg % 2]
            psv = ps.bitcast(BF16)
            k.op("pe", lambda e: e.transpose(out=psv[:, 0:32], in_=hcx[0:32, gg, :, :], identity=identb[0:32, 0:32]),
                 reads=[b_hcx, b_id], writes=[pb], inc=False)
            for tile in range(2):
                k.op("pe", lambda e, tile=tile: e.transpose(out=psv[:, 32 + tile * 128:32 + (tile + 1) * 128],
                                                            in_=hxc[:, tile, gg, :, :], identity=identb[:]),
                     reads=[b_hxc, b_id], writes=[pb], inc=(tile == 1))
            k.op("act", lambda e: e.copy(out=U[:, gg, 0:32], in_=psv[:, 0:32]), reads=[pb], writes=[b_U])
            k.op("act", lambda e: e.copy(out=U[:, gg, 288:320], in_=psv[:, 0:32]), reads=[pb], writes=[b_U])
            k.op("act", lambda e: e.copy(out=U[:, gg, 32:288].rearrange("p (t c r) -> p t r c", t=2, c=32, r=4),
                                         in_=psv[:, 32:288].rearrange("p (t r c) -> p t r c", t=2, r=4, c=32)),
                 reads=[pb], writes=[b_U])
        for tile in range(2):
            for l in range(8):
                k.op("dve", lambda e, tile=tile, l=l: e.tensor_tensor(out=hxc[:, tile, :, l, :], in0=hxc[:, tile, :, l, :],
                                                                      in1=dsk[:].rearrange("p (g c) -> p g c", c=16), op=ALU.mult),
                     reads=[b_hxc, b_c], writes=[b_hxc])
        for gp in range(32):
            it = cnt["it"] % 2
            cnt["it"] += 1
            wt, bwt, sp_, bsp = wts[it], b_wts[it], spv[it], b_spv[it]
            for d in range(2):
                tb_, btb = tab[d], b_tab[d]
                W, bW = wk[d], b_wk[d]
                k.dma("sp", wt[:, d], g.s5w[d, gp], reads=[g.bufs["s5w"]], writes=[bwt])
                k.dma("sp", tb_[:, 0, :], g.etab[d, 0, :, gp, :], reads=[g.bufs["etab"]], writes=[btb])
                k.dma("sp", tb_[:, 1, :], g.etab[d, 1, :, gp, :], reads=[g.bufs["etab"]], writes=[btb])
                cols = slice(0, 288) if d == 0 else slice(32, 320)
                pz = [(g.ps[2 * d], g.psb[2 * d]), (g.ps[2 * d + 1], g.psb[2 * d + 1])]
                for ri in range(2):
                    pZ, bZ = pz[ri]
                    for g2 in range(2):
                        k.op("pe", lambda e, g2=g2: e.matmul(pZ[g2 * 64:(g2 + 1) * 64, 0:288], lhsT=wt[:, d, 2 + ri, g2 * 64:(g2 + 1) * 64],
                                                             rhs=U[:, 2 * gp + g2, cols], start=True, stop=True),
                             reads=[bwt, b_U], writes=[bZ], inc=(g2 == 1))
                cosv, sinv = tb_[:, 0, :], tb_[:, 1, :]
                if d == 1:
                    cosv, sinv = rev_ap(cosv, 288), rev_ap(sinv, 288)
                Zr, bZr = pz[0][0][:, 0:288], pz[0][1]
                Zi, bZi = pz[1][0][:, 0:288], pz[1][1]
                za, zb, ztr, zti, sr, si, zc, zd = [w_[:] for w_ in W]
                bza, bzb, bztr, bzti, bsr, bsi, bzc, bzd = bW
                TT = lambda o, a_, b_, op, rd, wr: k.op("dve", lambda e: e.tensor_tensor(out=o, in0=a_, in1=b_, op=op), reads=rd, writes=wr)
                TT(za, Zr, cosv, ALU.mult, [bZr, btb], [bza])
                TT(zb, Zi, sinv, ALU.mult, [bZi, btb], [bzb])
                TT(zc, Zi, cosv, ALU.mult, [bZi, btb], [bzc])
                TT(zd, Zr, sinv, ALU.mult, [bZr, btb], [bzd])
                TT(ztr, za, zb, ALU.add, [bza, bzb], [bztr])
                TT(zti, zc, zd, ALU.subtract, [bzc, bzd], [bzti])
                rbc = bcf(rho[:, d, gp:gp + 1], 288)
                for (o_, i_, bo_, bi_) in ((sr, ztr, bsr, bztr), (si, zti, bsi, bzti)):
                    oo, ii = (o_, i_) if d == 0 else (rev_ap(o_, 288), rev_ap(i_, 288))
                    k.op("dve", lambda e, oo=oo, ii=ii: e.tensor_tensor_scan(out=oo, data0=rbc, data1=ii, initial=0.0,
                                                                             op0=ALU.mult, op1=ALU.add),
                         reads=[bi_, b_c], writes=[bo_])
                sl = slice(31, 287) if d == 0 else slice(1, 257)
                TT(za[:, 0:256], sr[:, sl], cosv[:, sl], ALU.mult, [bsr, btb], [bza])
                TT(zc[:, 0:256], sr[:, sl], sinv[:, sl], ALU.mult, [bsr, btb], [bzc])
                TT(zb[:, 0:256], si[:, sl], sinv[:, sl], ALU.mult, [bsi, btb], [bzb])
                TT(zd[:, 0:256], si[:, sl], cosv[:, sl], ALU.mult, [bsi, btb], [bzd])
                TT(sp_[:, d, 0, :], za[:, 0:256], zb[:, 0:256], ALU.subtract, [bza, bzb], [bsp])
                TT(sp_[:, d, 1, :], zd[:, 0:256], zc[:, 0:256], ALU.add, [bzc, bzd], [bsp])
            for g2 in range(2):
                L = slice(g2 * 64, (g2 + 1) * 64)
                gg = 2 * gp + g2
                yi = cnt["y"] % 2
                cnt["y"] += 1
                pY, bY = g.ps[4 + yi], g.psb[4 + yi]
                ops = [(wt[:, 0, g2, :], U[:, gg, 32:288]), (wt[:, 1, g2, :], U[:, gg, 32:288])]
                for d in range(2):
                    ops.append((wt[L, d, 4, :], sp_[L, d, 0, :]))
                    ops.append((wt[L, d, 5, :], sp_[L, d, 1, :]))
                for oi, (lw, rh) in enumerate(ops):
                    k.op("pe", lambda e, lw=lw, rh=rh, oi=oi: e.matmul(pY[:, 0:256], lhsT=lw, rhs=rh, start=(oi == 0), stop=(oi == 5)),
                         reads=[bwt, b_U, bsp], writes=[bY], inc=(oi == 5))
                ys, bys = ysb[yi], b_ysb[yi]
                k.op("act", lambda e: e.copy(out=ys[:].rearrange("p (t r c) -> p t c r", t=2, r=4, c=32),
                                             in_=pY[:, 0:256].rearrange("p (t c r) -> p t c r", t=2, c=32, r=4)),
                     reads=[bY], writes=[bys])
                pT, bT = g.ps[6 + yi], g.psb[6 + yi]
                pTv = pT.bitcast(BF16)
                for tile in range(2):
                    k.op("pe", lambda e, tile=tile: e.transpose(out=pTv[:, tile * 128:(tile + 1) * 128],
                                                                in_=ys[:, tile * 128:(tile + 1) * 128], identity=identb[:]),
                         reads=[bys, b_id], writes=[bT], inc=(tile == 1))
                tyv = hxc[:, :, gg, :, :]
                k.op("dve", lambda e: e.tensor_tensor(out=tyv, in0=tyv, in1=pTv[:, 0:256].rearrange("p (t l c) -> p t l c", t=2, l=8, c=16),
                                                       op=ALU.add),
                     reads=[bT, b_hxc], writes=[b_hxc])
        for kk in range(8):
            k.dma("pool", wglu[:, kk, :], g.gin("s5_w_glu")[kk * 128:(kk + 1) * 128, :], writes=[b_U])
        def d_pre(si_):
            tile, l = divmod(si_, 8)
            gb, bgb, gT, bgT = geb2[si_ % 2], b_geb2[si_ % 2], geT2[si_ % 2], b_geT2[si_ % 2]
            yv = hxc[:, tile, :, l, :]
            g0, g1, g2_ = [t_[:].rearrange("p (g c) -> p g c", c=16) for t_ in gt_]
            k.op("act", lambda e: e.activation(out=g0, in_=yv, func=AF.Square), reads=[b_hxc], writes=[b_gt[0]])
            k.op("dve", lambda e: e.tensor_scalar(out=g0, in0=g0, scalar1=0.044715, scalar2=1.0, op0=ALU.mult, op1=ALU.add),
                 reads=[b_gt[0]], writes=[b_gt[0]])
            k.op("dve", lambda e: e.tensor_tensor(out=g1, in0=g0, in1=yv, op=ALU.mult), reads=[b_gt[0], b_hxc], writes=[b_gt[1]])
            k.op("act", lambda e: e.activation(out=g2_, in_=g1, func=AF.Sigmoid, scale=1.5957691216057308),
                 reads=[b_gt[1]], writes=[b_gt[2]])
            k.op("dve", lambda e: e.tensor_tensor(out=gb[:].rearrange("p (g c) -> p g c", c=16), in0=g2_, in1=yv, op=ALU.mult),
                 reads=[b_gt[2], b_hxc], writes=[bgb])
            ps, pb = g.ps[6 + si_ % 2], g.psb[6 + si_ % 2]
            psv = ps.bitcast(BF16)
            for kk in range(8):
                k.op("pe", lambda e, kk=kk: e.transpose(out=psv[:, kk * 128:(kk + 1) * 128], in_=gb[:, kk * 128:(kk + 1) * 128],
                                                        identity=identb[:]),
                     reads=[bgb, b_id], writes=[pb], inc=(kk == 7))
            k.op("act", lambda e: e.copy(out=gT[:], in_=psv[:, 0:1024].rearrange("p (k t) -> p k t", t=128)),
                 reads=[pb], writes=[bgT])
            x_, bx = xt[si_ % 2], b_xt[si_ % 2]
            load_x_tile(x_, bx, g.x2[b], tile, l)

        def d_post(si_):
            tile, l = divmod(si_, 8)
            gT, bgT = geT2[si_ % 2], b_geT2[si_ % 2]
            x_, bx = xt[si_ % 2], b_xt[si_ % 2]
            for half in range(2):
                oi = cnt["o"] % 2
                cnt["o"] += 1
                pa, ba = g.ps[oi * 2], g.psb[oi * 2]
                pg_, bg_ = g.ps[oi * 2 + 1], g.psb[oi * 2 + 1]
                for (pp, bp, n0) in ((pa, ba, half * 512), (pg_, bg_, D + half * 512)):
                    for kk in range(8):
                        k.op("pe", lambda e, kk=kk, pp=pp, n0=n0: e.matmul(pp[:, :], lhsT=gT[:, kk, :], rhs=wglu[:, kk, n0:n0 + 512],
                                                                           start=(kk == 0), stop=(kk == 7)),
                             reads=[bgT, b_U], writes=[bp], inc=(kk == 7))
                hs = slice(half * 512, (half + 1) * 512)
                a_, ba_, g_, bg2 = av[oi], b_av[oi], gv[oi], b_gv[oi]
                k.op("dve", lambda e: e.tensor_tensor(out=a_[:], in0=pa[:, :], in1=bglu[:, hs], op=ALU.add),
                     reads=[ba, b_c], writes=[ba_])
                k.op("dve", lambda e: e.tensor_tensor(out=g_[:], in0=pg_[:, :], in1=bglu[:, D + half * 512:D + (half + 1) * 512], op=ALU.add),
                     reads=[bg_, b_c], writes=[bg2])
                k.op("act", lambda e: e.activation(out=g_[:], in_=g_[:], func=AF.Sigmoid), reads=[bg2], writes=[bg2])
                k.op("dve", lambda e: e.tensor_tensor(out=a_[:], in0=a_[:], in1=g_[:], op=ALU.mult), reads=[ba_, bg2], writes=[ba_])
                k.op("pool", lambda e: e.tensor_tensor(out=a_[:], in0=a_[:], in1=gatebc[:, hs], op=ALU.mult),
                     reads=[ba_, b_mod], writes=[ba_])
                k.op("pool", lambda e: e.tensor_tensor(out=x_[:, hs], in0=x_[:, hs], in1=a_[:], op=ALU.add),
                     reads=[ba_, bx], writes=[bx])
            for r4 in range(4):
                t0 = (r4 * 8 + l) * 64 + tile * 32
                k.dma("pool", g.x3[b, t0:t0 + 32, :], x_[r4 * 32:(r4 + 1) * 32, :], reads=[bx], writes=[g.bufs["x3"]])

        d_pre(0)
        for si_ in range(16):
            if si_ + 1 < 16:
                d_pre(si_ + 1)
            d_post(si_)


def phase_moe(g):
    k, nc = g.k, g.nc
    src = g.x3 if "s5" in g.phases or g.kinds.get("x3") == "in" else g.x2
    b_src = g.bufs["x3"] if "s5" in g.phases or g.kinds.get("x3") == "in" else g.bufs["x2"]
    NTL = T // 128
    NFG = EDIM // 512
    ns = NormSet(g, "m_")
    identf = sbt(g, "m_identf", [128, 128], F32)
    b_identf = Buf()
    k.dma("sp", identf[:], g.gin("ident")[:, :], writes=[b_identf])
    hT = sbt(g, "m_hT", [128, 8, T], BF16)
    b_hT = Buf()
    acc = sbt(g, "m_acc", [128, NTL, D], F32)
    b_acc = [Buf() for _ in range(NTL)]
    aT = sbt(g, "m_aT", [128, 4, T], BF16)
    b_aT = [Buf() for _ in range(4)]
    wg = [sbt(g, "m_wg%d" % i, [128, 8, 512], BF16) for i in range(2)]
    wu = [sbt(g, "m_wu%d" % i, [128, 8, 512], BF16) for i in range(2)]
    wo = [sbt(g, "m_wo%d" % i, [128, 4, D], BF16) for i in range(2)]
    b_w = [Buf(), Buf()]
    comb = sbt(g, "m_comb", [128, NTL, NEXP], F32)
    b_comb = Buf()
    rw = sbt(g, "m_rw", [128, 8, NEXP], F32)
    rb = sbt(g, "m_rb", [128, NEXP], F32)
    b_rw = Buf()
    k.dma("sp", rw[:], g.gin("moe_router_w").rearrange("(k p) e -> p k e", p=128), writes=[b_rw])
    k.dma("sp", rb[:], bcast_rows(g.gin("moe_router_b")[0:1, :], 128), writes=[b_rw])
    nfin = sbt(g, "m_nfin", [128, D], F32)
    b_nfin = Buf()
    k.dma("sp", nfin[:], bcast_rows(g.gin("norm_final")[0:1, :], 128), writes=[b_nfin])
    hTf2 = [sbt(g, "m_hTf%d" % i, [128, 8, 128], F32) for i in range(2)]
    b_hTf2 = [Buf(), Buf()]
    lg = [sbt(g, "m_lg%d" % i, [128, 40], F32) for i in range(2)]
    b_lg = [Buf(), Buf()]
    sg = [sbt(g, "m_sg%d" % i, [128, 512], F32) for i in range(2)]
    b_sg = [Buf(), Buf()]
    tmpo = [sbt(g, "m_to%d" % i, [128, 512], F32) for i in range(2)]
    b_to = [Buf(), Buf()]
    mods = ModBC(g, "m", 1, (3, 4, 5))
    wi = 0
    si = 0
    oi = 0
    pi = 0
    for b in range(NB):
        (shbc, gbc, gatebc), b_mod = mods.get(b)
        def a_part1(j):
                xt = acc[:, j, :]
                k.dma("sp", xt, src[b, j * 128:(j + 1) * 128, :], reads=[b_src], writes=[b_acc[j]])
                i = ns.i % ns.nbuf
                ns.i += 1
                rstd, b_st = rstd_of(g, ns, i, xt, b_acc[j])
                tmp, b_tmp, hb, b_hb = ns.tmp[i], ns.b_tmp[i], ns.hb[i], ns.b_hb[i]
                k.op("dve", lambda e: e.scalar_tensor_tensor(out=tmp[:], in0=xt, scalar=rstd, in1=gbc[:],
                                                              op0=ALU.mult, op1=ALU.mult),
                     reads=[b_acc[j], b_st, b_mod], writes=[b_tmp])
                k.op("pool", lambda e: e.tensor_tensor(out=tmp[:], in0=tmp[:], in1=shbc[:], op=ALU.add),
                     reads=[b_tmp, b_mod], writes=[b_tmp])
                k.op("act", lambda e: e.copy(out=hb[:], in_=tmp[:]), reads=[b_tmp], writes=[b_hb])
                ps, pb = g.ps[4 + (j % 2)], g.psb[4 + (j % 2)]
                psv = ps.bitcast(BF16)
                for kk in range(8):
                    k.op("pe", lambda e, kk=kk: e.transpose(out=psv[:, kk * 128:(kk + 1) * 128],
                                                            in_=hb[:, kk * 128:(kk + 1) * 128], identity=ns.ident[:]),
                         reads=[b_hb, ns.b_ident], writes=[pb], inc=(kk == 7))
                k.op("act", lambda e: e.copy(out=hT[:, :, j * 128:(j + 1) * 128],
                                             in_=psv[:, 0:1024].rearrange("p (k t) -> p k t", t=128)),
                     reads=[pb], writes=[b_hT])
                for hh in range(2):
                    psf, pbf = g.ps[6 + hh], g.psb[6 + hh]
                    for kk in range(4):
                        kf = hh * 4 + kk
                        k.op("pe", lambda e, kk=kk, kf=kf: e.transpose(out=psf[:, kk * 128:(kk + 1) * 128],
                                                                       in_=tmp[:, kf * 128:(kf + 1) * 128], identity=identf[:]),
                             reads=[b_tmp, b_identf], writes=[pbf], inc=(kk == 3))
                    k.op("act", lambda e, hh=hh: e.copy(out=hTf2[j % 2][:, hh * 4:(hh + 1) * 4, :],
                                                        in_=psf[:, :].rearrange("p (k t) -> p k t", t=128)),
                         reads=[pbf], writes=[b_hTf2[j % 2]])

        def a_part2(j):
                pl, pbl = g.ps[4 + (j % 2)], g.psb[4 + (j % 2)]
                for kk in range(8):
                    k.op("pe", lambda e, kk=kk: e.matmul(pl[:, 0:NEXP], lhsT=hTf2[j % 2][:, kk, :], rhs=rw[:, kk, :],
                                                         start=(kk == 0), stop=(kk == 7)),
                         reads=[b_hTf2[j % 2], b_rw], writes=[pbl], inc=(kk == 7))
                L = lg[j % 2]
                bL = b_lg[j % 2]
                k.op("dve", lambda e: e.tensor_tensor(out=L[:, 0:8], in0=pl[:, 0:NEXP], in1=rb[:], op=ALU.add),
                     reads=[pbl, b_rw], writes=[bL])
                k.op("dve", lambda e: e.max(out=L[:, 8:16], in_=L[:, 0:8]), reads=[bL], writes=[bL])
                k.op("dve", lambda e: e.tensor_scalar(out=L[:, 16:17], in0=L[:, 8:9], scalar1=-1.0, scalar2=None,
                                                       op0=ALU.mult), reads=[bL], writes=[bL])
                k.op("act", lambda e: e.activation(out=L[:, 17:18], in_=L[:, 9:10], func=AF.Exp, bias=L[:, 16:17]),
                     reads=[bL], writes=[bL])
                k.op("dve", lambda e: e.tensor_scalar(out=L[:, 20:21], in0=L[:, 17:18], scalar1=1.0, scalar2=None,
                                                       op0=ALU.add), reads=[bL], writes=[bL])
                k.op("dve", lambda e: e.reciprocal(out=L[:, 18:19], in_=L[:, 20:21]), reads=[bL], writes=[bL])
                k.op("dve", lambda e: e.tensor_tensor(out=L[:, 19:20], in0=L[:, 17:18], in1=L[:, 18:19], op=ALU.mult),
                     reads=[bL], writes=[bL])
                k.op("dve", lambda e: e.tensor_scalar(out=L[:, 24:32], in0=L[:, 0:8], scalar1=L[:, 8:9], scalar2=L[:, 18:19],
                                                       op0=ALU.is_equal, op1=ALU.mult), reads=[bL], writes=[bL])
                k.op("dve", lambda e: e.tensor_scalar(out=L[:, 32:40], in0=L[:, 0:8], scalar1=L[:, 9:10], scalar2=L[:, 19:20],
                                                       op0=ALU.is_equal, op1=ALU.mult), reads=[bL], writes=[bL])
                k.op("dve", lambda e: e.tensor_tensor(out=comb[:, j, :], in0=L[:, 24:32], in1=L[:, 32:40], op=ALU.add),
                     reads=[bL], writes=[b_comb])

        a_part1(0)
        for j in range(NTL):
            if j + 1 < NTL:
                a_part1(j + 1)
            a_part2(j)
        for ex in range(NEXP):
            for fg in range(NFG):
                w_i = wi % 2
                wi += 1
                bw = b_w[w_i]
                fs = slice(fg * 512, (fg + 1) * 512)
                k.dma("pool", wg[w_i][:], g.gin("moe_w_in")[ex, :, fg * 512:(fg + 1) * 512].rearrange("(k p) n -> p k n", p=128),
                      writes=[bw])
                k.dma("pool", wu[w_i][:], g.gin("moe_w_in")[ex, :, EDIM + fg * 512:EDIM + (fg + 1) * 512].rearrange("(k p) n -> p k n", p=128),
                      writes=[bw])
                k.dma("pool", wo[w_i][:], g.gin("moe_w_out")[ex, fg * 512:(fg + 1) * 512, :].rearrange("(c p) d -> p c d", p=128),
                      writes=[bw])
                for c4 in range(4):
                    k.op("pool", lambda e, c4=c4: e.tensor_tensor(out=wo[w_i][:, c4, :], in0=wo[w_i][:, c4, :], in1=gatebc[:], op=ALU.mult),
                         reads=[bw, b_mod], writes=[bw])
                for tg in range(4):
                    ts_ = slice(tg * 512, (tg + 1) * 512)
                    for fc in range(4):
                        pg, pu = g.ps[(pi % 2) * 2], g.ps[(pi % 2) * 2 + 1]
                        bg, bu = g.psb[(pi % 2) * 2], g.psb[(pi % 2) * 2 + 1]
                        pi += 1
                        for kk in range(8):
                            k.op("pe", lambda e, kk=kk: e.matmul(pg[:, :], lhsT=wg[w_i][:, kk, fc * 128:(fc + 1) * 128],
                                                                 rhs=hT[:, kk, ts_], start=(kk == 0), stop=(kk == 7)),
                                 reads=[bw, b_hT], writes=[bg], inc=(kk == 7))
                        for kk in range(8):
                            k.op("pe", lambda e, kk=kk: e.matmul(pu[:, :], lhsT=wu[w_i][:, kk, fc * 128:(fc + 1) * 128],
                                                                 rhs=hT[:, kk, ts_], start=(kk == 0), stop=(kk == 7)),
                                 reads=[bw, b_hT], writes=[bu], inc=(kk == 7))
                        s_i = si % 2
                        si += 1
                        k.op("act", lambda e: e.activation(out=sg[s_i][:], in_=pg[:, :], func=AF.Silu),
                             reads=[bg], writes=[b_sg[s_i]])
                        k.op("dve", lambda e: e.tensor_tensor(out=aT[:, fc, ts_], in0=sg[s_i][:], in1=pu[:, :], op=ALU.mult),
                             reads=[b_sg[s_i], bu], writes=[b_aT[tg]])
                for j in range(NTL):
                    for half in range(2):
                        hs = slice(half * 512, (half + 1) * 512)
                        po, pbo = g.ps[4 + (oi % 4)], g.psb[4 + (oi % 4)]
                        to, bto = tmpo[oi % 2], b_to[oi % 2]
                        oi += 1
                        for fc in range(4):
                            k.op("pe", lambda e, fc=fc: e.matmul(po[:, :], lhsT=aT[:, fc, j * 128:(j + 1) * 128],
                                                                 rhs=wo[w_i][:, fc, hs], start=(fc == 0), stop=(fc == 3)),
                                 reads=[b_aT[j // 4], bw], writes=[pbo], inc=(fc == 3))
                        k.op("dve", lambda e: e.scalar_tensor_tensor(out=acc[:, j, hs], in0=po[:, :], scalar=comb[:, j, ex:ex + 1],
                                                                      in1=acc[:, j, hs], op0=ALU.mult, op1=ALU.add),
                             reads=[pbo, b_comb, b_acc[j]], writes=[b_acc[j]])
        for j in range(NTL):
            xt = acc[:, j, :]
            i = ns.i % ns.nbuf
            ns.i += 1
            rstd, b_st = rstd_of(g, ns, i, xt, b_acc[j])
            k.op("dve", lambda e: e.scalar_tensor_tensor(out=xt, in0=xt, scalar=rstd, in1=nfin[:],
                                                          op0=ALU.mult, op1=ALU.mult),
                 reads=[b_acc[j], b_st, b_nfin], writes=[b_acc[j]])
            k.dma("pool", g.out[b, j * 128:(j + 1) * 128, :], xt, reads=[b_acc[j]], writes=[g.bufs["out"]])


_CACHE = {}


def host_consts():
    r = np.arange(128)[:, None]
    c = np.arange(128)[None, :]
    cmat = np.stack([(r <= c), (r > c), (r < c), (r >= c), np.ones((128, 128), bool)]).astype(np.float32)
    rr = np.arange(128)[:, None] // 16
    cc = np.arange(128)[None, :] // 16
    m8 = np.stack([(rr <= cc), (rr >= cc)]).astype(np.float32)
    return {"ident": np.eye(128, dtype=np.float32), "cmat": cmat, "m8": m8}


def s5_lane_tables(inp):
    out = np.empty((2, 3, 128, 32), np.float32)
    for d in range(2):
        for i, a in enumerate((inp["s5_lam_re"][0, d], inp["s5_lam_im"][0, d])):
            out[d, i] = a.reshape(32, 2, 64).transpose(1, 2, 0).reshape(128, 32)
        ls = np.repeat(inp["s5_log_step"][0, d][:, None], 64, axis=1)
        out[d, 2] = ls.reshape(32, 2, 64).transpose(1, 2, 0).reshape(128, 32)
    return out


def s5_c_lanes(inp):
    out = np.empty((2, 2, 128, 32, 16), np.float32)
    for d in range(2):
        for i, a in enumerate((inp["s5_c_re"][0, d], inp["s5_c_im"][0, d])):
            out[d, i] = a.reshape(32, 2, 16, 64).transpose(1, 3, 0, 2).reshape(128, 32, 16)
    return out


def core_inputs(inp, core):
    b0 = core * NB
    cT = np.ascontiguousarray(np.stack([inp["c"][b0], inp["c"][b0 + 1], inp["c_ctx"]], axis=1))
    m = {
        "x": np.ascontiguousarray(inp["x"][b0:b0 + NB]),
        "ctx": np.ascontiguousarray(inp["ctx"][b0:b0 + NB]),
        "cT": cT,
        "ada_w": inp["ada_w"], "ada_b": inp["ada_b"],
        "norm_mix": inp["norm_mix"], "norm_ffn": inp["norm_ffn"],
        "ffn_w_in": inp["ffn_w_in"][0], "ffn_w_out": inp["ffn_w_out"][0],
        "moe_router_w": inp["moe_router_w"][0], "moe_router_b": inp["moe_router_b"].reshape(1, NEXP),
        "moe_w_in": inp["moe_w_in"][0], "moe_w_out": inp["moe_w_out"][0],
        "norm_final": inp["norm_final"].reshape(1, D),
        "ssd_w_in": inp["ssd_w_in"][0],
        "convT": np.ascontiguousarray(np.concatenate([inp["ssd_conv_w"][0], inp["ssd_conv_b"]], axis=0).T),
        "ssd_dt_bias": inp["ssd_dt_bias"].reshape(1, 64), "ssd_a_log": inp["ssd_a_log"].reshape(1, 64),
        "ssd_d": inp["ssd_d"].reshape(1, 32), "ssd_norm": inp["ssd_norm"].reshape(1, 2048),
        "ssd_w_out": inp["ssd_w_out"][0],
        "s5_lamT": s5_lane_tables(inp),
        "s5_b": np.ascontiguousarray(np.stack([inp["s5_b_re"][0], inp["s5_b_im"][0]], axis=1).reshape(2, 2, 4096, 16)),
        "s5_cT": s5_c_lanes(inp),
        "s5_d": inp["s5_d"].reshape(1, D), "s5_w_glu": inp["s5_w_glu"][0], "s5_b_glu": inp["s5_b_glu"].reshape(1, 2 * D),
    }
    m.update(host_consts())
    return m


def kernel(**inp):
    inp = {kk: np.asarray(v) for kk, v in inp.items()}
    if "prog" not in _CACHE:
        _CACHE["prog"] = build_program(("ada", "ssd", "ffn", "s5", "moe"), {})
    nc, g = _CACHE["prog"]
    in_maps = []
    for core in range(8):
        m = core_inputs(inp, core)
        in_maps.append({kk: np.ascontiguousarray(v) for kk, v in m.items() if kk in g.inputs})
    res = run_bass_kernel_spmd(nc, in_maps, core_ids=list(range(8)))
    return np.concatenate([r["out"] for r in res.results], axis=0).astype(np.float32)
```
